# Optimizing a Trainium2 kernel written in Bass

```python
import math
import jax, jax.numpy as jnp
from jax import lax
import numpy as np

D_MODEL = 1024
BATCH = 2
SEQ = 8192
DEPTH = 4

SSD_EXPAND = 2
SSD_INNER = SSD_EXPAND * D_MODEL
SSD_HEADDIM = 64
SSD_HEADS = SSD_INNER // SSD_HEADDIM
SSD_GROUPS = 4
SSD_HEADS_PER_GROUP = SSD_HEADS // SSD_GROUPS
SSD_STATE = 128
SSD_CONV = 4
SSD_CHUNK = 256
SSD_CONV_DIM = SSD_INNER + 2 * SSD_GROUPS * SSD_STATE
DT_MIN = 1e-3
DT_MAX = 1e-1

ATTN_HEADS = 16
ATTN_HEAD_DIM = 64
ATTN_WIDTH = ATTN_HEADS * ATTN_HEAD_DIM
MOBA_BLOCK = 256
MOBA_TOPK = 3
Q_BLOCK = 128
ROPE_THETA = 10000.0

N_BRANCHES = 2
IN_SIZES = (SSD_INNER, SSD_CONV_DIM, SSD_HEADS, 3 * ATTN_WIDTH, N_BRANCHES * D_MODEL)
IN_COLS = sum(IN_SIZES)
IN_SPLITS = tuple(int(s) for s in np.cumsum(IN_SIZES)[:-1])

MOE_GROUPS = 4
MOE_EXPERTS_PER_GROUP = 8
N_EXPERTS = MOE_GROUPS * MOE_EXPERTS_PER_GROUP
MOE_TOPK = 2
EXPERT_FF = 512
MOE_BLOCK = 128

DEEPNORM_ALPHA = (2 * DEPTH) ** 0.25
DEEPNORM_BETA = (8 * DEPTH) ** -0.25
LN_EPS = 1e-5
NEG_INF = -1e30

kernel_name = "hybrid_ssd_moba_hmoe_deepnorm"


def layer_norm(x, g, b):
    xf = x.astype(jnp.float32)
    mu = jnp.mean(xf, axis=-1, keepdims=True)
    var = jnp.mean(jnp.square(xf - mu), axis=-1, keepdims=True)
    return ((xf - mu) * lax.rsqrt(var + LN_EPS) * g + b).astype(x.dtype)


def adaln(c, w, b):
    mod = jax.nn.silu(c) @ w + b
    shift, scale, gate = jnp.split(mod, 3, axis=-1)
    return shift[:, None, :], scale[:, None, :], gate[:, None, :]


def rope_tables(positions):
    inv_freq = ROPE_THETA ** (-jnp.arange(0, ATTN_HEAD_DIM, 2, dtype=jnp.float32) / ATTN_HEAD_DIM)
    ang = positions.astype(jnp.float32)[..., None] * inv_freq
    return jnp.cos(ang)[:, :, None, :], jnp.sin(ang)[:, :, None, :]


def apply_rope(x, cos, sin):
    xf = x.astype(jnp.float32)
    x1, x2 = jnp.split(xf, 2, axis=-1)
    return jnp.concatenate([x1 * cos - x2 * sin, x2 * cos + x1 * sin], axis=-1).astype(x.dtype)


def causal_depthwise_conv(x, w, b):
    out = lax.conv_general_dilated(
        x, w[:, None, :], window_strides=(1,), padding=[(SSD_CONV - 1, 0)],
        dimension_numbers=('NWC', 'WIO', 'NWC'), feature_group_count=x.shape[-1])
    return out + b


def ssd_chunked_scan(x, dt, a, b, c):
    bsz, seq, g, r, p = x.shape
    chunk = math.gcd(seq, SSD_CHUNK)
    nc = seq // chunk

    def to_chunks(t):
        return t.reshape((bsz, nc, chunk) + t.shape[2:]).swapaxes(0, 1)

    causal = jnp.tril(jnp.ones((chunk, chunk), dtype=bool))

    def step(state, inp):
        xc, dtc, bc, cc = inp
        xc = xc.astype(jnp.float32)
        bc = bc.astype(jnp.float32)
        cc = cc.astype(jnp.float32)
        cs = jnp.cumsum(dtc * a, axis=1)
        cs_t = jnp.moveaxis(cs, 1, -1)
        decay = jnp.exp(jnp.where(causal, cs_t[..., :, None] - cs_t[..., None, :], -jnp.inf))
        scores = jnp.einsum('blgn,bsgn->bgls', cc, bc)[:, :, None] * decay
        xdt = xc * dtc[..., None]
        y = jnp.einsum('bgrls,bsgrp->blgrp', scores, xdt)
        y = y + jnp.einsum('blgn,bgrpn->blgrp', cc, state) * jnp.exp(cs)[..., None]
        to_end = jnp.exp(cs[:, -1:] - cs)
        state = (state * jnp.exp(cs[:, -1])[..., None, None]
                 + jnp.einsum('bsgn,bsgrp->bgrpn', bc, xdt * to_end[..., None]))
        return state, y

    state0 = jnp.zeros((bsz, g, r, p, SSD_STATE), jnp.float32)
    _, y = lax.scan(step, state0, (to_chunks(x), to_chunks(dt), to_chunks(b), to_chunks(c)))
    return y.swapaxes(0, 1).reshape(bsz, seq, g, r, p)


def ssd_mixer(z, xbc, dt_raw, conv_w, conv_b, dt_bias, a_log, d_skip, norm_w):
    bsz, seq = z.shape[:2]
    xbc = jax.nn.silu(causal_depthwise_conv(xbc, conv_w, conv_b))
    xs, bs, cs = jnp.split(xbc, [SSD_INNER, SSD_INNER + SSD_GROUPS * SSD_STATE], axis=-1)
    xs = xs.reshape(bsz, seq, SSD_GROUPS, SSD_HEADS_PER_GROUP, SSD_HEADDIM)
    bs = bs.reshape(bsz, seq, SSD_GROUPS, SSD_STATE)
    cs = cs.reshape(bsz, seq, SSD_GROUPS, SSD_STATE)
    dt = jax.nn.softplus(dt_raw.astype(jnp.float32) + dt_bias.astype(jnp.float32))
    dt = dt.reshape(bsz, seq, SSD_GROUPS, SSD_HEADS_PER_GROUP)
    a = -jnp.exp(a_log.astype(jnp.float32)).reshape(SSD_GROUPS, SSD_HEADS_PER_GROUP)
    y = ssd_chunked_scan(xs, dt, a, bs, cs)
    y = y + d_skip.astype(jnp.float32).reshape(SSD_GROUPS, SSD_HEADS_PER_GROUP)[:, :, None] * xs.astype(jnp.float32)
    gy = y.reshape(bsz, seq, SSD_INNER) * jax.nn.silu(z.astype(jnp.float32))
    gy = gy.reshape(bsz, seq, SSD_GROUPS, SSD_INNER // SSD_GROUPS)
    gy = gy * lax.rsqrt(jnp.mean(jnp.square(gy), axis=-1, keepdims=True) + LN_EPS)
    return (gy.reshape(bsz, seq, SSD_INNER) * norm_w).astype(z.dtype)


def moba_attention(q, k, v):
    bsz, heads, seq, hd = q.shape
    nb = -(-seq // MOBA_BLOCK)
    pad = nb * MOBA_BLOCK - seq
    k_blocks = jnp.pad(k, ((0, 0), (0, 0), (0, pad), (0, 0))).reshape(bsz, heads, nb, MOBA_BLOCK, hd)
    v_blocks = jnp.pad(v, ((0, 0), (0, 0), (0, pad), (0, 0))).reshape(bsz, heads, nb, MOBA_BLOCK, hd)
    k_mean = jnp.mean(k_blocks.astype(jnp.float32), axis=3).astype(k.dtype)
    n_sel = min(MOBA_TOPK, nb)
    nq = seq // Q_BLOCK
    q_blocks = q.reshape(bsz, heads, nq, Q_BLOCK, hd).transpose(2, 0, 1, 3, 4)
    scale = hd ** -0.5
    b_ix = jnp.arange(bsz)[:, None, None, None]
    h_ix = jnp.arange(heads)[None, :, None, None]
    blk_ids = jnp.arange(nb)

    def attend(args):
        qi, qb = args
        own = (qi * Q_BLOCK) // MOBA_BLOCK
        gate = jnp.einsum('bhqd,bhnd->bhqn', qb, k_mean).astype(jnp.float32)
        gate = jnp.where(blk_ids < own, gate, -jnp.inf)
        _, sel = lax.top_k(gate, n_sel)
        valid = sel < own
        k_sel = k_blocks[b_ix, h_ix, sel]
        v_sel = v_blocks[b_ix, h_ix, sel]
        s_sel = jnp.einsum('bhqd,bhqnjd->bhqnj', qb, k_sel).astype(jnp.float32) * scale
        s_sel = jnp.where(valid[..., None], s_sel, NEG_INF)
        k_own = lax.dynamic_index_in_dim(k_blocks, own, axis=2, keepdims=False)
        v_own = lax.dynamic_index_in_dim(v_blocks, own, axis=2, keepdims=False)
        s_own = jnp.einsum('bhqd,bhjd->bhqj', qb, k_own).astype(jnp.float32) * scale
        q_pos = qi * Q_BLOCK + jnp.arange(Q_BLOCK)
        k_pos = own * MOBA_BLOCK + jnp.arange(MOBA_BLOCK)
        s_own = jnp.where(k_pos[None, :] <= q_pos[:, None], s_own, NEG_INF)
        s = jnp.concatenate([s_sel.reshape(bsz, heads, Q_BLOCK, n_sel * MOBA_BLOCK), s_own], axis=-1)
        p = jax.nn.softmax(s, axis=-1).astype(v.dtype)
        p_sel = p[..., :n_sel * MOBA_BLOCK].reshape(bsz, heads, Q_BLOCK, n_sel, MOBA_BLOCK)
        p_own = p[..., n_sel * MOBA_BLOCK:]
        return (jnp.einsum('bhqnj,bhqnjd->bhqd', p_sel, v_sel)
                + jnp.einsum('bhqj,bhjd->bhqd', p_own, v_own))

    out = lax.map(attend, (jnp.arange(nq), q_blocks))
    return out.transpose(1, 0, 3, 2, 4).reshape(bsz, seq, heads * hd)


def hybrid_mixer(h, cos, sin, w_in, conv_w, conv_b, dt_bias, a_log, d_skip, ssd_norm_w,
                 w_branch_ssd, w_branch_attn, w_out):
    bsz, seq, _ = h.shape
    proj = h @ w_in
    z, xbc, dt_raw, qkv, gates = jnp.split(proj, IN_SPLITS, axis=-1)
    y_ssd = ssd_mixer(z, xbc, dt_raw, conv_w, conv_b, dt_bias, a_log, d_skip, ssd_norm_w)
    q, k, v = jnp.split(qkv.reshape(bsz, seq, 3 * ATTN_HEADS, ATTN_HEAD_DIM), 3, axis=2)
    q = apply_rope(q, cos, sin).transpose(0, 2, 1, 3)
    k = apply_rope(k, cos, sin).transpose(0, 2, 1, 3)
    v = v.transpose(0, 2, 1, 3)
    y_attn = moba_attention(q, k, v)
    g_ssd, g_attn = jnp.split(gates, N_BRANCHES, axis=-1)
    merged = (jax.nn.sigmoid(g_ssd) * (y_ssd @ w_branch_ssd)
              + jax.nn.sigmoid(g_attn) * (y_attn @ w_branch_attn))
    return merged @ w_out


def hierarchical_moe(h, w_rg, b_rg, w_re, b_re, w_gate, w_up, w_down):
    bsz, seq, d = h.shape
    t = bsz * seq
    xt = h.reshape(t, d)
    g_prob = jax.nn.softmax((xt @ w_rg + b_rg).astype(jnp.float32), axis=-1)
    g_val, g_idx = lax.top_k(g_prob, 1)
    e_logits = (xt @ w_re + b_re).astype(jnp.float32).reshape(t, MOE_GROUPS, MOE_EXPERTS_PER_GROUP)
    e_in_group = jnp.take_along_axis(e_logits, g_idx[:, :, None], axis=1)[:, 0]
    e_val, e_idx = lax.top_k(e_in_group, MOE_TOPK)
    e_w = jax.nn.softmax(e_val, axis=-1) * g_val
    expert_id = g_idx * MOE_EXPERTS_PER_GROUP + e_idx

    n_assign = t * MOE_TOPK
    flat_e = expert_id.reshape(n_assign)
    flat_tok = jnp.repeat(jnp.arange(t, dtype=jnp.int32), MOE_TOPK)
    flat_w = e_w.reshape(n_assign)
    order = jnp.argsort(flat_e)
    se, stok, sw = flat_e[order], flat_tok[order], flat_w[order]
    counts = jnp.zeros((N_EXPERTS,), jnp.int32).at[flat_e].add(1)
    starts = jnp.cumsum(counts) - counts
    padded = (counts + MOE_BLOCK - 1) // MOE_BLOCK * MOE_BLOCK
    ends = jnp.cumsum(padded)
    pstarts = ends - padded
    dest = pstarts[se] + (jnp.arange(n_assign, dtype=jnp.int32) - starts[se])
    n_blocks = -(-n_assign // MOE_BLOCK) + N_EXPERTS
    n_rows = n_blocks * MOE_BLOCK
    row_tok = jnp.full((n_rows,), t, jnp.int32).at[dest].set(stok)
    row_w = jnp.zeros((n_rows,), jnp.float32).at[dest].set(sw)
    block_e = jnp.minimum(jnp.searchsorted(ends, jnp.arange(n_blocks) * MOE_BLOCK, side='right'),
                          N_EXPERTS - 1)
    x_pad = jnp.concatenate([xt, jnp.zeros((1, d), xt.dtype)], axis=0)
    x_rows = x_pad[row_tok].reshape(n_blocks, MOE_BLOCK, d)

    def run_expert(args):
        xb, e = args
        return (jax.nn.silu(xb @ w_gate[e]) * (xb @ w_up[e])) @ w_down[e]

    y_rows = lax.map(run_expert, (x_rows, block_e)).reshape(n_rows, d)
    y = jnp.zeros((t + 1, d), y_rows.dtype).at[row_tok].add(y_rows * row_w[:, None].astype(y_rows.dtype))
    return y[:t].reshape(bsz, seq, d)


def setup_inputs(seed: int = 0) -> dict:
    key = jax.random.key(seed)
    ks = jax.random.split(key, 27)

    def nrm(k, shape, std):
        return std * jax.random.normal(k, shape, jnp.float32)

    L, D = DEPTH, D_MODEL
    x = nrm(ks[0], (BATCH, SEQ, D), 1.0)
    c = nrm(ks[1], (BATCH, D), 1.0)
    positions = jnp.broadcast_to(jnp.arange(SEQ, dtype=jnp.int32)[None, :], (BATCH, SEQ))
    w_in = nrm(ks[2], (L, D, IN_COLS), D ** -0.5)
    conv_w = nrm(ks[3], (L, SSD_CONV, SSD_CONV_DIM), SSD_CONV ** -0.5)
    conv_b = nrm(ks[4], (L, SSD_CONV_DIM), 0.02)
    u = jax.random.uniform(ks[5], (L, SSD_HEADS), jnp.float32)
    dt0 = jnp.exp(u * (math.log(DT_MAX) - math.log(DT_MIN)) + math.log(DT_MIN))
    dt_bias = dt0 + jnp.log(-jnp.expm1(-dt0))
    a_log = jnp.log(jax.random.uniform(ks[6], (L, SSD_HEADS), jnp.float32, minval=1.0, maxval=16.0))
    d_skip = 1.0 + nrm(ks[7], (L, SSD_HEADS), 0.1)
    ssd_norm_w = 1.0 + nrm(ks[8], (L, SSD_INNER), 0.1)
    w_branch_ssd = nrm(ks[9], (L, SSD_INNER, D), DEEPNORM_BETA * SSD_INNER ** -0.5)
    w_branch_attn = nrm(ks[10], (L, ATTN_WIDTH, D), DEEPNORM_BETA * ATTN_WIDTH ** -0.5)
    w_out = nrm(ks[11], (L, D, D), DEEPNORM_BETA * D ** -0.5)
    w_ada_mix = nrm(ks[12], (L, D, 3 * D), 0.2 * D ** -0.5)
    b_ada_mix = nrm(ks[13], (L, 3 * D), 0.02)
    ln_mix_g = 1.0 + nrm(ks[14], (L, D), 0.1)
    ln_mix_b = nrm(ks[15], (L, D), 0.02)
    w_ada_ffn = nrm(ks[16], (L, D, 3 * D), 0.2 * D ** -0.5)
    b_ada_ffn = nrm(ks[17], (L, 3 * D), 0.02)
    w_router_group = nrm(ks[18], (L, D, MOE_GROUPS), D ** -0.5)
    b_router_group = nrm(ks[19], (L, MOE_GROUPS), 0.01)
    w_router_expert = nrm(ks[20], (L, D, N_EXPERTS), D ** -0.5)
    b_router_expert = nrm(ks[21], (L, N_EXPERTS), 0.01)
    w_expert_gate = nrm(ks[22], (L, N_EXPERTS, D, EXPERT_FF), D ** -0.5)
    w_expert_up = nrm(ks[23], (L, N_EXPERTS, D, EXPERT_FF), D ** -0.5)
    w_expert_down = nrm(ks[24], (L, N_EXPERTS, EXPERT_FF, D), DEEPNORM_BETA * EXPERT_FF ** -0.5)
    ln_ffn_g = 1.0 + nrm(ks[25], (L, D), 0.1)
    ln_ffn_b = nrm(ks[26], (L, D), 0.02)
    return {"x": x, "c": c, "positions": positions, "w_in": w_in, "conv_w": conv_w,
            "conv_b": conv_b, "dt_bias": dt_bias, "a_log": a_log, "d_skip": d_skip,
            "ssd_norm_w": ssd_norm_w, "w_branch_ssd": w_branch_ssd, "w_branch_attn": w_branch_attn,
            "w_out": w_out, "w_ada_mix": w_ada_mix, "b_ada_mix": b_ada_mix, "ln_mix_g": ln_mix_g,
            "ln_mix_b": ln_mix_b, "w_ada_ffn": w_ada_ffn, "b_ada_ffn": b_ada_ffn,
            "w_router_group": w_router_group, "b_router_group": b_router_group,
            "w_router_expert": w_router_expert, "b_router_expert": b_router_expert,
            "w_expert_gate": w_expert_gate, "w_expert_up": w_expert_up, "w_expert_down": w_expert_down,
            "ln_ffn_g": ln_ffn_g, "ln_ffn_b": ln_ffn_b}


def reference(x, c, positions, w_in, conv_w, conv_b, dt_bias, a_log, d_skip, ssd_norm_w,
              w_branch_ssd, w_branch_attn, w_out, w_ada_mix, b_ada_mix, ln_mix_g, ln_mix_b,
              w_ada_ffn, b_ada_ffn, w_router_group, b_router_group, w_router_expert,
              b_router_expert, w_expert_gate, w_expert_up, w_expert_down, ln_ffn_g, ln_ffn_b):
    cos, sin = rope_tables(positions)
    for l in range(DEPTH):
        shift, scale, gate = adaln(c, w_ada_mix[l], b_ada_mix[l])
        h = x * (1.0 + scale) + shift
        y = hybrid_mixer(h, cos, sin, w_in[l], conv_w[l], conv_b[l], dt_bias[l], a_log[l],
                         d_skip[l], ssd_norm_w[l], w_branch_ssd[l], w_branch_attn[l], w_out[l])
        x = layer_norm(DEEPNORM_ALPHA * x + (1.0 + gate) * y, ln_mix_g[l], ln_mix_b[l])
        shift, scale, gate = adaln(c, w_ada_ffn[l], b_ada_ffn[l])
        h = x * (1.0 + scale) + shift
        y = hierarchical_moe(h, w_router_group[l], b_router_group[l], w_router_expert[l],
                             b_router_expert[l], w_expert_gate[l], w_expert_up[l], w_expert_down[l])
        x = layer_norm(DEEPNORM_ALPHA * x + (1.0 + gate) * y, ln_ffn_g[l], ln_ffn_b[l])
    return x
```

```python
import numpy as np
import concourse.bass as bass
import concourse.mybir as mybir
from concourse.bass_utils import run_bass_kernel_spmd

F32 = mybir.dt.float32
BF16 = mybir.dt.bfloat16
I32 = mybir.dt.int32
AF = mybir.ActivationFunctionType
ALU = mybir.AluOpType
AX = mybir.AxisListType

ENGS = ("pe", "act", "dve", "pool", "sp")
DMA_POOL = 8


class T:
    __slots__ = ("ap", "w", "r", "name")

    def __init__(self, ap, name=""):
        self.ap = ap
        self.w = None
        self.r = []
        self.name = name

    def __getitem__(self, k):
        return self.ap[k]


class Op:
    __slots__ = ("eng", "fn", "deps", "dma", "idx", "needed", "sem", "val", "slot_prev")

    def __init__(self, eng, fn, deps, dma):
        self.eng = eng
        self.fn = fn
        self.deps = deps
        self.dma = dma
        self.needed = False
        self.sem = None
        self.val = None
        self.slot_prev = None


class Prog:
    def __init__(self, nc):
        self.nc = nc
        self.ops = {e: [] for e in ENGS}
        self.dma_count = {e: 0 for e in ENGS}
        self.dma_slots = {e: [None] * DMA_POOL for e in ENGS}
        self._ctx = []

    def sb(self, name, shape, dt=F32):
        g = self.nc.sbuf_tensor(name, list(shape), dt)
        t = g.__enter__()
        self._ctx.append(g)
        return T(t, name)

    def ps(self, name, shape, dt=F32):
        g = self.nc.psum_tensor(name, list(shape), dt)
        t = g.__enter__()
        self._ctx.append(g)
        return T(t, name)

    def alias(self, ap, name=""):
        return T(ap, name)

    def add(self, eng, fn, reads=(), writes=(), dma=False):
        deps = []
        for t in reads:
            if t.w is not None:
                deps.append((t.w, True))
        for t in writes:
            if t.w is not None:
                deps.append((t.w, False))
            for r in t.r:
                deps.append((r, False))
        op = Op(eng, fn, [], dma)
        seen = set()
        for d, raw in deps:
            if d is op or id(d) in seen:
                continue
            if d.eng == eng and not d.dma and not dma:
                if eng == "pe" or not raw:
                    continue
            seen.add(id(d))
            op.deps.append(d)
            d.needed = True
        if dma:
            k = self.dma_count[eng] % DMA_POOL
            self.dma_count[eng] += 1
            prev = self.dma_slots[eng][k]
            op.slot_prev = prev
            if prev is not None:
                prev.needed = True
            self.dma_slots[eng][k] = op
            op.sem = (eng, k)
            op.needed = True
        op.idx = len(self.ops[eng])
        self.ops[eng].append(op)
        for t in reads:
            t.r.append(op)
        for t in writes:
            t.w = op
            t.r = []
        return op

    def pe(self, fn, reads=(), writes=()):
        return self.add("pe", fn, reads, writes)

    def act(self, fn, reads=(), writes=()):
        return self.add("act", fn, reads, writes)

    def dve(self, fn, reads=(), writes=()):
        return self.add("dve", fn, reads, writes)

    def pool(self, fn, reads=(), writes=()):
        return self.add("pool", fn, reads, writes)

    def dma(self, eng, out_ap, in_ap, reads=(), writes=(), **kw):
        return self.add(eng, lambda e: e.dma_start(out=out_ap, in_=in_ap, **kw), reads, writes, dma=True)

    def finish(self, final_waits=()):
        nc = self.nc
        sem_objs = {}
        stack = []

        def getsem(key):
            if key not in sem_objs:
                g = nc.semaphore("s_%s_%s" % key if isinstance(key, tuple) else "s_%s" % key)
                sem_objs[key] = g.__enter__()
                stack.append(g)
            return sem_objs[key]

        for e in ENGS:
            cnt = 0
            dcnt = {}
            for op in self.ops[e]:
                if op.dma:
                    dcnt[op.sem] = dcnt.get(op.sem, 0) + 16
                    op.val = dcnt[op.sem]
                elif op.needed:
                    cnt += 1
                    op.sem = e
                    op.val = cnt
        for op in final_waits:
            op.needed = True
        engmap = {"pe": "tensor", "act": "scalar", "dve": "vector", "pool": "gpsimd", "sp": "sync"}
        prog = self

        def emit(e, engine):
            waited = {}
            ops = prog.ops[e]
            for op in ops:
                need = {}
                dl = list(op.deps)
                if op.slot_prev is not None:
                    dl.append(op.slot_prev)
                for d in dl:
                    if waited.get(d.sem, 0) >= d.val:
                        continue
                    if need.get(d.sem, 0) < d.val:
                        need[d.sem] = d.val
                for s, v in need.items():
                    engine.wait_ge(getsem(s), v)
                    waited[s] = v
                ins = op.fn(engine)
                if op.dma:
                    ins.then_inc(getsem(op.sem), 16)
                elif op.needed:
                    ins.then_inc(getsem(op.sem), 1)
            if e == "sp":
                for op in final_waits:
                    if waited.get(op.sem, 0) < op.val:
                        engine.wait_ge(getsem(op.sem), op.val)
                        waited[op.sem] = op.val

        for e in ENGS:
            for op in self.ops[e]:
                if op.sem is not None and (op.needed or op.dma):
                    getsem(op.sem)
        with nc.Block() as block:
            for e in ENGS:
                if not self.ops[e] and e != "sp":
                    continue
                getattr(block, engmap[e])(lambda engine, e=e: emit(e, engine))
        for g in reversed(stack):
            g.__exit__(None, None, None)
        for g in reversed(self._ctx):
            g.__exit__(None, None, None)
        self._ctx = []
        n = {e: len(self.ops[e]) for e in ENGS}
        return n


def _fence(P):
    f = []
    for e in ENGS:
        ops = P.ops[e]
        last_c = None
        nd = 0
        for op in reversed(ops):
            if op.dma:
                if nd < DMA_POOL:
                    f.append(op)
                    nd += 1
            elif last_c is None:
                last_c = op
                f.append(op)
            if nd >= DMA_POOL and last_c is not None:
                break
    return f


def _fenced(ap, fence, name=""):
    t = T(ap, name)
    t.r = list(fence)
    return t


D = 1024
KT = 8
ALPHA = float(8 ** 0.25)
EPS = 1e-5
NEG = -1.0e30
NEXP = 32


def _din(nc, name, shape, dt=F32):
    return nc.dram_tensor(name, list(shape), dt, kind="ExternalInput").ap()


def _dout(nc, name, shape, dt=F32):
    return nc.dram_tensor(name, list(shape), dt, kind="ExternalOutput").ap()


def _adaln(P, w_ap, b_sb, sc, wfull, ps, mod):
    for kt in range(KT):
        P.dma("sp", wfull[:, kt, :], w_ap[kt * 128:(kt + 1) * 128, :], writes=[wfull])
    for ft in range(24):
        for kt in range(KT):
            P.pe(lambda e, kt=kt, ft=ft: e.matmul(
                ps[:, ft:ft + 1], wfull[:, kt, ft * 128:(ft + 1) * 128], sc[:, kt:kt + 1],
                start=(kt == 0), stop=(kt == KT - 1)), reads=[wfull, sc], writes=[ps])
    P.dve(lambda e: e.tensor_tensor(out=mod[:, 0:24], in0=ps[:, 0:24], in1=b_sb[:, 0:24], op=ALU.add),
          reads=[ps, b_sb], writes=[mod])


def _layernorm(P, v, sq, ps1, ps2, ones, tmp, gcol, bcol, out, TB):
    P.act(lambda e: e.activation(out=sq[:, :, 0:TB], in_=v[:, :, 0:TB], func=AF.Square), reads=[v], writes=[sq])
    for ft in range(KT):
        P.pe(lambda e, ft=ft: e.matmul(ps1[:, 0:TB], ones[:, 0:128], v[:, ft, 0:TB], start=(ft == 0), stop=(ft == KT - 1)),
             reads=[v, ones], writes=[ps1])
    for ft in range(KT):
        P.pe(lambda e, ft=ft: e.matmul(ps2[:, 0:TB], ones[:, 0:128], sq[:, ft, 0:TB], start=(ft == 0), stop=(ft == KT - 1)),
             reads=[sq, ones], writes=[ps2])
    mean, msq, rstd = tmp
    P.dve(lambda e: e.tensor_scalar(out=mean[:, 0:TB], in0=ps1[:, 0:TB], scalar1=1.0 / D, scalar2=None, op0=ALU.mult),
          reads=[ps1], writes=[mean])
    P.dve(lambda e: e.tensor_tensor(out=msq[:, 0:TB], in0=mean[:, 0:TB], in1=mean[:, 0:TB], op=ALU.mult),
          reads=[mean], writes=[msq])
    P.dve(lambda e: e.scalar_tensor_tensor(out=rstd[:, 0:TB], in0=ps2[:, 0:TB], scalar=1.0 / D, in1=msq[:, 0:TB],
                                           op0=ALU.mult, op1=ALU.subtract), reads=[ps2, msq], writes=[rstd])
    P.dve(lambda e: e.tensor_scalar(out=rstd[:, 0:TB], in0=rstd[:, 0:TB], scalar1=EPS, scalar2=None,
                                    op0=ALU.add), reads=[rstd], writes=[rstd])
    P.act(lambda e: e.activation(out=rstd[:, 0:TB], in_=rstd[:, 0:TB], func=AF.Sqrt), reads=[rstd], writes=[rstd])
    P.dve(lambda e: e.reciprocal(out=rstd[:, 0:TB], in_=rstd[:, 0:TB]), reads=[rstd], writes=[rstd])
    def bc_t(t):
        return t[:, 0:TB].rearrange("p (o t) -> p o t", o=1).to_broadcast([128, KT, TB])

    def bc_f(t):
        return t[:, 0:KT].rearrange("p (k o) -> p k o", o=1).to_broadcast([128, KT, TB])
    P.dve(lambda e: e.tensor_tensor(out=sq[:, :, 0:TB], in0=v[:, :, 0:TB], in1=bc_t(mean), op=ALU.subtract),
          reads=[v, mean], writes=[sq])
    P.pool(lambda e: e.tensor_tensor(out=sq[:, :, 0:TB], in0=sq[:, :, 0:TB], in1=bc_t(rstd), op=ALU.mult),
           reads=[sq, rstd], writes=[sq])
    P.dve(lambda e: e.tensor_tensor(out=sq[:, :, 0:TB], in0=sq[:, :, 0:TB], in1=bc_f(gcol), op=ALU.mult),
          reads=[sq, gcol], writes=[sq])
    P.pool(lambda e: e.tensor_tensor(out=out[:, :, 0:TB], in0=sq[:, :, 0:TB], in1=bc_f(bcol), op=ALU.add),
           reads=[sq, bcol], writes=[out])


def _routing(P, psR, lgs, rt, Wt, ti):
    gmax, ngmax, gsel, gexp, gsum, gval, pen, msk, mx8, dd, ed, w1, w2, wa, wb = rt
    P.dve(lambda e: e.tensor_copy(lgs[:, 0:36], psR[:, 0:36]), reads=[psR], writes=[lgs])
    P.dve(lambda e: e.reduce_max(out=gmax[:, 0:1], in_=lgs[:, 0:4], axis=AX.X), reads=[lgs], writes=[gmax])
    P.dve(lambda e: e.tensor_scalar(out=ngmax[:, 0:1], in0=gmax[:, 0:1], scalar1=-1.0, scalar2=None, op0=ALU.mult),
          reads=[gmax], writes=[ngmax])
    P.dve(lambda e: e.tensor_scalar(out=gsel[:, 0:4], in0=lgs[:, 0:4], scalar1=gmax[:, 0:1], scalar2=None,
                                    op0=ALU.is_equal), reads=[lgs, gmax], writes=[gsel])
    P.act(lambda e: e.activation(out=gexp[:, 0:4], in_=lgs[:, 0:4], func=AF.Exp, bias=ngmax[:, 0:1], scale=1.0),
          reads=[lgs, ngmax], writes=[gexp])
    P.dve(lambda e: e.reduce_sum(out=gsum[:, 0:1], in_=gexp[:, 0:4], axis=AX.X), reads=[gexp], writes=[gsum])
    P.dve(lambda e: e.reciprocal(out=gval[:, 0:1], in_=gsum[:, 0:1]), reads=[gsum], writes=[gval])
    P.dve(lambda e: e.tensor_scalar(out=pen[:, 0:4], in0=gsel[:, 0:4], scalar1=-1.0, scalar2=-NEG,
                                    op0=ALU.add, op1=ALU.mult), reads=[gsel], writes=[pen])
    P.dve(lambda e: e.tensor_tensor(
        out=msk[:, 0:32].rearrange("p (g x) -> p g x", g=4),
        in0=lgs[:, 4:36].rearrange("p (g x) -> p g x", g=4),
        in1=pen[:, 0:4].rearrange("p (g o) -> p g o", o=1).to_broadcast([128, 4, 8]), op=ALU.add),
        reads=[lgs, pen], writes=[msk])
    P.dve(lambda e: e.max(out=mx8[:, 0:8], in_=msk[:, 0:32]), reads=[msk], writes=[mx8])
    P.dve(lambda e: e.tensor_tensor(out=dd[:, 0:1], in0=mx8[:, 1:2], in1=mx8[:, 0:1], op=ALU.subtract),
          reads=[mx8], writes=[dd])
    P.act(lambda e: e.activation(out=ed[:, 0:1], in_=dd[:, 0:1], func=AF.Exp), reads=[dd], writes=[ed])
    P.dve(lambda e: e.tensor_scalar(out=w1[:, 0:1], in0=ed[:, 0:1], scalar1=1.0, scalar2=None, op0=ALU.add),
          reads=[ed], writes=[w1])
    P.dve(lambda e: e.reciprocal(out=w1[:, 0:1], in_=w1[:, 0:1]), reads=[w1], writes=[w1])
    P.dve(lambda e: e.tensor_tensor(out=w1[:, 0:1], in0=w1[:, 0:1], in1=gval[:, 0:1], op=ALU.mult),
          reads=[w1, gval], writes=[w1])
    P.dve(lambda e: e.tensor_tensor(out=w2[:, 0:1], in0=w1[:, 0:1], in1=ed[:, 0:1], op=ALU.mult),
          reads=[w1, ed], writes=[w2])
    P.dve(lambda e: e.tensor_scalar(out=wa[:, 0:32], in0=msk[:, 0:32], scalar1=mx8[:, 0:1], scalar2=w1[:, 0:1],
                                    op0=ALU.is_equal, op1=ALU.mult), reads=[msk, mx8, w1], writes=[wa])
    P.dve(lambda e: e.tensor_scalar(out=wb[:, 0:32], in0=msk[:, 0:32], scalar1=mx8[:, 1:2], scalar2=w2[:, 0:1],
                                    op0=ALU.is_equal, op1=ALU.mult), reads=[msk, mx8, w2], writes=[wb])
    P.dve(lambda e, ti=ti: e.tensor_tensor(out=Wt[:, ti, 0:32], in0=wa[:, 0:32], in1=wb[:, 0:32], op=ALU.add),
          reads=[wa, wb], writes=[Wt])


def build_p2(NT, nexp=NEXP):
    nc = bass.Bass("TRN2", target_bir_lowering=False)
    TB = 256
    NB = NT // TB
    NTT = NT // 128
    TE = min(512, NT)
    NBE = NT // TE
    xT = _din(nc, "xT", [D, NT]); YsT = _din(nc, "YsT", [2048, NT]); YaT = _din(nc, "YaT", [1024, NT])
    ccol = _din(nc, "ccol", [128, 8])
    w_am = _din(nc, "w_am", [D, 3072]); b_am = _din(nc, "b_am", [128, 24])
    w_af = _din(nc, "w_af", [D, 3072]); b_af = _din(nc, "b_af", [128, 24])
    w_g = _din(nc, "w_g", [D, 2048]); w_bs = _din(nc, "w_bs", [2048, D])
    w_ba = _din(nc, "w_ba", [D, D]); w_o = _din(nc, "w_o", [D, D])
    lnp = _din(nc, "lnp", [128, 32])
    w_r = _din(nc, "w_r", [D, 36]); b_r = _din(nc, "b_r", [1, 36])
    w_eg = _din(nc, "w_eg", [NEXP, D, 512]); w_eu = _din(nc, "w_eu", [NEXP, D, 512]); w_ed = _din(nc, "w_ed", [NEXP, 512, D])
    cst = _din(nc, "cst", [128, 256])
    xoT = _dout(nc, "xoT", [D, NT])
    x1scr_ap = nc.dram_tensor("x1scr", [D, NT], F32, kind="Internal").ap()
    x1scr = T(x1scr_ap, "x1scr")

    P = Prog(nc)
    arena = P.sb("arena", [128, 49152], BF16)
    arena2 = P.sb("arena2", [128, 26624], BF16)
    h2raw = P.sb("h2raw", [128, 8 * NT], BF16)
    ident = P.sb("ident", [128, 128]); ones = P.sb("ones", [128, 128])
    sc = P.sb("sc", [128, 8]); lnv = [P.sb("lnv%d" % i, [128, 8]) for i in range(4)]
    bam = P.sb("bam", [128, 24]); baf = P.sb("baf", [128, 24])
    modm = P.sb("modm", [128, 24]); modf = P.sb("modf", [128, 24])
    sc1m = P.sb("sc1m", [128, 8]); g1pm = P.sb("g1pm", [128, 8]); sc1f = P.sb("sc1f", [128, 8]); g1pf = P.sb("g1pf", [128, 8])
    wr = P.sb("wr", [128, 8, 36]); br = P.sb("br", [1, 36])
    Wt = P.sb("Wt", [128, NTT, 32])
    gs = [P.sb("gs%d" % i, [128, 2, TB]) for i in range(2)]
    m1 = [P.sb("m1%d" % i, [128, TB]) for i in range(2)]
    m2 = [P.sb("m2%d" % i, [128, TB]) for i in range(2)]
    lntmp = [P.sb("lnt%d" % i, [128, TB]) for i in range(3)]
    lgs = P.sb("lgs", [128, 36])
    rt = [P.sb("rt%d" % i, [128, 32]) for i in range(15)]
    banks = [P.ps("bank%d" % i, [128, 512]) for i in range(8)]
    A0, A1, B0, B1, G0, G1, L, R = banks

    P.dma("sp", ident[:, :], cst[:, 0:128], writes=[ident])
    P.dma("sp", ones[:, :], cst[:, 128:256], writes=[ones])
    P.dma("sp", sc[:, :], ccol[:, :], writes=[sc])
    for i in range(4):
        P.dma("sp", lnv[i][:, :], lnp[:, i * 8:(i + 1) * 8], writes=[lnv[i]])
    P.dma("sp", bam[:, :], b_am[:, :], writes=[bam])
    P.dma("sp", baf[:, :], b_af[:, :], writes=[baf])
    P.dma("sp", wr[:, :, :], w_r.rearrange("(k p) n -> p k n", p=128), writes=[wr])
    P.dma("sp", br[:, :], b_r[:, :], writes=[br])
    P.act(lambda e: e.activation(out=sc[:, :], in_=sc[:, :], func=AF.Silu), reads=[sc], writes=[sc])
    wfull = T(arena.ap[:, 0:49152].bitcast(F32).rearrange("p (k f) -> p k f", k=8), "wfull")
    _adaln(P, w_am, bam, sc, wfull, R, modm)
    _adaln(P, w_af, baf, sc, wfull, R, modf)
    for (mod, s1, g1) in ((modm, sc1m, g1pm), (modf, sc1f, g1pf)):
        P.dve(lambda e, mod=mod, s1=s1: e.tensor_scalar(out=s1[:, :], in0=mod[:, 8:16], scalar1=1.0, scalar2=None, op0=ALU.add),
              reads=[mod], writes=[s1])
        P.dve(lambda e, mod=mod, g1=g1: e.tensor_scalar(out=g1[:, :], in0=mod[:, 16:24], scalar1=1.0, scalar2=None, op0=ALU.add),
              reads=[mod], writes=[g1])
    f0 = _fence(P)
    h2b = T(h2raw.ap[:, 0:8 * NT].rearrange("p (k t) -> p k t", k=8), "h2b")

    wg = _fenced(arena.ap[:, 0:16384].rearrange("p (k f) -> p k f", k=8), f0, "wg")
    wbs = _fenced(arena.ap[:, 16384:32768].rearrange("p (k f) -> p k f", k=16), f0, "wbs")
    wba = _fenced(arena.ap[:, 32768:40960].rearrange("p (k f) -> p k f", k=8), f0, "wba")
    wo = _fenced(arena.ap[:, 40960:49152].rearrange("p (k f) -> p k f", k=8), f0, "wo")
    for kt in range(8):
        P.dma("pool", wg[:, kt, :], w_g[kt * 128:(kt + 1) * 128, :], writes=[wg])
    for kt in range(16):
        P.dma("pool", wbs[:, kt, :], w_bs[kt * 128:(kt + 1) * 128, :], writes=[wbs])
    for kt in range(8):
        P.dma("pool", wba[:, kt, :], w_ba[kt * 128:(kt + 1) * 128, :], writes=[wba])
    for kt in range(8):
        P.dma("pool", wo[:, kt, :], w_o[kt * 128:(kt + 1) * 128, :], writes=[wo])

    def a2(off, n, dt, shape_k, name, fence=None):
        ap = arena2.ap[:, off:off + n]
        if dt == F32:
            ap = ap.bitcast(F32)
        ap = ap.rearrange("p (k t) -> p k t", k=shape_k)
        return _fenced(ap, fence, name) if fence is not None else T(ap, name)

    xb = a2(0, 4096, F32, 8, "xb"); v = a2(4096, 4096, F32, 8, "v"); sq = a2(8192, 4096, F32, 8, "sq")
    hT = a2(12288, 2048, BF16, 8, "hT"); ys = a2(14336, 4096, BF16, 16, "ys"); ya = a2(18432, 2048, BF16, 8, "ya")
    mg = a2(20480, 2048, BF16, 8, "mg"); h2f = a2(22528, 4096, F32, 8, "h2f")

    def bc_f(t, lo=0):
        return t[:, lo:lo + 8].rearrange("p (k o) -> p k o", o=1).to_broadcast([128, 8, TB])

    xTr = xT.rearrange("(k p) t -> p k t", p=128)
    YsTr = YsT.rearrange("(k p) t -> p k t", p=128)
    YaTr = YaT.rearrange("(k p) t -> p k t", p=128)
    x1r = x1scr_ap.rearrange("(k p) t -> p k t", p=128)
    xoTr = xoT.rearrange("(k p) t -> p k t", p=128)

    for tb in range(NB):
        t0 = tb * TB
        P.dma("sp", xb[:, :, :], xTr[:, :, t0:t0 + TB], writes=[xb])
        P.dma("pool", ys[:, :, :], YsTr[:, :, t0:t0 + TB], writes=[ys])
        P.dma("pool", ya[:, :, :], YaTr[:, :, t0:t0 + TB], writes=[ya])
        P.dve(lambda e: e.tensor_tensor(out=v[:, :, :], in0=xb[:, :, :], in1=bc_f(sc1m), op=ALU.mult),
              reads=[xb, sc1m], writes=[v])
        P.dve(lambda e: e.tensor_tensor(out=hT[:, :, :], in0=v[:, :, :], in1=bc_f(modm, 0), op=ALU.add),
              reads=[v, modm], writes=[hT])
        P.pool(lambda e: e.tensor_scalar(out=xb[:, :, :], in0=xb[:, :, :], scalar1=ALPHA, scalar2=None, op0=ALU.mult),
               reads=[xb], writes=[xb])
        for ft in range(8):
            pa, pb, pg = (A0, A1)[ft % 2], (B0, B1)[ft % 2], (G0, G1)[ft % 2]
            gsx, m1x, m2x = gs[ft % 2], m1[ft % 2], m2[ft % 2]
            fs = slice(ft * 128, (ft + 1) * 128)
            fs2 = slice(1024 + ft * 128, 1024 + (ft + 1) * 128)
            for kt in range(16):
                P.pe(lambda e, pa=pa, kt=kt, fs=fs: e.matmul(pa[:, 0:TB], wbs[:, kt, fs], ys[:, kt, :], start=(kt == 0), stop=(kt == 15)),
                     reads=[wbs, ys], writes=[pa])
            for kt in range(8):
                P.pe(lambda e, pb=pb, kt=kt, fs=fs: e.matmul(pb[:, 0:TB], wba[:, kt, fs], ya[:, kt, :], start=(kt == 0), stop=(kt == 7)),
                     reads=[wba, ya], writes=[pb])
            for kt in range(8):
                P.pe(lambda e, pg=pg, kt=kt, fs=fs: e.matmul(pg[:, 0:TB], wg[:, kt, fs], hT[:, kt, :], start=(kt == 0), stop=(kt == 7)),
                     reads=[wg, hT], writes=[pg])
            for kt in range(8):
                P.pe(lambda e, pg=pg, kt=kt, fs2=fs2: e.matmul(pg[:, TB:2 * TB], wg[:, kt, fs2], hT[:, kt, :], start=(kt == 0), stop=(kt == 7)),
                     reads=[wg, hT], writes=[pg])
            P.act(lambda e, pg=pg, gsx=gsx: e.activation(out=gsx[:, :, :].rearrange("p a t -> p (a t)"), in_=pg[:, 0:2 * TB], func=AF.Sigmoid),
                  reads=[pg], writes=[gsx])
            P.dve(lambda e, pa=pa, gsx=gsx, m1x=m1x: e.tensor_tensor(out=m1x[:, :], in0=pa[:, 0:TB], in1=gsx[:, 0, :], op=ALU.mult),
                  reads=[pa, gsx], writes=[m1x])
            P.dve(lambda e, pb=pb, gsx=gsx, m2x=m2x: e.tensor_tensor(out=m2x[:, :], in0=pb[:, 0:TB], in1=gsx[:, 1, :], op=ALU.mult),
                  reads=[pb, gsx], writes=[m2x])
            P.pool(lambda e, ft=ft, m1x=m1x, m2x=m2x: e.tensor_tensor(out=mg[:, ft, :], in0=m1x[:, :], in1=m2x[:, :], op=ALU.add),
                   reads=[m1x, m2x], writes=[mg])
        for ft in range(8):
            pa = (A0, A1)[ft % 2]
            fs = slice(ft * 128, (ft + 1) * 128)
            for kt in range(8):
                P.pe(lambda e, pa=pa, kt=kt, fs=fs: e.matmul(pa[:, 0:TB], wo[:, kt, fs], mg[:, kt, :], start=(kt == 0), stop=(kt == 7)),
                     reads=[wo, mg], writes=[pa])
            P.dve(lambda e, pa=pa, ft=ft: e.scalar_tensor_tensor(out=v[:, ft, :], in0=pa[:, 0:TB], scalar=g1pm[:, ft:ft + 1],
                                                                 in1=xb[:, ft, :], op0=ALU.mult, op1=ALU.add),
                  reads=[pa, g1pm, xb], writes=[v])
        _layernorm(P, v, sq, L, R, ones, lntmp, lnv[0], lnv[1], xb, TB)
        P.dma("sp", x1r[:, :, t0:t0 + TB], xb[:, :, :], reads=[xb], writes=[x1scr])
        P.dve(lambda e: e.tensor_tensor(out=v[:, :, :], in0=xb[:, :, :], in1=bc_f(sc1f), op=ALU.mult),
              reads=[xb, sc1f], writes=[v])
        P.dve(lambda e: e.tensor_tensor(out=h2f[:, :, :], in0=v[:, :, :], in1=bc_f(modf, 0), op=ALU.add),
              reads=[v, modf], writes=[h2f])
        P.act(lambda e, t0=t0: e.copy(h2b[:, :, t0:t0 + TB], h2f[:, :, :]), reads=[h2f], writes=[h2b])
        for tt in range(TB // 128):
            ti = tb * (TB // 128) + tt
            for kt in range(8):
                P.pe(lambda e, kt=kt, tt=tt: e.matmul(R[:, 0:36], h2f[:, kt, tt * 128:(tt + 1) * 128], wr[:, kt, :],
                                                     start=(kt == 0), stop=False), reads=[h2f, wr], writes=[R])
            P.pe(lambda e: e.matmul(R[:, 0:36], ones[0:1, 0:128], br[0:1, 0:36], start=False, stop=True),
                 reads=[ones, br], writes=[R])
            _routing(P, R, lgs, rt, Wt, ti)

    fAB = _fence(P)
    acc = _fenced(arena.ap[:, 0:NTT * 2048].bitcast(F32).rearrange("p (t d) -> p t d", t=NTT), fAB, "acc")
    slots = [_fenced(arena.ap[:, 32768:45056], fAB, "slot0"), _fenced(arena2.ap[:, 0:12288], fAB, "slot1")]
    act = _fenced(arena2.ap[:, 12288:12288 + 4 * TE].rearrange("p (k t) -> p k t", k=4), fAB, "act")
    sgs = [_fenced(arena2.ap[:, 14336 + i * 2 * TE:14336 + (i + 1) * 2 * TE].bitcast(F32), fAB, "sg%d" % i) for i in range(2)]
    for ex in range(nexp):
        slot = slots[ex % 2]
        wge = slot.ap[:, 0:4096].rearrange("p (k f) -> p k f", k=8)
        wue = slot.ap[:, 4096:8192].rearrange("p (k f) -> p k f", k=8)
        wde = slot.ap[:, 8192:12288].rearrange("p (k f) -> p k f", k=4)
        P.dma("pool", wge, w_eg[ex].rearrange("(k p) f -> p k f", p=128), writes=[slot])
        P.dma("pool", wue, w_eu[ex].rearrange("(k p) f -> p k f", p=128), writes=[slot])
        P.dma("pool", wde, w_ed[ex].rearrange("(k p) f -> p k f", p=128), writes=[slot])
        for tb in range(NBE):
            t0 = tb * TE
            for ff in range(4):
                pg, pu, sg = (A0, A1)[ff % 2], (B0, B1)[ff % 2], sgs[ff % 2]
                fs = slice(ff * 128, (ff + 1) * 128)
                for kt in range(8):
                    P.pe(lambda e, pg=pg, kt=kt, fs=fs, wge=wge, t0=t0: e.matmul(pg[:, 0:TE], wge[:, kt, fs], h2b[:, kt, t0:t0 + TE],
                                                                              start=(kt == 0), stop=(kt == 7)),
                         reads=[slot, h2b], writes=[pg])
                for kt in range(8):
                    P.pe(lambda e, pu=pu, kt=kt, fs=fs, wue=wue, t0=t0: e.matmul(pu[:, 0:TE], wue[:, kt, fs], h2b[:, kt, t0:t0 + TE],
                                                                              start=(kt == 0), stop=(kt == 7)),
                         reads=[slot, h2b], writes=[pu])
                P.act(lambda e, pg=pg, sg=sg: e.activation(out=sg[:, 0:TE], in_=pg[:, 0:TE], func=AF.Silu), reads=[pg], writes=[sg])
                P.dve(lambda e, pu=pu, sg=sg, ff=ff: e.tensor_tensor(out=act[:, ff, :], in0=pu[:, 0:TE], in1=sg[:, 0:TE], op=ALU.mult),
                      reads=[pu, sg], writes=[act])
            for tt in range(TE // 128):
                ti = tb * (TE // 128) + tt
                for dh in range(2):
                    pd = (G0, G1)[dh]
                    for ff in range(4):
                        P.pe(lambda e, pd=pd, ff=ff, tt=tt, dh=dh, wde=wde: e.matmul(
                            pd[:, 0:512], act[:, ff, tt * 128:(tt + 1) * 128], wde[:, ff, dh * 512:(dh + 1) * 512],
                            start=(ff == 0), stop=(ff == 3)), reads=[act, slot], writes=[pd])
                    if ex == 0:
                        P.dve(lambda e, pd=pd, ti=ti, dh=dh, ex=ex: e.tensor_scalar(
                            out=acc[:, ti, dh * 512:(dh + 1) * 512], in0=pd[:, 0:512], scalar1=Wt[:, ti, ex:ex + 1], scalar2=None,
                            op0=ALU.mult), reads=[pd, Wt], writes=[acc])
                    else:
                        P.dve(lambda e, pd=pd, ti=ti, dh=dh, ex=ex: e.scalar_tensor_tensor(
                            out=acc[:, ti, dh * 512:(dh + 1) * 512], in0=pd[:, 0:512], scalar=Wt[:, ti, ex:ex + 1],
                            in1=acc[:, ti, dh * 512:(dh + 1) * 512], op0=ALU.mult, op1=ALU.add), reads=[pd, Wt, acc], writes=[acc])

    fBC = _fence(P)
    xb2 = a2(0, 4096, F32, 8, "xb2", fBC); v2 = a2(4096, 4096, F32, 8, "v2", fBC); sq2 = a2(8192, 4096, F32, 8, "sq2", fBC)
    outs = []
    for tb in range(NB):
        t0 = tb * TB
        P.dma("sp", xb2[:, :, :], x1r[:, :, t0:t0 + TB], reads=[x1scr], writes=[xb2])
        P.pool(lambda e: e.tensor_scalar(out=xb2[:, :, :], in0=xb2[:, :, :], scalar1=ALPHA, scalar2=None, op0=ALU.mult),
               reads=[xb2], writes=[xb2])
        for ft in range(8):
            pa = (A0, A1)[ft % 2]
            for tt in range(TB // 128):
                ti = tb * (TB // 128) + tt
                P.pe(lambda e, pa=pa, ti=ti, tt=tt, ft=ft: e.matmul(pa[:, tt * 128:(tt + 1) * 128], acc[:, ti, ft * 128:(ft + 1) * 128],
                                                                   ident[:, 0:128], start=True, stop=True),
                     reads=[acc, ident], writes=[pa])
            P.dve(lambda e, pa=pa, ft=ft: e.scalar_tensor_tensor(out=v2[:, ft, :], in0=pa[:, 0:TB], scalar=g1pf[:, ft:ft + 1],
                                                                 in1=xb2[:, ft, :], op0=ALU.mult, op1=ALU.add),
                  reads=[pa, g1pf, xb2], writes=[v2])
        _layernorm(P, v2, sq2, L, R, ones, lntmp, lnv[2], lnv[3], xb2, TB)
        outs.append(P.dma("sp", xoTr[:, :, t0:t0 + TB], xb2[:, :, :], reads=[xb2]))
    counts = P.finish(outs)
    return nc, counts


def _col(vec, n):
    return np.ascontiguousarray(np.asarray(vec).reshape(n, 128).T)


def _consts():
    c = np.zeros((128, 256), np.float32)
    c[:, 0:128] = np.eye(128, dtype=np.float32)
    c[:, 128:256] = 1.0
    return c


def p2_inputs(inp, l, r, S, xT_b, YsT_b, YaT_b):
    b, g = r // 4, r % 4
    NT = S // 4
    sl = slice(g * NT, (g + 1) * NT)
    return {
        "xT": np.ascontiguousarray(xT_b[:, sl]), "YsT": np.ascontiguousarray(YsT_b[:, sl]),
        "YaT": np.ascontiguousarray(YaT_b[:, sl]),
        "ccol": _col(inp["c"][b], 8),
        "w_am": inp["w_ada_mix"][l], "b_am": _col(inp["b_ada_mix"][l], 24),
        "w_af": inp["w_ada_ffn"][l], "b_af": _col(inp["b_ada_ffn"][l], 24),
        "w_g": np.ascontiguousarray(inp["w_in"][l][:, 8224:10272]),
        "w_bs": inp["w_branch_ssd"][l], "w_ba": inp["w_branch_attn"][l], "w_o": inp["w_out"][l],
        "lnp": np.ascontiguousarray(np.concatenate([_col(inp["ln_mix_g"][l], 8), _col(inp["ln_mix_b"][l], 8),
                                                    _col(inp["ln_ffn_g"][l], 8), _col(inp["ln_ffn_b"][l], 8)], axis=1)),
        "w_r": np.ascontiguousarray(np.concatenate([inp["w_router_group"][l], inp["w_router_expert"][l]], axis=1)),
        "b_r": np.ascontiguousarray(np.concatenate([inp["b_router_group"][l], inp["b_router_expert"][l]])[None, :]),
        "w_eg": inp["w_expert_gate"][l], "w_eu": inp["w_expert_up"][l], "w_ed": inp["w_expert_down"][l],
        "cst": _consts(),
    }


C1_ID, C1_ONES, C1_U, C1_UW0, C1_UW1, C1_CM, C1_CB, C1_MISC, C1_N = 0, 128, 256, 384, 640, 896, 1408, 1920, 1928
W1_NF = 2304
PI = float(np.pi)


def _consts1():
    c = np.zeros((128, C1_N), np.float32)
    c[:, C1_ID:C1_ID + 128] = np.eye(128, dtype=np.float32)
    c[:, C1_ONES:C1_ONES + 128] = 1.0
    U = np.triu(np.ones((128, 128), np.float32))
    c[:, C1_U:C1_U + 128] = U
    c[:, C1_UW0:C1_UW0 + 128] = U
    c[:, C1_UW0 + 128:C1_UW0 + 256] = 1.0
    c[:, C1_UW1 + 128:C1_UW1 + 256] = U
    s = np.arange(128)[:, None]
    l = np.arange(256)[None, :]
    m0 = (l >= s).astype(np.float32)
    m1 = (l >= s + 128).astype(np.float32)
    c[:, C1_CM:C1_CM + 256] = m0
    c[:, C1_CM + 256:C1_CM + 512] = m1
    c[:, C1_CB:C1_CB + 256] = (m0 - 1.0) * 1.0e30
    c[:, C1_CB + 256:C1_CB + 512] = (m1 - 1.0) * 1.0e30
    p = np.arange(128)
    inv_freq = (10000.0 ** (-np.arange(0, 64, 2, dtype=np.float32) / 64)).astype(np.float32)
    c[:, C1_MISC + 0] = inv_freq[p % 32]
    c[:, C1_MISC + 1] = np.where((p % 64) < 32, -1.0, 1.0)
    c[:, C1_MISC + 2] = -PI
    return c


def _esel():
    e = np.zeros((32, 32, 128), np.float32)
    for n in range(32):
        e[n, n, :] = 1.0
    return e.reshape(32, 4096)


def build_p1(S):
    nc = bass.Bass("TRN2", target_bir_lowering=False)
    NCH = S // 256
    xT = _din(nc, "xT", [D, S]); ccol = _din(nc, "ccol", [128, 8])
    w_am = _din(nc, "w_am", [D, 3072]); b_am = _din(nc, "b_am", [128, 24])
    w1 = _din(nc, "w1", [D, 2568])
    convw = _din(nc, "convw", [128, 24]); convb = _din(nc, "convb", [128, 6])
    pvec = _din(nc, "pvec", [128, 24])
    pos = _din(nc, "pos", [1, S], I32)
    cst = _din(nc, "cst", [128, C1_N]); esel = _din(nc, "esel", [32, 4096])
    YT = _dout(nc, "YT", [768, S])
    P = Prog(nc)

    big = P.sb("big", [128, max(49152, 20480 + 4 * S)], BF16)
    cs = P.sb("cs", [128, C1_N])
    sc = P.sb("sc", [128, 8]); bam = P.sb("bam", [128, 24]); modm = P.sb("modm", [128, 24]); sc1 = P.sb("sc1", [128, 8])
    cw = P.sb("cw", [128, 24]); cb = P.sb("cb", [128, 6]); pv = P.sb("pv", [128, 24])
    aneg = P.sb("aneg", [128, 8]); wdt = P.sb("wdt", [128, 8, 8])
    banks = [P.ps("bank%d" % i, [128, 512]) for i in range(8)]
    BJ, PM, PG, PY, PS0, PS1, PO, PD = banks

    P.dma("sp", cs[:, :], cst[:, :], writes=[cs])
    P.dma("sp", sc[:, :], ccol[:, :], writes=[sc])
    P.dma("sp", bam[:, :], b_am[:, :], writes=[bam])
    P.dma("sp", cw[:, :], convw[:, :], writes=[cw])
    P.dma("sp", cb[:, :], convb[:, :], writes=[cb])
    P.dma("sp", pv[:, :], pvec[:, :], writes=[pv])
    P.dma("sp", wdt[:, :, :], w1.rearrange("(k p) n -> p k n", p=128)[:, :, 2560:2568], writes=[wdt])
    P.act(lambda e: e.activation(out=sc[:, :], in_=sc[:, :], func=AF.Silu), reads=[sc], writes=[sc])
    P.act(lambda e: e.activation(out=aneg[:, :], in_=pv[:, 16:24], func=AF.Exp), reads=[pv], writes=[aneg])
    P.dve(lambda e: e.tensor_scalar(out=aneg[:, :], in0=aneg[:, :], scalar1=-1.0, scalar2=None, op0=ALU.mult),
          reads=[aneg], writes=[aneg])
    wfull = T(big.ap[:, 0:49152].bitcast(F32).rearrange("p (k f) -> p k f", k=8), "wfull")
    _adaln(P, w_am, bam, sc, wfull, PM, modm)
    P.dve(lambda e: e.tensor_scalar(out=sc1[:, :], in0=modm[:, 8:16], scalar1=1.0, scalar2=None, op0=ALU.add),
          reads=[modm], writes=[sc1])
    f0 = _fence(P)
    wsb = _fenced(big.ap[:, 0:20480].rearrange("p (k f) -> p k f", k=8), f0, "wsb")
    kT_ap = big.ap[:, 20480:20480 + 2 * S].rearrange("p (i t) -> p i t", i=2)
    V_ap = big.ap[:, 20480 + 2 * S:20480 + 4 * S].rearrange("p (n f) -> p n f", f=256)
    kTt = [_fenced(kT_ap, f0, "kT%d" % c) for c in range(NCH)]
    Vt = [_fenced(V_ap, f0, "V%d" % c) for c in range(NCH)]
    for kt in range(8):
        P.dma("pool", wsb[:, kt, :], w1[kt * 128:(kt + 1) * 128, 0:2560], writes=[wsb])

    xc = P.sb("xc", [128, 8, 256]); hTf = xc; hTb = P.sb("hTb", [128, 8, 256], BF16)
    zs = P.sb("zs", [128, 4, 256]); cin = P.sb("cin", [128, 6, 259]); xcv = P.sb("xcv", [128, 6, 256])
    BTb = P.sb("BTb", [128, 256], BF16); CTb = P.sb("CTb", [128, 256], BF16)
    posi = P.sb("posi", [128, 256], I32); ang = P.sb("ang", [128, 256]); tm = P.sb("tm", [128, 256])
    cosT = P.sb("cosT", [128, 256]); sinT = P.sb("sinT", [128, 256])
    tA = P.sb("tA", [128, 256]); tB = P.sb("tB", [128, 256]); cacc = tA
    qTf = P.sb("qTf", [128, 2, 256]); qTb = P.sb("qTb", [128, 2, 256], BF16); kTf = P.sb("kTf", [128, 2, 256])
    kmean = P.sb("kmean", [128, 2, 32]); ksum = P.sb("ksum", [128, 2])
    dtr = P.sb("dtr", [128, 2, 8]); dta_ = P.sb("dta", [128, 2, 8]); dtt = [P.sb("dtt%d" % i, [128, 2, 8]) for i in range(4)]
    cssb = P.sb("cssb", [128, 24]); negcs = P.sb("negcs", [128, 2, 8]); wend = P.sb("wend", [128, 2, 8])
    dtw = P.sb("dtw", [128, 2, 8]); dec = P.sb("dec", [128, 8])
    xtok = P.sb("xtok", [128, 2, 512]); XE = P.sb("XE", [128, 2, 512], BF16); XO = P.sb("XO", [128, 2, 512], BF16)
    xdtw = P.sb("xdtw", [128, 2, 512], BF16); Btok = P.sb("Btok", [128, 2, 128], BF16)
    Gm = P.sb("Gm", [128, 2, 256]); Dm = P.sb("Dm", [128, 2, 256]); dcy = Dm
    scT = [P.sb("scT%d" % i, [128, 2, 256], BF16) for i in range(2)]
    E1 = P.sb("E1", [128, 256]); CE = [P.sb("CE%d" % i, [128, 256], BF16) for i in range(2)]
    state = P.sb("state", [128, 512]); stE = P.sb("stE", [128, 512], BF16); stO = P.sb("stO", [128, 512], BF16)
    yD = P.sb("yD", [128, 256]); gy = P.sb("gy", [128, 4, 256]); sqg = P.sb("sqg", [128, 4, 256])
    rstd = P.sb("rstd", [128, 256]); yout = sqg
    gate = P.sb("gate", [128, 32]); mx8 = P.sb("mx8", [128, 8]); selm = P.sb("selm", [128, 32])
    selb = [P.sb("selb%d" % i, [128, 32]) for i in range(2)]
    selT = P.sb("selT", [32, 4, 256], BF16)
    pT = [P.sb("pT%d" % i, [128, 2, 256], BF16) for i in range(2)]
    rden = P.sb("rden", [64, 256]); yatt = P.sb("yatt", [64, 4, 256])
    onesb = P.sb("onesb", [128, 64], BF16); identb = P.sb("identb", [128, 128], BF16)
    cbias = P.sb("cbias", [128, 2, 256], BF16)

    ident = cs; invf = cs
    def C(off, n):
        return cs[:, off:off + n]

    P.dve(lambda e: e.tensor_copy(onesb[:, :], C(C1_ONES, 64)), reads=[cs], writes=[onesb])
    P.dve(lambda e: e.tensor_copy(identb[:, :], C(C1_ID, 128)), reads=[cs], writes=[identb])
    P.dve(lambda e: e.tensor_copy(cbias[:, :, :].rearrange("p a t -> p (a t)"), C(C1_CB, 512)), reads=[cs], writes=[cbias])
    P.dve(lambda e: e.memset(cin[:, :, :], 0.0), writes=[cin])
    P.pool(lambda e: e.memset(XE[:, :, :], 0.0), writes=[XE])
    P.pool(lambda e: e.memset(XO[:, :, :], 0.0), writes=[XO])
    P.pool(lambda e: e.memset(stE[:, :], 0.0), writes=[stE])
    P.pool(lambda e: e.memset(stO[:, :], 0.0), writes=[stO])
    P.dve(lambda e: e.memset(gate[:, :], NEG), writes=[gate])
    for i in range(2):
        P.dve(lambda e, i=i: e.memset(selb[i][:, :], 0.0), writes=[selb[i]])

    xTr = xT.rearrange("(k p) t -> p k t", p=128)
    YsO = YT[0:512, :].rearrange("(i p) t -> p i t", p=128)
    YaO = YT[512:768, :].rearrange("(h d) t -> d h t", d=64)
    outs = []

    def bc8(ap):
        return ap.rearrange("p (r o) -> p r o", o=1).to_broadcast([128, 8, 64])

    def do_chunk(c):
        t0 = c * 256
        P.dma("sp", xc[:, :, :], xTr[:, :, t0:t0 + 256], writes=[xc])
        P.dma("sp", posi[:, :], pos[0:1, t0:t0 + 256].to_broadcast([128, 256]), writes=[posi])
        P.dve(lambda e: e.tensor_tensor(out=hTf[:, :, :], in0=xc[:, :, :],
                                        in1=sc1[:, 0:8].rearrange("p (k o) -> p k o", o=1).to_broadcast([128, 8, 256]), op=ALU.mult),
              reads=[xc, sc1], writes=[xc])
        P.dve(lambda e: e.tensor_tensor(out=hTf[:, :, :], in0=hTf[:, :, :],
                                        in1=modm[:, 0:8].rearrange("p (k o) -> p k o", o=1).to_broadcast([128, 8, 256]), op=ALU.add),
              reads=[hTf, modm], writes=[hTf])
        P.act(lambda e: e.copy(hTb[:, :, :], hTf[:, :, :]), reads=[hTf], writes=[hTb])
        P.dve(lambda e: e.tensor_copy(ang[:, :], posi[:, :]), reads=[posi], writes=[ang])
        P.dve(lambda e: e.tensor_scalar(out=ang[:, :], in0=ang[:, :], scalar1=cs[:, C1_MISC:C1_MISC + 1], scalar2=None, op0=ALU.mult),
              reads=[ang, cs], writes=[ang])
        C1_, C2_ = 6.28125, 2 * PI - 6.28125
        P.dve(lambda e: e.tensor_scalar(out=tm[:, :], in0=ang[:, :], scalar1=1.0 / (2 * PI), scalar2=None, op0=ALU.mult), reads=[ang], writes=[tm])
        P.dve(lambda e: e.tensor_copy(posi[:, :], tm[:, :]), reads=[tm], writes=[posi])
        P.dve(lambda e: e.tensor_copy(tm[:, :], posi[:, :]), reads=[posi], writes=[tm])
        P.dve(lambda e: e.scalar_tensor_tensor(out=ang[:, :], in0=tm[:, :], scalar=-C1_, in1=ang[:, :], op0=ALU.mult, op1=ALU.add),
              reads=[tm, ang], writes=[ang])
        P.dve(lambda e: e.scalar_tensor_tensor(out=ang[:, :], in0=tm[:, :], scalar=-C2_, in1=ang[:, :], op0=ALU.mult, op1=ALU.add),
              reads=[tm, ang], writes=[ang])
        for (shift, dstT) in ((0.0, sinT), (0.5 * PI, cosT)):
            if shift != 0.0:
                P.dve(lambda e, shift=shift: e.tensor_scalar(out=ang[:, :], in0=ang[:, :], scalar1=shift, scalar2=None, op0=ALU.add),
                      reads=[ang], writes=[ang])
            P.dve(lambda e: e.tensor_scalar(out=tm[:, :], in0=ang[:, :], scalar1=PI, scalar2=-2 * PI, op0=ALU.is_gt, op1=ALU.mult),
                  reads=[ang], writes=[tm])
            P.dve(lambda e: e.tensor_tensor(out=ang[:, :], in0=ang[:, :], in1=tm[:, :], op=ALU.add), reads=[ang, tm], writes=[ang])
            P.dve(lambda e: e.tensor_scalar(out=tm[:, :], in0=ang[:, :], scalar1=-PI, scalar2=2 * PI, op0=ALU.is_lt, op1=ALU.mult),
                  reads=[ang], writes=[tm])
            P.dve(lambda e: e.tensor_tensor(out=ang[:, :], in0=ang[:, :], in1=tm[:, :], op=ALU.add), reads=[ang, tm], writes=[ang])
            P.act(lambda e, dstT=dstT: e.activation(out=dstT[:, :], in_=ang[:, :], func=AF.Sin), reads=[ang], writes=[dstT])
        P.dve(lambda e: e.tensor_scalar(out=sinT[:, :], in0=sinT[:, :], scalar1=cs[:, C1_MISC + 1:C1_MISC + 2], scalar2=None, op0=ALU.mult),
              reads=[sinT, cs], writes=[sinT])

        def proj(j, half):
            for kt in range(8):
                P.pe(lambda e, kt=kt: e.matmul(BJ[:, half * 256:(half + 1) * 256], wsb[:, kt, j * 128:(j + 1) * 128], hTb[:, kt, :],
                                               start=(kt == 0), stop=(kt == 7)), reads=[wsb, hTb], writes=[BJ])
        for j in range(4):
            proj(j, j % 2)
            P.act(lambda e, j=j: e.activation(out=zs[:, j, :], in_=BJ[:, (j % 2) * 256:(j % 2 + 1) * 256], func=AF.Silu),
                  reads=[BJ], writes=[zs])
        for jj in range(6):
            proj(4 + jj, jj % 2)
            P.act(lambda e, jj=jj: e.copy(cin[:, jj, 3:259], BJ[:, (jj % 2) * 256:(jj % 2 + 1) * 256]), reads=[BJ], writes=[cin])
        for i in range(2):
            for (jq, js, dst) in ((10 + i, 14 + i, "q"), (12 + i, 16 + i, "k")):
                proj(jq, 0)
                proj(js, 1)
                P.dve(lambda e: e.tensor_tensor(out=tA[:, :], in0=BJ[:, 0:256], in1=cosT[:, :], op=ALU.mult),
                      reads=[BJ, cosT], writes=[tA])
                P.dve(lambda e: e.tensor_tensor(out=tB[:, :], in0=BJ[:, 256:512], in1=sinT[:, :], op=ALU.mult),
                      reads=[BJ, sinT], writes=[tB])
                if dst == "q":
                    P.pool(lambda e, i=i: e.tensor_tensor(out=qTf[:, i, :], in0=tA[:, :], in1=tB[:, :], op=ALU.add),
                           reads=[tA, tB], writes=[qTf])
                    P.act(lambda e, i=i: e.mul(qTb[:, i, :], qTf[:, i, :], 0.125), reads=[qTf], writes=[qTb])
                else:
                    P.pool(lambda e, i=i: e.tensor_tensor(out=kTf[:, i, :], in0=tA[:, :], in1=tB[:, :], op=ALU.add),
                           reads=[tA, tB], writes=[kTf])
                    P.act(lambda e, i=i: e.copy(kT_ap[:, i, t0:t0 + 256], kTf[:, i, :]), reads=[kTf], writes=[kTt[c]])
        P.dve(lambda e: e.reduce_sum(out=ksum[:, 0:2], in_=kTf[:, :, :], axis=AX.X), reads=[kTf], writes=[ksum])
        P.dve(lambda e: e.tensor_scalar(out=kmean[:, :, c], in0=ksum[:, 0:2], scalar1=1.0 / 256, scalar2=None, op0=ALU.mult),
              reads=[ksum], writes=[kmean])
        for tt in range(2):
            for kt in range(8):
                P.pe(lambda e, kt=kt, tt=tt: e.matmul(BJ[:, tt * 256:(tt + 1) * 256], hTb[:, kt, tt * 128:(tt + 1) * 128], wsb[:, kt, 2304:2560],
                                                     start=(kt == 0), stop=(kt == 7)), reads=[hTb, wsb], writes=[BJ])
            P.act(lambda e, tt=tt: e.copy(V_ap[:, 2 * c + tt, :], BJ[:, tt * 256:(tt + 1) * 256]), reads=[BJ], writes=[Vt[c]])
        for tt in range(2):
            for kt in range(8):
                P.pe(lambda e, kt=kt, tt=tt: e.matmul(PM[:, tt * 8:(tt + 1) * 8], hTf[:, kt, tt * 128:(tt + 1) * 128], wdt[:, kt, :],
                                                     start=(kt == 0), stop=(kt == 7)), reads=[hTf, wdt], writes=[PM])
        a_, ab_, e_, l_ = dtt
        P.dve(lambda e: e.tensor_tensor(out=a_[:, :, :], in0=PM[:, 0:16].rearrange("p (t r) -> p t r", t=2),
                                        in1=pv[:, 8:16].rearrange("p (o r) -> p o r", o=1).to_broadcast([128, 2, 8]), op=ALU.add),
              reads=[PM, pv], writes=[a_])
        P.dve(lambda e: e.tensor_scalar(out=ab_[:, :, :], in0=a_[:, :, :], scalar1=-1.0, scalar2=None, op0=ALU.mult), reads=[a_], writes=[ab_])
        P.dve(lambda e: e.tensor_tensor(out=ab_[:, :, :], in0=ab_[:, :, :], in1=a_[:, :, :], op=ALU.min), reads=[a_, ab_], writes=[ab_])
        P.act(lambda e: e.activation(out=e_[:, :, :], in_=ab_[:, :, :], func=AF.Exp), reads=[ab_], writes=[e_])
        P.dve(lambda e: e.tensor_scalar(out=e_[:, :, :], in0=e_[:, :, :], scalar1=1.0, scalar2=None, op0=ALU.add), reads=[e_], writes=[e_])
        P.act(lambda e: e.activation(out=l_[:, :, :], in_=e_[:, :, :], func=AF.Ln), reads=[e_], writes=[l_])
        P.dve(lambda e: e.tensor_scalar(out=a_[:, :, :], in0=a_[:, :, :], scalar1=0.0, scalar2=None, op0=ALU.max), reads=[a_], writes=[a_])
        P.dve(lambda e: e.tensor_tensor(out=dtr[:, :, :], in0=a_[:, :, :], in1=l_[:, :, :], op=ALU.add), reads=[a_, l_], writes=[dtr])
        P.dve(lambda e: e.tensor_tensor(out=dta_[:, :, :], in0=dtr[:, :, :],
                                        in1=aneg[:, 0:8].rearrange("p (o r) -> p o r", o=1).to_broadcast([128, 2, 8]), op=ALU.mult),
              reads=[dtr, aneg], writes=[dta_])
        for jj in range(6):
            P.dve(lambda e, jj=jj: e.tensor_scalar(out=cacc[:, :], in0=cin[:, jj, 0:256], scalar1=cw[:, jj * 4:jj * 4 + 1], scalar2=None, op0=ALU.mult),
                  reads=[cin, cw], writes=[cacc])
            for k in range(1, 4):
                P.dve(lambda e, jj=jj, k=k: e.scalar_tensor_tensor(out=cacc[:, :], in0=cin[:, jj, k:k + 256], scalar=cw[:, jj * 4 + k:jj * 4 + k + 1],
                                                                  in1=cacc[:, :], op0=ALU.mult, op1=ALU.add), reads=[cin, cw, cacc], writes=[cacc])
            P.act(lambda e, jj=jj: e.activation(out=xcv[:, jj, :], in_=cacc[:, :], func=AF.Silu, bias=cb[:, jj:jj + 1], scale=1.0),
                  reads=[cacc, cb], writes=[xcv])
        P.pool(lambda e: e.tensor_copy(cin[:, :, 0:3], cin[:, :, 256:259]), reads=[cin], writes=[cin])
        P.act(lambda e: e.copy(BTb[:, :], xcv[:, 4, :]), reads=[xcv], writes=[BTb])
        P.act(lambda e: e.copy(CTb[:, :], xcv[:, 5, :]), reads=[xcv], writes=[CTb])
        Uc = C(C1_U, 128); On = C(C1_ONES, 128)
        P.pe(lambda e: e.matmul(PM[:, 32:40], Uc, dta_[:, 0, :], start=True, stop=True), reads=[cs, dta_], writes=[PM])
        P.pe(lambda e: e.matmul(PM[:, 40:48], On, dta_[:, 0, :], start=True, stop=False), reads=[cs, dta_], writes=[PM])
        P.pe(lambda e: e.matmul(PM[:, 40:48], Uc, dta_[:, 1, :], start=False, stop=True), reads=[cs, dta_], writes=[PM])
        P.pe(lambda e: e.matmul(PM[:, 48:56], On, dta_[:, 0, :], start=True, stop=False), reads=[cs, dta_], writes=[PM])
        P.pe(lambda e: e.matmul(PM[:, 48:56], On, dta_[:, 1, :], start=False, stop=True), reads=[cs, dta_], writes=[PM])
        P.dve(lambda e: e.tensor_copy(cssb[:, 0:24], PM[:, 32:56]), reads=[PM], writes=[cssb])
        csv = cssb[:, 0:16].rearrange("p (t r) -> p t r", t=2)
        clb = cssb[:, 16:24].rearrange("p (o r) -> p o r", o=1).to_broadcast([128, 2, 8])
        P.dve(lambda e: e.tensor_scalar(out=negcs[:, :, :], in0=csv, scalar1=-1.0, scalar2=None, op0=ALU.mult), reads=[cssb], writes=[negcs])
        P.dve(lambda e: e.tensor_tensor(out=wend[:, :, :], in0=clb, in1=csv, op=ALU.subtract), reads=[cssb], writes=[wend])
        P.act(lambda e: e.activation(out=wend[:, :, :], in_=wend[:, :, :], func=AF.Exp), reads=[wend], writes=[wend])
        P.dve(lambda e: e.tensor_tensor(out=dtw[:, :, :], in0=dtr[:, :, :], in1=wend[:, :, :], op=ALU.mult), reads=[dtr, wend], writes=[dtw])
        P.act(lambda e: e.activation(out=dec[:, :], in_=cssb[:, 16:24], func=AF.Exp), reads=[cssb], writes=[dec])
        for tt in range(2):
            for jj in range(4):
                P.pe(lambda e, tt=tt, jj=jj: e.matmul(PG[:, jj * 128:(jj + 1) * 128], xcv[:, jj, tt * 128:(tt + 1) * 128], C(C1_ID, 128),
                                                     start=True, stop=True), reads=[xcv, cs], writes=[PG])
            P.act(lambda e, tt=tt: e.copy(xtok[:, tt, :], PG[:, 0:512]), reads=[PG], writes=[xtok])
        for tt in range(2):
            P.pe(lambda e, tt=tt: e.matmul(PM[:, 64 + tt * 128:64 + (tt + 1) * 128], xcv[:, 4, tt * 128:(tt + 1) * 128], C(C1_ID, 128),
                                           start=True, stop=True), reads=[xcv, cs], writes=[PM])
        P.act(lambda e: e.copy(Btok[:, :, :].rearrange("p t n -> p (t n)"), PM[:, 64:320]), reads=[PM], writes=[Btok])
        for tt in range(2):
            xv = xtok[:, tt, :].rearrange("p (i two q) -> p i two q", two=2, q=64)
            dv = dtr[:, tt, :].rearrange("p (i two) -> p i two", two=2)
            for par, X in ((0, XE), (1, XO)):
                P.dve(lambda e, tt=tt, par=par, X=X, xv=xv, dv=dv: e.tensor_tensor(
                    out=X[:, tt, :].rearrange("p (i two q) -> p i two q", two=2, q=64)[:, :, par, :],
                    in0=xv[:, :, par, :], in1=dv[:, :, par:par + 1].to_broadcast([128, 4, 64]), op=ALU.mult),
                    reads=[xtok, dtr], writes=[X])
            P.pool(lambda e, tt=tt: e.tensor_tensor(out=xdtw[:, tt, :].rearrange("p (r q) -> p r q", q=64),
                                                   in0=xtok[:, tt, :].rearrange("p (r q) -> p r q", q=64),
                                                   in1=bc8(dtw[:, tt, :]), op=ALU.mult), reads=[xtok, dtw], writes=[xdtw])
        for st in range(2):
            P.pe(lambda e, st=st: e.matmul(PG[:, st * 256:(st + 1) * 256], BTb[:, st * 128:(st + 1) * 128], CTb[:, 0:256],
                                           start=True, stop=True), reads=[BTb, CTb], writes=[PG])
        P.dve(lambda e: e.tensor_tensor(out=Gm[:, :, :].rearrange("p a t -> p (a t)"), in0=PG[:, 0:512], in1=C(C1_CM, 512), op=ALU.mult),
              reads=[PG, cs], writes=[Gm])
        for r in range(8):
            i, par = r // 2, r % 2
            sc_, ce_ = scT[r % 2], CE[r % 2]
            PT = PM
            P.pe(lambda e, r=r: e.matmul(PT[:, 256:512], dta_[:, 0, r:r + 1].to_broadcast([128, 128]), C(C1_UW0, 256), start=True, stop=False),
                 reads=[dta_, cs], writes=[PM])
            P.pe(lambda e, r=r: e.matmul(PT[:, 256:512], dta_[:, 1, r:r + 1].to_broadcast([128, 128]), C(C1_UW1, 256), start=False, stop=True),
                 reads=[dta_, cs], writes=[PM])
            for st in range(2):
                P.dve(lambda e, r=r, st=st: e.tensor_scalar(out=Dm[:, st, :], in0=PT[:, 256:512], scalar1=negcs[:, st, r:r + 1], scalar2=0.0,
                                                           op0=ALU.add, op1=ALU.min), reads=[PM, negcs], writes=[Dm])
            P.act(lambda e: e.activation(out=dcy[:, :, :], in_=Dm[:, :, :], func=AF.Exp), reads=[Dm], writes=[Dm])
            P.pool(lambda e, sc_=sc_: e.tensor_tensor(out=sc_[:, :, :], in0=Gm[:, :, :], in1=dcy[:, :, :], op=ALU.mult),
                   reads=[Gm, dcy], writes=[sc_])
            if c > 0:
                P.act(lambda e: e.activation(out=E1[:, :], in_=PT[:, 256:512], func=AF.Exp), reads=[PM], writes=[E1])
                P.pool(lambda e, ce_=ce_: e.tensor_tensor(out=ce_[:, :], in0=xcv[:, 5, :], in1=E1[:, :], op=ALU.mult),
                       reads=[xcv, E1], writes=[ce_])
            X = XE if par == 0 else XO
            st_ = stE if par == 0 else stO
            pys = slice((i % 2) * 256, (i % 2 + 1) * 256)
            P.pe(lambda e, X=X, i=i, sc_=sc_, pys=pys, par=par: e.matmul(PY[:, pys], X[:, 0, i * 128:(i + 1) * 128], sc_[:, 0, :],
                                                                      start=(par == 0), stop=False), reads=[X, sc_], writes=[PY])
            P.pe(lambda e, X=X, i=i, sc_=sc_, pys=pys, par=par: e.matmul(PY[:, pys], X[:, 1, i * 128:(i + 1) * 128], sc_[:, 1, :],
                                                                      start=False, stop=(par == 1 and c == 0)), reads=[X, sc_], writes=[PY])
            if c > 0:
                P.pe(lambda e, st_=st_, i=i, ce_=ce_, pys=pys, par=par: e.matmul(PY[:, pys], st_[:, i * 128:(i + 1) * 128], ce_[:, :],
                                                                              start=False, stop=(par == 1)), reads=[st_, ce_], writes=[PY])
            if par == 1:
                P.dve(lambda e, i=i, pys=pys: e.scalar_tensor_tensor(out=yD[:, :], in0=xcv[:, i, :], scalar=pv[:, i:i + 1], in1=PY[:, pys],
                                                                    op0=ALU.mult, op1=ALU.add), reads=[xcv, pv, PY], writes=[yD])
                P.pool(lambda e, i=i: e.tensor_tensor(out=gy[:, i, :], in0=yD[:, :], in1=zs[:, i, :], op=ALU.mult),
                       reads=[yD, zs], writes=[gy])
        P.act(lambda e: e.activation(out=sqg[:, :, :], in_=gy[:, :, :], func=AF.Square), reads=[gy], writes=[sqg])
        for i in range(4):
            P.pe(lambda e, i=i: e.matmul(BJ[:, 0:256], C(C1_ONES, 128), sqg[:, i, :], start=(i == 0), stop=(i == 3)),
                 reads=[cs, sqg], writes=[BJ])
        P.dve(lambda e: e.tensor_scalar(out=rstd[:, :], in0=BJ[:, 0:256], scalar1=1.0 / 512, scalar2=EPS, op0=ALU.mult, op1=ALU.add),
              reads=[BJ], writes=[rstd])
        P.act(lambda e: e.activation(out=rstd[:, :], in_=rstd[:, :], func=AF.Sqrt), reads=[rstd], writes=[rstd])
        P.dve(lambda e: e.reciprocal(out=rstd[:, :], in_=rstd[:, :]), reads=[rstd], writes=[rstd])
        for i in range(4):
            P.dve(lambda e, i=i: e.scalar_tensor_tensor(out=yout[:, i, :], in0=gy[:, i, :], scalar=pv[:, 4 + i:5 + i], in1=rstd[:, :],
                                                       op0=ALU.mult, op1=ALU.mult), reads=[gy, pv, rstd], writes=[yout])
        outs.append(P.dma("sp", YsO[:, :, t0:t0 + 256], yout[:, :, :], reads=[yout]))
        if c < NCH - 1:
            for tt in range(2):
                P.pe(lambda e, tt=tt: e.matmul(PG[:, 0:512], Btok[:, tt, :], xdtw[:, tt, :], start=(tt == 0), stop=(tt == 1)),
                     reads=[Btok, xdtw], writes=[PG])
            if c == 0:
                P.dve(lambda e: e.tensor_copy(state[:, :], PG[:, 0:512]), reads=[PG], writes=[state])
            else:
                P.pool(lambda e: e.tensor_tensor(out=state[:, :].rearrange("p (r q) -> p r q", q=64),
                                                in0=state[:, :].rearrange("p (r q) -> p r q", q=64), in1=bc8(dec[:, 0:8]), op=ALU.mult),
                       reads=[state, dec], writes=[state])
                P.dve(lambda e: e.tensor_tensor(out=state[:, :], in0=state[:, :], in1=PG[:, 0:512], op=ALU.add), reads=[state, PG], writes=[state])
            sv = state[:, :].rearrange("p (i two q) -> p i two q", two=2, q=64)
            P.act(lambda e, sv=sv: e.copy(stE[:, :].rearrange("p (i two q) -> p i two q", two=2, q=64)[:, :, 0, :], sv[:, :, 0, :]),
                  reads=[state], writes=[stE])
            P.act(lambda e, sv=sv: e.copy(stO[:, :].rearrange("p (i two q) -> p i two q", two=2, q=64)[:, :, 1, :], sv[:, :, 1, :]),
                  reads=[state], writes=[stO])
        use_gate = c > 3
        if use_gate:
            for h in range(4):
                i, po = h // 2, (h % 2) * 64
                for tt in range(2):
                    sb_ = selb[tt]
                    P.pe(lambda e, i=i, po=po, tt=tt: e.matmul(PM[:, 0:c], qTf[po:po + 64, i, tt * 128:(tt + 1) * 128], kmean[po:po + 64, i, 0:c],
                                                              start=True, stop=True), reads=[qTf, kmean], writes=[PM])
                    P.dve(lambda e: e.tensor_copy(gate[:, 0:c], PM[:, 0:c]), reads=[PM], writes=[gate])
                    P.dve(lambda e: e.max(out=mx8[:, 0:8], in_=gate[:, 0:32]), reads=[gate], writes=[mx8])
                    P.dve(lambda e: e.tensor_scalar(out=selm[:, 0:c], in0=gate[:, 0:c], scalar1=mx8[:, 2:3], scalar2=None, op0=ALU.is_ge),
                          reads=[gate, mx8], writes=[selm])
                    P.dve(lambda e, sb_=sb_: e.tensor_scalar(out=sb_[:, 0:c], in0=selm[:, 0:c], scalar1=-1.0, scalar2=-NEG, op0=ALU.add, op1=ALU.mult),
                          reads=[selm], writes=[sb_])
                    P.pe(lambda e, sb_=sb_, tt=tt: e.matmul(PM[0:32, 64 + tt * 128:64 + (tt + 1) * 128], sb_[:, 0:32], C(C1_ID, 128), start=True, stop=True),
                         reads=[sb_, cs], writes=[PM])
                P.act(lambda e, h=h: e.copy(selT[:, h, :], PM[0:32, 64:320]), reads=[PM], writes=[selT])
        for h in range(4):
            i, po = h // 2, (h % 2) * 64
            for n in range(c + 1):
                PSx = (PS0, PS1)[n % 2]
                pTx = pT[n % 2]
                own = (n == c)
                for kt in range(2):
                    ks = slice(n * 256 + kt * 128, n * 256 + (kt + 1) * 128)
                    has_bias = own or use_gate
                    P.pe(lambda e, PSx=PSx, kt=kt, ks=ks, i=i, po=po, has_bias=has_bias: e.matmul(
                        PSx[:, kt * 256:(kt + 1) * 256], kT_ap[po:po + 64, i, ks], qTb[po:po + 64, i, :], start=True, stop=(not has_bias)),
                        reads=[kTt[n], qTb], writes=[PSx])
                    if own:
                        P.pe(lambda e, PSx=PSx, kt=kt: e.matmul(PSx[:, kt * 256:(kt + 1) * 256], identb[:, :], cbias[:, kt, :], start=False, stop=True),
                             reads=[identb, cbias], writes=[PSx])
                    elif use_gate:
                        P.pe(lambda e, PSx=PSx, kt=kt, n=n, h=h: e.matmul(PSx[:, kt * 256:(kt + 1) * 256], identb[0:32, n:n + 1].to_broadcast([32, 128]), selT[:, h, :],
                                                                        start=False, stop=True), reads=[identb, selT], writes=[PSx])
                P.act(lambda e, PSx=PSx, pTx=pTx: e.activation(out=pTx[:, :, :].rearrange("p a t -> p (a t)"), in_=PSx[:, 0:512], func=AF.Exp),
                      reads=[PSx], writes=[pTx])
                for kt in range(2):
                    first = (n == 0 and kt == 0)
                    last = (n == c and kt == 1)
                    P.pe(lambda e, pTx=pTx, kt=kt, n=n, h=h, first=first, last=last: e.matmul(
                        PO[0:64, 0:256], V_ap[:, 2 * n + kt, h * 64:(h + 1) * 64], pTx[:, kt, :], start=first, stop=last),
                        reads=[Vt[n], pTx], writes=[PO])
                    P.pe(lambda e, pTx=pTx, kt=kt, first=first, last=last: e.matmul(
                        PD[0:64, 0:256], onesb[:, 0:64], pTx[:, kt, :], start=first, stop=last), reads=[onesb, pTx], writes=[PD])
            P.dve(lambda e: e.reciprocal(out=rden[:, :], in_=PD[0:64, 0:256]), reads=[PD], writes=[rden])
            P.dve(lambda e, h=h: e.tensor_tensor(out=yatt[:, h, :], in0=PO[0:64, 0:256], in1=rden[:, :], op=ALU.mult),
                  reads=[PO, rden], writes=[yatt])
        outs.append(P.dma("sp", YaO[:, :, t0:t0 + 256], yatt[:, :, :], reads=[yatt]))
    for c in range(NCH):
        do_chunk(c)
    counts = P.finish(outs)
    return nc, counts


def p1_inputs(inp, l, r, S, xT_b):
    b, g = r // 4, r % 4
    w = inp["w_in"][l]
    hq = 5152 + 4 * g * 64
    hk = 5152 + 1024 + 4 * g * 64
    hv = 5152 + 2048 + 4 * g * 64
    swap = np.concatenate([np.arange(h * 64 + 32, h * 64 + 64).tolist() + np.arange(h * 64, h * 64 + 32).tolist() for h in range(4)])
    w1 = np.concatenate([
        w[:, g * 512:(g + 1) * 512],
        w[:, 2048 + g * 512:2048 + (g + 1) * 512],
        w[:, 4096 + g * 128:4096 + (g + 1) * 128],
        w[:, 4608 + g * 128:4608 + (g + 1) * 128],
        w[:, hq:hq + 256], w[:, hk:hk + 256],
        w[:, hq:hq + 256][:, swap], w[:, hk:hk + 256][:, swap],
        w[:, hv:hv + 256],
        w[:, 5120 + g * 8:5120 + (g + 1) * 8],
    ], axis=1)
    ch = np.concatenate([g * 512 + np.arange(512), 2048 + g * 128 + np.arange(128), 2560 + g * 128 + np.arange(128)])
    cwl = inp["conv_w"][l][:, ch]
    convw = np.ascontiguousarray(cwl.reshape(4, 6, 128).transpose(2, 1, 0).reshape(128, 24))
    convb = np.ascontiguousarray(inp["conv_b"][l][ch].reshape(6, 128).T)
    heads = g * 8 + np.arange(8)
    p = np.arange(128)
    dvec = np.stack([inp["d_skip"][l][g * 8 + 2 * i + (p >= 64)] for i in range(4)], axis=1)
    normw = inp["ssd_norm_w"][l][g * 512:(g + 1) * 512].reshape(4, 128).T
    dtb = np.broadcast_to(inp["dt_bias"][l][heads][None, :], (128, 8))
    alog = np.broadcast_to(inp["a_log"][l][heads][None, :], (128, 8))
    pvec = np.ascontiguousarray(np.concatenate([dvec, normw, dtb, alog], axis=1).astype(np.float32))
    return {
        "xT": xT_b, "ccol": _col(inp["c"][b], 8),
        "w_am": inp["w_ada_mix"][l], "b_am": _col(inp["b_ada_mix"][l], 24),
        "w1": np.ascontiguousarray(w1), "convw": convw, "convb": convb, "pvec": pvec,
        "pos": np.ascontiguousarray(inp["positions"][b, :S][None, :]),
        "cst": _consts1(), "esel": _esel(),
    }


_NC_CACHE = {}


def _get_nc(kind, n):
    key = (kind, n)
    if key not in _NC_CACHE:
        _NC_CACHE[key] = (build_p1(n) if kind == "p1" else build_p2(n))[0]
    return _NC_CACHE[key]


def _run(nc, maps):
    import concourse.bass_utils as _bu
    return _bu.run_bass_kernel_spmd(nc, maps, core_ids=list(range(8))).results


def kernel(**inputs):
    inp = {k: np.asarray(v) for k, v in inputs.items()}
    B, S, _ = inp["x"].shape
    L = inp["w_in"].shape[0]
    assert B == 2
    nc1 = _get_nc("p1", S)
    nc2 = _get_nc("p2", S // 4)
    xT = [np.ascontiguousarray(inp["x"][b].T) for b in range(2)]
    for l in range(L):
        res = _run(nc1, [p1_inputs(inp, l, r, S, xT[r // 4]) for r in range(8)])
        YsT = [np.ascontiguousarray(np.concatenate([res[b * 4 + g]["YT"][0:512] for g in range(4)], axis=0)) for b in range(2)]
        YaT = [np.ascontiguousarray(np.concatenate([res[b * 4 + g]["YT"][512:768] for g in range(4)], axis=0)) for b in range(2)]
        res = _run(nc2, [p2_inputs(inp, l, r, S, xT[r // 4], YsT[r // 4], YaT[r // 4]) for r in range(8)])
        xT = [np.ascontiguousarray(np.concatenate([res[b * 4 + g]["xoT"] for g in range(4)], axis=1)) for b in range(2)]
    out = np.stack([xT[b].T for b in range(2)], axis=0)
    return np.ascontiguousarray(out).astype(np.float32)
```

```python
import numpy as np
import concourse.bass as bass
import concourse.mybir as mybir
from concourse.bass_utils import run_bass_kernel_spmd

F32 = mybir.dt.float32
BF16 = mybir.dt.bfloat16
I32 = mybir.dt.int32
AF = mybir.ActivationFunctionType
ALU = mybir.AluOpType
AX = mybir.AxisListType

ENGS = ("pe", "act", "dve", "pool", "sp")
DMA_POOL = 8


class T:
    __slots__ = ("ap", "w", "r", "name")

    def __init__(self, ap, name=""):
        self.ap = ap
        self.w = None
        self.r = []
        self.name = name

    def __getitem__(self, k):
        return self.ap[k]


class Op:
    __slots__ = ("eng", "fn", "deps", "dma", "idx", "needed", "sem", "val", "slot_prev", "inc")

    def __init__(self, eng, fn, deps, dma):
        self.eng = eng
        self.fn = fn
        self.deps = deps
        self.dma = dma
        self.needed = False
        self.sem = None
        self.val = None
        self.slot_prev = None
        self.inc = 16


class Prog:
    def __init__(self, nc):
        self.nc = nc
        self.ops = {e: [] for e in ENGS}
        self.dma_count = {e: 0 for e in ENGS + ("cc",)}
        self.dma_slots = {e: [None] * DMA_POOL for e in ENGS + ("cc",)}
        self._ctx = []
        self.arena = None
        self.off = 0
        self.fence = []
        self._rank = {}

    def rank(self, engine):
        k = id(engine)
        if k not in self._rank:
            self._rank[k] = engine.snap(engine.partition_id() % 4, min_val=0, max_val=3)
        return self._rank[k]

    def use_arena(self, nbytes):
        g = self.nc.sbuf_tensor("arena_all", [128, nbytes // 2], BF16)
        self.arena = g.__enter__()
        self._ctx.append(g)
        self.arena_bytes = nbytes

    def begin_phase(self):
        self.off = 0
        self.fence = _fence(self)

    def sb(self, name, shape, dt=F32):
        if self.arena is not None:
            esz = 2 if dt == BF16 else 4
            free = 1
            for d_ in shape[1:]:
                free *= d_
            nb = (free * esz + 63) // 64 * 64
            assert self.off + nb <= self.arena_bytes, "SBUF arena overflow at %s (%d + %d)" % (name, self.off, nb)
            ap = self.arena[0:shape[0], self.off // 2:self.off // 2 + free * esz // 2]
            self.off += nb
            if dt != BF16:
                ap = ap.bitcast(dt)
            if len(shape) == 3:
                ap = ap.rearrange("p (a b) -> p a b", a=shape[1])
            t = T(ap, name)
            t.r = list(self.fence)
            return t
        g = self.nc.sbuf_tensor(name, list(shape), dt)
        t = g.__enter__()
        self._ctx.append(g)
        return T(t, name)

    def ps(self, name, shape, dt=F32):
        g = self.nc.psum_tensor(name, list(shape), dt)
        t = g.__enter__()
        self._ctx.append(g)
        return T(t, name)

    def alias(self, ap, name=""):
        return T(ap, name)

    def add(self, eng, fn, reads=(), writes=(), dma=False, inc=16, semgroup=None):
        deps = []
        for t in reads:
            if t.w is not None:
                deps.append((t.w, True))
        for t in writes:
            if t.w is not None:
                deps.append((t.w, False))
            for r in t.r:
                deps.append((r, False))
        op = Op(eng, fn, [], dma)
        op.inc = inc
        seen = set()
        for d, raw in deps:
            if d is op or id(d) in seen:
                continue
            if d.eng == eng and not d.dma and not dma:
                if eng == "pe" or not raw:
                    continue
            seen.add(id(d))
            op.deps.append(d)
            d.needed = True
        if dma:
            sg = semgroup or eng
            k = self.dma_count[sg] % DMA_POOL
            self.dma_count[sg] += 1
            prev = self.dma_slots[sg][k]
            op.slot_prev = prev
            if prev is not None:
                prev.needed = True
            self.dma_slots[sg][k] = op
            op.sem = (sg, k)
            op.needed = True
        op.idx = len(self.ops[eng])
        self.ops[eng].append(op)
        for t in reads:
            t.r.append(op)
        for t in writes:
            t.w = op
            t.r = []
        return op

    def pe(self, fn, reads=(), writes=()):
        return self.add("pe", fn, reads, writes)

    def act(self, fn, reads=(), writes=()):
        return self.add("act", fn, reads, writes)

    def dve(self, fn, reads=(), writes=()):
        return self.add("dve", fn, reads, writes)

    def pool(self, fn, reads=(), writes=()):
        return self.add("pool", fn, reads, writes)

    def dma(self, eng, out_ap, in_ap, reads=(), writes=(), **kw):
        return self.add(eng, lambda e: e.dma_start(out=out_ap, in_=in_ap, **kw), reads, writes, dma=True)

    def finish(self, final_waits=()):
        nc = self.nc
        sem_objs = {}
        stack = []

        def getsem(key):
            if key not in sem_objs:
                g = nc.semaphore("s_%s_%s" % key if isinstance(key, tuple) else "s_%s" % key)
                sem_objs[key] = g.__enter__()
                stack.append(g)
            return sem_objs[key]

        for e in ENGS:
            cnt = 0
            dcnt = {}
            for op in self.ops[e]:
                if op.dma:
                    dcnt[op.sem] = dcnt.get(op.sem, 0) + op.inc
                    op.val = dcnt[op.sem]
                elif op.needed:
                    cnt += 1
                    op.sem = e
                    op.val = cnt
        for op in final_waits:
            op.needed = True
        engmap = {"pe": "tensor", "act": "scalar", "dve": "vector", "pool": "gpsimd", "sp": "sync"}
        prog = self

        def emit(e, engine):
            waited = {}
            ops = prog.ops[e]
            for op in ops:
                need = {}
                dl = list(op.deps)
                if op.slot_prev is not None:
                    dl.append(op.slot_prev)
                for d in dl:
                    if waited.get(d.sem, 0) >= d.val:
                        continue
                    if need.get(d.sem, 0) < d.val:
                        need[d.sem] = d.val
                for s, v in need.items():
                    engine.wait_ge(getsem(s), v)
                    waited[s] = v
                ins = op.fn(engine)
                if op.dma:
                    ins.then_inc(getsem(op.sem), op.inc)
                elif op.needed:
                    ins.then_inc(getsem(op.sem), 1)
            if e == "sp":
                for op in final_waits:
                    if waited.get(op.sem, 0) < op.val:
                        engine.wait_ge(getsem(op.sem), op.val)
                        waited[op.sem] = op.val

        for e in ENGS:
            for op in self.ops[e]:
                if op.sem is not None and (op.needed or op.dma):
                    getsem(op.sem)
        with nc.Block() as block:
            for e in ENGS:
                if not self.ops[e] and e != "sp":
                    continue
                getattr(block, engmap[e])(lambda engine, e=e: emit(e, engine))
        for g in reversed(stack):
            g.__exit__(None, None, None)
        for g in reversed(self._ctx):
            g.__exit__(None, None, None)
        self._ctx = []
        n = {e: len(self.ops[e]) for e in ENGS}
        return n


def _fence(P):
    f = []
    for e in ENGS:
        ops = P.ops[e]
        last_c = None
        nd = 0
        for op in reversed(ops):
            if op.dma:
                if nd < DMA_POOL:
                    f.append(op)
                    nd += 1
            elif last_c is None:
                last_c = op
                f.append(op)
            if nd >= DMA_POOL and last_c is not None:
                break
    return f


def _fenced(ap, fence, name=""):
    t = T(ap, name)
    t.r = list(fence)
    return t


D = 1024
KT = 8
ALPHA = float(8 ** 0.25)
EPS = 1e-5
NEG = -1.0e30
NEXP = 32


def _din(nc, name, shape, dt=F32):
    return nc.dram_tensor(name, list(shape), dt, kind="ExternalInput").ap()


def _dout(nc, name, shape, dt=F32):
    return nc.dram_tensor(name, list(shape), dt, kind="ExternalOutput").ap()


def _adaln(P, w_ap, b_sb, sc, wfull, ps, mod):
    for kt in range(KT):
        P.dma("sp", wfull[:, kt, :], w_ap[kt * 128:(kt + 1) * 128, :], writes=[wfull])
    for ft in range(24):
        for kt in range(KT):
            P.pe(lambda e, kt=kt, ft=ft: e.matmul(
                ps[:, ft:ft + 1], wfull[:, kt, ft * 128:(ft + 1) * 128], sc[:, kt:kt + 1],
                start=(kt == 0), stop=(kt == KT - 1)), reads=[wfull, sc], writes=[ps])
    P.dve(lambda e: e.tensor_tensor(out=mod[:, 0:24], in0=ps[:, 0:24], in1=b_sb[:, 0:24], op=ALU.add),
          reads=[ps, b_sb], writes=[mod])


def _layernorm(P, v, sq, ps1, ps2, ones, tmp, gcol, bcol, out, TB):
    P.act(lambda e: e.activation(out=sq[:, :, 0:TB], in_=v[:, :, 0:TB], func=AF.Square), reads=[v], writes=[sq])
    for ft in range(KT):
        P.pe(lambda e, ft=ft: e.matmul(ps1[:, 0:TB], ones[:, 0:128], v[:, ft, 0:TB], start=(ft == 0), stop=(ft == KT - 1)),
             reads=[v, ones], writes=[ps1])
    for ft in range(KT):
        P.pe(lambda e, ft=ft: e.matmul(ps2[:, 0:TB], ones[:, 0:128], sq[:, ft, 0:TB], start=(ft == 0), stop=(ft == KT - 1)),
             reads=[sq, ones], writes=[ps2])
    mean, msq, rstd = tmp
    P.dve(lambda e: e.tensor_scalar(out=mean[:, 0:TB], in0=ps1[:, 0:TB], scalar1=1.0 / D, scalar2=None, op0=ALU.mult),
          reads=[ps1], writes=[mean])
    P.dve(lambda e: e.tensor_tensor(out=msq[:, 0:TB], in0=mean[:, 0:TB], in1=mean[:, 0:TB], op=ALU.mult),
          reads=[mean], writes=[msq])
    P.dve(lambda e: e.scalar_tensor_tensor(out=rstd[:, 0:TB], in0=ps2[:, 0:TB], scalar=1.0 / D, in1=msq[:, 0:TB],
                                           op0=ALU.mult, op1=ALU.subtract), reads=[ps2, msq], writes=[rstd])
    P.dve(lambda e: e.tensor_scalar(out=rstd[:, 0:TB], in0=rstd[:, 0:TB], scalar1=EPS, scalar2=None,
                                    op0=ALU.add), reads=[rstd], writes=[rstd])
    P.act(lambda e: e.activation(out=rstd[:, 0:TB], in_=rstd[:, 0:TB], func=AF.Sqrt), reads=[rstd], writes=[rstd])
    P.dve(lambda e: e.reciprocal(out=rstd[:, 0:TB], in_=rstd[:, 0:TB]), reads=[rstd], writes=[rstd])
    def bc_t(t):
        return t[:, 0:TB].rearrange("p (o t) -> p o t", o=1).to_broadcast([128, KT, TB])

    def bc_f(t):
        return t[:, 0:KT].rearrange("p (k o) -> p k o", o=1).to_broadcast([128, KT, TB])
    P.dve(lambda e: e.tensor_tensor(out=sq[:, :, 0:TB], in0=v[:, :, 0:TB], in1=bc_t(mean), op=ALU.subtract),
          reads=[v, mean], writes=[sq])
    P.pool(lambda e: e.tensor_tensor(out=sq[:, :, 0:TB], in0=sq[:, :, 0:TB], in1=bc_t(rstd), op=ALU.mult),
           reads=[sq, rstd], writes=[sq])
    P.dve(lambda e: e.tensor_tensor(out=sq[:, :, 0:TB], in0=sq[:, :, 0:TB], in1=bc_f(gcol), op=ALU.mult),
          reads=[sq, gcol], writes=[sq])
    P.pool(lambda e: e.tensor_tensor(out=out[:, :, 0:TB], in0=sq[:, :, 0:TB], in1=bc_f(bcol), op=ALU.add),
           reads=[sq, bcol], writes=[out])


def _routing(P, psR, lgs, rt, Wt, ti):
    gmax, ngmax, gsel, gexp, gsum, gval, pen, msk, mx8, dd, ed, w1, w2, wa, wb = rt
    P.dve(lambda e: e.tensor_copy(lgs[:, 0:36], psR[:, 0:36]), reads=[psR], writes=[lgs])
    P.dve(lambda e: e.reduce_max(out=gmax[:, 0:1], in_=lgs[:, 0:4], axis=AX.X), reads=[lgs], writes=[gmax])
    P.dve(lambda e: e.tensor_scalar(out=ngmax[:, 0:1], in0=gmax[:, 0:1], scalar1=-1.0, scalar2=None, op0=ALU.mult),
          reads=[gmax], writes=[ngmax])
    P.dve(lambda e: e.tensor_scalar(out=gsel[:, 0:4], in0=lgs[:, 0:4], scalar1=gmax[:, 0:1], scalar2=None,
                                    op0=ALU.is_equal), reads=[lgs, gmax], writes=[gsel])
    P.act(lambda e: e.activation(out=gexp[:, 0:4], in_=lgs[:, 0:4], func=AF.Exp, bias=ngmax[:, 0:1], scale=1.0),
          reads=[lgs, ngmax], writes=[gexp])
    P.dve(lambda e: e.reduce_sum(out=gsum[:, 0:1], in_=gexp[:, 0:4], axis=AX.X), reads=[gexp], writes=[gsum])
    P.dve(lambda e: e.reciprocal(out=gval[:, 0:1], in_=gsum[:, 0:1]), reads=[gsum], writes=[gval])
    P.dve(lambda e: e.tensor_scalar(out=pen[:, 0:4], in0=gsel[:, 0:4], scalar1=-1.0, scalar2=-NEG,
                                    op0=ALU.add, op1=ALU.mult), reads=[gsel], writes=[pen])
    P.dve(lambda e: e.tensor_tensor(
        out=msk[:, 0:32].rearrange("p (g x) -> p g x", g=4),
        in0=lgs[:, 4:36].rearrange("p (g x) -> p g x", g=4),
        in1=pen[:, 0:4].rearrange("p (g o) -> p g o", o=1).to_broadcast([128, 4, 8]), op=ALU.add),
        reads=[lgs, pen], writes=[msk])
    P.dve(lambda e: e.max(out=mx8[:, 0:8], in_=msk[:, 0:32]), reads=[msk], writes=[mx8])
    P.dve(lambda e: e.tensor_tensor(out=dd[:, 0:1], in0=mx8[:, 1:2], in1=mx8[:, 0:1], op=ALU.subtract),
          reads=[mx8], writes=[dd])
    P.act(lambda e: e.activation(out=ed[:, 0:1], in_=dd[:, 0:1], func=AF.Exp), reads=[dd], writes=[ed])
    P.dve(lambda e: e.tensor_scalar(out=w1[:, 0:1], in0=ed[:, 0:1], scalar1=1.0, scalar2=None, op0=ALU.add),
          reads=[ed], writes=[w1])
    P.dve(lambda e: e.reciprocal(out=w1[:, 0:1], in_=w1[:, 0:1]), reads=[w1], writes=[w1])
    P.dve(lambda e: e.tensor_tensor(out=w1[:, 0:1], in0=w1[:, 0:1], in1=gval[:, 0:1], op=ALU.mult),
          reads=[w1, gval], writes=[w1])
    P.dve(lambda e: e.tensor_tensor(out=w2[:, 0:1], in0=w1[:, 0:1], in1=ed[:, 0:1], op=ALU.mult),
          reads=[w1, ed], writes=[w2])
    P.dve(lambda e: e.tensor_scalar(out=wa[:, 0:32], in0=msk[:, 0:32], scalar1=mx8[:, 0:1], scalar2=w1[:, 0:1],
                                    op0=ALU.is_equal, op1=ALU.mult), reads=[msk, mx8, w1], writes=[wa])
    P.dve(lambda e: e.tensor_scalar(out=wb[:, 0:32], in0=msk[:, 0:32], scalar1=mx8[:, 1:2], scalar2=w2[:, 0:1],
                                    op0=ALU.is_equal, op1=ALU.mult), reads=[msk, mx8, w2], writes=[wb])
    P.dve(lambda e, ti=ti: e.tensor_tensor(out=Wt[:, ti, 0:32], in0=wa[:, 0:32], in1=wb[:, 0:32], op=ALU.add),
          reads=[wa, wb], writes=[Wt])


def emit_p2(P, nc, banks, NT, io, nexp=NEXP):
    TB = 256
    NB = NT // TB
    NTT = NT // 128
    TE = min(512, NT)
    NBE = NT // TE
    xT = io["xs"]; ccol = io["ccol"]
    w_am = io["w_am"]; b_am = io["b_am"]; w_af = io["w_af"]; b_af = io["b_af"]
    w_g = io["w_g"]; w_bs = io["w_bs"]; w_ba = io["w_ba"]; w_o = io["w_o"]
    lnp = io["lnp"]; w_r = io["w_r"]; b_r = io["b_r"]
    w_eg = io["w_eg"]; w_eu = io["w_eu"]; w_ed = io["w_ed"]
    cst = io["cst2"]; xoT = io["xo"]; Yg = io["Yg"]; Yg_t = io["Yg_t"]
    x1scr_ap = io["x1scr"]; x1scr = io["x1scr_t"]
    xs_t = [io["xs_t"]] if io.get("xs_t") is not None else []
    xo_t = [io["xo_t"]] if io.get("xo_t") is not None else []
    P.begin_phase()
    Ym = io["Ym"]; Ym_t = io["Ym_t"]
    for rt in range(6):
        def cp(e, rt=rt):
            return e.dma_start(out=Ym[rt].rearrange("g p t -> (g p) t"), in_=Yg[P.rank(e), rt].rearrange("g p t -> (g p) t"))
        P.add("sp", cp, reads=[Yg_t], writes=[Ym_t], dma=True)
    arena = P.sb("arena", [128, 49152], BF16)
    arena2 = P.sb("arena2", [128, 26624], BF16)
    h2raw = P.sb("h2raw", [128, 8 * NT], BF16)
    ident = P.sb("ident", [128, 128]); ones = P.sb("ones", [128, 128])
    sc = P.sb("sc", [128, 8]); lnv = [P.sb("lnv%d" % i, [128, 8]) for i in range(4)]
    bam = P.sb("bam", [128, 24]); baf = P.sb("baf", [128, 24])
    modm = P.sb("modm", [128, 24]); modf = P.sb("modf", [128, 24])
    sc1m = P.sb("sc1m", [128, 8]); g1pm = P.sb("g1pm", [128, 8]); sc1f = P.sb("sc1f", [128, 8]); g1pf = P.sb("g1pf", [128, 8])
    wr = P.sb("wr", [128, 8, 36]); br = P.sb("br", [1, 36])
    Wt = P.sb("Wt", [128, NTT, 32])
    gs = [P.sb("gs%d" % i, [128, 2, TB]) for i in range(2)]
    m1 = [P.sb("m1%d" % i, [128, TB]) for i in range(2)]
    m2 = [P.sb("m2%d" % i, [128, TB]) for i in range(2)]
    lntmp = [P.sb("lnt%d" % i, [128, TB]) for i in range(3)]
    lgs = P.sb("lgs", [128, 36])
    rt = [P.sb("rt%d" % i, [128, 32]) for i in range(15)]
    A0, A1, B0, B1, G0, G1, L, R = banks

    P.dma("sp", ident[:, :], cst[:, 0:128], writes=[ident])
    P.dma("sp", ones[:, :], cst[:, 128:256], writes=[ones])
    P.dma("sp", sc[:, :], ccol[:, :], writes=[sc])
    for i in range(4):
        P.dma("sp", lnv[i][:, :], lnp[:, i * 8:(i + 1) * 8], writes=[lnv[i]])
    P.dma("sp", bam[:, :], b_am[:, :], writes=[bam])
    P.dma("sp", baf[:, :], b_af[:, :], writes=[baf])
    P.dma("sp", wr[:, :, :], w_r.rearrange("(k p) n -> p k n", p=128), writes=[wr])
    P.dma("sp", br[:, :], b_r[:, :], writes=[br])
    P.act(lambda e: e.activation(out=sc[:, :], in_=sc[:, :], func=AF.Silu), reads=[sc], writes=[sc])
    wfull = T(arena.ap[:, 0:49152].bitcast(F32).rearrange("p (k f) -> p k f", k=8), "wfull")
    _adaln(P, w_am, bam, sc, wfull, R, modm)
    _adaln(P, w_af, baf, sc, wfull, R, modf)
    for (mod, s1, g1) in ((modm, sc1m, g1pm), (modf, sc1f, g1pf)):
        P.dve(lambda e, mod=mod, s1=s1: e.tensor_scalar(out=s1[:, :], in0=mod[:, 8:16], scalar1=1.0, scalar2=None, op0=ALU.add),
              reads=[mod], writes=[s1])
        P.dve(lambda e, mod=mod, g1=g1: e.tensor_scalar(out=g1[:, :], in0=mod[:, 16:24], scalar1=1.0, scalar2=None, op0=ALU.add),
              reads=[mod], writes=[g1])
    f0 = _fence(P)
    h2b = T(h2raw.ap[:, 0:8 * NT].rearrange("p (k t) -> p k t", k=8), "h2b")

    wg = _fenced(arena.ap[:, 0:16384].rearrange("p (k f) -> p k f", k=8), f0, "wg")
    wbs = _fenced(arena.ap[:, 16384:32768].rearrange("p (k f) -> p k f", k=16), f0, "wbs")
    wba = _fenced(arena.ap[:, 32768:40960].rearrange("p (k f) -> p k f", k=8), f0, "wba")
    wo = _fenced(arena.ap[:, 40960:49152].rearrange("p (k f) -> p k f", k=8), f0, "wo")
    for kt in range(8):
        P.dma("pool", wg[:, kt, :], w_g[kt * 128:(kt + 1) * 128, :], writes=[wg])
    for kt in range(16):
        P.dma("pool", wbs[:, kt, :], w_bs[kt * 128:(kt + 1) * 128, :], writes=[wbs])
    for kt in range(8):
        P.dma("pool", wba[:, kt, :], w_ba[kt * 128:(kt + 1) * 128, :], writes=[wba])
    for kt in range(8):
        P.dma("pool", wo[:, kt, :], w_o[kt * 128:(kt + 1) * 128, :], writes=[wo])

    def a2(off, n, dt, shape_k, name, fence=None):
        ap = arena2.ap[:, off:off + n]
        if dt == F32:
            ap = ap.bitcast(F32)
        ap = ap.rearrange("p (k t) -> p k t", k=shape_k)
        return _fenced(ap, fence, name) if fence is not None else T(ap, name)

    xb = a2(0, 4096, F32, 8, "xb"); v = a2(4096, 4096, F32, 8, "v"); sq = a2(8192, 4096, F32, 8, "sq")
    hT = a2(12288, 2048, BF16, 8, "hT"); ys = a2(14336, 4096, BF16, 16, "ys"); ya = a2(18432, 2048, BF16, 8, "ya")
    mg = a2(20480, 2048, BF16, 8, "mg"); h2f = a2(22528, 4096, F32, 8, "h2f")

    def bc_f(t, lo=0):
        return t[:, lo:lo + 8].rearrange("p (k o) -> p k o", o=1).to_broadcast([128, 8, TB])

    xTr = xT.rearrange("(k p) t -> p k t", p=128)
    x1r = x1scr_ap.rearrange("(k p) t -> p k t", p=128)
    xoTr = xoT.rearrange("(k p) t -> p k t", p=128)

    for tb in range(NB):
        t0 = tb * TB
        P.dma("sp", xb[:, :, :], xTr[:, :, t0:t0 + TB], reads=xs_t, writes=[xb])
        for gp in range(4):
            P.dma("sp", ys[:, gp * 4:(gp + 1) * 4, :], Ym[0:4, gp, :, t0:t0 + TB].rearrange("i p t -> p i t"), reads=[Ym_t], writes=[ys])
            P.dma("sp", ya[:, gp * 2:(gp + 1) * 2, :], Ym[4:6, gp, :, t0:t0 + TB].rearrange("i p t -> p i t"), reads=[Ym_t], writes=[ya])
        P.dve(lambda e: e.tensor_tensor(out=v[:, :, :], in0=xb[:, :, :], in1=bc_f(sc1m), op=ALU.mult),
              reads=[xb, sc1m], writes=[v])
        P.dve(lambda e: e.tensor_tensor(out=hT[:, :, :], in0=v[:, :, :], in1=bc_f(modm, 0), op=ALU.add),
              reads=[v, modm], writes=[hT])
        P.pool(lambda e: e.tensor_scalar(out=xb[:, :, :], in0=xb[:, :, :], scalar1=ALPHA, scalar2=None, op0=ALU.mult),
               reads=[xb], writes=[xb])
        for ft in range(8):
            pa, pb, pg = (A0, A1)[ft % 2], (B0, B1)[ft % 2], (G0, G1)[ft % 2]
            gsx, m1x, m2x = gs[ft % 2], m1[ft % 2], m2[ft % 2]
            fs = slice(ft * 128, (ft + 1) * 128)
            fs2 = slice(1024 + ft * 128, 1024 + (ft + 1) * 128)
            for kt in range(16):
                P.pe(lambda e, pa=pa, kt=kt, fs=fs: e.matmul(pa[:, 0:TB], wbs[:, kt, fs], ys[:, kt, :], start=(kt == 0), stop=(kt == 15)),
                     reads=[wbs, ys], writes=[pa])
            for kt in range(8):
                P.pe(lambda e, pb=pb, kt=kt, fs=fs: e.matmul(pb[:, 0:TB], wba[:, kt, fs], ya[:, kt, :], start=(kt == 0), stop=(kt == 7)),
                     reads=[wba, ya], writes=[pb])
            for kt in range(8):
                P.pe(lambda e, pg=pg, kt=kt, fs=fs: e.matmul(pg[:, 0:TB], wg[:, kt, fs], hT[:, kt, :], start=(kt == 0), stop=(kt == 7)),
                     reads=[wg, hT], writes=[pg])
            for kt in range(8):
                P.pe(lambda e, pg=pg, kt=kt, fs2=fs2: e.matmul(pg[:, TB:2 * TB], wg[:, kt, fs2], hT[:, kt, :], start=(kt == 0), stop=(kt == 7)),
                     reads=[wg, hT], writes=[pg])
            P.act(lambda e, pg=pg, gsx=gsx: e.activation(out=gsx[:, :, :].rearrange("p a t -> p (a t)"), in_=pg[:, 0:2 * TB], func=AF.Sigmoid),
                  reads=[pg], writes=[gsx])
            P.dve(lambda e, pa=pa, gsx=gsx, m1x=m1x: e.tensor_tensor(out=m1x[:, :], in0=pa[:, 0:TB], in1=gsx[:, 0, :], op=ALU.mult),
                  reads=[pa, gsx], writes=[m1x])
            P.dve(lambda e, pb=pb, gsx=gsx, m2x=m2x: e.tensor_tensor(out=m2x[:, :], in0=pb[:, 0:TB], in1=gsx[:, 1, :], op=ALU.mult),
                  reads=[pb, gsx], writes=[m2x])
            P.pool(lambda e, ft=ft, m1x=m1x, m2x=m2x: e.tensor_tensor(out=mg[:, ft, :], in0=m1x[:, :], in1=m2x[:, :], op=ALU.add),
                   reads=[m1x, m2x], writes=[mg])
        for ft in range(8):
            pa = (A0, A1)[ft % 2]
            fs = slice(ft * 128, (ft + 1) * 128)
            for kt in range(8):
                P.pe(lambda e, pa=pa, kt=kt, fs=fs: e.matmul(pa[:, 0:TB], wo[:, kt, fs], mg[:, kt, :], start=(kt == 0), stop=(kt == 7)),
                     reads=[wo, mg], writes=[pa])
            P.dve(lambda e, pa=pa, ft=ft: e.scalar_tensor_tensor(out=v[:, ft, :], in0=pa[:, 0:TB], scalar=g1pm[:, ft:ft + 1],
                                                                 in1=xb[:, ft, :], op0=ALU.mult, op1=ALU.add),
                  reads=[pa, g1pm, xb], writes=[v])
        _layernorm(P, v, sq, L, R, ones, lntmp, lnv[0], lnv[1], xb, TB)
        P.dma("sp", x1r[:, :, t0:t0 + TB], xb[:, :, :], reads=[xb], writes=[x1scr])
        P.dve(lambda e: e.tensor_tensor(out=v[:, :, :], in0=xb[:, :, :], in1=bc_f(sc1f), op=ALU.mult),
              reads=[xb, sc1f], writes=[v])
        P.dve(lambda e: e.tensor_tensor(out=h2f[:, :, :], in0=v[:, :, :], in1=bc_f(modf, 0), op=ALU.add),
              reads=[v, modf], writes=[h2f])
        P.act(lambda e, t0=t0: e.copy(h2b[:, :, t0:t0 + TB], h2f[:, :, :]), reads=[h2f], writes=[h2b])
        for tt in range(TB // 128):
            ti = tb * (TB // 128) + tt
            for kt in range(8):
                P.pe(lambda e, kt=kt, tt=tt: e.matmul(R[:, 0:36], h2f[:, kt, tt * 128:(tt + 1) * 128], wr[:, kt, :],
                                                     start=(kt == 0), stop=False), reads=[h2f, wr], writes=[R])
            P.pe(lambda e: e.matmul(R[:, 0:36], ones[0:1, 0:128], br[0:1, 0:36], start=False, stop=True),
                 reads=[ones, br], writes=[R])
            _routing(P, R, lgs, rt, Wt, ti)

    fAB = _fence(P)
    acc = _fenced(arena.ap[:, 0:NTT * 2048].bitcast(F32).rearrange("p (t d) -> p t d", t=NTT), fAB, "acc")
    slots = [_fenced(arena.ap[:, 32768:45056], fAB, "slot0"), _fenced(arena2.ap[:, 0:12288], fAB, "slot1")]
    act = _fenced(arena2.ap[:, 12288:12288 + 4 * TE].rearrange("p (k t) -> p k t", k=4), fAB, "act")
    sgs = [_fenced(arena2.ap[:, 14336 + i * 2 * TE:14336 + (i + 1) * 2 * TE].bitcast(F32), fAB, "sg%d" % i) for i in range(2)]
    for ex in range(nexp):
        slot = slots[ex % 2]
        wge = slot.ap[:, 0:4096].rearrange("p (k f) -> p k f", k=8)
        wue = slot.ap[:, 4096:8192].rearrange("p (k f) -> p k f", k=8)
        wde = slot.ap[:, 8192:12288].rearrange("p (k f) -> p k f", k=4)
        P.dma("pool", wge, w_eg[ex].rearrange("(k p) f -> p k f", p=128), writes=[slot])
        P.dma("pool", wue, w_eu[ex].rearrange("(k p) f -> p k f", p=128), writes=[slot])
        P.dma("pool", wde, w_ed[ex].rearrange("(k p) f -> p k f", p=128), writes=[slot])
        for tb in range(NBE):
            t0 = tb * TE
            for ff in range(4):
                pg, pu, sg = (A0, A1)[ff % 2], (B0, B1)[ff % 2], sgs[ff % 2]
                fs = slice(ff * 128, (ff + 1) * 128)
                for kt in range(8):
                    P.pe(lambda e, pg=pg, kt=kt, fs=fs, wge=wge, t0=t0: e.matmul(pg[:, 0:TE], wge[:, kt, fs], h2b[:, kt, t0:t0 + TE],
                                                                              start=(kt == 0), stop=(kt == 7)),
                         reads=[slot, h2b], writes=[pg])
                for kt in range(8):
                    P.pe(lambda e, pu=pu, kt=kt, fs=fs, wue=wue, t0=t0: e.matmul(pu[:, 0:TE], wue[:, kt, fs], h2b[:, kt, t0:t0 + TE],
                                                                              start=(kt == 0), stop=(kt == 7)),
                         reads=[slot, h2b], writes=[pu])
                P.act(lambda e, pg=pg, sg=sg: e.activation(out=sg[:, 0:TE], in_=pg[:, 0:TE], func=AF.Silu), reads=[pg], writes=[sg])
                P.dve(lambda e, pu=pu, sg=sg, ff=ff: e.tensor_tensor(out=act[:, ff, :], in0=pu[:, 0:TE], in1=sg[:, 0:TE], op=ALU.mult),
                      reads=[pu, sg], writes=[act])
            for tt in range(TE // 128):
                ti = tb * (TE // 128) + tt
                for dh in range(2):
                    pd = (G0, G1)[dh]
                    for ff in range(4):
                        P.pe(lambda e, pd=pd, ff=ff, tt=tt, dh=dh, wde=wde: e.matmul(
                            pd[:, 0:512], act[:, ff, tt * 128:(tt + 1) * 128], wde[:, ff, dh * 512:(dh + 1) * 512],
                            start=(ff == 0), stop=(ff == 3)), reads=[act, slot], writes=[pd])
                    if ex == 0:
                        P.dve(lambda e, pd=pd, ti=ti, dh=dh, ex=ex: e.tensor_scalar(
                            out=acc[:, ti, dh * 512:(dh + 1) * 512], in0=pd[:, 0:512], scalar1=Wt[:, ti, ex:ex + 1], scalar2=None,
                            op0=ALU.mult), reads=[pd, Wt], writes=[acc])
                    else:
                        P.dve(lambda e, pd=pd, ti=ti, dh=dh, ex=ex: e.scalar_tensor_tensor(
                            out=acc[:, ti, dh * 512:(dh + 1) * 512], in0=pd[:, 0:512], scalar=Wt[:, ti, ex:ex + 1],
                            in1=acc[:, ti, dh * 512:(dh + 1) * 512], op0=ALU.mult, op1=ALU.add), reads=[pd, Wt, acc], writes=[acc])

    fBC = _fence(P)
    xb2 = a2(0, 4096, F32, 8, "xb2", fBC); v2 = a2(4096, 4096, F32, 8, "v2", fBC); sq2 = a2(8192, 4096, F32, 8, "sq2", fBC)
    outs = []
    for tb in range(NB):
        t0 = tb * TB
        P.dma("sp", xb2[:, :, :], x1r[:, :, t0:t0 + TB], reads=[x1scr], writes=[xb2])
        P.pool(lambda e: e.tensor_scalar(out=xb2[:, :, :], in0=xb2[:, :, :], scalar1=ALPHA, scalar2=None, op0=ALU.mult),
               reads=[xb2], writes=[xb2])
        for ft in range(8):
            pa = (A0, A1)[ft % 2]
            for tt in range(TB // 128):
                ti = tb * (TB // 128) + tt
                P.pe(lambda e, pa=pa, ti=ti, tt=tt, ft=ft: e.matmul(pa[:, tt * 128:(tt + 1) * 128], acc[:, ti, ft * 128:(ft + 1) * 128],
                                                                   ident[:, 0:128], start=True, stop=True),
                     reads=[acc, ident], writes=[pa])
            P.dve(lambda e, pa=pa, ft=ft: e.scalar_tensor_tensor(out=v2[:, ft, :], in0=pa[:, 0:TB], scalar=g1pf[:, ft:ft + 1],
                                                                 in1=xb2[:, ft, :], op0=ALU.mult, op1=ALU.add),
                  reads=[pa, g1pf, xb2], writes=[v2])
        _layernorm(P, v2, sq2, L, R, ones, lntmp, lnv[2], lnv[3], xb2, TB)
        outs.append(P.dma("sp", xoTr[:, :, t0:t0 + TB], xb2[:, :, :], reads=[xb2], writes=xo_t))
    return outs


def _col(vec, n):
    return np.ascontiguousarray(np.asarray(vec).reshape(n, 128).T)


def _consts():
    c = np.zeros((128, 256), np.float32)
    c[:, 0:128] = np.eye(128, dtype=np.float32)
    c[:, 128:256] = 1.0
    return c


def p2_inputs(inp, l):
    return {
        "w_af": inp["w_ada_ffn"][l], "b_af": _col(inp["b_ada_ffn"][l], 24),
        "w_g": np.ascontiguousarray(inp["w_in"][l][:, 8224:10272]),
        "w_bs": inp["w_branch_ssd"][l], "w_ba": inp["w_branch_attn"][l], "w_o": inp["w_out"][l],
        "lnp": np.ascontiguousarray(np.concatenate([_col(inp["ln_mix_g"][l], 8), _col(inp["ln_mix_b"][l], 8),
                                                    _col(inp["ln_ffn_g"][l], 8), _col(inp["ln_ffn_b"][l], 8)], axis=1)),
        "w_r": np.ascontiguousarray(np.concatenate([inp["w_router_group"][l], inp["w_router_expert"][l]], axis=1)),
        "b_r": np.ascontiguousarray(np.concatenate([inp["b_router_group"][l], inp["b_router_expert"][l]])[None, :]),
        "w_eg": inp["w_expert_gate"][l], "w_eu": inp["w_expert_up"][l], "w_ed": inp["w_expert_down"][l],
    }


C1_ID, C1_ONES, C1_U, C1_UW0, C1_UW1, C1_CM, C1_CB, C1_MISC, C1_N = 0, 128, 256, 384, 640, 896, 1408, 1920, 1928
W1_NF = 2304
PI = float(np.pi)


def _consts1():
    c = np.zeros((128, C1_N), np.float32)
    c[:, C1_ID:C1_ID + 128] = np.eye(128, dtype=np.float32)
    c[:, C1_ONES:C1_ONES + 128] = 1.0
    U = np.triu(np.ones((128, 128), np.float32))
    c[:, C1_U:C1_U + 128] = U
    c[:, C1_UW0:C1_UW0 + 128] = U
    c[:, C1_UW0 + 128:C1_UW0 + 256] = 1.0
    c[:, C1_UW1 + 128:C1_UW1 + 256] = U
    s = np.arange(128)[:, None]
    l = np.arange(256)[None, :]
    m0 = (l >= s).astype(np.float32)
    m1 = (l >= s + 128).astype(np.float32)
    c[:, C1_CM:C1_CM + 256] = m0
    c[:, C1_CM + 256:C1_CM + 512] = m1
    c[:, C1_CB:C1_CB + 256] = (m0 - 1.0) * 1.0e30
    c[:, C1_CB + 256:C1_CB + 512] = (m1 - 1.0) * 1.0e30
    p = np.arange(128)
    inv_freq = (10000.0 ** (-np.arange(0, 64, 2, dtype=np.float32) / 64)).astype(np.float32)
    c[:, C1_MISC + 0] = inv_freq[p % 32]
    c[:, C1_MISC + 1] = np.where((p % 64) < 32, -1.0, 1.0)
    c[:, C1_MISC + 2] = -PI
    return c


def _esel():
    e = np.zeros((32, 32, 128), np.float32)
    for n in range(32):
        e[n, n, :] = 1.0
    return e.reshape(32, 4096)


def emit_p1(P, nc, banks, S, io):
    NCH = S // 256
    H2 = S // 4
    ccol = io["ccol"]; w_am = io["w_am"]; b_am = io["b_am"]; w1 = io["w1"]
    convw = io["convw"]; convb = io["convb"]; pvec = io["pvec"]; pos = io["pos"]; cst = io["cst1"]
    Yloc = io["Yloc"]; Yloc_t = io["Yloc_t"]
    x_t = [io["x_t"]] if io.get("x_t") is not None else []
    P.begin_phase()
    big = P.sb("big", [128, max(49152, 20480 + 4 * S)], BF16)
    cs = P.sb("cs", [128, C1_N])
    sc = P.sb("sc", [128, 8]); bam = P.sb("bam", [128, 24]); modm = P.sb("modm", [128, 24]); sc1 = P.sb("sc1", [128, 8])
    cw = P.sb("cw", [128, 24]); cb = P.sb("cb", [128, 6]); pv = P.sb("pv", [128, 24])
    aneg = P.sb("aneg", [128, 8]); wdt = P.sb("wdt", [128, 8, 8])
    BJ, PM, PG, PY, PS0, PS1, PO, PD = banks

    P.dma("sp", cs[:, :], cst[:, :], writes=[cs])
    P.dma("sp", sc[:, :], ccol[:, :], writes=[sc])
    P.dma("sp", bam[:, :], b_am[:, :], writes=[bam])
    P.dma("sp", cw[:, :], convw[:, :], writes=[cw])
    P.dma("sp", cb[:, :], convb[:, :], writes=[cb])
    P.dma("sp", pv[:, :], pvec[:, :], writes=[pv])
    P.dma("sp", wdt[:, :, :], w1.rearrange("(k p) n -> p k n", p=128)[:, :, 2560:2568], writes=[wdt])
    P.act(lambda e: e.activation(out=sc[:, :], in_=sc[:, :], func=AF.Silu), reads=[sc], writes=[sc])
    P.act(lambda e: e.activation(out=aneg[:, :], in_=pv[:, 16:24], func=AF.Exp), reads=[pv], writes=[aneg])
    P.dve(lambda e: e.tensor_scalar(out=aneg[:, :], in0=aneg[:, :], scalar1=-1.0, scalar2=None, op0=ALU.mult),
          reads=[aneg], writes=[aneg])
    wfull = T(big.ap[:, 0:49152].bitcast(F32).rearrange("p (k f) -> p k f", k=8), "wfull")
    _adaln(P, w_am, bam, sc, wfull, PM, modm)
    P.dve(lambda e: e.tensor_scalar(out=sc1[:, :], in0=modm[:, 8:16], scalar1=1.0, scalar2=None, op0=ALU.add),
          reads=[modm], writes=[sc1])
    f0 = _fence(P)
    wsb = _fenced(big.ap[:, 0:20480].rearrange("p (k f) -> p k f", k=8), f0, "wsb")
    kT_ap = big.ap[:, 20480:20480 + 2 * S].rearrange("p (i t) -> p i t", i=2)
    V_ap = big.ap[:, 20480 + 2 * S:20480 + 4 * S].rearrange("p (n f) -> p n f", f=256)
    kTt = [_fenced(kT_ap, f0, "kT%d" % c) for c in range(NCH)]
    Vt = [_fenced(V_ap, f0, "V%d" % c) for c in range(NCH)]
    for kt in range(8):
        P.dma("pool", wsb[:, kt, :], w1[kt * 128:(kt + 1) * 128, 0:2560], writes=[wsb])

    xc = P.sb("xc", [128, 8, 256]); hTf = xc; hTb = P.sb("hTb", [128, 8, 256], BF16)
    zs = P.sb("zs", [128, 4, 256]); cin = P.sb("cin", [128, 6, 259]); xcv = P.sb("xcv", [128, 6, 256])
    BTb = P.sb("BTb", [128, 256], BF16); CTb = P.sb("CTb", [128, 256], BF16)
    posi = P.sb("posi", [128, 256], I32); ang = P.sb("ang", [128, 256]); tm = P.sb("tm", [128, 256])
    cosT = P.sb("cosT", [128, 256]); sinT = P.sb("sinT", [128, 256])
    tA = P.sb("tA", [128, 256]); tB = P.sb("tB", [128, 256]); cacc = tA
    qTf = P.sb("qTf", [128, 2, 256]); qTb = P.sb("qTb", [128, 2, 256], BF16); kTf = P.sb("kTf", [128, 2, 256])
    kmean = P.sb("kmean", [128, 2, 32]); ksum = P.sb("ksum", [128, 2])
    dtr = P.sb("dtr", [128, 2, 8]); dta_ = P.sb("dta", [128, 2, 8]); dtt = [P.sb("dtt%d" % i, [128, 2, 8]) for i in range(4)]
    cssb = P.sb("cssb", [128, 24]); negcs = P.sb("negcs", [128, 2, 8]); wend = P.sb("wend", [128, 2, 8])
    dtw = P.sb("dtw", [128, 2, 8]); dec = P.sb("dec", [128, 8])
    xtok = P.sb("xtok", [128, 2, 512]); XE = P.sb("XE", [128, 2, 512], BF16); XO = P.sb("XO", [128, 2, 512], BF16)
    xdtw = P.sb("xdtw", [128, 2, 512], BF16); Btok = P.sb("Btok", [128, 2, 128], BF16)
    Gm = P.sb("Gm", [128, 2, 256]); Dm = P.sb("Dm", [128, 2, 256]); dcy = Dm
    scT = [P.sb("scT%d" % i, [128, 2, 256], BF16) for i in range(2)]
    E1 = P.sb("E1", [128, 256]); CE = [P.sb("CE%d" % i, [128, 256], BF16) for i in range(2)]
    state = P.sb("state", [128, 512]); stE = P.sb("stE", [128, 512], BF16); stO = P.sb("stO", [128, 512], BF16)
    yD = P.sb("yD", [128, 256]); gy = P.sb("gy", [128, 4, 256]); sqg = P.sb("sqg", [128, 4, 256])
    rstd = P.sb("rstd", [128, 256]); yout = P.sb("yout", [128, 4, 256], BF16)
    gate = P.sb("gate", [128, 32]); mx8 = P.sb("mx8", [128, 8]); selm = P.sb("selm", [128, 32])
    selb = [P.sb("selb%d" % i, [128, 32]) for i in range(2)]
    selT = P.sb("selT", [32, 4, 256], BF16)
    pT = [P.sb("pT%d" % i, [128, 2, 256], BF16) for i in range(2)]
    rden = P.sb("rden", [64, 256]); yatt = P.sb("yatt", [64, 4, 256], BF16)
    onesb = P.sb("onesb", [128, 64], BF16); identb = P.sb("identb", [128, 128], BF16)
    cbias = P.sb("cbias", [128, 2, 256], BF16)

    ident = cs; invf = cs
    def C(off, n):
        return cs[:, off:off + n]

    P.dve(lambda e: e.tensor_copy(onesb[:, :], C(C1_ONES, 64)), reads=[cs], writes=[onesb])
    P.dve(lambda e: e.tensor_copy(identb[:, :], C(C1_ID, 128)), reads=[cs], writes=[identb])
    P.dve(lambda e: e.tensor_copy(cbias[:, :, :].rearrange("p a t -> p (a t)"), C(C1_CB, 512)), reads=[cs], writes=[cbias])
    P.dve(lambda e: e.memset(cin[:, :, :], 0.0), writes=[cin])
    P.pool(lambda e: e.memset(XE[:, :, :], 0.0), writes=[XE])
    P.pool(lambda e: e.memset(XO[:, :, :], 0.0), writes=[XO])
    P.pool(lambda e: e.memset(stE[:, :], 0.0), writes=[stE])
    P.pool(lambda e: e.memset(stO[:, :], 0.0), writes=[stO])
    P.dve(lambda e: e.memset(gate[:, :], NEG), writes=[gate])
    for i in range(2):
        P.dve(lambda e, i=i: e.memset(selb[i][:, :], 0.0), writes=[selb[i]])

    outs = []

    def bc8(ap):
        return ap.rearrange("p (r o) -> p r o", o=1).to_broadcast([128, 8, 64])

    def do_chunk(c):
        t0 = c * 256
        hh, col0 = t0 // H2, t0 % H2
        YsO = Yloc[hh, 0:512, :].rearrange("(i p) t -> p i t", p=128)
        YaO = Yloc[hh, 512:768, :].rearrange("(h d) t -> d h t", d=64)
        P.dma("sp", xc[:, :, :], io["x_src"](c), reads=x_t, writes=[xc])
        P.dma("sp", posi[:, :], pos[0:1, t0:t0 + 256].to_broadcast([128, 256]), writes=[posi])
        P.dve(lambda e: e.tensor_tensor(out=hTf[:, :, :], in0=xc[:, :, :],
                                        in1=sc1[:, 0:8].rearrange("p (k o) -> p k o", o=1).to_broadcast([128, 8, 256]), op=ALU.mult),
              reads=[xc, sc1], writes=[xc])
        P.dve(lambda e: e.tensor_tensor(out=hTf[:, :, :], in0=hTf[:, :, :],
                                        in1=modm[:, 0:8].rearrange("p (k o) -> p k o", o=1).to_broadcast([128, 8, 256]), op=ALU.add),
              reads=[hTf, modm], writes=[hTf])
        P.act(lambda e: e.copy(hTb[:, :, :], hTf[:, :, :]), reads=[hTf], writes=[hTb])
        P.dve(lambda e: e.tensor_copy(ang[:, :], posi[:, :]), reads=[posi], writes=[ang])
        P.dve(lambda e: e.tensor_scalar(out=ang[:, :], in0=ang[:, :], scalar1=cs[:, C1_MISC:C1_MISC + 1], scalar2=None, op0=ALU.mult),
              reads=[ang, cs], writes=[ang])
        C1_, C2_ = 6.28125, 2 * PI - 6.28125
        P.dve(lambda e: e.tensor_scalar(out=tm[:, :], in0=ang[:, :], scalar1=1.0 / (2 * PI), scalar2=None, op0=ALU.mult), reads=[ang], writes=[tm])
        P.dve(lambda e: e.tensor_copy(posi[:, :], tm[:, :]), reads=[tm], writes=[posi])
        P.dve(lambda e: e.tensor_copy(tm[:, :], posi[:, :]), reads=[posi], writes=[tm])
        P.dve(lambda e: e.scalar_tensor_tensor(out=ang[:, :], in0=tm[:, :], scalar=-C1_, in1=ang[:, :], op0=ALU.mult, op1=ALU.add),
              reads=[tm, ang], writes=[ang])
        P.dve(lambda e: e.scalar_tensor_tensor(out=ang[:, :], in0=tm[:, :], scalar=-C2_, in1=ang[:, :], op0=ALU.mult, op1=ALU.add),
              reads=[tm, ang], writes=[ang])
        for (shift, dstT) in ((0.0, sinT), (0.5 * PI, cosT)):
            if shift != 0.0:
                P.dve(lambda e, shift=shift: e.tensor_scalar(out=ang[:, :], in0=ang[:, :], scalar1=shift, scalar2=None, op0=ALU.add),
                      reads=[ang], writes=[ang])
            P.dve(lambda e: e.tensor_scalar(out=tm[:, :], in0=ang[:, :], scalar1=PI, scalar2=-2 * PI, op0=ALU.is_gt, op1=ALU.mult),
                  reads=[ang], writes=[tm])
            P.dve(lambda e: e.tensor_tensor(out=ang[:, :], in0=ang[:, :], in1=tm[:, :], op=ALU.add), reads=[ang, tm], writes=[ang])
            P.dve(lambda e: e.tensor_scalar(out=tm[:, :], in0=ang[:, :], scalar1=-PI, scalar2=2 * PI, op0=ALU.is_lt, op1=ALU.mult),
                  reads=[ang], writes=[tm])
            P.dve(lambda e: e.tensor_tensor(out=ang[:, :], in0=ang[:, :], in1=tm[:, :], op=ALU.add), reads=[ang, tm], writes=[ang])
            P.act(lambda e, dstT=dstT: e.activation(out=dstT[:, :], in_=ang[:, :], func=AF.Sin), reads=[ang], writes=[dstT])
        P.dve(lambda e: e.tensor_scalar(out=sinT[:, :], in0=sinT[:, :], scalar1=cs[:, C1_MISC + 1:C1_MISC + 2], scalar2=None, op0=ALU.mult),
              reads=[sinT, cs], writes=[sinT])

        def proj(j, half):
            for kt in range(8):
                P.pe(lambda e, kt=kt: e.matmul(BJ[:, half * 256:(half + 1) * 256], wsb[:, kt, j * 128:(j + 1) * 128], hTb[:, kt, :],
                                               start=(kt == 0), stop=(kt == 7)), reads=[wsb, hTb], writes=[BJ])
        for j in range(4):
            proj(j, j % 2)
            P.act(lambda e, j=j: e.activation(out=zs[:, j, :], in_=BJ[:, (j % 2) * 256:(j % 2 + 1) * 256], func=AF.Silu),
                  reads=[BJ], writes=[zs])
        for jj in range(6):
            proj(4 + jj, jj % 2)
            P.act(lambda e, jj=jj: e.copy(cin[:, jj, 3:259], BJ[:, (jj % 2) * 256:(jj % 2 + 1) * 256]), reads=[BJ], writes=[cin])
        for i in range(2):
            for (jq, js, dst) in ((10 + i, 14 + i, "q"), (12 + i, 16 + i, "k")):
                proj(jq, 0)
                proj(js, 1)
                P.dve(lambda e: e.tensor_tensor(out=tA[:, :], in0=BJ[:, 0:256], in1=cosT[:, :], op=ALU.mult),
                      reads=[BJ, cosT], writes=[tA])
                P.dve(lambda e: e.tensor_tensor(out=tB[:, :], in0=BJ[:, 256:512], in1=sinT[:, :], op=ALU.mult),
                      reads=[BJ, sinT], writes=[tB])
                if dst == "q":
                    P.pool(lambda e, i=i: e.tensor_tensor(out=qTf[:, i, :], in0=tA[:, :], in1=tB[:, :], op=ALU.add),
                           reads=[tA, tB], writes=[qTf])
                    P.act(lambda e, i=i: e.mul(qTb[:, i, :], qTf[:, i, :], 0.125), reads=[qTf], writes=[qTb])
                else:
                    P.pool(lambda e, i=i: e.tensor_tensor(out=kTf[:, i, :], in0=tA[:, :], in1=tB[:, :], op=ALU.add),
                           reads=[tA, tB], writes=[kTf])
                    P.act(lambda e, i=i: e.copy(kT_ap[:, i, t0:t0 + 256], kTf[:, i, :]), reads=[kTf], writes=[kTt[c]])
        P.dve(lambda e: e.reduce_sum(out=ksum[:, 0:2], in_=kTf[:, :, :], axis=AX.X), reads=[kTf], writes=[ksum])
        P.dve(lambda e: e.tensor_scalar(out=kmean[:, :, c], in0=ksum[:, 0:2], scalar1=1.0 / 256, scalar2=None, op0=ALU.mult),
              reads=[ksum], writes=[kmean])
        for tt in range(2):
            for kt in range(8):
                P.pe(lambda e, kt=kt, tt=tt: e.matmul(BJ[:, tt * 256:(tt + 1) * 256], hTb[:, kt, tt * 128:(tt + 1) * 128], wsb[:, kt, 2304:2560],
                                                     start=(kt == 0), stop=(kt == 7)), reads=[hTb, wsb], writes=[BJ])
            P.act(lambda e, tt=tt: e.copy(V_ap[:, 2 * c + tt, :], BJ[:, tt * 256:(tt + 1) * 256]), reads=[BJ], writes=[Vt[c]])
        for tt in range(2):
            for kt in range(8):
                P.pe(lambda e, kt=kt, tt=tt: e.matmul(PM[:, tt * 8:(tt + 1) * 8], hTf[:, kt, tt * 128:(tt + 1) * 128], wdt[:, kt, :],
                                                     start=(kt == 0), stop=(kt == 7)), reads=[hTf, wdt], writes=[PM])
        a_, ab_, e_, l_ = dtt
        P.dve(lambda e: e.tensor_tensor(out=a_[:, :, :], in0=PM[:, 0:16].rearrange("p (t r) -> p t r", t=2),
                                        in1=pv[:, 8:16].rearrange("p (o r) -> p o r", o=1).to_broadcast([128, 2, 8]), op=ALU.add),
              reads=[PM, pv], writes=[a_])
        P.dve(lambda e: e.tensor_scalar(out=ab_[:, :, :], in0=a_[:, :, :], scalar1=-1.0, scalar2=None, op0=ALU.mult), reads=[a_], writes=[ab_])
        P.dve(lambda e: e.tensor_tensor(out=ab_[:, :, :], in0=ab_[:, :, :], in1=a_[:, :, :], op=ALU.min), reads=[a_, ab_], writes=[ab_])
        P.act(lambda e: e.activation(out=e_[:, :, :], in_=ab_[:, :, :], func=AF.Exp), reads=[ab_], writes=[e_])
        P.dve(lambda e: e.tensor_scalar(out=e_[:, :, :], in0=e_[:, :, :], scalar1=1.0, scalar2=None, op0=ALU.add), reads=[e_], writes=[e_])
        P.act(lambda e: e.activation(out=l_[:, :, :], in_=e_[:, :, :], func=AF.Ln), reads=[e_], writes=[l_])
        P.dve(lambda e: e.tensor_scalar(out=a_[:, :, :], in0=a_[:, :, :], scalar1=0.0, scalar2=None, op0=ALU.max), reads=[a_], writes=[a_])
        P.dve(lambda e: e.tensor_tensor(out=dtr[:, :, :], in0=a_[:, :, :], in1=l_[:, :, :], op=ALU.add), reads=[a_, l_], writes=[dtr])
        P.dve(lambda e: e.tensor_tensor(out=dta_[:, :, :], in0=dtr[:, :, :],
                                        in1=aneg[:, 0:8].rearrange("p (o r) -> p o r", o=1).to_broadcast([128, 2, 8]), op=ALU.mult),
              reads=[dtr, aneg], writes=[dta_])
        for jj in range(6):
            P.dve(lambda e, jj=jj: e.tensor_scalar(out=cacc[:, :], in0=cin[:, jj, 0:256], scalar1=cw[:, jj * 4:jj * 4 + 1], scalar2=None, op0=ALU.mult),
                  reads=[cin, cw], writes=[cacc])
            for k in range(1, 4):
                P.dve(lambda e, jj=jj, k=k: e.scalar_tensor_tensor(out=cacc[:, :], in0=cin[:, jj, k:k + 256], scalar=cw[:, jj * 4 + k:jj * 4 + k + 1],
                                                                  in1=cacc[:, :], op0=ALU.mult, op1=ALU.add), reads=[cin, cw, cacc], writes=[cacc])
            P.act(lambda e, jj=jj: e.activation(out=xcv[:, jj, :], in_=cacc[:, :], func=AF.Silu, bias=cb[:, jj:jj + 1], scale=1.0),
                  reads=[cacc, cb], writes=[xcv])
        P.pool(lambda e: e.tensor_copy(cin[:, :, 0:3], cin[:, :, 256:259]), reads=[cin], writes=[cin])
        P.act(lambda e: e.copy(BTb[:, :], xcv[:, 4, :]), reads=[xcv], writes=[BTb])
        P.act(lambda e: e.copy(CTb[:, :], xcv[:, 5, :]), reads=[xcv], writes=[CTb])
        Uc = C(C1_U, 128); On = C(C1_ONES, 128)
        P.pe(lambda e: e.matmul(PM[:, 32:40], Uc, dta_[:, 0, :], start=True, stop=True), reads=[cs, dta_], writes=[PM])
        P.pe(lambda e: e.matmul(PM[:, 40:48], On, dta_[:, 0, :], start=True, stop=False), reads=[cs, dta_], writes=[PM])
        P.pe(lambda e: e.matmul(PM[:, 40:48], Uc, dta_[:, 1, :], start=False, stop=True), reads=[cs, dta_], writes=[PM])
        P.pe(lambda e: e.matmul(PM[:, 48:56], On, dta_[:, 0, :], start=True, stop=False), reads=[cs, dta_], writes=[PM])
        P.pe(lambda e: e.matmul(PM[:, 48:56], On, dta_[:, 1, :], start=False, stop=True), reads=[cs, dta_], writes=[PM])
        P.dve(lambda e: e.tensor_copy(cssb[:, 0:24], PM[:, 32:56]), reads=[PM], writes=[cssb])
        csv = cssb[:, 0:16].rearrange("p (t r) -> p t r", t=2)
        clb = cssb[:, 16:24].rearrange("p (o r) -> p o r", o=1).to_broadcast([128, 2, 8])
        P.dve(lambda e: e.tensor_scalar(out=negcs[:, :, :], in0=csv, scalar1=-1.0, scalar2=None, op0=ALU.mult), reads=[cssb], writes=[negcs])
        P.dve(lambda e: e.tensor_tensor(out=wend[:, :, :], in0=clb, in1=csv, op=ALU.subtract), reads=[cssb], writes=[wend])
        P.act(lambda e: e.activation(out=wend[:, :, :], in_=wend[:, :, :], func=AF.Exp), reads=[wend], writes=[wend])
        P.dve(lambda e: e.tensor_tensor(out=dtw[:, :, :], in0=dtr[:, :, :], in1=wend[:, :, :], op=ALU.mult), reads=[dtr, wend], writes=[dtw])
        P.act(lambda e: e.activation(out=dec[:, :], in_=cssb[:, 16:24], func=AF.Exp), reads=[cssb], writes=[dec])
        for tt in range(2):
            for jj in range(4):
                P.pe(lambda e, tt=tt, jj=jj: e.matmul(PG[:, jj * 128:(jj + 1) * 128], xcv[:, jj, tt * 128:(tt + 1) * 128], C(C1_ID, 128),
                                                     start=True, stop=True), reads=[xcv, cs], writes=[PG])
            P.act(lambda e, tt=tt: e.copy(xtok[:, tt, :], PG[:, 0:512]), reads=[PG], writes=[xtok])
        for tt in range(2):
            P.pe(lambda e, tt=tt: e.matmul(PM[:, 64 + tt * 128:64 + (tt + 1) * 128], xcv[:, 4, tt * 128:(tt + 1) * 128], C(C1_ID, 128),
                                           start=True, stop=True), reads=[xcv, cs], writes=[PM])
        P.act(lambda e: e.copy(Btok[:, :, :].rearrange("p t n -> p (t n)"), PM[:, 64:320]), reads=[PM], writes=[Btok])
        for tt in range(2):
            xv = xtok[:, tt, :].rearrange("p (i two q) -> p i two q", two=2, q=64)
            dv = dtr[:, tt, :].rearrange("p (i two) -> p i two", two=2)
            for par, X in ((0, XE), (1, XO)):
                P.dve(lambda e, tt=tt, par=par, X=X, xv=xv, dv=dv: e.tensor_tensor(
                    out=X[:, tt, :].rearrange("p (i two q) -> p i two q", two=2, q=64)[:, :, par, :],
                    in0=xv[:, :, par, :], in1=dv[:, :, par:par + 1].to_broadcast([128, 4, 64]), op=ALU.mult),
                    reads=[xtok, dtr], writes=[X])
            P.pool(lambda e, tt=tt: e.tensor_tensor(out=xdtw[:, tt, :].rearrange("p (r q) -> p r q", q=64),
                                                   in0=xtok[:, tt, :].rearrange("p (r q) -> p r q", q=64),
                                                   in1=bc8(dtw[:, tt, :]), op=ALU.mult), reads=[xtok, dtw], writes=[xdtw])
        for st in range(2):
            P.pe(lambda e, st=st: e.matmul(PG[:, st * 256:(st + 1) * 256], BTb[:, st * 128:(st + 1) * 128], CTb[:, 0:256],
                                           start=True, stop=True), reads=[BTb, CTb], writes=[PG])
        P.dve(lambda e: e.tensor_tensor(out=Gm[:, :, :].rearrange("p a t -> p (a t)"), in0=PG[:, 0:512], in1=C(C1_CM, 512), op=ALU.mult),
              reads=[PG, cs], writes=[Gm])
        for r in range(8):
            i, par = r // 2, r % 2
            sc_, ce_ = scT[r % 2], CE[r % 2]
            PT = PM
            P.pe(lambda e, r=r: e.matmul(PT[:, 256:512], dta_[:, 0, r:r + 1].to_broadcast([128, 128]), C(C1_UW0, 256), start=True, stop=False),
                 reads=[dta_, cs], writes=[PM])
            P.pe(lambda e, r=r: e.matmul(PT[:, 256:512], dta_[:, 1, r:r + 1].to_broadcast([128, 128]), C(C1_UW1, 256), start=False, stop=True),
                 reads=[dta_, cs], writes=[PM])
            for st in range(2):
                P.dve(lambda e, r=r, st=st: e.tensor_scalar(out=Dm[:, st, :], in0=PT[:, 256:512], scalar1=negcs[:, st, r:r + 1], scalar2=0.0,
                                                           op0=ALU.add, op1=ALU.min), reads=[PM, negcs], writes=[Dm])
            P.act(lambda e: e.activation(out=dcy[:, :, :], in_=Dm[:, :, :], func=AF.Exp), reads=[Dm], writes=[Dm])
            P.pool(lambda e, sc_=sc_: e.tensor_tensor(out=sc_[:, :, :], in0=Gm[:, :, :], in1=dcy[:, :, :], op=ALU.mult),
                   reads=[Gm, dcy], writes=[sc_])
            if c > 0:
                P.act(lambda e: e.activation(out=E1[:, :], in_=PT[:, 256:512], func=AF.Exp), reads=[PM], writes=[E1])
                P.pool(lambda e, ce_=ce_: e.tensor_tensor(out=ce_[:, :], in0=xcv[:, 5, :], in1=E1[:, :], op=ALU.mult),
                       reads=[xcv, E1], writes=[ce_])
            X = XE if par == 0 else XO
            st_ = stE if par == 0 else stO
            pys = slice((i % 2) * 256, (i % 2 + 1) * 256)
            P.pe(lambda e, X=X, i=i, sc_=sc_, pys=pys, par=par: e.matmul(PY[:, pys], X[:, 0, i * 128:(i + 1) * 128], sc_[:, 0, :],
                                                                      start=(par == 0), stop=False), reads=[X, sc_], writes=[PY])
            P.pe(lambda e, X=X, i=i, sc_=sc_, pys=pys, par=par: e.matmul(PY[:, pys], X[:, 1, i * 128:(i + 1) * 128], sc_[:, 1, :],
                                                                      start=False, stop=(par == 1 and c == 0)), reads=[X, sc_], writes=[PY])
            if c > 0:
                P.pe(lambda e, st_=st_, i=i, ce_=ce_, pys=pys, par=par: e.matmul(PY[:, pys], st_[:, i * 128:(i + 1) * 128], ce_[:, :],
                                                                              start=False, stop=(par == 1)), reads=[st_, ce_], writes=[PY])
            if par == 1:
                P.dve(lambda e, i=i, pys=pys: e.scalar_tensor_tensor(out=yD[:, :], in0=xcv[:, i, :], scalar=pv[:, i:i + 1], in1=PY[:, pys],
                                                                    op0=ALU.mult, op1=ALU.add), reads=[xcv, pv, PY], writes=[yD])
                P.pool(lambda e, i=i: e.tensor_tensor(out=gy[:, i, :], in0=yD[:, :], in1=zs[:, i, :], op=ALU.mult),
                       reads=[yD, zs], writes=[gy])
        P.act(lambda e: e.activation(out=sqg[:, :, :], in_=gy[:, :, :], func=AF.Square), reads=[gy], writes=[sqg])
        for i in range(4):
            P.pe(lambda e, i=i: e.matmul(BJ[:, 0:256], C(C1_ONES, 128), sqg[:, i, :], start=(i == 0), stop=(i == 3)),
                 reads=[cs, sqg], writes=[BJ])
        P.dve(lambda e: e.tensor_scalar(out=rstd[:, :], in0=BJ[:, 0:256], scalar1=1.0 / 512, scalar2=EPS, op0=ALU.mult, op1=ALU.add),
              reads=[BJ], writes=[rstd])
        P.act(lambda e: e.activation(out=rstd[:, :], in_=rstd[:, :], func=AF.Sqrt), reads=[rstd], writes=[rstd])
        P.dve(lambda e: e.reciprocal(out=rstd[:, :], in_=rstd[:, :]), reads=[rstd], writes=[rstd])
        for i in range(4):
            P.dve(lambda e, i=i: e.scalar_tensor_tensor(out=yout[:, i, :], in0=gy[:, i, :], scalar=pv[:, 4 + i:5 + i], in1=rstd[:, :],
                                                       op0=ALU.mult, op1=ALU.mult), reads=[gy, pv, rstd], writes=[yout])
        outs.append(P.dma("sp", YsO[:, :, col0:col0 + 256], yout[:, :, :], reads=[yout], writes=[Yloc_t]))
        if c < NCH - 1:
            for tt in range(2):
                P.pe(lambda e, tt=tt: e.matmul(PG[:, 0:512], Btok[:, tt, :], xdtw[:, tt, :], start=(tt == 0), stop=(tt == 1)),
                     reads=[Btok, xdtw], writes=[PG])
            if c == 0:
                P.dve(lambda e: e.tensor_copy(state[:, :], PG[:, 0:512]), reads=[PG], writes=[state])
            else:
                P.pool(lambda e: e.tensor_tensor(out=state[:, :].rearrange("p (r q) -> p r q", q=64),
                                                in0=state[:, :].rearrange("p (r q) -> p r q", q=64), in1=bc8(dec[:, 0:8]), op=ALU.mult),
                       reads=[state, dec], writes=[state])
                P.dve(lambda e: e.tensor_tensor(out=state[:, :], in0=state[:, :], in1=PG[:, 0:512], op=ALU.add), reads=[state, PG], writes=[state])
            sv = state[:, :].rearrange("p (i two q) -> p i two q", two=2, q=64)
            P.act(lambda e, sv=sv: e.copy(stE[:, :].rearrange("p (i two q) -> p i two q", two=2, q=64)[:, :, 0, :], sv[:, :, 0, :]),
                  reads=[state], writes=[stE])
            P.act(lambda e, sv=sv: e.copy(stO[:, :].rearrange("p (i two q) -> p i two q", two=2, q=64)[:, :, 1, :], sv[:, :, 1, :]),
                  reads=[state], writes=[stO])
        use_gate = c > 3
        if use_gate:
            for h in range(4):
                i, po = h // 2, (h % 2) * 64
                for tt in range(2):
                    sb_ = selb[tt]
                    P.pe(lambda e, i=i, po=po, tt=tt: e.matmul(PM[:, 0:c], qTf[po:po + 64, i, tt * 128:(tt + 1) * 128], kmean[po:po + 64, i, 0:c],
                                                              start=True, stop=True), reads=[qTf, kmean], writes=[PM])
                    P.dve(lambda e: e.tensor_copy(gate[:, 0:c], PM[:, 0:c]), reads=[PM], writes=[gate])
                    P.dve(lambda e: e.max(out=mx8[:, 0:8], in_=gate[:, 0:32]), reads=[gate], writes=[mx8])
                    P.dve(lambda e: e.tensor_scalar(out=selm[:, 0:c], in0=gate[:, 0:c], scalar1=mx8[:, 2:3], scalar2=None, op0=ALU.is_ge),
                          reads=[gate, mx8], writes=[selm])
                    P.dve(lambda e, sb_=sb_: e.tensor_scalar(out=sb_[:, 0:c], in0=selm[:, 0:c], scalar1=-1.0, scalar2=-NEG, op0=ALU.add, op1=ALU.mult),
                          reads=[selm], writes=[sb_])
                    P.pe(lambda e, sb_=sb_, tt=tt: e.matmul(PM[0:32, 64 + tt * 128:64 + (tt + 1) * 128], sb_[:, 0:32], C(C1_ID, 128), start=True, stop=True),
                         reads=[sb_, cs], writes=[PM])
                P.act(lambda e, h=h: e.copy(selT[:, h, :], PM[0:32, 64:320]), reads=[PM], writes=[selT])
        for h in range(4):
            i, po = h // 2, (h % 2) * 64
            for n in range(c + 1):
                PSx = (PS0, PS1)[n % 2]
                pTx = pT[n % 2]
                own = (n == c)
                for kt in range(2):
                    ks = slice(n * 256 + kt * 128, n * 256 + (kt + 1) * 128)
                    has_bias = own or use_gate
                    P.pe(lambda e, PSx=PSx, kt=kt, ks=ks, i=i, po=po, has_bias=has_bias: e.matmul(
                        PSx[:, kt * 256:(kt + 1) * 256], kT_ap[po:po + 64, i, ks], qTb[po:po + 64, i, :], start=True, stop=(not has_bias)),
                        reads=[kTt[n], qTb], writes=[PSx])
                    if own:
                        P.pe(lambda e, PSx=PSx, kt=kt: e.matmul(PSx[:, kt * 256:(kt + 1) * 256], identb[:, :], cbias[:, kt, :], start=False, stop=True),
                             reads=[identb, cbias], writes=[PSx])
                    elif use_gate:
                        P.pe(lambda e, PSx=PSx, kt=kt, n=n, h=h: e.matmul(PSx[:, kt * 256:(kt + 1) * 256], identb[0:32, n:n + 1].to_broadcast([32, 128]), selT[:, h, :],
                                                                        start=False, stop=True), reads=[identb, selT], writes=[PSx])
                P.act(lambda e, PSx=PSx, pTx=pTx: e.activation(out=pTx[:, :, :].rearrange("p a t -> p (a t)"), in_=PSx[:, 0:512], func=AF.Exp),
                      reads=[PSx], writes=[pTx])
                for kt in range(2):
                    first = (n == 0 and kt == 0)
                    last = (n == c and kt == 1)
                    P.pe(lambda e, pTx=pTx, kt=kt, n=n, h=h, first=first, last=last: e.matmul(
                        PO[0:64, 0:256], V_ap[:, 2 * n + kt, h * 64:(h + 1) * 64], pTx[:, kt, :], start=first, stop=last),
                        reads=[Vt[n], pTx], writes=[PO])
                    P.pe(lambda e, pTx=pTx, kt=kt, first=first, last=last: e.matmul(
                        PD[0:64, 0:256], onesb[:, 0:64], pTx[:, kt, :], start=first, stop=last), reads=[onesb, pTx], writes=[PD])
            P.dve(lambda e: e.reciprocal(out=rden[:, :], in_=PD[0:64, 0:256]), reads=[PD], writes=[rden])
            P.dve(lambda e, h=h: e.tensor_tensor(out=yatt[:, h, :], in0=PO[0:64, 0:256], in1=rden[:, :], op=ALU.mult),
                  reads=[PO, rden], writes=[yatt])
        outs.append(P.dma("sp", YaO[:, :, col0:col0 + 256], yatt[:, :, :], reads=[yatt], writes=[Yloc_t]))
    for c in range(NCH):
        do_chunk(c)
    return outs


def p1_inputs(inp, l, g):
    w = inp["w_in"][l]
    hq = 5152 + 4 * g * 64
    hk = 5152 + 1024 + 4 * g * 64
    hv = 5152 + 2048 + 4 * g * 64
    swap = np.concatenate([np.arange(h * 64 + 32, h * 64 + 64).tolist() + np.arange(h * 64, h * 64 + 32).tolist() for h in range(4)])
    w1 = np.concatenate([
        w[:, g * 512:(g + 1) * 512],
        w[:, 2048 + g * 512:2048 + (g + 1) * 512],
        w[:, 4096 + g * 128:4096 + (g + 1) * 128],
        w[:, 4608 + g * 128:4608 + (g + 1) * 128],
        w[:, hq:hq + 256], w[:, hk:hk + 256],
        w[:, hq:hq + 256][:, swap], w[:, hk:hk + 256][:, swap],
        w[:, hv:hv + 256],
        w[:, 5120 + g * 8:5120 + (g + 1) * 8],
    ], axis=1)
    ch = np.concatenate([g * 512 + np.arange(512), 2048 + g * 128 + np.arange(128), 2560 + g * 128 + np.arange(128)])
    cwl = inp["conv_w"][l][:, ch]
    convw = np.ascontiguousarray(cwl.reshape(4, 6, 128).transpose(2, 1, 0).reshape(128, 24))
    convb = np.ascontiguousarray(inp["conv_b"][l][ch].reshape(6, 128).T)
    heads = g * 8 + np.arange(8)
    p = np.arange(128)
    dvec = np.stack([inp["d_skip"][l][g * 8 + 2 * i + (p >= 64)] for i in range(4)], axis=1)
    normw = inp["ssd_norm_w"][l][g * 512:(g + 1) * 512].reshape(4, 128).T
    dtb = np.broadcast_to(inp["dt_bias"][l][heads][None, :], (128, 8))
    alog = np.broadcast_to(inp["a_log"][l][heads][None, :], (128, 8))
    pvec = np.ascontiguousarray(np.concatenate([dvec, normw, dtb, alog], axis=1).astype(np.float32))
    return {
        "w_am": inp["w_ada_mix"][l], "b_am": _col(inp["b_ada_mix"][l], 24),
        "w1": np.ascontiguousarray(w1), "convw": convw, "convb": convb, "pvec": pvec,
    }


P1_SHAPES = {"w_am": [D, 3072], "b_am": [128, 24], "w1": [D, 2568], "convw": [128, 24], "convb": [128, 6], "pvec": [128, 24]}
P2_SHAPES = {"w_af": [D, 3072], "b_af": [128, 24], "w_g": [D, 2048], "w_bs": [2048, D], "w_ba": [D, D], "w_o": [D, D],
             "lnp": [128, 32], "w_r": [D, 36], "b_r": [1, 36], "w_eg": [NEXP, D, 512], "w_eu": [NEXP, D, 512], "w_ed": [NEXP, 512, D]}
GROUPS = [[0, 1, 2, 3], [4, 5, 6, 7]]


def _allgather(P, in_ap, out_ap, reads, writes):
    def fn(e):
        return e.collective_compute("AllGather", ALU.bypass, replica_groups=GROUPS, ins=[in_ap.opt()], outs=[out_ap.opt()])
    return P.add("pool", fn, reads=reads, writes=writes, dma=True, inc=1, semgroup="cc")


def build_fused(S, L, nexp=NEXP):
    nc = bass.Bass("TRN2", target_bir_lowering=False)
    NT = S // 4
    H2 = S // 2
    xT_in = _din(nc, "xT", [D, S]); xs_in = _din(nc, "xs", [D, NT])
    ccol = _din(nc, "ccol", [128, 8]); pos = _din(nc, "pos", [1, S], I32)
    cst1 = _din(nc, "cst1", [128, C1_N]); cst2 = _din(nc, "cst2", [128, 256])
    lw = []
    for l in range(L):
        dct = {k: _din(nc, "%s_%d" % (k, l), shp) for k, shp in P1_SHAPES.items()}
        dct.update({k: _din(nc, "%s_%d" % (k, l), shp) for k, shp in P2_SHAPES.items()})
        lw.append(dct)
    out = _dout(nc, "xoT", [D, NT])
    Yloc = [nc.dram_tensor("Yloc%d" % i, [4, 768, NT], BF16) for i in range(2)]
    Yg = [nc.dram_tensor("Yg%d" % i, [4, 6, 4, 128, NT], BF16) for i in range(2)]
    xo = [nc.dram_tensor("xo%d" % i, [D, NT], F32) for i in range(2)]
    xg = [nc.dram_tensor("xg%d" % i, [8, 4, 128, NT], F32) for i in range(2)]
    x1scr = nc.dram_tensor("x1scr", [D, NT], F32)
    Ym = nc.dram_tensor("Ym", [6, 4, 128, NT], BF16)
    Ym_t = T(None, "Ym")
    Yloc_t = [T(None, "Yloc%d" % i) for i in range(2)]; Yg_t = [T(None, "Yg%d" % i) for i in range(2)]
    xo_t = [T(None, "xo%d" % i) for i in range(2)]; xg_t = [T(None, "xg%d" % i) for i in range(2)]
    x1_t = T(None, "x1scr")

    P = Prog(nc)
    P.use_arena(206 * 1024)
    banks = [P.ps("bank%d" % i, [128, 512]) for i in range(8)]
    outs = []
    for l in range(L):
        par = l % 2
        io = dict(lw[l])
        io.update(ccol=ccol, pos=pos, cst1=cst1, cst2=cst2)
        if l == 0:
            xr = xT_in.rearrange("(k p) t -> p k t", p=128)
            io["x_src"] = lambda c, xr=xr: xr[:, :, c * 256:(c + 1) * 256]
            io["x_t"] = None
        else:
            xga = xg[1 - par].ap()
            io["x_src"] = lambda c, xga=xga: xga[:, (c * 256) // NT, :, (c * 256) % NT:(c * 256) % NT + 256].rearrange("k p t -> p k t")
            io["x_t"] = xg_t[1 - par]
        io["Yloc"] = Yloc[par].ap(); io["Yloc_t"] = Yloc_t[par]
        emit_p1(P, nc, banks, S, io)
        for hh in range(4):
            for rt in range(6):
                _allgather(P, Yloc[par].ap()[hh, rt * 128:(rt + 1) * 128, :],
                           Yg[par].ap()[hh, rt].rearrange("g p t -> (g p) t"), [Yloc_t[par]], [Yg_t[par]])
        io["Yg"] = Yg[par].ap(); io["Yg_t"] = Yg_t[par]
        io["x1scr"] = x1scr.ap(); io["x1scr_t"] = x1_t
        io["Ym"] = Ym.ap(); io["Ym_t"] = Ym_t
        if l == 0:
            io["xs"] = xs_in; io["xs_t"] = None
        else:
            io["xs"] = xo[1 - par].ap(); io["xs_t"] = xo_t[1 - par]
        if l == L - 1:
            io["xo"] = out; io["xo_t"] = None
        else:
            io["xo"] = xo[par].ap(); io["xo_t"] = xo_t[par]
        o2 = emit_p2(P, nc, banks, NT, io, nexp=nexp)
        if l == L - 1:
            outs = o2
        else:
            for kt in range(8):
                _allgather(P, xo[par].ap()[kt * 128:(kt + 1) * 128, :], xg[par].ap()[kt].rearrange("g p t -> (g p) t"),
                           [xo_t[par]], [xg_t[par]])
    counts = P.finish(outs)
    return nc, counts


def fused_inputs(inp, r, S):
    b, g = r // 4, r % 4
    NT = S // 4
    L = inp["w_in"].shape[0]
    xT_b = np.ascontiguousarray(inp["x"][b].T)
    m = {"xT": xT_b, "xs": np.ascontiguousarray(xT_b[:, g * NT:(g + 1) * NT]), "ccol": _col(inp["c"][b], 8),
         "pos": np.ascontiguousarray(inp["positions"][b][None, :]).astype(np.int32), "cst1": _consts1(), "cst2": _consts()}
    for l in range(L):
        for k, v in p1_inputs(inp, l, g).items():
            m["%s_%d" % (k, l)] = np.ascontiguousarray(v, dtype=np.float32)
        for k, v in p2_inputs(inp, l).items():
            m["%s_%d" % (k, l)] = v
    return m


_NC_CACHE = {}


def kernel(**inputs):
    inp = {k: np.asarray(v) for k, v in inputs.items()}
    B, S, _ = inp["x"].shape
    L = inp["w_in"].shape[0]
    assert B == 2
    key = (S, L)
    if key not in _NC_CACHE:
        _NC_CACHE[key] = build_fused(S, L)[0]
    nc = _NC_CACHE[key]
    import concourse.bass_utils as _bu
    shared = {}
    maps = []
    for r in range(8):
        m = fused_inputs(inp, r, S)
        for k in list(m):
            if k.startswith(("w_a", "b_a", "w_g", "w_b", "w_o", "lnp", "w_r", "b_r", "w_e", "cst")):
                m[k] = shared.setdefault(k, m[k])
        maps.append(m)
    res = _bu.run_bass_kernel_spmd(nc, maps, core_ids=list(range(8))).results
    NT = S // 4
    out = np.zeros((2, S, D), np.float32)
    for r in range(8):
        b, g = r // 4, r % 4
        out[b, g * NT:(g + 1) * NT, :] = res[r]["xoT"].T
    return out
```

```python
import numpy as np
import concourse.bass as bass
import concourse.mybir as mybir
from concourse.bass_utils import run_bass_kernel_spmd

F32 = mybir.dt.float32
BF16 = mybir.dt.bfloat16
I32 = mybir.dt.int32
AF = mybir.ActivationFunctionType
ALU = mybir.AluOpType
AX = mybir.AxisListType

ENGS = ("pe", "act", "dve", "pool", "sp")
DMA_POOL = 8


class T:
    __slots__ = ("ap", "w", "r", "name")

    def __init__(self, ap, name=""):
        self.ap = ap
        self.w = None
        self.r = []
        self.name = name

    def __getitem__(self, k):
        return self.ap[k]


class Op:
    __slots__ = ("eng", "fn", "deps", "dma", "idx", "needed", "sem", "val", "slot_prev", "inc")

    def __init__(self, eng, fn, deps, dma):
        self.eng = eng
        self.fn = fn
        self.deps = deps
        self.dma = dma
        self.needed = False
        self.sem = None
        self.val = None
        self.slot_prev = None
        self.inc = 16


class Prog:
    def __init__(self, nc):
        self.nc = nc
        self.ops = {e: [] for e in ENGS}
        self.dma_count = {e: 0 for e in ENGS + ("cc",)}
        self.dma_slots = {e: [None] * DMA_POOL for e in ENGS + ("cc",)}
        self._ctx = []
        self.arena = None
        self.off = 0
        self.fence = []
        self._rank = {}

    def rank(self, engine):
        k = id(engine)
        if k not in self._rank:
            self._rank[k] = engine.snap(engine.partition_id() % 4, min_val=0, max_val=3)
        return self._rank[k]

    def use_arena(self, nbytes):
        g = self.nc.sbuf_tensor("arena_all", [128, nbytes // 2], BF16)
        self.arena = g.__enter__()
        self._ctx.append(g)
        self.arena_bytes = nbytes

    def begin_phase(self):
        self.off = 0
        self.fence = _fence(self)

    def sb(self, name, shape, dt=F32):
        if self.arena is not None:
            esz = 2 if dt == BF16 else 4
            free = 1
            for d_ in shape[1:]:
                free *= d_
            nb = (free * esz + 63) // 64 * 64
            assert self.off + nb <= self.arena_bytes, "SBUF arena overflow at %s (%d + %d)" % (name, self.off, nb)
            ap = self.arena[0:shape[0], self.off // 2:self.off // 2 + free * esz // 2]
            self.off += nb
            if dt != BF16:
                ap = ap.bitcast(dt)
            if len(shape) == 3:
                ap = ap.rearrange("p (a b) -> p a b", a=shape[1])
            t = T(ap, name)
            t.r = list(self.fence)
            return t
        g = self.nc.sbuf_tensor(name, list(shape), dt)
        t = g.__enter__()
        self._ctx.append(g)
        return T(t, name)

    def ps(self, name, shape, dt=F32):
        g = self.nc.psum_tensor(name, list(shape), dt)
        t = g.__enter__()
        self._ctx.append(g)
        return T(t, name)

    def alias(self, ap, name=""):
        return T(ap, name)

    def add(self, eng, fn, reads=(), writes=(), dma=False, inc=16, semgroup=None):
        deps = []
        for t in reads:
            if t.w is not None:
                deps.append((t.w, True))
        for t in writes:
            if t.w is not None:
                deps.append((t.w, False))
            for r in t.r:
                deps.append((r, False))
        op = Op(eng, fn, [], dma)
        op.inc = inc
        seen = set()
        for d, raw in deps:
            if d is op or id(d) in seen:
                continue
            if d.eng == eng and not d.dma and not dma:
                if eng == "pe" or not raw:
                    continue
            seen.add(id(d))
            op.deps.append(d)
            d.needed = True
        if dma:
            sg = semgroup or eng
            k = self.dma_count[sg] % DMA_POOL
            self.dma_count[sg] += 1
            prev = self.dma_slots[sg][k]
            op.slot_prev = prev
            if prev is not None:
                prev.needed = True
            self.dma_slots[sg][k] = op
            op.sem = (sg, k)
            op.needed = True
        op.idx = len(self.ops[eng])
        self.ops[eng].append(op)
        for t in reads:
            t.r.append(op)
        for t in writes:
            t.w = op
            t.r = []
        return op

    def pe(self, fn, reads=(), writes=()):
        return self.add("pe", fn, reads, writes)

    def act(self, fn, reads=(), writes=()):
        return self.add("act", fn, reads, writes)

    def dve(self, fn, reads=(), writes=()):
        return self.add("dve", fn, reads, writes)

    def pool(self, fn, reads=(), writes=()):
        return self.add("pool", fn, reads, writes)

    def dma(self, eng, out_ap, in_ap, reads=(), writes=(), **kw):
        return self.add(eng, lambda e: e.dma_start(out=out_ap, in_=in_ap, **kw), reads, writes, dma=True)

    def finish(self, final_waits=()):
        nc = self.nc
        sem_objs = {}
        stack = []

        def getsem(key):
            if key not in sem_objs:
                g = nc.semaphore("s_%s_%s" % key if isinstance(key, tuple) else "s_%s" % key)
                sem_objs[key] = g.__enter__()
                stack.append(g)
            return sem_objs[key]

        for e in ENGS:
            cnt = 0
            dcnt = {}
            for op in self.ops[e]:
                if op.dma:
                    dcnt[op.sem] = dcnt.get(op.sem, 0) + op.inc
                    op.val = dcnt[op.sem]
                elif op.needed:
                    cnt += 1
                    op.sem = e
                    op.val = cnt
        for op in final_waits:
            op.needed = True
        engmap = {"pe": "tensor", "act": "scalar", "dve": "vector", "pool": "gpsimd", "sp": "sync"}
        prog = self

        def emit(e, engine):
            waited = {}
            ops = prog.ops[e]
            for op in ops:
                need = {}
                dl = list(op.deps)
                if op.slot_prev is not None:
                    dl.append(op.slot_prev)
                for d in dl:
                    if waited.get(d.sem, 0) >= d.val:
                        continue
                    if need.get(d.sem, 0) < d.val:
                        need[d.sem] = d.val
                for s, v in need.items():
                    engine.wait_ge(getsem(s), v)
                    waited[s] = v
                ins = op.fn(engine)
                if op.dma:
                    ins.then_inc(getsem(op.sem), op.inc)
                elif op.needed:
                    ins.then_inc(getsem(op.sem), 1)
            if e == "sp":
                for op in final_waits:
                    if waited.get(op.sem, 0) < op.val:
                        engine.wait_ge(getsem(op.sem), op.val)
                        waited[op.sem] = op.val

        for e in ENGS:
            for op in self.ops[e]:
                if op.sem is not None and (op.needed or op.dma):
                    getsem(op.sem)
        with nc.Block() as block:
            for e in ENGS:
                if not self.ops[e] and e != "sp":
                    continue
                getattr(block, engmap[e])(lambda engine, e=e: emit(e, engine))
        for g in reversed(stack):
            g.__exit__(None, None, None)
        for g in reversed(self._ctx):
            g.__exit__(None, None, None)
        self._ctx = []
        n = {e: len(self.ops[e]) for e in ENGS}
        return n


def _fence(P):
    f = []
    for e in ENGS:
        ops = P.ops[e]
        last_c = None
        nd = 0
        for op in reversed(ops):
            if op.dma:
                if nd < DMA_POOL:
                    f.append(op)
                    nd += 1
            elif last_c is None:
                last_c = op
                f.append(op)
            if nd >= DMA_POOL and last_c is not None:
                break
    return f


def _fenced(ap, fence, name=""):
    t = T(ap, name)
    t.r = list(fence)
    return t


D = 1024
KT = 8
ALPHA = float(8 ** 0.25)
EPS = 1e-5
NEG = -1.0e30
NEXP = 32


def _din(nc, name, shape, dt=F32):
    return nc.dram_tensor(name, list(shape), dt, kind="ExternalInput").ap()


def _dout(nc, name, shape, dt=F32):
    return nc.dram_tensor(name, list(shape), dt, kind="ExternalOutput").ap()


def _adaln(P, w_ap, b_sb, sc, wfull, ps, mod):
    for kt in range(KT):
        P.dma("sp", wfull[:, kt, :], w_ap[kt * 128:(kt + 1) * 128, :], writes=[wfull])
    for ft in range(24):
        for kt in range(KT):
            P.pe(lambda e, kt=kt, ft=ft: e.matmul(
                ps[:, ft:ft + 1], wfull[:, kt, ft * 128:(ft + 1) * 128], sc[:, kt:kt + 1],
                start=(kt == 0), stop=(kt == KT - 1)), reads=[wfull, sc], writes=[ps])
    P.dve(lambda e: e.tensor_tensor(out=mod[:, 0:24], in0=ps[:, 0:24], in1=b_sb[:, 0:24], op=ALU.add),
          reads=[ps, b_sb], writes=[mod])


def _layernorm(P, v, sq, ps1, ps2, ones, tmp, gcol, bcol, out, TB):
    P.act(lambda e: e.activation(out=sq[:, :, 0:TB], in_=v[:, :, 0:TB], func=AF.Square), reads=[v], writes=[sq])
    for ft in range(KT):
        P.pe(lambda e, ft=ft: e.matmul(ps1[:, 0:TB], ones[:, 0:128], v[:, ft, 0:TB], start=(ft == 0), stop=(ft == KT - 1)),
             reads=[v, ones], writes=[ps1])
    for ft in range(KT):
        P.pe(lambda e, ft=ft: e.matmul(ps2[:, 0:TB], ones[:, 0:128], sq[:, ft, 0:TB], start=(ft == 0), stop=(ft == KT - 1)),
             reads=[sq, ones], writes=[ps2])
    mean, msq, rstd = tmp
    P.dve(lambda e: e.tensor_scalar(out=mean[:, 0:TB], in0=ps1[:, 0:TB], scalar1=1.0 / D, scalar2=None, op0=ALU.mult),
          reads=[ps1], writes=[mean])
    P.dve(lambda e: e.tensor_tensor(out=msq[:, 0:TB], in0=mean[:, 0:TB], in1=mean[:, 0:TB], op=ALU.mult),
          reads=[mean], writes=[msq])
    P.dve(lambda e: e.scalar_tensor_tensor(out=rstd[:, 0:TB], in0=ps2[:, 0:TB], scalar=1.0 / D, in1=msq[:, 0:TB],
                                           op0=ALU.mult, op1=ALU.subtract), reads=[ps2, msq], writes=[rstd])
    P.dve(lambda e: e.tensor_scalar(out=rstd[:, 0:TB], in0=rstd[:, 0:TB], scalar1=EPS, scalar2=None,
                                    op0=ALU.add), reads=[rstd], writes=[rstd])
    P.act(lambda e: e.activation(out=rstd[:, 0:TB], in_=rstd[:, 0:TB], func=AF.Sqrt), reads=[rstd], writes=[rstd])
    P.dve(lambda e: e.reciprocal(out=rstd[:, 0:TB], in_=rstd[:, 0:TB]), reads=[rstd], writes=[rstd])
    def bc_t(t):
        return t[:, 0:TB].rearrange("p (o t) -> p o t", o=1).to_broadcast([128, KT, TB])

    def bc_f(t):
        return t[:, 0:KT].rearrange("p (k o) -> p k o", o=1).to_broadcast([128, KT, TB])
    P.dve(lambda e: e.tensor_tensor(out=sq[:, :, 0:TB], in0=v[:, :, 0:TB], in1=bc_t(mean), op=ALU.subtract),
          reads=[v, mean], writes=[sq])
    P.pool(lambda e: e.tensor_tensor(out=sq[:, :, 0:TB], in0=sq[:, :, 0:TB], in1=bc_t(rstd), op=ALU.mult),
           reads=[sq, rstd], writes=[sq])
    P.dve(lambda e: e.tensor_tensor(out=sq[:, :, 0:TB], in0=sq[:, :, 0:TB], in1=bc_f(gcol), op=ALU.mult),
          reads=[sq, gcol], writes=[sq])
    P.pool(lambda e: e.tensor_tensor(out=out[:, :, 0:TB], in0=sq[:, :, 0:TB], in1=bc_f(bcol), op=ALU.add),
           reads=[sq, bcol], writes=[out])


def _routing(P, psR, lgs, rt, Wt, ti):
    gmax, ngmax, gsel, gexp, gsum, gval, pen, msk, mx8, dd, ed, w1, w2, wa, wb = rt
    P.dve(lambda e: e.tensor_copy(lgs[:, 0:36], psR[:, 0:36]), reads=[psR], writes=[lgs])
    P.dve(lambda e: e.reduce_max(out=gmax[:, 0:1], in_=lgs[:, 0:4], axis=AX.X), reads=[lgs], writes=[gmax])
    P.dve(lambda e: e.tensor_scalar(out=ngmax[:, 0:1], in0=gmax[:, 0:1], scalar1=-1.0, scalar2=None, op0=ALU.mult),
          reads=[gmax], writes=[ngmax])
    P.dve(lambda e: e.tensor_scalar(out=gsel[:, 0:4], in0=lgs[:, 0:4], scalar1=gmax[:, 0:1], scalar2=None,
                                    op0=ALU.is_equal), reads=[lgs, gmax], writes=[gsel])
    P.act(lambda e: e.activation(out=gexp[:, 0:4], in_=lgs[:, 0:4], func=AF.Exp, bias=ngmax[:, 0:1], scale=1.0),
          reads=[lgs, ngmax], writes=[gexp])
    P.dve(lambda e: e.reduce_sum(out=gsum[:, 0:1], in_=gexp[:, 0:4], axis=AX.X), reads=[gexp], writes=[gsum])
    P.dve(lambda e: e.reciprocal(out=gval[:, 0:1], in_=gsum[:, 0:1]), reads=[gsum], writes=[gval])
    P.dve(lambda e: e.tensor_scalar(out=pen[:, 0:4], in0=gsel[:, 0:4], scalar1=-1.0, scalar2=-NEG,
                                    op0=ALU.add, op1=ALU.mult), reads=[gsel], writes=[pen])
    P.dve(lambda e: e.tensor_tensor(
        out=msk[:, 0:32].rearrange("p (g x) -> p g x", g=4),
        in0=lgs[:, 4:36].rearrange("p (g x) -> p g x", g=4),
        in1=pen[:, 0:4].rearrange("p (g o) -> p g o", o=1).to_broadcast([128, 4, 8]), op=ALU.add),
        reads=[lgs, pen], writes=[msk])
    P.dve(lambda e: e.max(out=mx8[:, 0:8], in_=msk[:, 0:32]), reads=[msk], writes=[mx8])
    P.dve(lambda e: e.tensor_tensor(out=dd[:, 0:1], in0=mx8[:, 1:2], in1=mx8[:, 0:1], op=ALU.subtract),
          reads=[mx8], writes=[dd])
    P.act(lambda e: e.activation(out=ed[:, 0:1], in_=dd[:, 0:1], func=AF.Exp), reads=[dd], writes=[ed])
    P.dve(lambda e: e.tensor_scalar(out=w1[:, 0:1], in0=ed[:, 0:1], scalar1=1.0, scalar2=None, op0=ALU.add),
          reads=[ed], writes=[w1])
    P.dve(lambda e: e.reciprocal(out=w1[:, 0:1], in_=w1[:, 0:1]), reads=[w1], writes=[w1])
    P.dve(lambda e: e.tensor_tensor(out=w1[:, 0:1], in0=w1[:, 0:1], in1=gval[:, 0:1], op=ALU.mult),
          reads=[w1, gval], writes=[w1])
    P.dve(lambda e: e.tensor_tensor(out=w2[:, 0:1], in0=w1[:, 0:1], in1=ed[:, 0:1], op=ALU.mult),
          reads=[w1, ed], writes=[w2])
    P.dve(lambda e: e.tensor_scalar(out=wa[:, 0:32], in0=msk[:, 0:32], scalar1=mx8[:, 0:1], scalar2=w1[:, 0:1],
                                    op0=ALU.is_equal, op1=ALU.mult), reads=[msk, mx8, w1], writes=[wa])
    P.dve(lambda e: e.tensor_scalar(out=wb[:, 0:32], in0=msk[:, 0:32], scalar1=mx8[:, 1:2], scalar2=w2[:, 0:1],
                                    op0=ALU.is_equal, op1=ALU.mult), reads=[msk, mx8, w2], writes=[wb])
    P.dve(lambda e, ti=ti: e.tensor_tensor(out=Wt[:, ti, 0:32], in0=wa[:, 0:32], in1=wb[:, 0:32], op=ALU.add),
          reads=[wa, wb], writes=[Wt])


def emit_p2(P, nc, banks, NT, io, nexp=NEXP):
    TB = 256
    NB = NT // TB
    NTT = NT // 128
    TE = min(512, NT)
    NBE = NT // TE
    xT = io["xs"]; ccol = io["ccol"]
    w_am = io["w_am"]; b_am = io["b_am"]; w_af = io["w_af"]; b_af = io["b_af"]
    w_g = io["w_g"]; w_bs = io["w_bs"]; w_ba = io["w_ba"]; w_o = io["w_o"]
    lnp = io["lnp"]; w_r = io["w_r"]; b_r = io["b_r"]
    w_eg = io["w_eg"]; w_eu = io["w_eu"]; w_ed = io["w_ed"]
    cst = io["cst2"]; xoT = io["xo"]; Yg_t = io["Yg_t"]
    x1scr_ap = io["x1scr"]; x1scr = io["x1scr_t"]
    xs_t = [io["xs_t"]] if io.get("xs_t") is not None else []
    xo_t = [io["xo_t"]] if io.get("xo_t") is not None else []
    P.begin_phase()
    Yms = io["Yms"]; Yma = io["Yma"]; Ygs = io["Ygs"]; Yga = io["Yga"]; Ym_t = io["Ym_t"]
    CB = io["CB"]
    for ch in range((NT // 256) // CB):
        def cps(e, ch=ch):
            return e.dma_start(out=Yms[ch].rearrange("g c p f -> (g c p f)").rearrange("(a b) -> a b", a=128),
                               in_=Ygs[P.rank(e), ch].rearrange("g c p f -> (g c p f)").rearrange("(a b) -> a b", a=128))
        P.add("sp", cps, reads=[Yg_t], writes=[Ym_t], dma=True)

    def cpa(e):
        return e.dma_start(out=Yma.rearrange("i g p t -> (i g p t)").rearrange("(a b) -> a b", a=128),
                           in_=Yga[P.rank(e)].rearrange("i g p t -> (i g p t)").rearrange("(a b) -> a b", a=128))
    P.add("sp", cpa, reads=[Yg_t], writes=[Ym_t], dma=True)
    arena = P.sb("arena", [128, 49152], BF16)
    arena2 = P.sb("arena2", [128, 26624], BF16)
    h2raw = P.sb("h2raw", [128, 8 * NT], BF16)
    ident = P.sb("ident", [128, 128]); ones = P.sb("ones", [128, 128])
    sc = P.sb("sc", [128, 8]); lnv = [P.sb("lnv%d" % i, [128, 8]) for i in range(4)]
    bam = P.sb("bam", [128, 24]); baf = P.sb("baf", [128, 24])
    modm = P.sb("modm", [128, 24]); modf = P.sb("modf", [128, 24])
    sc1m = P.sb("sc1m", [128, 8]); g1pm = P.sb("g1pm", [128, 8]); sc1f = P.sb("sc1f", [128, 8]); g1pf = P.sb("g1pf", [128, 8])
    wr = P.sb("wr", [128, 8, 36]); br = P.sb("br", [1, 36])
    Wt = P.sb("Wt", [128, NTT, 32])
    gs = [P.sb("gs%d" % i, [128, 2, TB]) for i in range(2)]
    m1 = [P.sb("m1%d" % i, [128, TB]) for i in range(2)]
    m2 = [P.sb("m2%d" % i, [128, TB]) for i in range(2)]
    lntmp = [P.sb("lnt%d" % i, [128, TB]) for i in range(3)]
    lgs = P.sb("lgs", [128, 36])
    rt = [P.sb("rt%d" % i, [128, 32]) for i in range(15)]
    A0, A1, B0, B1, G0, G1, L, R = banks

    P.dma("sp", ident[:, :], cst[:, 0:128], writes=[ident])
    P.dma("sp", ones[:, :], cst[:, 128:256], writes=[ones])
    P.dma("sp", sc[:, :], ccol[:, :], writes=[sc])
    for i in range(4):
        P.dma("sp", lnv[i][:, :], lnp[:, i * 8:(i + 1) * 8], writes=[lnv[i]])
    P.dma("sp", bam[:, :], b_am[:, :], writes=[bam])
    P.dma("sp", baf[:, :], b_af[:, :], writes=[baf])
    P.dma("sp", wr[:, :, :], w_r.rearrange("(k p) n -> p k n", p=128), writes=[wr])
    P.dma("sp", br[:, :], b_r[:, :], writes=[br])
    P.act(lambda e: e.activation(out=sc[:, :], in_=sc[:, :], func=AF.Silu), reads=[sc], writes=[sc])
    wfull = T(arena.ap[:, 0:49152].bitcast(F32).rearrange("p (k f) -> p k f", k=8), "wfull")
    _adaln(P, w_am, bam, sc, wfull, R, modm)
    _adaln(P, w_af, baf, sc, wfull, R, modf)
    for (mod, s1, g1) in ((modm, sc1m, g1pm), (modf, sc1f, g1pf)):
        P.dve(lambda e, mod=mod, s1=s1: e.tensor_scalar(out=s1[:, :], in0=mod[:, 8:16], scalar1=1.0, scalar2=None, op0=ALU.add),
              reads=[mod], writes=[s1])
        P.dve(lambda e, mod=mod, g1=g1: e.tensor_scalar(out=g1[:, :], in0=mod[:, 16:24], scalar1=1.0, scalar2=None, op0=ALU.add),
              reads=[mod], writes=[g1])
    f0 = _fence(P)
    h2b = T(h2raw.ap[:, 0:8 * NT].rearrange("p (k t) -> p k t", k=8), "h2b")

    wg = _fenced(arena.ap[:, 0:16384].rearrange("p (k f) -> p k f", k=8), f0, "wg")
    wbs = _fenced(arena.ap[:, 16384:32768].rearrange("p (k f) -> p k f", k=16), f0, "wbs")
    wba = _fenced(arena.ap[:, 32768:40960].rearrange("p (k f) -> p k f", k=8), f0, "wba")
    wo = _fenced(arena.ap[:, 40960:49152].rearrange("p (k f) -> p k f", k=8), f0, "wo")
    for kt in range(8):
        P.dma("pool", wg[:, kt, :], w_g[kt * 128:(kt + 1) * 128, :], writes=[wg])
    for kt in range(16):
        P.dma("pool", wbs[:, kt, :], w_bs[kt * 128:(kt + 1) * 128, :], writes=[wbs])
    for kt in range(8):
        P.dma("pool", wba[:, kt, :], w_ba[kt * 128:(kt + 1) * 128, :], writes=[wba])
    for kt in range(8):
        P.dma("pool", wo[:, kt, :], w_o[kt * 128:(kt + 1) * 128, :], writes=[wo])

    def a2(off, n, dt, shape_k, name, fence=None):
        ap = arena2.ap[:, off:off + n]
        if dt == F32:
            ap = ap.bitcast(F32)
        ap = ap.rearrange("p (k t) -> p k t", k=shape_k)
        return _fenced(ap, fence, name) if fence is not None else T(ap, name)

    xb = a2(0, 4096, F32, 8, "xb"); v = a2(4096, 4096, F32, 8, "v"); sq = a2(8192, 4096, F32, 8, "sq")
    hT = a2(12288, 2048, BF16, 8, "hT"); ys = a2(14336, 4096, BF16, 16, "ys"); ya = a2(18432, 2048, BF16, 8, "ya")
    mg = a2(20480, 2048, BF16, 8, "mg"); h2f = a2(22528, 4096, F32, 8, "h2f")

    def bc_f(t, lo=0):
        return t[:, lo:lo + 8].rearrange("p (k o) -> p k o", o=1).to_broadcast([128, 8, TB])

    def blk(ap, tb):
        return ap[tb].rearrange("p (k t) -> p k t", k=8)

    for tb in range(NB):
        t0 = tb * TB
        P.dma("sp", xb[:, :, :], blk(xT, tb), reads=xs_t, writes=[xb])
        for gp in range(4):
            P.dma("sp", ys[:, gp * 4:(gp + 1) * 4, :], Yms[tb // CB, gp, tb % CB].rearrange("p (i t) -> p i t", i=4), reads=[Ym_t], writes=[ys])
            P.dma("sp", ya[:, gp * 2:(gp + 1) * 2, :], Yma[0:2, gp, :, t0:t0 + TB].rearrange("i p t -> p i t"), reads=[Ym_t], writes=[ya])
        P.dve(lambda e: e.tensor_tensor(out=v[:, :, :], in0=xb[:, :, :], in1=bc_f(sc1m), op=ALU.mult),
              reads=[xb, sc1m], writes=[v])
        P.dve(lambda e: e.tensor_tensor(out=hT[:, :, :], in0=v[:, :, :], in1=bc_f(modm, 0), op=ALU.add),
              reads=[v, modm], writes=[hT])
        P.pool(lambda e: e.tensor_scalar(out=xb[:, :, :], in0=xb[:, :, :], scalar1=ALPHA, scalar2=None, op0=ALU.mult),
               reads=[xb], writes=[xb])
        for ft in range(8):
            pa, pb, pg = (A0, A1)[ft % 2], (B0, B1)[ft % 2], (G0, G1)[ft % 2]
            gsx, m1x, m2x = gs[ft % 2], m1[ft % 2], m2[ft % 2]
            fs = slice(ft * 128, (ft + 1) * 128)
            fs2 = slice(1024 + ft * 128, 1024 + (ft + 1) * 128)
            for kt in range(16):
                P.pe(lambda e, pa=pa, kt=kt, fs=fs: e.matmul(pa[:, 0:TB], wbs[:, kt, fs], ys[:, kt, :], start=(kt == 0), stop=(kt == 15)),
                     reads=[wbs, ys], writes=[pa])
            for kt in range(8):
                P.pe(lambda e, pb=pb, kt=kt, fs=fs: e.matmul(pb[:, 0:TB], wba[:, kt, fs], ya[:, kt, :], start=(kt == 0), stop=(kt == 7)),
                     reads=[wba, ya], writes=[pb])
            for kt in range(8):
                P.pe(lambda e, pg=pg, kt=kt, fs=fs: e.matmul(pg[:, 0:TB], wg[:, kt, fs], hT[:, kt, :], start=(kt == 0), stop=(kt == 7)),
                     reads=[wg, hT], writes=[pg])
            for kt in range(8):
                P.pe(lambda e, pg=pg, kt=kt, fs2=fs2: e.matmul(pg[:, TB:2 * TB], wg[:, kt, fs2], hT[:, kt, :], start=(kt == 0), stop=(kt == 7)),
                     reads=[wg, hT], writes=[pg])
            P.act(lambda e, pg=pg, gsx=gsx: e.activation(out=gsx[:, :, :].rearrange("p a t -> p (a t)"), in_=pg[:, 0:2 * TB], func=AF.Sigmoid),
                  reads=[pg], writes=[gsx])
            P.dve(lambda e, pa=pa, gsx=gsx, m1x=m1x: e.tensor_tensor(out=m1x[:, :], in0=pa[:, 0:TB], in1=gsx[:, 0, :], op=ALU.mult),
                  reads=[pa, gsx], writes=[m1x])
            P.dve(lambda e, pb=pb, gsx=gsx, m2x=m2x: e.tensor_tensor(out=m2x[:, :], in0=pb[:, 0:TB], in1=gsx[:, 1, :], op=ALU.mult),
                  reads=[pb, gsx], writes=[m2x])
            P.pool(lambda e, ft=ft, m1x=m1x, m2x=m2x: e.tensor_tensor(out=mg[:, ft, :], in0=m1x[:, :], in1=m2x[:, :], op=ALU.add),
                   reads=[m1x, m2x], writes=[mg])
        for ft in range(8):
            pa = (A0, A1)[ft % 2]
            fs = slice(ft * 128, (ft + 1) * 128)
            for kt in range(8):
                P.pe(lambda e, pa=pa, kt=kt, fs=fs: e.matmul(pa[:, 0:TB], wo[:, kt, fs], mg[:, kt, :], start=(kt == 0), stop=(kt == 7)),
                     reads=[wo, mg], writes=[pa])
            P.dve(lambda e, pa=pa, ft=ft: e.scalar_tensor_tensor(out=v[:, ft, :], in0=pa[:, 0:TB], scalar=g1pm[:, ft:ft + 1],
                                                                 in1=xb[:, ft, :], op0=ALU.mult, op1=ALU.add),
                  reads=[pa, g1pm, xb], writes=[v])
        _layernorm(P, v, sq, L, R, ones, lntmp, lnv[0], lnv[1], xb, TB)
        P.dma("sp", blk(x1scr_ap, tb), xb[:, :, :], reads=[xb], writes=[x1scr])
        P.dve(lambda e: e.tensor_tensor(out=v[:, :, :], in0=xb[:, :, :], in1=bc_f(sc1f), op=ALU.mult),
              reads=[xb, sc1f], writes=[v])
        P.dve(lambda e: e.tensor_tensor(out=h2f[:, :, :], in0=v[:, :, :], in1=bc_f(modf, 0), op=ALU.add),
              reads=[v, modf], writes=[h2f])
        P.act(lambda e, t0=t0: e.copy(h2b[:, :, t0:t0 + TB], h2f[:, :, :]), reads=[h2f], writes=[h2b])
        for tt in range(TB // 128):
            ti = tb * (TB // 128) + tt
            for kt in range(8):
                P.pe(lambda e, kt=kt, tt=tt: e.matmul(R[:, 0:36], h2f[:, kt, tt * 128:(tt + 1) * 128], wr[:, kt, :],
                                                     start=(kt == 0), stop=False), reads=[h2f, wr], writes=[R])
            P.pe(lambda e: e.matmul(R[:, 0:36], ones[0:1, 0:128], br[0:1, 0:36], start=False, stop=True),
                 reads=[ones, br], writes=[R])
            _routing(P, R, lgs, rt, Wt, ti)

    fAB = _fence(P)
    acc = _fenced(arena.ap[:, 0:NTT * 2048].bitcast(F32).rearrange("p (t d) -> p t d", t=NTT), fAB, "acc")
    slots = [_fenced(arena.ap[:, 32768:45056], fAB, "slot0"), _fenced(arena2.ap[:, 0:12288], fAB, "slot1")]
    act = _fenced(arena2.ap[:, 12288:12288 + 4 * TE].rearrange("p (k t) -> p k t", k=4), fAB, "act")
    sgs = [_fenced(arena2.ap[:, 14336 + i * 2 * TE:14336 + (i + 1) * 2 * TE].bitcast(F32), fAB, "sg%d" % i) for i in range(2)]
    for ex in range(nexp):
        slot = slots[ex % 2]
        wge = slot.ap[:, 0:4096].rearrange("p (k f) -> p k f", k=8)
        wue = slot.ap[:, 4096:8192].rearrange("p (k f) -> p k f", k=8)
        wde = slot.ap[:, 8192:12288].rearrange("p (k f) -> p k f", k=4)
        P.dma("pool", slot.ap[:, 0:4096], w_eg[ex], writes=[slot])
        P.dma("pool", slot.ap[:, 4096:8192], w_eu[ex], writes=[slot])
        P.dma("pool", slot.ap[:, 8192:12288], w_ed[ex], writes=[slot])
        for tb in range(NBE):
            t0 = tb * TE
            for ff in range(4):
                pg, pu, sg = (A0, A1)[ff % 2], (B0, B1)[ff % 2], sgs[ff % 2]
                fs = slice(ff * 128, (ff + 1) * 128)
                for kt in range(8):
                    P.pe(lambda e, pg=pg, kt=kt, fs=fs, wge=wge, t0=t0: e.matmul(pg[:, 0:TE], wge[:, kt, fs], h2b[:, kt, t0:t0 + TE],
                                                                              start=(kt == 0), stop=(kt == 7)),
                         reads=[slot, h2b], writes=[pg])
                for kt in range(8):
                    P.pe(lambda e, pu=pu, kt=kt, fs=fs, wue=wue, t0=t0: e.matmul(pu[:, 0:TE], wue[:, kt, fs], h2b[:, kt, t0:t0 + TE],
                                                                              start=(kt == 0), stop=(kt == 7)),
                         reads=[slot, h2b], writes=[pu])
                P.act(lambda e, pg=pg, sg=sg: e.activation(out=sg[:, 0:TE], in_=pg[:, 0:TE], func=AF.Silu), reads=[pg], writes=[sg])
                P.dve(lambda e, pu=pu, sg=sg, ff=ff: e.tensor_tensor(out=act[:, ff, :], in0=pu[:, 0:TE], in1=sg[:, 0:TE], op=ALU.mult),
                      reads=[pu, sg], writes=[act])
            for tt in range(TE // 128):
                ti = tb * (TE // 128) + tt
                for dh in range(2):
                    pd = (G0, G1)[dh]
                    for ff in range(4):
                        P.pe(lambda e, pd=pd, ff=ff, tt=tt, dh=dh, wde=wde: e.matmul(
                            pd[:, 0:512], act[:, ff, tt * 128:(tt + 1) * 128], wde[:, ff, dh * 512:(dh + 1) * 512],
                            start=(ff == 0), stop=(ff == 3)), reads=[act, slot], writes=[pd])
                    if ex == 0:
                        P.dve(lambda e, pd=pd, ti=ti, dh=dh, ex=ex: e.tensor_scalar(
                            out=acc[:, ti, dh * 512:(dh + 1) * 512], in0=pd[:, 0:512], scalar1=Wt[:, ti, ex:ex + 1], scalar2=None,
                            op0=ALU.mult), reads=[pd, Wt], writes=[acc])
                    else:
                        P.dve(lambda e, pd=pd, ti=ti, dh=dh, ex=ex: e.scalar_tensor_tensor(
                            out=acc[:, ti, dh * 512:(dh + 1) * 512], in0=pd[:, 0:512], scalar=Wt[:, ti, ex:ex + 1],
                            in1=acc[:, ti, dh * 512:(dh + 1) * 512], op0=ALU.mult, op1=ALU.add), reads=[pd, Wt, acc], writes=[acc])

    fBC = _fence(P)
    xb2 = a2(0, 4096, F32, 8, "xb2", fBC); v2 = a2(4096, 4096, F32, 8, "v2", fBC); sq2 = a2(8192, 4096, F32, 8, "sq2", fBC)
    outs = []
    for tb in range(NB):
        t0 = tb * TB
        P.dma("sp", xb2[:, :, :], blk(x1scr_ap, tb), reads=[x1scr], writes=[xb2])
        P.pool(lambda e: e.tensor_scalar(out=xb2[:, :, :], in0=xb2[:, :, :], scalar1=ALPHA, scalar2=None, op0=ALU.mult),
               reads=[xb2], writes=[xb2])
        for ft in range(8):
            pa = (A0, A1)[ft % 2]
            for tt in range(TB // 128):
                ti = tb * (TB // 128) + tt
                P.pe(lambda e, pa=pa, ti=ti, tt=tt, ft=ft: e.matmul(pa[:, tt * 128:(tt + 1) * 128], acc[:, ti, ft * 128:(ft + 1) * 128],
                                                                   ident[:, 0:128], start=True, stop=True),
                     reads=[acc, ident], writes=[pa])
            P.dve(lambda e, pa=pa, ft=ft: e.scalar_tensor_tensor(out=v2[:, ft, :], in0=pa[:, 0:TB], scalar=g1pf[:, ft:ft + 1],
                                                                 in1=xb2[:, ft, :], op0=ALU.mult, op1=ALU.add),
                  reads=[pa, g1pf, xb2], writes=[v2])
        _layernorm(P, v2, sq2, L, R, ones, lntmp, lnv[2], lnv[3], xb2, TB)
        outs.append(P.dma("sp", blk(xoT, tb), xb2[:, :, :], reads=[xb2], writes=xo_t))
    return outs


def _col(vec, n):
    return np.ascontiguousarray(np.asarray(vec).reshape(n, 128).T)


def _pmajor(w, k):
    E, _, F = w.shape
    return np.ascontiguousarray(w.reshape(E, k, 128, F).transpose(0, 2, 1, 3).reshape(E, 128, k * F))


def _xblocks(xts):
    n = xts.shape[0] // 256
    return np.ascontiguousarray(xts.reshape(n, 256, 8, 128).transpose(0, 3, 2, 1).reshape(n, 128, 2048))


def _xunblocks(xb):
    n = xb.shape[0]
    return np.ascontiguousarray(xb.reshape(n, 128, 8, 256).transpose(0, 3, 2, 1).reshape(n * 256, 1024))


def _consts():
    c = np.zeros((128, 256), np.float32)
    c[:, 0:128] = np.eye(128, dtype=np.float32)
    c[:, 128:256] = 1.0
    return c


def p2_inputs(inp, l):
    return {
        "w_af": inp["w_ada_ffn"][l], "b_af": _col(inp["b_ada_ffn"][l], 24),
        "w_g": np.ascontiguousarray(inp["w_in"][l][:, 8224:10272]),
        "w_bs": inp["w_branch_ssd"][l], "w_ba": inp["w_branch_attn"][l], "w_o": inp["w_out"][l],
        "lnp": np.ascontiguousarray(np.concatenate([_col(inp["ln_mix_g"][l], 8), _col(inp["ln_mix_b"][l], 8),
                                                    _col(inp["ln_ffn_g"][l], 8), _col(inp["ln_ffn_b"][l], 8)], axis=1)),
        "w_r": np.ascontiguousarray(np.concatenate([inp["w_router_group"][l], inp["w_router_expert"][l]], axis=1)),
        "b_r": np.ascontiguousarray(np.concatenate([inp["b_router_group"][l], inp["b_router_expert"][l]])[None, :]),
        "w_eg": _pmajor(inp["w_expert_gate"][l], 8), "w_eu": _pmajor(inp["w_expert_up"][l], 8),
        "w_ed": _pmajor(inp["w_expert_down"][l], 4),
    }


C1_ID, C1_ONES, C1_U, C1_UW0, C1_UW1, C1_CM, C1_CB, C1_MISC, C1_N = 0, 128, 256, 384, 640, 896, 1408, 1920, 1928
W1_NF = 2304
PI = float(np.pi)


def _consts1():
    c = np.zeros((128, C1_N), np.float32)
    c[:, C1_ID:C1_ID + 128] = np.eye(128, dtype=np.float32)
    c[:, C1_ONES:C1_ONES + 128] = 1.0
    U = np.triu(np.ones((128, 128), np.float32))
    c[:, C1_U:C1_U + 128] = U
    c[:, C1_UW0:C1_UW0 + 128] = U
    c[:, C1_UW0 + 128:C1_UW0 + 256] = 1.0
    c[:, C1_UW1 + 128:C1_UW1 + 256] = U
    s = np.arange(128)[:, None]
    l = np.arange(256)[None, :]
    m0 = (l >= s).astype(np.float32)
    m1 = (l >= s + 128).astype(np.float32)
    c[:, C1_CM:C1_CM + 256] = m0
    c[:, C1_CM + 256:C1_CM + 512] = m1
    c[:, C1_CB:C1_CB + 256] = (m0 - 1.0) * 1.0e30
    c[:, C1_CB + 256:C1_CB + 512] = (m1 - 1.0) * 1.0e30
    p = np.arange(128)
    inv_freq = (10000.0 ** (-np.arange(0, 64, 2, dtype=np.float32) / 64)).astype(np.float32)
    c[:, C1_MISC + 0] = inv_freq[p % 32]
    c[:, C1_MISC + 1] = np.where((p % 64) < 32, -1.0, 1.0)
    c[:, C1_MISC + 2] = -PI
    return c


def _esel():
    e = np.zeros((32, 32, 128), np.float32)
    for n in range(32):
        e[n, n, :] = 1.0
    return e.reshape(32, 4096)


def emit_p1(P, nc, banks, S, io):
    NCH = S // 256
    H2 = S // 4
    ccol = io["ccol"]; w_am = io["w_am"]; b_am = io["b_am"]; w1 = io["w1"]
    convw = io["convw"]; convb = io["convb"]; pvec = io["pvec"]; pos = io["pos"]; cst = io["cst1"]
    Yls = io["Yls"]; Yla = io["Yla"]; Yloc_t = io["Yloc_t"]
    NBq = (S // 4) // 256
    x_t = [io["x_t"]] if io.get("x_t") is not None else []
    P.begin_phase()
    big = P.sb("big", [128, max(49152, 20480 + 4 * S)], BF16)
    cs = P.sb("cs", [128, C1_N])
    sc = P.sb("sc", [128, 8]); bam = P.sb("bam", [128, 24]); modm = P.sb("modm", [128, 24]); sc1 = P.sb("sc1", [128, 8])
    cw = P.sb("cw", [128, 24]); cb = P.sb("cb", [128, 6]); pv = P.sb("pv", [128, 24])
    aneg = P.sb("aneg", [128, 8]); wdt = P.sb("wdt", [128, 8, 8])
    BJ, PM, PG, PY, PS0, PS1, PO, PD = banks

    P.dma("sp", cs[:, :], cst[:, :], writes=[cs])
    P.dma("sp", sc[:, :], ccol[:, :], writes=[sc])
    P.dma("sp", bam[:, :], b_am[:, :], writes=[bam])
    P.dma("sp", cw[:, :], convw[:, :], writes=[cw])
    P.dma("sp", cb[:, :], convb[:, :], writes=[cb])
    P.dma("sp", pv[:, :], pvec[:, :], writes=[pv])
    P.dma("sp", wdt[:, :, :], w1.rearrange("(k p) n -> p k n", p=128)[:, :, 2560:2568], writes=[wdt])
    P.act(lambda e: e.activation(out=sc[:, :], in_=sc[:, :], func=AF.Silu), reads=[sc], writes=[sc])
    P.act(lambda e: e.activation(out=aneg[:, :], in_=pv[:, 16:24], func=AF.Exp), reads=[pv], writes=[aneg])
    P.dve(lambda e: e.tensor_scalar(out=aneg[:, :], in0=aneg[:, :], scalar1=-1.0, scalar2=None, op0=ALU.mult),
          reads=[aneg], writes=[aneg])
    wfull = T(big.ap[:, 0:49152].bitcast(F32).rearrange("p (k f) -> p k f", k=8), "wfull")
    _adaln(P, w_am, bam, sc, wfull, PM, modm)
    P.dve(lambda e: e.tensor_scalar(out=sc1[:, :], in0=modm[:, 8:16], scalar1=1.0, scalar2=None, op0=ALU.add),
          reads=[modm], writes=[sc1])
    f0 = _fence(P)
    wsb = _fenced(big.ap[:, 0:20480].rearrange("p (k f) -> p k f", k=8), f0, "wsb")
    kT_ap = big.ap[:, 20480:20480 + 2 * S].rearrange("p (i t) -> p i t", i=2)
    V_ap = big.ap[:, 20480 + 2 * S:20480 + 4 * S].rearrange("p (n f) -> p n f", f=256)
    kTt = [_fenced(kT_ap, f0, "kT%d" % c) for c in range(NCH)]
    Vt = [_fenced(V_ap, f0, "V%d" % c) for c in range(NCH)]
    for kt in range(8):
        P.dma("pool", wsb[:, kt, :], w1[kt * 128:(kt + 1) * 128, 0:2560], writes=[wsb])

    xc = P.sb("xc", [128, 8, 256]); hTf = xc; hTb = P.sb("hTb", [128, 8, 256], BF16)
    zs = P.sb("zs", [128, 4, 256]); cin = P.sb("cin", [128, 6, 259]); xcv = P.sb("xcv", [128, 6, 256])
    BTb = P.sb("BTb", [128, 256], BF16); CTb = P.sb("CTb", [128, 256], BF16)
    posi = P.sb("posi", [128, 256], I32); ang = P.sb("ang", [128, 256]); tm = P.sb("tm", [128, 256])
    cosT = P.sb("cosT", [128, 256]); sinT = P.sb("sinT", [128, 256])
    tA = P.sb("tA", [128, 256]); tB = P.sb("tB", [128, 256]); cacc = tA
    qTf = P.sb("qTf", [128, 2, 256]); qTb = P.sb("qTb", [128, 2, 256], BF16); kTf = P.sb("kTf", [128, 2, 256])
    kmean = P.sb("kmean", [128, 2, 32]); ksum = P.sb("ksum", [128, 2])
    dtr = P.sb("dtr", [128, 2, 8]); dta_ = P.sb("dta", [128, 2, 8]); dtt = [P.sb("dtt%d" % i, [128, 2, 8]) for i in range(4)]
    cssb = P.sb("cssb", [128, 24]); negcs = P.sb("negcs", [128, 2, 8]); wend = P.sb("wend", [128, 2, 8])
    dtw = P.sb("dtw", [128, 2, 8]); dec = P.sb("dec", [128, 8])
    xtok = P.sb("xtok", [128, 2, 512]); XE = P.sb("XE", [128, 2, 512], BF16); XO = P.sb("XO", [128, 2, 512], BF16)
    xdtw = P.sb("xdtw", [128, 2, 512], BF16); Btok = P.sb("Btok", [128, 2, 128], BF16)
    Gm = P.sb("Gm", [128, 2, 256]); Dm = P.sb("Dm", [128, 2, 256]); dcy = Dm
    scT = [P.sb("scT%d" % i, [128, 2, 256], BF16) for i in range(2)]
    E1 = P.sb("E1", [128, 256]); CE = [P.sb("CE%d" % i, [128, 256], BF16) for i in range(2)]
    state = P.sb("state", [128, 512]); stE = P.sb("stE", [128, 512], BF16); stO = P.sb("stO", [128, 512], BF16)
    yD = P.sb("yD", [128, 256]); gy = P.sb("gy", [128, 4, 256]); sqg = P.sb("sqg", [128, 4, 256])
    rstd = P.sb("rstd", [128, 256]); yout = P.sb("yout", [128, 4, 256], BF16)
    gate = P.sb("gate", [128, 32]); mx8 = P.sb("mx8", [128, 8]); selm = P.sb("selm", [128, 32])
    selb = [P.sb("selb%d" % i, [128, 32]) for i in range(2)]
    selT = P.sb("selT", [32, 4, 256], BF16)
    pT = [P.sb("pT%d" % i, [128, 2, 256], BF16) for i in range(2)]
    rden = P.sb("rden", [64, 256]); yatt = P.sb("yatt", [64, 4, 256], BF16)
    onesb = P.sb("onesb", [128, 64], BF16); identb = P.sb("identb", [128, 128], BF16)
    cbias = P.sb("cbias", [128, 2, 256], BF16)

    ident = cs; invf = cs
    def C(off, n):
        return cs[:, off:off + n]

    P.dve(lambda e: e.tensor_copy(onesb[:, :], C(C1_ONES, 64)), reads=[cs], writes=[onesb])
    P.dve(lambda e: e.tensor_copy(identb[:, :], C(C1_ID, 128)), reads=[cs], writes=[identb])
    P.dve(lambda e: e.tensor_copy(cbias[:, :, :].rearrange("p a t -> p (a t)"), C(C1_CB, 512)), reads=[cs], writes=[cbias])
    P.dve(lambda e: e.memset(cin[:, :, :], 0.0), writes=[cin])
    P.pool(lambda e: e.memset(XE[:, :, :], 0.0), writes=[XE])
    P.pool(lambda e: e.memset(XO[:, :, :], 0.0), writes=[XO])
    P.pool(lambda e: e.memset(stE[:, :], 0.0), writes=[stE])
    P.pool(lambda e: e.memset(stO[:, :], 0.0), writes=[stO])
    P.dve(lambda e: e.memset(gate[:, :], NEG), writes=[gate])
    for i in range(2):
        P.dve(lambda e, i=i: e.memset(selb[i][:, :], 0.0), writes=[selb[i]])

    outs = []

    def bc8(ap):
        return ap.rearrange("p (r o) -> p r o", o=1).to_broadcast([128, 8, 64])

    def do_chunk(c):
        t0 = c * 256
        hh, col0 = t0 // H2, t0 % H2
        YsO = Yls[c // NBq, c % NBq].rearrange("p (i t) -> p i t", i=4)
        YaO = Yla[hh].rearrange("(h d) t -> d h t", d=64)
        P.dma("sp", xc[:, :, :], io["x_src"](c), reads=x_t, writes=[xc])
        P.dma("sp", posi[:, :], pos[0:1, t0:t0 + 256].to_broadcast([128, 256]), writes=[posi])
        P.dve(lambda e: e.tensor_tensor(out=hTf[:, :, :], in0=xc[:, :, :],
                                        in1=sc1[:, 0:8].rearrange("p (k o) -> p k o", o=1).to_broadcast([128, 8, 256]), op=ALU.mult),
              reads=[xc, sc1], writes=[xc])
        P.dve(lambda e: e.tensor_tensor(out=hTf[:, :, :], in0=hTf[:, :, :],
                                        in1=modm[:, 0:8].rearrange("p (k o) -> p k o", o=1).to_broadcast([128, 8, 256]), op=ALU.add),
              reads=[hTf, modm], writes=[hTf])
        P.act(lambda e: e.copy(hTb[:, :, :], hTf[:, :, :]), reads=[hTf], writes=[hTb])
        P.dve(lambda e: e.tensor_copy(ang[:, :], posi[:, :]), reads=[posi], writes=[ang])
        P.dve(lambda e: e.tensor_scalar(out=ang[:, :], in0=ang[:, :], scalar1=cs[:, C1_MISC:C1_MISC + 1], scalar2=None, op0=ALU.mult),
              reads=[ang, cs], writes=[ang])
        C1_, C2_ = 6.28125, 2 * PI - 6.28125
        P.dve(lambda e: e.tensor_scalar(out=tm[:, :], in0=ang[:, :], scalar1=1.0 / (2 * PI), scalar2=None, op0=ALU.mult), reads=[ang], writes=[tm])
        P.dve(lambda e: e.tensor_copy(posi[:, :], tm[:, :]), reads=[tm], writes=[posi])
        P.dve(lambda e: e.tensor_copy(tm[:, :], posi[:, :]), reads=[posi], writes=[tm])
        P.dve(lambda e: e.scalar_tensor_tensor(out=ang[:, :], in0=tm[:, :], scalar=-C1_, in1=ang[:, :], op0=ALU.mult, op1=ALU.add),
              reads=[tm, ang], writes=[ang])
        P.dve(lambda e: e.scalar_tensor_tensor(out=ang[:, :], in0=tm[:, :], scalar=-C2_, in1=ang[:, :], op0=ALU.mult, op1=ALU.add),
              reads=[tm, ang], writes=[ang])
        for (shift, dstT) in ((0.0, sinT), (0.5 * PI, cosT)):
            if shift != 0.0:
                P.dve(lambda e, shift=shift: e.tensor_scalar(out=ang[:, :], in0=ang[:, :], scalar1=shift, scalar2=None, op0=ALU.add),
                      reads=[ang], writes=[ang])
            P.dve(lambda e: e.tensor_scalar(out=tm[:, :], in0=ang[:, :], scalar1=PI, scalar2=-2 * PI, op0=ALU.is_gt, op1=ALU.mult),
                  reads=[ang], writes=[tm])
            P.dve(lambda e: e.tensor_tensor(out=ang[:, :], in0=ang[:, :], in1=tm[:, :], op=ALU.add), reads=[ang, tm], writes=[ang])
            P.dve(lambda e: e.tensor_scalar(out=tm[:, :], in0=ang[:, :], scalar1=-PI, scalar2=2 * PI, op0=ALU.is_lt, op1=ALU.mult),
                  reads=[ang], writes=[tm])
            P.dve(lambda e: e.tensor_tensor(out=ang[:, :], in0=ang[:, :], in1=tm[:, :], op=ALU.add), reads=[ang, tm], writes=[ang])
            P.act(lambda e, dstT=dstT: e.activation(out=dstT[:, :], in_=ang[:, :], func=AF.Sin), reads=[ang], writes=[dstT])
        P.dve(lambda e: e.tensor_scalar(out=sinT[:, :], in0=sinT[:, :], scalar1=cs[:, C1_MISC + 1:C1_MISC + 2], scalar2=None, op0=ALU.mult),
              reads=[sinT, cs], writes=[sinT])

        def proj(j, half):
            for kt in range(8):
                P.pe(lambda e, kt=kt: e.matmul(BJ[:, half * 256:(half + 1) * 256], wsb[:, kt, j * 128:(j + 1) * 128], hTb[:, kt, :],
                                               start=(kt == 0), stop=(kt == 7)), reads=[wsb, hTb], writes=[BJ])
        for j in range(4):
            proj(j, j % 2)
            P.act(lambda e, j=j: e.activation(out=zs[:, j, :], in_=BJ[:, (j % 2) * 256:(j % 2 + 1) * 256], func=AF.Silu),
                  reads=[BJ], writes=[zs])
        for jj in range(6):
            proj(4 + jj, jj % 2)
            P.act(lambda e, jj=jj: e.copy(cin[:, jj, 3:259], BJ[:, (jj % 2) * 256:(jj % 2 + 1) * 256]), reads=[BJ], writes=[cin])
        for i in range(2):
            for (jq, js, dst) in ((10 + i, 14 + i, "q"), (12 + i, 16 + i, "k")):
                proj(jq, 0)
                proj(js, 1)
                P.dve(lambda e: e.tensor_tensor(out=tA[:, :], in0=BJ[:, 0:256], in1=cosT[:, :], op=ALU.mult),
                      reads=[BJ, cosT], writes=[tA])
                P.dve(lambda e: e.tensor_tensor(out=tB[:, :], in0=BJ[:, 256:512], in1=sinT[:, :], op=ALU.mult),
                      reads=[BJ, sinT], writes=[tB])
                if dst == "q":
                    P.pool(lambda e, i=i: e.tensor_tensor(out=qTf[:, i, :], in0=tA[:, :], in1=tB[:, :], op=ALU.add),
                           reads=[tA, tB], writes=[qTf])
                    P.act(lambda e, i=i: e.mul(qTb[:, i, :], qTf[:, i, :], 0.125), reads=[qTf], writes=[qTb])
                else:
                    P.pool(lambda e, i=i: e.tensor_tensor(out=kTf[:, i, :], in0=tA[:, :], in1=tB[:, :], op=ALU.add),
                           reads=[tA, tB], writes=[kTf])
                    P.act(lambda e, i=i: e.copy(kT_ap[:, i, t0:t0 + 256], kTf[:, i, :]), reads=[kTf], writes=[kTt[c]])
        P.dve(lambda e: e.reduce_sum(out=ksum[:, 0:2], in_=kTf[:, :, :], axis=AX.X), reads=[kTf], writes=[ksum])
        P.dve(lambda e: e.tensor_scalar(out=kmean[:, :, c], in0=ksum[:, 0:2], scalar1=1.0 / 256, scalar2=None, op0=ALU.mult),
              reads=[ksum], writes=[kmean])
        for tt in range(2):
            for kt in range(8):
                P.pe(lambda e, kt=kt, tt=tt: e.matmul(BJ[:, tt * 256:(tt + 1) * 256], hTb[:, kt, tt * 128:(tt + 1) * 128], wsb[:, kt, 2304:2560],
                                                     start=(kt == 0), stop=(kt == 7)), reads=[hTb, wsb], writes=[BJ])
            P.act(lambda e, tt=tt: e.copy(V_ap[:, 2 * c + tt, :], BJ[:, tt * 256:(tt + 1) * 256]), reads=[BJ], writes=[Vt[c]])
        for tt in range(2):
            for kt in range(8):
                P.pe(lambda e, kt=kt, tt=tt: e.matmul(PM[:, tt * 8:(tt + 1) * 8], hTf[:, kt, tt * 128:(tt + 1) * 128], wdt[:, kt, :],
                                                     start=(kt == 0), stop=(kt == 7)), reads=[hTf, wdt], writes=[PM])
        a_, ab_, e_, l_ = dtt
        P.dve(lambda e: e.tensor_tensor(out=a_[:, :, :], in0=PM[:, 0:16].rearrange("p (t r) -> p t r", t=2),
                                        in1=pv[:, 8:16].rearrange("p (o r) -> p o r", o=1).to_broadcast([128, 2, 8]), op=ALU.add),
              reads=[PM, pv], writes=[a_])
        P.dve(lambda e: e.tensor_scalar(out=ab_[:, :, :], in0=a_[:, :, :], scalar1=-1.0, scalar2=None, op0=ALU.mult), reads=[a_], writes=[ab_])
        P.dve(lambda e: e.tensor_tensor(out=ab_[:, :, :], in0=ab_[:, :, :], in1=a_[:, :, :], op=ALU.min), reads=[a_, ab_], writes=[ab_])
        P.act(lambda e: e.activation(out=e_[:, :, :], in_=ab_[:, :, :], func=AF.Exp), reads=[ab_], writes=[e_])
        P.dve(lambda e: e.tensor_scalar(out=e_[:, :, :], in0=e_[:, :, :], scalar1=1.0, scalar2=None, op0=ALU.add), reads=[e_], writes=[e_])
        P.act(lambda e: e.activation(out=l_[:, :, :], in_=e_[:, :, :], func=AF.Ln), reads=[e_], writes=[l_])
        P.dve(lambda e: e.tensor_scalar(out=a_[:, :, :], in0=a_[:, :, :], scalar1=0.0, scalar2=None, op0=ALU.max), reads=[a_], writes=[a_])
        P.dve(lambda e: e.tensor_tensor(out=dtr[:, :, :], in0=a_[:, :, :], in1=l_[:, :, :], op=ALU.add), reads=[a_, l_], writes=[dtr])
        P.dve(lambda e: e.tensor_tensor(out=dta_[:, :, :], in0=dtr[:, :, :],
                                        in1=aneg[:, 0:8].rearrange("p (o r) -> p o r", o=1).to_broadcast([128, 2, 8]), op=ALU.mult),
              reads=[dtr, aneg], writes=[dta_])
        for jj in range(6):
            P.dve(lambda e, jj=jj: e.tensor_scalar(out=cacc[:, :], in0=cin[:, jj, 0:256], scalar1=cw[:, jj * 4:jj * 4 + 1], scalar2=None, op0=ALU.mult),
                  reads=[cin, cw], writes=[cacc])
            for k in range(1, 4):
                P.dve(lambda e, jj=jj, k=k: e.scalar_tensor_tensor(out=cacc[:, :], in0=cin[:, jj, k:k + 256], scalar=cw[:, jj * 4 + k:jj * 4 + k + 1],
                                                                  in1=cacc[:, :], op0=ALU.mult, op1=ALU.add), reads=[cin, cw, cacc], writes=[cacc])
            P.act(lambda e, jj=jj: e.activation(out=xcv[:, jj, :], in_=cacc[:, :], func=AF.Silu, bias=cb[:, jj:jj + 1], scale=1.0),
                  reads=[cacc, cb], writes=[xcv])
        P.pool(lambda e: e.tensor_copy(cin[:, :, 0:3], cin[:, :, 256:259]), reads=[cin], writes=[cin])
        P.act(lambda e: e.copy(BTb[:, :], xcv[:, 4, :]), reads=[xcv], writes=[BTb])
        P.act(lambda e: e.copy(CTb[:, :], xcv[:, 5, :]), reads=[xcv], writes=[CTb])
        Uc = C(C1_U, 128); On = C(C1_ONES, 128)
        P.pe(lambda e: e.matmul(PM[:, 32:40], Uc, dta_[:, 0, :], start=True, stop=True), reads=[cs, dta_], writes=[PM])
        P.pe(lambda e: e.matmul(PM[:, 40:48], On, dta_[:, 0, :], start=True, stop=False), reads=[cs, dta_], writes=[PM])
        P.pe(lambda e: e.matmul(PM[:, 40:48], Uc, dta_[:, 1, :], start=False, stop=True), reads=[cs, dta_], writes=[PM])
        P.pe(lambda e: e.matmul(PM[:, 48:56], On, dta_[:, 0, :], start=True, stop=False), reads=[cs, dta_], writes=[PM])
        P.pe(lambda e: e.matmul(PM[:, 48:56], On, dta_[:, 1, :], start=False, stop=True), reads=[cs, dta_], writes=[PM])
        P.dve(lambda e: e.tensor_copy(cssb[:, 0:24], PM[:, 32:56]), reads=[PM], writes=[cssb])
        csv = cssb[:, 0:16].rearrange("p (t r) -> p t r", t=2)
        clb = cssb[:, 16:24].rearrange("p (o r) -> p o r", o=1).to_broadcast([128, 2, 8])
        P.dve(lambda e: e.tensor_scalar(out=negcs[:, :, :], in0=csv, scalar1=-1.0, scalar2=None, op0=ALU.mult), reads=[cssb], writes=[negcs])
        P.dve(lambda e: e.tensor_tensor(out=wend[:, :, :], in0=clb, in1=csv, op=ALU.subtract), reads=[cssb], writes=[wend])
        P.act(lambda e: e.activation(out=wend[:, :, :], in_=wend[:, :, :], func=AF.Exp), reads=[wend], writes=[wend])
        P.dve(lambda e: e.tensor_tensor(out=dtw[:, :, :], in0=dtr[:, :, :], in1=wend[:, :, :], op=ALU.mult), reads=[dtr, wend], writes=[dtw])
        P.act(lambda e: e.activation(out=dec[:, :], in_=cssb[:, 16:24], func=AF.Exp), reads=[cssb], writes=[dec])
        for tt in range(2):
            for jj in range(4):
                P.pe(lambda e, tt=tt, jj=jj: e.matmul(PG[:, jj * 128:(jj + 1) * 128], xcv[:, jj, tt * 128:(tt + 1) * 128], C(C1_ID, 128),
                                                     start=True, stop=True), reads=[xcv, cs], writes=[PG])
            P.act(lambda e, tt=tt: e.copy(xtok[:, tt, :], PG[:, 0:512]), reads=[PG], writes=[xtok])
        for tt in range(2):
            P.pe(lambda e, tt=tt: e.matmul(PM[:, 64 + tt * 128:64 + (tt + 1) * 128], xcv[:, 4, tt * 128:(tt + 1) * 128], C(C1_ID, 128),
                                           start=True, stop=True), reads=[xcv, cs], writes=[PM])
        P.act(lambda e: e.copy(Btok[:, :, :].rearrange("p t n -> p (t n)"), PM[:, 64:320]), reads=[PM], writes=[Btok])
        for tt in range(2):
            xv = xtok[:, tt, :].rearrange("p (i two q) -> p i two q", two=2, q=64)
            dv = dtr[:, tt, :].rearrange("p (i two) -> p i two", two=2)
            for par, X in ((0, XE), (1, XO)):
                P.dve(lambda e, tt=tt, par=par, X=X, xv=xv, dv=dv: e.tensor_tensor(
                    out=X[:, tt, :].rearrange("p (i two q) -> p i two q", two=2, q=64)[:, :, par, :],
                    in0=xv[:, :, par, :], in1=dv[:, :, par:par + 1].to_broadcast([128, 4, 64]), op=ALU.mult),
                    reads=[xtok, dtr], writes=[X])
            P.pool(lambda e, tt=tt: e.tensor_tensor(out=xdtw[:, tt, :].rearrange("p (r q) -> p r q", q=64),
                                                   in0=xtok[:, tt, :].rearrange("p (r q) -> p r q", q=64),
                                                   in1=bc8(dtw[:, tt, :]), op=ALU.mult), reads=[xtok, dtw], writes=[xdtw])
        for st in range(2):
            P.pe(lambda e, st=st: e.matmul(PG[:, st * 256:(st + 1) * 256], BTb[:, st * 128:(st + 1) * 128], CTb[:, 0:256],
                                           start=True, stop=True), reads=[BTb, CTb], writes=[PG])
        P.dve(lambda e: e.tensor_tensor(out=Gm[:, :, :].rearrange("p a t -> p (a t)"), in0=PG[:, 0:512], in1=C(C1_CM, 512), op=ALU.mult),
              reads=[PG, cs], writes=[Gm])
        for r in range(8):
            i, par = r // 2, r % 2
            sc_, ce_ = scT[r % 2], CE[r % 2]
            PT = PM
            P.pe(lambda e, r=r: e.matmul(PT[:, 256:512], dta_[:, 0, r:r + 1].to_broadcast([128, 128]), C(C1_UW0, 256), start=True, stop=False),
                 reads=[dta_, cs], writes=[PM])
            P.pe(lambda e, r=r: e.matmul(PT[:, 256:512], dta_[:, 1, r:r + 1].to_broadcast([128, 128]), C(C1_UW1, 256), start=False, stop=True),
                 reads=[dta_, cs], writes=[PM])
            for st in range(2):
                P.dve(lambda e, r=r, st=st: e.tensor_scalar(out=Dm[:, st, :], in0=PT[:, 256:512], scalar1=negcs[:, st, r:r + 1], scalar2=0.0,
                                                           op0=ALU.add, op1=ALU.min), reads=[PM, negcs], writes=[Dm])
            P.act(lambda e: e.activation(out=dcy[:, :, :], in_=Dm[:, :, :], func=AF.Exp), reads=[Dm], writes=[Dm])
            P.pool(lambda e, sc_=sc_: e.tensor_tensor(out=sc_[:, :, :], in0=Gm[:, :, :], in1=dcy[:, :, :], op=ALU.mult),
                   reads=[Gm, dcy], writes=[sc_])
            if c > 0:
                P.act(lambda e: e.activation(out=E1[:, :], in_=PT[:, 256:512], func=AF.Exp), reads=[PM], writes=[E1])
                P.pool(lambda e, ce_=ce_: e.tensor_tensor(out=ce_[:, :], in0=xcv[:, 5, :], in1=E1[:, :], op=ALU.mult),
                       reads=[xcv, E1], writes=[ce_])
            X = XE if par == 0 else XO
            st_ = stE if par == 0 else stO
            pys = slice((i % 2) * 256, (i % 2 + 1) * 256)
            P.pe(lambda e, X=X, i=i, sc_=sc_, pys=pys, par=par: e.matmul(PY[:, pys], X[:, 0, i * 128:(i + 1) * 128], sc_[:, 0, :],
                                                                      start=(par == 0), stop=False), reads=[X, sc_], writes=[PY])
            P.pe(lambda e, X=X, i=i, sc_=sc_, pys=pys, par=par: e.matmul(PY[:, pys], X[:, 1, i * 128:(i + 1) * 128], sc_[:, 1, :],
                                                                      start=False, stop=(par == 1 and c == 0)), reads=[X, sc_], writes=[PY])
            if c > 0:
                P.pe(lambda e, st_=st_, i=i, ce_=ce_, pys=pys, par=par: e.matmul(PY[:, pys], st_[:, i * 128:(i + 1) * 128], ce_[:, :],
                                                                              start=False, stop=(par == 1)), reads=[st_, ce_], writes=[PY])
            if par == 1:
                P.dve(lambda e, i=i, pys=pys: e.scalar_tensor_tensor(out=yD[:, :], in0=xcv[:, i, :], scalar=pv[:, i:i + 1], in1=PY[:, pys],
                                                                    op0=ALU.mult, op1=ALU.add), reads=[xcv, pv, PY], writes=[yD])
                P.pool(lambda e, i=i: e.tensor_tensor(out=gy[:, i, :], in0=yD[:, :], in1=zs[:, i, :], op=ALU.mult),
                       reads=[yD, zs], writes=[gy])
        P.act(lambda e: e.activation(out=sqg[:, :, :], in_=gy[:, :, :], func=AF.Square), reads=[gy], writes=[sqg])
        for i in range(4):
            P.pe(lambda e, i=i: e.matmul(BJ[:, 0:256], C(C1_ONES, 128), sqg[:, i, :], start=(i == 0), stop=(i == 3)),
                 reads=[cs, sqg], writes=[BJ])
        P.dve(lambda e: e.tensor_scalar(out=rstd[:, :], in0=BJ[:, 0:256], scalar1=1.0 / 512, scalar2=EPS, op0=ALU.mult, op1=ALU.add),
              reads=[BJ], writes=[rstd])
        P.act(lambda e: e.activation(out=rstd[:, :], in_=rstd[:, :], func=AF.Sqrt), reads=[rstd], writes=[rstd])
        P.dve(lambda e: e.reciprocal(out=rstd[:, :], in_=rstd[:, :]), reads=[rstd], writes=[rstd])
        for i in range(4):
            P.dve(lambda e, i=i: e.scalar_tensor_tensor(out=yout[:, i, :], in0=gy[:, i, :], scalar=pv[:, 4 + i:5 + i], in1=rstd[:, :],
                                                       op0=ALU.mult, op1=ALU.mult), reads=[gy, pv, rstd], writes=[yout])
        outs.append(P.dma("sp", YsO, yout[:, :, :], reads=[yout], writes=[Yloc_t]))
        if c < NCH - 1:
            for tt in range(2):
                P.pe(lambda e, tt=tt: e.matmul(PG[:, 0:512], Btok[:, tt, :], xdtw[:, tt, :], start=(tt == 0), stop=(tt == 1)),
                     reads=[Btok, xdtw], writes=[PG])
            if c == 0:
                P.dve(lambda e: e.tensor_copy(state[:, :], PG[:, 0:512]), reads=[PG], writes=[state])
            else:
                P.pool(lambda e: e.tensor_tensor(out=state[:, :].rearrange("p (r q) -> p r q", q=64),
                                                in0=state[:, :].rearrange("p (r q) -> p r q", q=64), in1=bc8(dec[:, 0:8]), op=ALU.mult),
                       reads=[state, dec], writes=[state])
                P.dve(lambda e: e.tensor_tensor(out=state[:, :], in0=state[:, :], in1=PG[:, 0:512], op=ALU.add), reads=[state, PG], writes=[state])
            sv = state[:, :].rearrange("p (i two q) -> p i two q", two=2, q=64)
            P.act(lambda e, sv=sv: e.copy(stE[:, :].rearrange("p (i two q) -> p i two q", two=2, q=64)[:, :, 0, :], sv[:, :, 0, :]),
                  reads=[state], writes=[stE])
            P.act(lambda e, sv=sv: e.copy(stO[:, :].rearrange("p (i two q) -> p i two q", two=2, q=64)[:, :, 1, :], sv[:, :, 1, :]),
                  reads=[state], writes=[stO])
        use_gate = c > 3
        if use_gate:
            for h in range(4):
                i, po = h // 2, (h % 2) * 64
                for tt in range(2):
                    sb_ = selb[tt]
                    P.pe(lambda e, i=i, po=po, tt=tt: e.matmul(PM[:, 0:c], qTf[po:po + 64, i, tt * 128:(tt + 1) * 128], kmean[po:po + 64, i, 0:c],
                                                              start=True, stop=True), reads=[qTf, kmean], writes=[PM])
                    P.dve(lambda e: e.tensor_copy(gate[:, 0:c], PM[:, 0:c]), reads=[PM], writes=[gate])
                    P.dve(lambda e: e.max(out=mx8[:, 0:8], in_=gate[:, 0:32]), reads=[gate], writes=[mx8])
                    P.dve(lambda e: e.tensor_scalar(out=selm[:, 0:c], in0=gate[:, 0:c], scalar1=mx8[:, 2:3], scalar2=None, op0=ALU.is_ge),
                          reads=[gate, mx8], writes=[selm])
                    P.dve(lambda e, sb_=sb_: e.tensor_scalar(out=sb_[:, 0:c], in0=selm[:, 0:c], scalar1=-1.0, scalar2=-NEG, op0=ALU.add, op1=ALU.mult),
                          reads=[selm], writes=[sb_])
                    P.pe(lambda e, sb_=sb_, tt=tt: e.matmul(PM[0:32, 64 + tt * 128:64 + (tt + 1) * 128], sb_[:, 0:32], C(C1_ID, 128), start=True, stop=True),
                         reads=[sb_, cs], writes=[PM])
                P.act(lambda e, h=h: e.copy(selT[:, h, :], PM[0:32, 64:320]), reads=[PM], writes=[selT])
        for h in range(4):
            i, po = h // 2, (h % 2) * 64
            for n in range(c + 1):
                PSx = (PS0, PS1)[n % 2]
                pTx = pT[n % 2]
                own = (n == c)
                for kt in range(2):
                    ks = slice(n * 256 + kt * 128, n * 256 + (kt + 1) * 128)
                    has_bias = own or use_gate
                    P.pe(lambda e, PSx=PSx, kt=kt, ks=ks, i=i, po=po, has_bias=has_bias: e.matmul(
                        PSx[:, kt * 256:(kt + 1) * 256], kT_ap[po:po + 64, i, ks], qTb[po:po + 64, i, :], start=True, stop=(not has_bias)),
                        reads=[kTt[n], qTb], writes=[PSx])
                    if own:
                        P.pe(lambda e, PSx=PSx, kt=kt: e.matmul(PSx[:, kt * 256:(kt + 1) * 256], identb[:, :], cbias[:, kt, :], start=False, stop=True),
                             reads=[identb, cbias], writes=[PSx])
                    elif use_gate:
                        P.pe(lambda e, PSx=PSx, kt=kt, n=n, h=h: e.matmul(PSx[:, kt * 256:(kt + 1) * 256], identb[0:32, n:n + 1].to_broadcast([32, 128]), selT[:, h, :],
                                                                        start=False, stop=True), reads=[identb, selT], writes=[PSx])
                P.act(lambda e, PSx=PSx, pTx=pTx: e.activation(out=pTx[:, :, :].rearrange("p a t -> p (a t)"), in_=PSx[:, 0:512], func=AF.Exp),
                      reads=[PSx], writes=[pTx])
                for kt in range(2):
                    first = (n == 0 and kt == 0)
                    last = (n == c and kt == 1)
                    P.pe(lambda e, pTx=pTx, kt=kt, n=n, h=h, first=first, last=last: e.matmul(
                        PO[0:64, 0:256], V_ap[:, 2 * n + kt, h * 64:(h + 1) * 64], pTx[:, kt, :], start=first, stop=last),
                        reads=[Vt[n], pTx], writes=[PO])
                    P.pe(lambda e, pTx=pTx, kt=kt, first=first, last=last: e.matmul(
                        PD[0:64, 0:256], onesb[:, 0:64], pTx[:, kt, :], start=first, stop=last), reads=[onesb, pTx], writes=[PD])
            P.dve(lambda e: e.reciprocal(out=rden[:, :], in_=PD[0:64, 0:256]), reads=[PD], writes=[rden])
            P.dve(lambda e, h=h: e.tensor_tensor(out=yatt[:, h, :], in0=PO[0:64, 0:256], in1=rden[:, :], op=ALU.mult),
                  reads=[PO, rden], writes=[yatt])
        outs.append(P.dma("sp", YaO[:, :, col0:col0 + 256], yatt[:, :, :], reads=[yatt], writes=[Yloc_t]))
    for c in range(NCH):
        do_chunk(c)
    return outs


def p1_inputs(inp, l, g):
    w = inp["w_in"][l]
    hq = 5152 + 4 * g * 64
    hk = 5152 + 1024 + 4 * g * 64
    hv = 5152 + 2048 + 4 * g * 64
    swap = np.concatenate([np.arange(h * 64 + 32, h * 64 + 64).tolist() + np.arange(h * 64, h * 64 + 32).tolist() for h in range(4)])
    w1 = np.concatenate([
        w[:, g * 512:(g + 1) * 512],
        w[:, 2048 + g * 512:2048 + (g + 1) * 512],
        w[:, 4096 + g * 128:4096 + (g + 1) * 128],
        w[:, 4608 + g * 128:4608 + (g + 1) * 128],
        w[:, hq:hq + 256], w[:, hk:hk + 256],
        w[:, hq:hq + 256][:, swap], w[:, hk:hk + 256][:, swap],
        w[:, hv:hv + 256],
        w[:, 5120 + g * 8:5120 + (g + 1) * 8],
    ], axis=1)
    ch = np.concatenate([g * 512 + np.arange(512), 2048 + g * 128 + np.arange(128), 2560 + g * 128 + np.arange(128)])
    cwl = inp["conv_w"][l][:, ch]
    convw = np.ascontiguousarray(cwl.reshape(4, 6, 128).transpose(2, 1, 0).reshape(128, 24))
    convb = np.ascontiguousarray(inp["conv_b"][l][ch].reshape(6, 128).T)
    heads = g * 8 + np.arange(8)
    p = np.arange(128)
    dvec = np.stack([inp["d_skip"][l][g * 8 + 2 * i + (p >= 64)] for i in range(4)], axis=1)
    normw = inp["ssd_norm_w"][l][g * 512:(g + 1) * 512].reshape(4, 128).T
    dtb = np.broadcast_to(inp["dt_bias"][l][heads][None, :], (128, 8))
    alog = np.broadcast_to(inp["a_log"][l][heads][None, :], (128, 8))
    pvec = np.ascontiguousarray(np.concatenate([dvec, normw, dtb, alog], axis=1).astype(np.float32))
    return {
        "w_am": inp["w_ada_mix"][l], "b_am": _col(inp["b_ada_mix"][l], 24),
        "w1": np.ascontiguousarray(w1), "convw": convw, "convb": convb, "pvec": pvec,
    }


P1_SHAPES = {"w_am": [D, 3072], "b_am": [128, 24], "w1": [D, 2568], "convw": [128, 24], "convb": [128, 6], "pvec": [128, 24]}
P2_SHAPES = {"w_af": [D, 3072], "b_af": [128, 24], "w_g": [D, 2048], "w_bs": [2048, D], "w_ba": [D, D], "w_o": [D, D],
             "lnp": [128, 32], "w_r": [D, 36], "b_r": [1, 36], "w_eg": [NEXP, 128, 4096], "w_eu": [NEXP, 128, 4096], "w_ed": [NEXP, 128, 4096]}
GROUPS = [[0, 1, 2, 3], [4, 5, 6, 7]]


def _allgather(P, in_ap, out_ap, reads, writes):
    def fn(e):
        return e.collective_compute("AllGather", ALU.bypass, replica_groups=GROUPS, ins=[in_ap.opt()], outs=[out_ap.opt()])
    return P.add("pool", fn, reads=reads, writes=writes, dma=True, inc=1, semgroup="cc")


def build_fused(S, L, nexp=NEXP):
    nc = bass.Bass("TRN2", target_bir_lowering=False)
    NT = S // 4
    H2 = S // 2
    NB = NT // 256
    xT_in = _din(nc, "xT", [S // 256, 128, 2048]); xs_in = _din(nc, "xs", [NB, 128, 2048])
    ccol = _din(nc, "ccol", [128, 8]); pos = _din(nc, "pos", [1, S], I32)
    cst1 = _din(nc, "cst1", [128, C1_N]); cst2 = _din(nc, "cst2", [128, 256])
    lw = []
    for l in range(L):
        dct = {k: _din(nc, "%s_%d" % (k, l), shp) for k, shp in P1_SHAPES.items()}
        dct.update({k: _din(nc, "%s_%d" % (k, l), shp) for k, shp in P2_SHAPES.items()})
        lw.append(dct)
    out = _dout(nc, "xoT", [NB, 128, 2048])
    CB = min(4, NB)
    NCK = NB // CB
    Yls = [nc.dram_tensor("Yls%d" % i, [4, NB, 128, 1024], BF16) for i in range(2)]
    Yla = [nc.dram_tensor("Yla%d" % i, [4, 256, NT], BF16) for i in range(2)]
    Ygs = [nc.dram_tensor("Ygs%d" % i, [4, NCK, 4, CB, 128, 1024], BF16) for i in range(2)]
    Yga = [nc.dram_tensor("Yga%d" % i, [4, 2, 4, 128, NT], BF16) for i in range(2)]
    xo = [nc.dram_tensor("xo%d" % i, [NB, 128, 2048], F32) for i in range(2)]
    xg = [nc.dram_tensor("xg%d" % i, [NB, 4, 128, 2048], F32) for i in range(2)]
    x1scr = nc.dram_tensor("x1scr", [NB, 128, 2048], F32)
    Yms = nc.dram_tensor("Yms", [NCK, 4, CB, 128, 1024], BF16)
    Yma = nc.dram_tensor("Yma", [2, 4, 128, NT], BF16)
    Ym_t = T(None, "Ym")
    Yloc_t = [T(None, "Yloc%d" % i) for i in range(2)]; Yg_t = [T(None, "Yg%d" % i) for i in range(2)]
    xo_t = [T(None, "xo%d" % i) for i in range(2)]; xg_t = [T(None, "xg%d" % i) for i in range(2)]
    x1_t = T(None, "x1scr")

    P = Prog(nc)
    P.use_arena(206 * 1024)
    banks = [P.ps("bank%d" % i, [128, 512]) for i in range(8)]
    outs = []
    for l in range(L):
        par = l % 2
        io = dict(lw[l])
        io.update(ccol=ccol, pos=pos, cst1=cst1, cst2=cst2)
        if l == 0:
            io["x_src"] = lambda c: xT_in[c].rearrange("p (k t) -> p k t", k=8)
            io["x_t"] = None
        else:
            xga = xg[1 - par].ap()
            io["x_src"] = lambda c, xga=xga: xga[c % NB, c // NB].rearrange("p (k t) -> p k t", k=8)
            io["x_t"] = xg_t[1 - par]
        io["Yls"] = Yls[par].ap(); io["Yla"] = Yla[par].ap(); io["Yloc_t"] = Yloc_t[par]
        emit_p1(P, nc, banks, S, io)
        for hh in range(4):
            for ck in range(NCK):
                _allgather(P, Yls[par].ap()[hh, ck * CB:(ck + 1) * CB].rearrange("c p f -> (c p) f"),
                           Ygs[par].ap()[hh, ck].rearrange("g c p f -> (g c p) f"), [Yloc_t[par]], [Yg_t[par]])
            for rt in range(2):
                _allgather(P, Yla[par].ap()[hh, rt * 128:(rt + 1) * 128, :],
                           Yga[par].ap()[hh, rt].rearrange("g p t -> (g p) t"), [Yloc_t[par]], [Yg_t[par]])
        io["Ygs"] = Ygs[par].ap(); io["Yga"] = Yga[par].ap(); io["Yg_t"] = Yg_t[par]; io["CB"] = CB
        io["x1scr"] = x1scr.ap(); io["x1scr_t"] = x1_t
        io["Yms"] = Yms.ap(); io["Yma"] = Yma.ap(); io["Ym_t"] = Ym_t
        if l == 0:
            io["xs"] = xs_in; io["xs_t"] = None
        else:
            io["xs"] = xo[1 - par].ap(); io["xs_t"] = xo_t[1 - par]
        if l == L - 1:
            io["xo"] = out; io["xo_t"] = None
        else:
            io["xo"] = xo[par].ap(); io["xo_t"] = xo_t[par]
        o2 = emit_p2(P, nc, banks, NT, io, nexp=nexp)
        if l == L - 1:
            outs = o2
        else:
            for tb in range(NB):
                _allgather(P, xo[par].ap()[tb], xg[par].ap()[tb].rearrange("g p f -> (g p) f"), [xo_t[par]], [xg_t[par]])
    counts = P.finish(outs)
    return nc, counts


def fused_inputs(inp, r, S):
    b, g = r // 4, r % 4
    NT = S // 4
    L = inp["w_in"].shape[0]
    xblk = _xblocks(np.asarray(inp["x"][b]))
    nb = NT // 256
    m = {"xT": xblk, "xs": np.ascontiguousarray(xblk[g * nb:(g + 1) * nb]), "ccol": _col(inp["c"][b], 8),
         "pos": np.ascontiguousarray(inp["positions"][b][None, :]).astype(np.int32), "cst1": _consts1(), "cst2": _consts()}
    for l in range(L):
        for k, v in p1_inputs(inp, l, g).items():
            m["%s_%d" % (k, l)] = np.ascontiguousarray(v, dtype=np.float32)
        for k, v in p2_inputs(inp, l).items():
            m["%s_%d" % (k, l)] = v
    return m


_NC_CACHE = {}


def kernel(**inputs):
    inp = {k: np.asarray(v) for k, v in inputs.items()}
    B, S, _ = inp["x"].shape
    L = inp["w_in"].shape[0]
    assert B == 2
    key = (S, L)
    if key not in _NC_CACHE:
        _NC_CACHE[key] = build_fused(S, L)[0]
    nc = _NC_CACHE[key]
    import concourse.bass_utils as _bu
    shared = {}
    maps = []
    for r in range(8):
        m = fused_inputs(inp, r, S)
        for k in list(m):
            if k.startswith(("w_a", "b_a", "w_g", "w_b", "w_o", "lnp", "w_r", "b_r", "w_e", "cst")):
                m[k] = shared.setdefault(k, m[k])
        maps.append(m)
    res = _bu.run_bass_kernel_spmd(nc, maps, core_ids=list(range(8))).results
    NT = S // 4
    out = np.zeros((2, S, D), np.float32)
    for r in range(8):
        b, g = r // 4, r % 4
        out[b, g * NT:(g + 1) * NT, :] = _xunblocks(np.asarray(res[r]["xoT"]))
    return out
```

```python
import numpy as np
import concourse.bass as bass
import concourse.mybir as mybir
from concourse.bass_utils import run_bass_kernel_spmd

F32 = mybir.dt.float32
BF16 = mybir.dt.bfloat16
I32 = mybir.dt.int32
AF = mybir.ActivationFunctionType
ALU = mybir.AluOpType
AX = mybir.AxisListType

ENGS = ("pe", "act", "dve", "pool", "sp")
DMA_POOL = 8


class T:
    __slots__ = ("ap", "w", "r", "name")

    def __init__(self, ap, name=""):
        self.ap = ap
        self.w = None
        self.r = []
        self.name = name

    def __getitem__(self, k):
        return self.ap[k]


class Op:
    __slots__ = ("eng", "fn", "deps", "dma", "idx", "needed", "sem", "val", "slot_prev", "inc")

    def __init__(self, eng, fn, deps, dma):
        self.eng = eng
        self.fn = fn
        self.deps = deps
        self.dma = dma
        self.needed = False
        self.sem = None
        self.val = None
        self.slot_prev = None
        self.inc = 16


class Prog:
    def __init__(self, nc):
        self.nc = nc
        self.ops = {e: [] for e in ENGS}
        self.dma_count = {e: 0 for e in ENGS + ("cc",)}
        self.dma_slots = {e: [None] * DMA_POOL for e in ENGS + ("cc",)}
        self._ctx = []
        self.arena = None
        self.off = 0
        self.fence = []
        self._rank = {}

    def rank(self, engine):
        k = id(engine)
        if k not in self._rank:
            self._rank[k] = engine.snap(engine.partition_id() % 4, min_val=0, max_val=3)
        return self._rank[k]

    def use_arena(self, nbytes):
        g = self.nc.sbuf_tensor("arena_all", [128, nbytes // 2], BF16)
        self.arena = g.__enter__()
        self._ctx.append(g)
        self.arena_bytes = nbytes

    def begin_phase(self):
        self.off = 0
        self.fence = _fence(self)

    def sb(self, name, shape, dt=F32):
        if self.arena is not None:
            esz = 2 if dt == BF16 else 4
            free = 1
            for d_ in shape[1:]:
                free *= d_
            nb = (free * esz + 63) // 64 * 64
            assert self.off + nb <= self.arena_bytes, "SBUF arena overflow at %s (%d + %d)" % (name, self.off, nb)
            ap = self.arena[0:shape[0], self.off // 2:self.off // 2 + free * esz // 2]
            self.off += nb
            if dt != BF16:
                ap = ap.bitcast(dt)
            if len(shape) == 3:
                ap = ap.rearrange("p (a b) -> p a b", a=shape[1])
            t = T(ap, name)
            t.r = list(self.fence)
            return t
        g = self.nc.sbuf_tensor(name, list(shape), dt)
        t = g.__enter__()
        self._ctx.append(g)
        return T(t, name)

    def ps(self, name, shape, dt=F32):
        g = self.nc.psum_tensor(name, list(shape), dt)
        t = g.__enter__()
        self._ctx.append(g)
        return T(t, name)

    def alias(self, ap, name=""):
        return T(ap, name)

    def add(self, eng, fn, reads=(), writes=(), dma=False, inc=16, semgroup=None):
        deps = []
        for t in reads:
            if t.w is not None:
                deps.append((t.w, True))
        for t in writes:
            if t.w is not None:
                deps.append((t.w, False))
            for r in t.r:
                deps.append((r, False))
        op = Op(eng, fn, [], dma)
        op.inc = inc
        seen = set()
        for d, raw in deps:
            if d is op or id(d) in seen:
                continue
            if d.eng == eng and not d.dma and not dma:
                if eng == "pe" or not raw:
                    continue
            seen.add(id(d))
            op.deps.append(d)
            d.needed = True
        if dma:
            sg = semgroup or eng
            k = self.dma_count[sg] % DMA_POOL
            self.dma_count[sg] += 1
            prev = self.dma_slots[sg][k]
            op.slot_prev = prev
            if prev is not None:
                prev.needed = True
            self.dma_slots[sg][k] = op
            op.sem = (sg, k)
            op.needed = True
        op.idx = len(self.ops[eng])
        self.ops[eng].append(op)
        for t in reads:
            t.r.append(op)
        for t in writes:
            t.w = op
            t.r = []
        return op

    def pe(self, fn, reads=(), writes=()):
        return self.add("pe", fn, reads, writes)

    def act(self, fn, reads=(), writes=()):
        return self.add("act", fn, reads, writes)

    def dve(self, fn, reads=(), writes=()):
        return self.add("dve", fn, reads, writes)

    def pool(self, fn, reads=(), writes=()):
        return self.add("pool", fn, reads, writes)

    def dma(self, eng, out_ap, in_ap, reads=(), writes=(), **kw):
        return self.add(eng, lambda e: e.dma_start(out=out_ap, in_=in_ap, **kw), reads, writes, dma=True)

    def finish(self, final_waits=()):
        nc = self.nc
        sem_objs = {}
        stack = []

        def getsem(key):
            if key not in sem_objs:
                g = nc.semaphore("s_%s_%s" % key if isinstance(key, tuple) else "s_%s" % key)
                sem_objs[key] = g.__enter__()
                stack.append(g)
            return sem_objs[key]

        for e in ENGS:
            cnt = 0
            dcnt = {}
            for op in self.ops[e]:
                if op.dma:
                    dcnt[op.sem] = dcnt.get(op.sem, 0) + op.inc
                    op.val = dcnt[op.sem]
                elif op.needed:
                    cnt += 1
                    op.sem = e
                    op.val = cnt
        for op in final_waits:
            op.needed = True
        engmap = {"pe": "tensor", "act": "scalar", "dve": "vector", "pool": "gpsimd", "sp": "sync"}
        prog = self

        def emit(e, engine):
            waited = {}
            ops = prog.ops[e]
            for op in ops:
                need = {}
                dl = list(op.deps)
                if op.slot_prev is not None:
                    dl.append(op.slot_prev)
                for d in dl:
                    if waited.get(d.sem, 0) >= d.val:
                        continue
                    if need.get(d.sem, 0) < d.val:
                        need[d.sem] = d.val
                for s, v in need.items():
                    engine.wait_ge(getsem(s), v)
                    waited[s] = v
                ins = op.fn(engine)
                if op.dma:
                    ins.then_inc(getsem(op.sem), op.inc)
                elif op.needed:
                    ins.then_inc(getsem(op.sem), 1)
            if e == "sp":
                for op in final_waits:
                    if waited.get(op.sem, 0) < op.val:
                        engine.wait_ge(getsem(op.sem), op.val)
                        waited[op.sem] = op.val

        for e in ENGS:
            for op in self.ops[e]:
                if op.sem is not None and (op.needed or op.dma):
                    getsem(op.sem)
        with nc.Block() as block:
            for e in ENGS:
                if not self.ops[e] and e != "sp":
                    continue
                getattr(block, engmap[e])(lambda engine, e=e: emit(e, engine))
        for g in reversed(stack):
            g.__exit__(None, None, None)
        for g in reversed(self._ctx):
            g.__exit__(None, None, None)
        self._ctx = []
        n = {e: len(self.ops[e]) for e in ENGS}
        return n


def _fence(P):
    f = []
    for e in ENGS:
        ops = P.ops[e]
        last_c = None
        nd = 0
        for op in reversed(ops):
            if op.dma:
                if nd < DMA_POOL:
                    f.append(op)
                    nd += 1
            elif last_c is None:
                last_c = op
                f.append(op)
            if nd >= DMA_POOL and last_c is not None:
                break
    return f


def _fenced(ap, fence, name=""):
    t = T(ap, name)
    t.r = list(fence)
    return t


D = 1024
KT = 8
ALPHA = float(8 ** 0.25)
EPS = 1e-5
NEG = -1.0e30
NEXP = 32


def _din(nc, name, shape, dt=F32):
    return nc.dram_tensor(name, list(shape), dt, kind="ExternalInput").ap()


def _dout(nc, name, shape, dt=F32):
    return nc.dram_tensor(name, list(shape), dt, kind="ExternalOutput").ap()


def _adaln(P, w_ap, b_sb, sc, wfull, ps, mod):
    for kt in range(KT):
        P.dma("sp", wfull[:, kt, :], w_ap[kt * 128:(kt + 1) * 128, :], writes=[wfull])
    for ft in range(24):
        for kt in range(KT):
            P.pe(lambda e, kt=kt, ft=ft: e.matmul(
                ps[:, ft:ft + 1], wfull[:, kt, ft * 128:(ft + 1) * 128], sc[:, kt:kt + 1],
                start=(kt == 0), stop=(kt == KT - 1)), reads=[wfull, sc], writes=[ps])
    P.dve(lambda e: e.tensor_tensor(out=mod[:, 0:24], in0=ps[:, 0:24], in1=b_sb[:, 0:24], op=ALU.add),
          reads=[ps, b_sb], writes=[mod])


def _layernorm(P, v, sq, ps1, ps2, ones, tmp, gcol, bcol, out, TB):
    P.act(lambda e: e.activation(out=sq[:, :, 0:TB], in_=v[:, :, 0:TB], func=AF.Square), reads=[v], writes=[sq])
    for ft in range(KT):
        P.pe(lambda e, ft=ft: e.matmul(ps1[:, 0:TB], ones[:, 0:128], v[:, ft, 0:TB], start=(ft == 0), stop=(ft == KT - 1)),
             reads=[v, ones], writes=[ps1])
    for ft in range(KT):
        P.pe(lambda e, ft=ft: e.matmul(ps2[:, 0:TB], ones[:, 0:128], sq[:, ft, 0:TB], start=(ft == 0), stop=(ft == KT - 1)),
             reads=[sq, ones], writes=[ps2])
    mean, msq, rstd = tmp
    P.dve(lambda e: e.tensor_scalar(out=mean[:, 0:TB], in0=ps1[:, 0:TB], scalar1=1.0 / D, scalar2=None, op0=ALU.mult),
          reads=[ps1], writes=[mean])
    P.dve(lambda e: e.tensor_tensor(out=msq[:, 0:TB], in0=mean[:, 0:TB], in1=mean[:, 0:TB], op=ALU.mult),
          reads=[mean], writes=[msq])
    P.dve(lambda e: e.scalar_tensor_tensor(out=rstd[:, 0:TB], in0=ps2[:, 0:TB], scalar=1.0 / D, in1=msq[:, 0:TB],
                                           op0=ALU.mult, op1=ALU.subtract), reads=[ps2, msq], writes=[rstd])
    P.dve(lambda e: e.tensor_scalar(out=rstd[:, 0:TB], in0=rstd[:, 0:TB], scalar1=EPS, scalar2=None,
                                    op0=ALU.add), reads=[rstd], writes=[rstd])
    P.act(lambda e: e.activation(out=rstd[:, 0:TB], in_=rstd[:, 0:TB], func=AF.Sqrt), reads=[rstd], writes=[rstd])
    P.dve(lambda e: e.reciprocal(out=rstd[:, 0:TB], in_=rstd[:, 0:TB]), reads=[rstd], writes=[rstd])
    def bc_t(t):
        return t[:, 0:TB].rearrange("p (o t) -> p o t", o=1).to_broadcast([128, KT, TB])

    def bc_f(t):
        return t[:, 0:KT].rearrange("p (k o) -> p k o", o=1).to_broadcast([128, KT, TB])
    P.dve(lambda e: e.tensor_tensor(out=sq[:, :, 0:TB], in0=v[:, :, 0:TB], in1=bc_t(mean), op=ALU.subtract),
          reads=[v, mean], writes=[sq])
    P.pool(lambda e: e.tensor_tensor(out=sq[:, :, 0:TB], in0=sq[:, :, 0:TB], in1=bc_t(rstd), op=ALU.mult),
           reads=[sq, rstd], writes=[sq])
    P.dve(lambda e: e.tensor_tensor(out=sq[:, :, 0:TB], in0=sq[:, :, 0:TB], in1=bc_f(gcol), op=ALU.mult),
          reads=[sq, gcol], writes=[sq])
    P.pool(lambda e: e.tensor_tensor(out=out[:, :, 0:TB], in0=sq[:, :, 0:TB], in1=bc_f(bcol), op=ALU.add),
           reads=[sq, bcol], writes=[out])


def _routing(P, psR, lgs, rt, Wt, ti):
    gmax, ngmax, gsel, gexp, gsum, gval, pen, msk, mx8, dd, ed, w1, w2, wa, wb = rt
    P.dve(lambda e: e.tensor_copy(lgs[:, 0:36], psR[:, 0:36]), reads=[psR], writes=[lgs])
    P.dve(lambda e: e.reduce_max(out=gmax[:, 0:1], in_=lgs[:, 0:4], axis=AX.X), reads=[lgs], writes=[gmax])
    P.dve(lambda e: e.tensor_scalar(out=ngmax[:, 0:1], in0=gmax[:, 0:1], scalar1=-1.0, scalar2=None, op0=ALU.mult),
          reads=[gmax], writes=[ngmax])
    P.dve(lambda e: e.tensor_scalar(out=gsel[:, 0:4], in0=lgs[:, 0:4], scalar1=gmax[:, 0:1], scalar2=None,
                                    op0=ALU.is_equal), reads=[lgs, gmax], writes=[gsel])
    P.act(lambda e: e.activation(out=gexp[:, 0:4], in_=lgs[:, 0:4], func=AF.Exp, bias=ngmax[:, 0:1], scale=1.0),
          reads=[lgs, ngmax], writes=[gexp])
    P.dve(lambda e: e.reduce_sum(out=gsum[:, 0:1], in_=gexp[:, 0:4], axis=AX.X), reads=[gexp], writes=[gsum])
    P.dve(lambda e: e.reciprocal(out=gval[:, 0:1], in_=gsum[:, 0:1]), reads=[gsum], writes=[gval])
    P.dve(lambda e: e.tensor_scalar(out=pen[:, 0:4], in0=gsel[:, 0:4], scalar1=-1.0, scalar2=-NEG,
                                    op0=ALU.add, op1=ALU.mult), reads=[gsel], writes=[pen])
    P.dve(lambda e: e.tensor_tensor(
        out=msk[:, 0:32].rearrange("p (g x) -> p g x", g=4),
        in0=lgs[:, 4:36].rearrange("p (g x) -> p g x", g=4),
        in1=pen[:, 0:4].rearrange("p (g o) -> p g o", o=1).to_broadcast([128, 4, 8]), op=ALU.add),
        reads=[lgs, pen], writes=[msk])
    P.dve(lambda e: e.max(out=mx8[:, 0:8], in_=msk[:, 0:32]), reads=[msk], writes=[mx8])
    P.dve(lambda e: e.tensor_tensor(out=dd[:, 0:1], in0=mx8[:, 1:2], in1=mx8[:, 0:1], op=ALU.subtract),
          reads=[mx8], writes=[dd])
    P.act(lambda e: e.activation(out=ed[:, 0:1], in_=dd[:, 0:1], func=AF.Exp), reads=[dd], writes=[ed])
    P.dve(lambda e: e.tensor_scalar(out=w1[:, 0:1], in0=ed[:, 0:1], scalar1=1.0, scalar2=None, op0=ALU.add),
          reads=[ed], writes=[w1])
    P.dve(lambda e: e.reciprocal(out=w1[:, 0:1], in_=w1[:, 0:1]), reads=[w1], writes=[w1])
    P.dve(lambda e: e.tensor_tensor(out=w1[:, 0:1], in0=w1[:, 0:1], in1=gval[:, 0:1], op=ALU.mult),
          reads=[w1, gval], writes=[w1])
    P.dve(lambda e: e.tensor_tensor(out=w2[:, 0:1], in0=w1[:, 0:1], in1=ed[:, 0:1], op=ALU.mult),
          reads=[w1, ed], writes=[w2])
    P.dve(lambda e: e.tensor_scalar(out=wa[:, 0:32], in0=msk[:, 0:32], scalar1=mx8[:, 0:1], scalar2=w1[:, 0:1],
                                    op0=ALU.is_equal, op1=ALU.mult), reads=[msk, mx8, w1], writes=[wa])
    P.dve(lambda e: e.tensor_scalar(out=wb[:, 0:32], in0=msk[:, 0:32], scalar1=mx8[:, 1:2], scalar2=w2[:, 0:1],
                                    op0=ALU.is_equal, op1=ALU.mult), reads=[msk, mx8, w2], writes=[wb])
    P.dve(lambda e, ti=ti: e.tensor_tensor(out=Wt[:, ti, 0:32], in0=wa[:, 0:32], in1=wb[:, 0:32], op=ALU.add),
          reads=[wa, wb], writes=[Wt])


def emit_p2(P, nc, banks, NT, io, nexp=NEXP):
    TB = 256
    NB = NT // TB
    NTT = NT // 128
    TE = min(512, NT)
    NBE = NT // TE
    xT = io["xs"]; ccol = io["ccol"]
    w_am = io["w_am"]; b_am = io["b_am"]; w_af = io["w_af"]; b_af = io["b_af"]
    w_g = io["w_g"]; w_bs = io["w_bs"]; w_ba = io["w_ba"]; w_o = io["w_o"]
    lnp = io["lnp"]; w_r = io["w_r"]; b_r = io["b_r"]
    w_eg = io["w_eg"]; w_eu = io["w_eu"]; w_ed = io["w_ed"]
    cst = io["cst2"]; xoT = io["xo"]; Yg_t = io["Yg_t"]
    x1scr_ap = io["x1scr"]; x1scr = io["x1scr_t"]
    xs_t = [io["xs_t"]] if io.get("xs_t") is not None else []
    xo_t = [io["xo_t"]] if io.get("xo_t") is not None else []
    P.begin_phase()
    Yms = io["Yms"]; Yma = io["Yma"]; Ygs = io["Ygs"]; Yga = io["Yga"]; Ym_t = io["Ym_t"]
    CB = io["CB"]
    for ch in range((NT // 256) // CB):
        def cps(e, ch=ch):
            return e.dma_start(out=Yms[ch].rearrange("g c p f -> (g c p f)").rearrange("(a b) -> a b", a=128),
                               in_=Ygs[P.rank(e), ch].rearrange("g c p f -> (g c p f)").rearrange("(a b) -> a b", a=128))
        P.add("sp", cps, reads=[Yg_t], writes=[Ym_t], dma=True)

    def cpa(e):
        return e.dma_start(out=Yma.rearrange("i g p t -> (i g p t)").rearrange("(a b) -> a b", a=128),
                           in_=Yga[P.rank(e)].rearrange("i g p t -> (i g p t)").rearrange("(a b) -> a b", a=128))
    P.add("sp", cpa, reads=[Yg_t], writes=[Ym_t], dma=True)
    arena = P.sb("arena", [128, 49152], BF16)
    arena2 = P.sb("arena2", [128, 26624], BF16)
    h2raw = P.sb("h2raw", [128, 8 * NT], BF16)
    ident = P.sb("ident", [128, 128]); ones = P.sb("ones", [128, 128])
    sc = P.sb("sc", [128, 8]); lnv = [P.sb("lnv%d" % i, [128, 8]) for i in range(4)]
    bam = P.sb("bam", [128, 24]); baf = P.sb("baf", [128, 24])
    modm = P.sb("modm", [128, 24]); modf = P.sb("modf", [128, 24])
    sc1m = P.sb("sc1m", [128, 8]); g1pm = P.sb("g1pm", [128, 8]); sc1f = P.sb("sc1f", [128, 8]); g1pf = P.sb("g1pf", [128, 8])
    wr = P.sb("wr", [128, 8, 36]); br = P.sb("br", [1, 36])
    Wt = P.sb("Wt", [128, NTT, 32])
    gs = [P.sb("gs%d" % i, [128, 2, TB]) for i in range(2)]
    m1 = [P.sb("m1%d" % i, [128, TB]) for i in range(2)]
    m2 = [P.sb("m2%d" % i, [128, TB]) for i in range(2)]
    lntmp = [P.sb("lnt%d" % i, [128, TB]) for i in range(3)]
    lgs = P.sb("lgs", [128, 36])
    rt = [P.sb("rt%d" % i, [128, 32]) for i in range(15)]
    A0, A1, B0, B1, G0, G1, L, R = banks

    P.dma("sp", ident[:, :], cst[:, 0:128], writes=[ident])
    P.dma("sp", ones[:, :], cst[:, 128:256], writes=[ones])
    P.dma("sp", sc[:, :], ccol[:, :], writes=[sc])
    for i in range(4):
        P.dma("sp", lnv[i][:, :], lnp[:, i * 8:(i + 1) * 8], writes=[lnv[i]])
    P.dma("sp", bam[:, :], b_am[:, :], writes=[bam])
    P.dma("sp", baf[:, :], b_af[:, :], writes=[baf])
    P.dma("sp", wr[:, :, :], w_r.rearrange("(k p) n -> p k n", p=128), writes=[wr])
    P.dma("sp", br[:, :], b_r[:, :], writes=[br])
    P.act(lambda e: e.activation(out=sc[:, :], in_=sc[:, :], func=AF.Silu), reads=[sc], writes=[sc])
    wfull = T(arena.ap[:, 0:49152].bitcast(F32).rearrange("p (k f) -> p k f", k=8), "wfull")
    _adaln(P, w_am, bam, sc, wfull, R, modm)
    _adaln(P, w_af, baf, sc, wfull, R, modf)
    for (mod, s1, g1) in ((modm, sc1m, g1pm), (modf, sc1f, g1pf)):
        P.dve(lambda e, mod=mod, s1=s1: e.tensor_scalar(out=s1[:, :], in0=mod[:, 8:16], scalar1=1.0, scalar2=None, op0=ALU.add),
              reads=[mod], writes=[s1])
        P.dve(lambda e, mod=mod, g1=g1: e.tensor_scalar(out=g1[:, :], in0=mod[:, 16:24], scalar1=1.0, scalar2=None, op0=ALU.add),
              reads=[mod], writes=[g1])
    f0 = _fence(P)
    h2b = T(h2raw.ap[:, 0:8 * NT].rearrange("p (k t) -> p k t", k=8), "h2b")

    wg = _fenced(arena.ap[:, 0:16384].rearrange("p (k f) -> p k f", k=8), f0, "wg")
    wbs = _fenced(arena.ap[:, 16384:32768].rearrange("p (k f) -> p k f", k=16), f0, "wbs")
    wba = _fenced(arena.ap[:, 32768:40960].rearrange("p (k f) -> p k f", k=8), f0, "wba")
    wo = _fenced(arena.ap[:, 40960:49152].rearrange("p (k f) -> p k f", k=8), f0, "wo")
    for kt in range(8):
        P.dma("pool", wg[:, kt, :], w_g[kt * 128:(kt + 1) * 128, :], writes=[wg])
    for kt in range(16):
        P.dma("pool", wbs[:, kt, :], w_bs[kt * 128:(kt + 1) * 128, :], writes=[wbs])
    for kt in range(8):
        P.dma("pool", wba[:, kt, :], w_ba[kt * 128:(kt + 1) * 128, :], writes=[wba])
    for kt in range(8):
        P.dma("pool", wo[:, kt, :], w_o[kt * 128:(kt + 1) * 128, :], writes=[wo])

    def a2(off, n, dt, shape_k, name, fence=None):
        ap = arena2.ap[:, off:off + n]
        if dt == F32:
            ap = ap.bitcast(F32)
        ap = ap.rearrange("p (k t) -> p k t", k=shape_k)
        return _fenced(ap, fence, name) if fence is not None else T(ap, name)

    xb = a2(0, 4096, F32, 8, "xb"); v = a2(4096, 4096, F32, 8, "v"); sq = a2(8192, 4096, F32, 8, "sq")
    hT = a2(12288, 2048, BF16, 8, "hT"); ys = a2(14336, 4096, BF16, 16, "ys"); ya = a2(18432, 2048, BF16, 8, "ya")
    mg = a2(20480, 2048, BF16, 8, "mg"); h2f = a2(22528, 4096, F32, 8, "h2f")

    def bc_f(t, lo=0):
        return t[:, lo:lo + 8].rearrange("p (k o) -> p k o", o=1).to_broadcast([128, 8, TB])

    def blk(ap, tb):
        return ap[tb].rearrange("p (k t) -> p k t", k=8)

    for tb in range(NB):
        t0 = tb * TB
        P.dma("sp", xb[:, :, :], blk(xT, tb), reads=xs_t, writes=[xb])
        for gp in range(4):
            P.dma("sp", ys[:, gp * 4:(gp + 1) * 4, :], Yms[tb // CB, gp, tb % CB].rearrange("p (i t) -> p i t", i=4), reads=[Ym_t], writes=[ys])
            P.dma("sp", ya[:, gp * 2:(gp + 1) * 2, :], Yma[0:2, gp, :, t0:t0 + TB].rearrange("i p t -> p i t"), reads=[Ym_t], writes=[ya])
        P.dve(lambda e: e.tensor_tensor(out=v[:, :, :], in0=xb[:, :, :], in1=bc_f(sc1m), op=ALU.mult),
              reads=[xb, sc1m], writes=[v])
        P.dve(lambda e: e.tensor_tensor(out=hT[:, :, :], in0=v[:, :, :], in1=bc_f(modm, 0), op=ALU.add),
              reads=[v, modm], writes=[hT])
        P.pool(lambda e: e.tensor_scalar(out=xb[:, :, :], in0=xb[:, :, :], scalar1=ALPHA, scalar2=None, op0=ALU.mult),
               reads=[xb], writes=[xb])
        for ft in range(8):
            pa, pb, pg = (A0, A1)[ft % 2], (B0, B1)[ft % 2], (G0, G1)[ft % 2]
            gsx, m1x, m2x = gs[ft % 2], m1[ft % 2], m2[ft % 2]
            fs = slice(ft * 128, (ft + 1) * 128)
            fs2 = slice(1024 + ft * 128, 1024 + (ft + 1) * 128)
            for kt in range(16):
                P.pe(lambda e, pa=pa, kt=kt, fs=fs: e.matmul(pa[:, 0:TB], wbs[:, kt, fs], ys[:, kt, :], start=(kt == 0), stop=(kt == 15)),
                     reads=[wbs, ys], writes=[pa])
            for kt in range(8):
                P.pe(lambda e, pb=pb, kt=kt, fs=fs: e.matmul(pb[:, 0:TB], wba[:, kt, fs], ya[:, kt, :], start=(kt == 0), stop=(kt == 7)),
                     reads=[wba, ya], writes=[pb])
            for kt in range(8):
                P.pe(lambda e, pg=pg, kt=kt, fs=fs: e.matmul(pg[:, 0:TB], wg[:, kt, fs], hT[:, kt, :], start=(kt == 0), stop=(kt == 7)),
                     reads=[wg, hT], writes=[pg])
            for kt in range(8):
                P.pe(lambda e, pg=pg, kt=kt, fs2=fs2: e.matmul(pg[:, TB:2 * TB], wg[:, kt, fs2], hT[:, kt, :], start=(kt == 0), stop=(kt == 7)),
                     reads=[wg, hT], writes=[pg])
            P.act(lambda e, pg=pg, gsx=gsx: e.activation(out=gsx[:, :, :].rearrange("p a t -> p (a t)"), in_=pg[:, 0:2 * TB], func=AF.Sigmoid),
                  reads=[pg], writes=[gsx])
            P.dve(lambda e, pa=pa, gsx=gsx, m1x=m1x: e.tensor_tensor(out=m1x[:, :], in0=pa[:, 0:TB], in1=gsx[:, 0, :], op=ALU.mult),
                  reads=[pa, gsx], writes=[m1x])
            P.dve(lambda e, pb=pb, gsx=gsx, m2x=m2x: e.tensor_tensor(out=m2x[:, :], in0=pb[:, 0:TB], in1=gsx[:, 1, :], op=ALU.mult),
                  reads=[pb, gsx], writes=[m2x])
            P.pool(lambda e, ft=ft, m1x=m1x, m2x=m2x: e.tensor_tensor(out=mg[:, ft, :], in0=m1x[:, :], in1=m2x[:, :], op=ALU.add),
                   reads=[m1x, m2x], writes=[mg])
        for ft in range(8):
            pa = (A0, A1)[ft % 2]
            fs = slice(ft * 128, (ft + 1) * 128)
            for kt in range(8):
                P.pe(lambda e, pa=pa, kt=kt, fs=fs: e.matmul(pa[:, 0:TB], wo[:, kt, fs], mg[:, kt, :], start=(kt == 0), stop=(kt == 7)),
                     reads=[wo, mg], writes=[pa])
            P.dve(lambda e, pa=pa, ft=ft: e.scalar_tensor_tensor(out=v[:, ft, :], in0=pa[:, 0:TB], scalar=g1pm[:, ft:ft + 1],
                                                                 in1=xb[:, ft, :], op0=ALU.mult, op1=ALU.add),
                  reads=[pa, g1pm, xb], writes=[v])
        _layernorm(P, v, sq, L, R, ones, lntmp, lnv[0], lnv[1], xb, TB)
        P.dma("sp", blk(x1scr_ap, tb), xb[:, :, :], reads=[xb], writes=[x1scr])
        P.dve(lambda e: e.tensor_tensor(out=v[:, :, :], in0=xb[:, :, :], in1=bc_f(sc1f), op=ALU.mult),
              reads=[xb, sc1f], writes=[v])
        P.dve(lambda e: e.tensor_tensor(out=h2f[:, :, :], in0=v[:, :, :], in1=bc_f(modf, 0), op=ALU.add),
              reads=[v, modf], writes=[h2f])
        P.act(lambda e, t0=t0: e.copy(h2b[:, :, t0:t0 + TB], h2f[:, :, :]), reads=[h2f], writes=[h2b])
        for tt in range(TB // 128):
            ti = tb * (TB // 128) + tt
            for kt in range(8):
                P.pe(lambda e, kt=kt, tt=tt: e.matmul(R[:, 0:36], h2f[:, kt, tt * 128:(tt + 1) * 128], wr[:, kt, :],
                                                     start=(kt == 0), stop=False), reads=[h2f, wr], writes=[R])
            P.pe(lambda e: e.matmul(R[:, 0:36], ones[0:1, 0:128], br[0:1, 0:36], start=False, stop=True),
                 reads=[ones, br], writes=[R])
            _routing(P, R, lgs, rt, Wt, ti)

    fAB = _fence(P)
    acc = _fenced(arena.ap[:, 0:NTT * 2048].bitcast(F32).rearrange("p (t d) -> p t d", t=NTT), fAB, "acc")
    slots = [_fenced(arena.ap[:, 32768:45056], fAB, "slot0"), _fenced(arena2.ap[:, 0:12288], fAB, "slot1")]
    act = _fenced(arena2.ap[:, 12288:12288 + 4 * TE].rearrange("p (k t) -> p k t", k=4), fAB, "act")
    sgs = [_fenced(arena2.ap[:, 14336 + i * 2 * TE:14336 + (i + 1) * 2 * TE].bitcast(F32), fAB, "sg%d" % i) for i in range(2)]
    for ex in range(nexp):
        slot = slots[ex % 2]
        wge = slot.ap[:, 0:4096].rearrange("p (k f) -> p k f", k=8)
        wue = slot.ap[:, 4096:8192].rearrange("p (k f) -> p k f", k=8)
        wde = slot.ap[:, 8192:12288].rearrange("p (k f) -> p k f", k=4)
        P.dma("pool", slot.ap[:, 0:4096], w_eg[ex], writes=[slot])
        P.dma("pool", slot.ap[:, 4096:8192], w_eu[ex], writes=[slot])
        P.dma("pool", slot.ap[:, 8192:12288], w_ed[ex], writes=[slot])
        for tb in range(NBE):
            t0 = tb * TE
            for ff in range(4):
                pg, pu, sg = (A0, A1)[ff % 2], (B0, B1)[ff % 2], sgs[ff % 2]
                fs = slice(ff * 128, (ff + 1) * 128)
                for kt in range(8):
                    P.pe(lambda e, pg=pg, kt=kt, fs=fs, wge=wge, t0=t0: e.matmul(pg[:, 0:TE], wge[:, kt, fs], h2b[:, kt, t0:t0 + TE],
                                                                              start=(kt == 0), stop=(kt == 7)),
                         reads=[slot, h2b], writes=[pg])
                for kt in range(8):
                    P.pe(lambda e, pu=pu, kt=kt, fs=fs, wue=wue, t0=t0: e.matmul(pu[:, 0:TE], wue[:, kt, fs], h2b[:, kt, t0:t0 + TE],
                                                                              start=(kt == 0), stop=(kt == 7)),
                         reads=[slot, h2b], writes=[pu])
                P.act(lambda e, pg=pg, sg=sg: e.activation(out=sg[:, 0:TE], in_=pg[:, 0:TE], func=AF.Silu), reads=[pg], writes=[sg])
                P.dve(lambda e, pu=pu, sg=sg, ff=ff: e.tensor_tensor(out=act[:, ff, :], in0=pu[:, 0:TE], in1=sg[:, 0:TE], op=ALU.mult),
                      reads=[pu, sg], writes=[act])
            for tt in range(TE // 128):
                ti = tb * (TE // 128) + tt
                for dh in range(2):
                    pd = (G0, G1)[dh]
                    for ff in range(4):
                        P.pe(lambda e, pd=pd, ff=ff, tt=tt, dh=dh, wde=wde: e.matmul(
                            pd[:, 0:512], act[:, ff, tt * 128:(tt + 1) * 128], wde[:, ff, dh * 512:(dh + 1) * 512],
                            start=(ff == 0), stop=(ff == 3)), reads=[act, slot], writes=[pd])
                    if ex == 0:
                        P.dve(lambda e, pd=pd, ti=ti, dh=dh, ex=ex: e.tensor_scalar(
                            out=acc[:, ti, dh * 512:(dh + 1) * 512], in0=pd[:, 0:512], scalar1=Wt[:, ti, ex:ex + 1], scalar2=None,
                            op0=ALU.mult), reads=[pd, Wt], writes=[acc])
                    else:
                        P.dve(lambda e, pd=pd, ti=ti, dh=dh, ex=ex: e.scalar_tensor_tensor(
                            out=acc[:, ti, dh * 512:(dh + 1) * 512], in0=pd[:, 0:512], scalar=Wt[:, ti, ex:ex + 1],
                            in1=acc[:, ti, dh * 512:(dh + 1) * 512], op0=ALU.mult, op1=ALU.add), reads=[pd, Wt, acc], writes=[acc])

    fBC = _fence(P)
    xb2 = a2(0, 4096, F32, 8, "xb2", fBC); v2 = a2(4096, 4096, F32, 8, "v2", fBC); sq2 = a2(8192, 4096, F32, 8, "sq2", fBC)
    outs = []
    for tb in range(NB):
        t0 = tb * TB
        P.dma("sp", xb2[:, :, :], blk(x1scr_ap, tb), reads=[x1scr], writes=[xb2])
        P.pool(lambda e: e.tensor_scalar(out=xb2[:, :, :], in0=xb2[:, :, :], scalar1=ALPHA, scalar2=None, op0=ALU.mult),
               reads=[xb2], writes=[xb2])
        for ft in range(8):
            pa = (A0, A1)[ft % 2]
            for tt in range(TB // 128):
                ti = tb * (TB // 128) + tt
                P.pe(lambda e, pa=pa, ti=ti, tt=tt, ft=ft: e.matmul(pa[:, tt * 128:(tt + 1) * 128], acc[:, ti, ft * 128:(ft + 1) * 128],
                                                                   ident[:, 0:128], start=True, stop=True),
                     reads=[acc, ident], writes=[pa])
            P.dve(lambda e, pa=pa, ft=ft: e.scalar_tensor_tensor(out=v2[:, ft, :], in0=pa[:, 0:TB], scalar=g1pf[:, ft:ft + 1],
                                                                 in1=xb2[:, ft, :], op0=ALU.mult, op1=ALU.add),
                  reads=[pa, g1pf, xb2], writes=[v2])
        _layernorm(P, v2, sq2, L, R, ones, lntmp, lnv[2], lnv[3], xb2, TB)
        outs.append(P.dma("sp", blk(xoT, tb), xb2[:, :, :], reads=[xb2], writes=xo_t))
    return outs


def _col(vec, n):
    return np.ascontiguousarray(np.asarray(vec).reshape(n, 128).T)


def _pmajor(w, k):
    E, _, F = w.shape
    return np.ascontiguousarray(w.reshape(E, k, 128, F).transpose(0, 2, 1, 3).reshape(E, 128, k * F))


def _xblocks(xts):
    n = xts.shape[0] // 256
    return np.ascontiguousarray(xts.reshape(n, 256, 8, 128).transpose(0, 3, 2, 1).reshape(n, 128, 2048))


def _xunblocks(xb):
    n = xb.shape[0]
    return np.ascontiguousarray(xb.reshape(n, 128, 8, 256).transpose(0, 3, 2, 1).reshape(n * 256, 1024))


def _consts():
    c = np.zeros((128, 256), np.float32)
    c[:, 0:128] = np.eye(128, dtype=np.float32)
    c[:, 128:256] = 1.0
    return c


def p2_inputs(inp, l):
    return {
        "w_af": inp["w_ada_ffn"][l], "b_af": _col(inp["b_ada_ffn"][l], 24),
        "w_g": np.ascontiguousarray(inp["w_in"][l][:, 8224:10272]),
        "w_bs": inp["w_branch_ssd"][l], "w_ba": inp["w_branch_attn"][l], "w_o": inp["w_out"][l],
        "lnp": np.ascontiguousarray(np.concatenate([_col(inp["ln_mix_g"][l], 8), _col(inp["ln_mix_b"][l], 8),
                                                    _col(inp["ln_ffn_g"][l], 8), _col(inp["ln_ffn_b"][l], 8)], axis=1)),
        "w_r": np.ascontiguousarray(np.concatenate([inp["w_router_group"][l], inp["w_router_expert"][l]], axis=1)),
        "b_r": np.ascontiguousarray(np.concatenate([inp["b_router_group"][l], inp["b_router_expert"][l]])[None, :]),
        "w_eg": _pmajor(inp["w_expert_gate"][l], 8), "w_eu": _pmajor(inp["w_expert_up"][l], 8),
        "w_ed": _pmajor(inp["w_expert_down"][l], 4),
    }


C1_ID, C1_ONES, C1_U, C1_UW0, C1_UW1, C1_CM, C1_CB, C1_MISC, C1_N = 0, 128, 256, 384, 640, 896, 1408, 1920, 1928
W1_NF = 2304
PI = float(np.pi)


def _consts1():
    c = np.zeros((128, C1_N), np.float32)
    c[:, C1_ID:C1_ID + 128] = np.eye(128, dtype=np.float32)
    c[:, C1_ONES:C1_ONES + 128] = 1.0
    U = np.triu(np.ones((128, 128), np.float32))
    c[:, C1_U:C1_U + 128] = U
    c[:, C1_UW0:C1_UW0 + 128] = U
    c[:, C1_UW0 + 128:C1_UW0 + 256] = 1.0
    c[:, C1_UW1 + 128:C1_UW1 + 256] = U
    s = np.arange(128)[:, None]
    l = np.arange(256)[None, :]
    m0 = (l >= s).astype(np.float32)
    m1 = (l >= s + 128).astype(np.float32)
    c[:, C1_CM:C1_CM + 256] = m0
    c[:, C1_CM + 256:C1_CM + 512] = m1
    c[:, C1_CB:C1_CB + 256] = (m0 - 1.0) * 1.0e30
    c[:, C1_CB + 256:C1_CB + 512] = (m1 - 1.0) * 1.0e30
    p = np.arange(128)
    inv_freq = (10000.0 ** (-np.arange(0, 64, 2, dtype=np.float32) / 64)).astype(np.float32)
    c[:, C1_MISC + 0] = inv_freq[p % 32]
    c[:, C1_MISC + 1] = np.where((p % 64) < 32, -1.0, 1.0)
    c[:, C1_MISC + 2] = -PI
    return c


def _esel():
    e = np.zeros((32, 32, 128), np.float32)
    for n in range(32):
        e[n, n, :] = 1.0
    return e.reshape(32, 4096)


def emit_p1(P, nc, banks, S, io):
    NCH = S // 256
    H2 = S // 4
    ccol = io["ccol"]; w_am = io["w_am"]; b_am = io["b_am"]; w1 = io["w1"]
    convw = io["convw"]; convb = io["convb"]; pvec = io["pvec"]; pos = io["pos"]; cst = io["cst1"]
    Yls = io["Yls"]; Yla = io["Yla"]; Yloc_t = io["Yloc_t"]
    NBq = (S // 4) // 256
    x_t = [io["x_t"]] if io.get("x_t") is not None else []
    P.begin_phase()
    big = P.sb("big", [128, max(49152, 20480 + 4 * S)], BF16)
    cs = P.sb("cs", [128, C1_N])
    sc = P.sb("sc", [128, 8]); bam = P.sb("bam", [128, 24]); modm = P.sb("modm", [128, 24]); sc1 = P.sb("sc1", [128, 8])
    cw = P.sb("cw", [128, 24]); cb = P.sb("cb", [128, 6]); pv = P.sb("pv", [128, 24])
    aneg = P.sb("aneg", [128, 8]); wdt = P.sb("wdt", [128, 8, 8])
    BJ, PM, PG, PY, PS0, PS1, PO, PD = banks

    P.dma("sp", cs[:, :], cst[:, :], writes=[cs])
    P.dma("sp", sc[:, :], ccol[:, :], writes=[sc])
    P.dma("sp", bam[:, :], b_am[:, :], writes=[bam])
    P.dma("sp", cw[:, :], convw[:, :], writes=[cw])
    P.dma("sp", cb[:, :], convb[:, :], writes=[cb])
    P.dma("sp", pv[:, :], pvec[:, :], writes=[pv])
    P.dma("sp", wdt[:, :, :], w1.rearrange("(k p) n -> p k n", p=128)[:, :, 2560:2568], writes=[wdt])
    P.act(lambda e: e.activation(out=sc[:, :], in_=sc[:, :], func=AF.Silu), reads=[sc], writes=[sc])
    P.act(lambda e: e.activation(out=aneg[:, :], in_=pv[:, 16:24], func=AF.Exp), reads=[pv], writes=[aneg])
    P.dve(lambda e: e.tensor_scalar(out=aneg[:, :], in0=aneg[:, :], scalar1=-1.0, scalar2=None, op0=ALU.mult),
          reads=[aneg], writes=[aneg])
    wfull = T(big.ap[:, 0:49152].bitcast(F32).rearrange("p (k f) -> p k f", k=8), "wfull")
    _adaln(P, w_am, bam, sc, wfull, PM, modm)
    P.dve(lambda e: e.tensor_scalar(out=sc1[:, :], in0=modm[:, 8:16], scalar1=1.0, scalar2=None, op0=ALU.add),
          reads=[modm], writes=[sc1])
    f0 = _fence(P)
    wsb = _fenced(big.ap[:, 0:20480].rearrange("p (k f) -> p k f", k=8), f0, "wsb")
    kT_ap = big.ap[:, 20480:20480 + 2 * S].rearrange("p (i t) -> p i t", i=2)
    V_ap = big.ap[:, 20480 + 2 * S:20480 + 4 * S].rearrange("p (n f) -> p n f", f=256)
    kTt = [_fenced(kT_ap, f0, "kT%d" % c) for c in range(NCH)]
    Vt = [_fenced(V_ap, f0, "V%d" % c) for c in range(NCH)]
    for kt in range(8):
        P.dma("pool", wsb[:, kt, :], w1[kt * 128:(kt + 1) * 128, 0:2560], writes=[wsb])

    xc = P.sb("xc", [128, 8, 256]); hTf = xc; hTb = P.sb("hTb", [128, 8, 256], BF16)
    zs = P.sb("zs", [128, 4, 256]); cin = P.sb("cin", [128, 6, 259]); xcv = P.sb("xcv", [128, 6, 256])
    BTb = P.sb("BTb", [128, 256], BF16); CTb = P.sb("CTb", [128, 256], BF16)
    posi = P.sb("posi", [128, 256], I32); ang = P.sb("ang", [128, 256]); tm = P.sb("tm", [128, 256])
    cosT = P.sb("cosT", [128, 256]); sinT = P.sb("sinT", [128, 256])
    tA = P.sb("tA", [128, 256]); tB = P.sb("tB", [128, 256]); cacc = tA
    qTf = P.sb("qTf", [128, 2, 256]); qTb = P.sb("qTb", [128, 2, 256], BF16); kTf = P.sb("kTf", [128, 2, 256])
    kmean = P.sb("kmean", [128, 2, 32]); ksum = P.sb("ksum", [128, 2])
    dtr = P.sb("dtr", [128, 2, 8]); dta_ = P.sb("dta", [128, 2, 8]); dtt = [P.sb("dtt%d" % i, [128, 2, 8]) for i in range(4)]
    cssb = P.sb("cssb", [128, 24]); negcs = P.sb("negcs", [128, 2, 8]); wend = P.sb("wend", [128, 2, 8])
    dtw = P.sb("dtw", [128, 2, 8]); dec = P.sb("dec", [128, 8])
    xtok = P.sb("xtok", [128, 2, 512]); XE = P.sb("XE", [128, 2, 512], BF16); XO = P.sb("XO", [128, 2, 512], BF16)
    xdtw = P.sb("xdtw", [128, 2, 512], BF16); Btok = P.sb("Btok", [128, 2, 128], BF16)
    Gm = P.sb("Gm", [128, 2, 256]); Dm = P.sb("Dm", [128, 2, 256]); dcy = Dm
    scT = [P.sb("scT%d" % i, [128, 2, 256], BF16) for i in range(2)]
    E1 = P.sb("E1", [128, 256]); CE = [P.sb("CE%d" % i, [128, 256], BF16) for i in range(2)]
    state = P.sb("state", [128, 512]); stE = P.sb("stE", [128, 512], BF16); stO = P.sb("stO", [128, 512], BF16)
    yD = P.sb("yD", [128, 256]); gy = P.sb("gy", [128, 4, 256]); sqg = P.sb("sqg", [128, 4, 256])
    rstd = P.sb("rstd", [128, 256]); yout = P.sb("yout", [128, 4, 256], BF16)
    gate = P.sb("gate", [128, 32]); mx8 = P.sb("mx8", [128, 8]); selm = P.sb("selm", [128, 32])
    selb = [P.sb("selb%d" % i, [128, 32]) for i in range(2)]
    selT = P.sb("selT", [32, 4, 256], BF16)
    pT = [P.sb("pT%d" % i, [128, 2, 256], BF16) for i in range(4)]
    rden = P.sb("rden", [64, 256]); yatt = P.sb("yatt", [64, 4, 256], BF16)
    onesb = P.sb("onesb", [128, 64], BF16); identb = P.sb("identb", [128, 128], BF16)
    cbias = P.sb("cbias", [128, 2, 256], BF16)

    ident = cs; invf = cs
    def C(off, n):
        return cs[:, off:off + n]

    P.dve(lambda e: e.tensor_copy(onesb[:, :], C(C1_ONES, 64)), reads=[cs], writes=[onesb])
    P.dve(lambda e: e.tensor_copy(identb[:, :], C(C1_ID, 128)), reads=[cs], writes=[identb])
    P.dve(lambda e: e.tensor_copy(cbias[:, :, :].rearrange("p a t -> p (a t)"), C(C1_CB, 512)), reads=[cs], writes=[cbias])
    P.dve(lambda e: e.memset(cin[:, :, :], 0.0), writes=[cin])
    P.pool(lambda e: e.memset(XE[:, :, :], 0.0), writes=[XE])
    P.pool(lambda e: e.memset(XO[:, :, :], 0.0), writes=[XO])
    P.pool(lambda e: e.memset(stE[:, :], 0.0), writes=[stE])
    P.pool(lambda e: e.memset(stO[:, :], 0.0), writes=[stO])
    P.dve(lambda e: e.memset(gate[:, :], NEG), writes=[gate])
    for i in range(2):
        P.dve(lambda e, i=i: e.memset(selb[i][:, :], 0.0), writes=[selb[i]])

    outs = []

    def bc8(ap):
        return ap.rearrange("p (r o) -> p r o", o=1).to_broadcast([128, 8, 64])

    def do_chunk(c):
        t0 = c * 256
        hh, col0 = t0 // H2, t0 % H2
        YsO = Yls[c // NBq, c % NBq].rearrange("p (i t) -> p i t", i=4)
        YaO = Yla[hh].rearrange("(h d) t -> d h t", d=64)
        P.dma("sp", xc[:, :, :], io["x_src"](c), reads=x_t, writes=[xc])
        P.dma("sp", posi[:, :], pos[0:1, t0:t0 + 256].to_broadcast([128, 256]), writes=[posi])
        P.dve(lambda e: e.tensor_tensor(out=hTf[:, :, :], in0=xc[:, :, :],
                                        in1=sc1[:, 0:8].rearrange("p (k o) -> p k o", o=1).to_broadcast([128, 8, 256]), op=ALU.mult),
              reads=[xc, sc1], writes=[xc])
        P.dve(lambda e: e.tensor_tensor(out=hTf[:, :, :], in0=hTf[:, :, :],
                                        in1=modm[:, 0:8].rearrange("p (k o) -> p k o", o=1).to_broadcast([128, 8, 256]), op=ALU.add),
              reads=[hTf, modm], writes=[hTf])
        P.act(lambda e: e.copy(hTb[:, :, :], hTf[:, :, :]), reads=[hTf], writes=[hTb])
        P.dve(lambda e: e.tensor_copy(ang[:, :], posi[:, :]), reads=[posi], writes=[ang])
        P.dve(lambda e: e.tensor_scalar(out=ang[:, :], in0=ang[:, :], scalar1=cs[:, C1_MISC:C1_MISC + 1], scalar2=None, op0=ALU.mult),
              reads=[ang, cs], writes=[ang])
        C1_, C2_ = 6.28125, 2 * PI - 6.28125
        P.dve(lambda e: e.tensor_scalar(out=tm[:, :], in0=ang[:, :], scalar1=1.0 / (2 * PI), scalar2=None, op0=ALU.mult), reads=[ang], writes=[tm])
        P.dve(lambda e: e.tensor_copy(posi[:, :], tm[:, :]), reads=[tm], writes=[posi])
        P.dve(lambda e: e.tensor_copy(tm[:, :], posi[:, :]), reads=[posi], writes=[tm])
        P.dve(lambda e: e.scalar_tensor_tensor(out=ang[:, :], in0=tm[:, :], scalar=-C1_, in1=ang[:, :], op0=ALU.mult, op1=ALU.add),
              reads=[tm, ang], writes=[ang])
        P.dve(lambda e: e.scalar_tensor_tensor(out=ang[:, :], in0=tm[:, :], scalar=-C2_, in1=ang[:, :], op0=ALU.mult, op1=ALU.add),
              reads=[tm, ang], writes=[ang])
        for (shift, dstT) in ((0.0, sinT), (0.5 * PI, cosT)):
            if shift != 0.0:
                P.dve(lambda e, shift=shift: e.tensor_scalar(out=ang[:, :], in0=ang[:, :], scalar1=shift, scalar2=None, op0=ALU.add),
                      reads=[ang], writes=[ang])
            P.dve(lambda e: e.tensor_scalar(out=tm[:, :], in0=ang[:, :], scalar1=PI, scalar2=-2 * PI, op0=ALU.is_gt, op1=ALU.mult),
                  reads=[ang], writes=[tm])
            P.dve(lambda e: e.tensor_tensor(out=ang[:, :], in0=ang[:, :], in1=tm[:, :], op=ALU.add), reads=[ang, tm], writes=[ang])
            P.dve(lambda e: e.tensor_scalar(out=tm[:, :], in0=ang[:, :], scalar1=-PI, scalar2=2 * PI, op0=ALU.is_lt, op1=ALU.mult),
                  reads=[ang], writes=[tm])
            P.dve(lambda e: e.tensor_tensor(out=ang[:, :], in0=ang[:, :], in1=tm[:, :], op=ALU.add), reads=[ang, tm], writes=[ang])
            P.act(lambda e, dstT=dstT: e.activation(out=dstT[:, :], in_=ang[:, :], func=AF.Sin), reads=[ang], writes=[dstT])
        P.dve(lambda e: e.tensor_scalar(out=sinT[:, :], in0=sinT[:, :], scalar1=cs[:, C1_MISC + 1:C1_MISC + 2], scalar2=None, op0=ALU.mult),
              reads=[sinT, cs], writes=[sinT])

        def proj(j, half):
            for kt in range(8):
                P.pe(lambda e, kt=kt: e.matmul(BJ[:, half * 256:(half + 1) * 256], wsb[:, kt, j * 128:(j + 1) * 128], hTb[:, kt, :],
                                               start=(kt == 0), stop=(kt == 7)), reads=[wsb, hTb], writes=[BJ])
        for j in range(4):
            proj(j, j % 2)
            P.act(lambda e, j=j: e.activation(out=zs[:, j, :], in_=BJ[:, (j % 2) * 256:(j % 2 + 1) * 256], func=AF.Silu),
                  reads=[BJ], writes=[zs])
        for jj in range(6):
            proj(4 + jj, jj % 2)
            P.act(lambda e, jj=jj: e.copy(cin[:, jj, 3:259], BJ[:, (jj % 2) * 256:(jj % 2 + 1) * 256]), reads=[BJ], writes=[cin])
        for i in range(2):
            for (jq, js, dst) in ((10 + i, 14 + i, "q"), (12 + i, 16 + i, "k")):
                proj(jq, 0)
                proj(js, 1)
                P.dve(lambda e: e.tensor_tensor(out=tA[:, :], in0=BJ[:, 0:256], in1=cosT[:, :], op=ALU.mult),
                      reads=[BJ, cosT], writes=[tA])
                P.dve(lambda e: e.tensor_tensor(out=tB[:, :], in0=BJ[:, 256:512], in1=sinT[:, :], op=ALU.mult),
                      reads=[BJ, sinT], writes=[tB])
                if dst == "q":
                    P.pool(lambda e, i=i: e.tensor_tensor(out=qTf[:, i, :], in0=tA[:, :], in1=tB[:, :], op=ALU.add),
                           reads=[tA, tB], writes=[qTf])
                    P.act(lambda e, i=i: e.mul(qTb[:, i, :], qTf[:, i, :], 0.125), reads=[qTf], writes=[qTb])
                else:
                    P.pool(lambda e, i=i: e.tensor_tensor(out=kTf[:, i, :], in0=tA[:, :], in1=tB[:, :], op=ALU.add),
                           reads=[tA, tB], writes=[kTf])
                    P.act(lambda e, i=i: e.copy(kT_ap[:, i, t0:t0 + 256], kTf[:, i, :]), reads=[kTf], writes=[kTt[c]])
        P.dve(lambda e: e.reduce_sum(out=ksum[:, 0:2], in_=kTf[:, :, :], axis=AX.X), reads=[kTf], writes=[ksum])
        P.dve(lambda e: e.tensor_scalar(out=kmean[:, :, c], in0=ksum[:, 0:2], scalar1=1.0 / 256, scalar2=None, op0=ALU.mult),
              reads=[ksum], writes=[kmean])
        for tt in range(2):
            for kt in range(8):
                P.pe(lambda e, kt=kt, tt=tt: e.matmul(BJ[:, tt * 256:(tt + 1) * 256], hTb[:, kt, tt * 128:(tt + 1) * 128], wsb[:, kt, 2304:2560],
                                                     start=(kt == 0), stop=(kt == 7)), reads=[hTb, wsb], writes=[BJ])
            P.act(lambda e, tt=tt: e.copy(V_ap[:, 2 * c + tt, :], BJ[:, tt * 256:(tt + 1) * 256]), reads=[BJ], writes=[Vt[c]])
        for tt in range(2):
            for kt in range(8):
                P.pe(lambda e, kt=kt, tt=tt: e.matmul(PM[:, tt * 8:(tt + 1) * 8], hTf[:, kt, tt * 128:(tt + 1) * 128], wdt[:, kt, :],
                                                     start=(kt == 0), stop=(kt == 7)), reads=[hTf, wdt], writes=[PM])
        a_, ab_, e_, l_ = dtt
        P.dve(lambda e: e.tensor_tensor(out=a_[:, :, :], in0=PM[:, 0:16].rearrange("p (t r) -> p t r", t=2),
                                        in1=pv[:, 8:16].rearrange("p (o r) -> p o r", o=1).to_broadcast([128, 2, 8]), op=ALU.add),
              reads=[PM, pv], writes=[a_])
        P.dve(lambda e: e.tensor_scalar(out=ab_[:, :, :], in0=a_[:, :, :], scalar1=-1.0, scalar2=None, op0=ALU.mult), reads=[a_], writes=[ab_])
        P.dve(lambda e: e.tensor_tensor(out=ab_[:, :, :], in0=ab_[:, :, :], in1=a_[:, :, :], op=ALU.min), reads=[a_, ab_], writes=[ab_])
        P.act(lambda e: e.activation(out=e_[:, :, :], in_=ab_[:, :, :], func=AF.Exp), reads=[ab_], writes=[e_])
        P.dve(lambda e: e.tensor_scalar(out=e_[:, :, :], in0=e_[:, :, :], scalar1=1.0, scalar2=None, op0=ALU.add), reads=[e_], writes=[e_])
        P.act(lambda e: e.activation(out=l_[:, :, :], in_=e_[:, :, :], func=AF.Ln), reads=[e_], writes=[l_])
        P.dve(lambda e: e.tensor_scalar(out=a_[:, :, :], in0=a_[:, :, :], scalar1=0.0, scalar2=None, op0=ALU.max), reads=[a_], writes=[a_])
        P.dve(lambda e: e.tensor_tensor(out=dtr[:, :, :], in0=a_[:, :, :], in1=l_[:, :, :], op=ALU.add), reads=[a_, l_], writes=[dtr])
        P.dve(lambda e: e.tensor_tensor(out=dta_[:, :, :], in0=dtr[:, :, :],
                                        in1=aneg[:, 0:8].rearrange("p (o r) -> p o r", o=1).to_broadcast([128, 2, 8]), op=ALU.mult),
              reads=[dtr, aneg], writes=[dta_])
        for jj in range(6):
            P.dve(lambda e, jj=jj: e.tensor_scalar(out=cacc[:, :], in0=cin[:, jj, 0:256], scalar1=cw[:, jj * 4:jj * 4 + 1], scalar2=None, op0=ALU.mult),
                  reads=[cin, cw], writes=[cacc])
            for k in range(1, 4):
                P.dve(lambda e, jj=jj, k=k: e.scalar_tensor_tensor(out=cacc[:, :], in0=cin[:, jj, k:k + 256], scalar=cw[:, jj * 4 + k:jj * 4 + k + 1],
                                                                  in1=cacc[:, :], op0=ALU.mult, op1=ALU.add), reads=[cin, cw, cacc], writes=[cacc])
            P.act(lambda e, jj=jj: e.activation(out=xcv[:, jj, :], in_=cacc[:, :], func=AF.Silu, bias=cb[:, jj:jj + 1], scale=1.0),
                  reads=[cacc, cb], writes=[xcv])
        P.pool(lambda e: e.tensor_copy(cin[:, :, 0:3], cin[:, :, 256:259]), reads=[cin], writes=[cin])
        P.act(lambda e: e.copy(BTb[:, :], xcv[:, 4, :]), reads=[xcv], writes=[BTb])
        P.act(lambda e: e.copy(CTb[:, :], xcv[:, 5, :]), reads=[xcv], writes=[CTb])
        Uc = C(C1_U, 128); On = C(C1_ONES, 128)
        P.pe(lambda e: e.matmul(PM[:, 32:40], Uc, dta_[:, 0, :], start=True, stop=True), reads=[cs, dta_], writes=[PM])
        P.pe(lambda e: e.matmul(PM[:, 40:48], On, dta_[:, 0, :], start=True, stop=False), reads=[cs, dta_], writes=[PM])
        P.pe(lambda e: e.matmul(PM[:, 40:48], Uc, dta_[:, 1, :], start=False, stop=True), reads=[cs, dta_], writes=[PM])
        P.pe(lambda e: e.matmul(PM[:, 48:56], On, dta_[:, 0, :], start=True, stop=False), reads=[cs, dta_], writes=[PM])
        P.pe(lambda e: e.matmul(PM[:, 48:56], On, dta_[:, 1, :], start=False, stop=True), reads=[cs, dta_], writes=[PM])
        P.dve(lambda e: e.tensor_copy(cssb[:, 0:24], PM[:, 32:56]), reads=[PM], writes=[cssb])
        csv = cssb[:, 0:16].rearrange("p (t r) -> p t r", t=2)
        clb = cssb[:, 16:24].rearrange("p (o r) -> p o r", o=1).to_broadcast([128, 2, 8])
        P.dve(lambda e: e.tensor_scalar(out=negcs[:, :, :], in0=csv, scalar1=-1.0, scalar2=None, op0=ALU.mult), reads=[cssb], writes=[negcs])
        P.dve(lambda e: e.tensor_tensor(out=wend[:, :, :], in0=clb, in1=csv, op=ALU.subtract), reads=[cssb], writes=[wend])
        P.act(lambda e: e.activation(out=wend[:, :, :], in_=wend[:, :, :], func=AF.Exp), reads=[wend], writes=[wend])
        P.dve(lambda e: e.tensor_tensor(out=dtw[:, :, :], in0=dtr[:, :, :], in1=wend[:, :, :], op=ALU.mult), reads=[dtr, wend], writes=[dtw])
        P.act(lambda e: e.activation(out=dec[:, :], in_=cssb[:, 16:24], func=AF.Exp), reads=[cssb], writes=[dec])
        for tt in range(2):
            for jj in range(4):
                P.pe(lambda e, tt=tt, jj=jj: e.matmul(PG[:, jj * 128:(jj + 1) * 128], xcv[:, jj, tt * 128:(tt + 1) * 128], C(C1_ID, 128),
                                                     start=True, stop=True), reads=[xcv, cs], writes=[PG])
            P.act(lambda e, tt=tt: e.copy(xtok[:, tt, :], PG[:, 0:512]), reads=[PG], writes=[xtok])
        for tt in range(2):
            P.pe(lambda e, tt=tt: e.matmul(PM[:, 64 + tt * 128:64 + (tt + 1) * 128], xcv[:, 4, tt * 128:(tt + 1) * 128], C(C1_ID, 128),
                                           start=True, stop=True), reads=[xcv, cs], writes=[PM])
        P.act(lambda e: e.copy(Btok[:, :, :].rearrange("p t n -> p (t n)"), PM[:, 64:320]), reads=[PM], writes=[Btok])
        for tt in range(2):
            xv = xtok[:, tt, :].rearrange("p (i two q) -> p i two q", two=2, q=64)
            dv = dtr[:, tt, :].rearrange("p (i two) -> p i two", two=2)
            for par, X in ((0, XE), (1, XO)):
                P.dve(lambda e, tt=tt, par=par, X=X, xv=xv, dv=dv: e.tensor_tensor(
                    out=X[:, tt, :].rearrange("p (i two q) -> p i two q", two=2, q=64)[:, :, par, :],
                    in0=xv[:, :, par, :], in1=dv[:, :, par:par + 1].to_broadcast([128, 4, 64]), op=ALU.mult),
                    reads=[xtok, dtr], writes=[X])
            P.pool(lambda e, tt=tt: e.tensor_tensor(out=xdtw[:, tt, :].rearrange("p (r q) -> p r q", q=64),
                                                   in0=xtok[:, tt, :].rearrange("p (r q) -> p r q", q=64),
                                                   in1=bc8(dtw[:, tt, :]), op=ALU.mult), reads=[xtok, dtw], writes=[xdtw])
        for st in range(2):
            P.pe(lambda e, st=st: e.matmul(PG[:, st * 256:(st + 1) * 256], BTb[:, st * 128:(st + 1) * 128], CTb[:, 0:256],
                                           start=True, stop=True), reads=[BTb, CTb], writes=[PG])
        P.dve(lambda e: e.tensor_tensor(out=Gm[:, :, :].rearrange("p a t -> p (a t)"), in0=PG[:, 0:512], in1=C(C1_CM, 512), op=ALU.mult),
              reads=[PG, cs], writes=[Gm])
        for r in range(8):
            i, par = r // 2, r % 2
            sc_, ce_ = scT[r % 2], CE[r % 2]
            PT = PM
            P.pe(lambda e, r=r: e.matmul(PT[:, 256:512], dta_[:, 0, r:r + 1].to_broadcast([128, 128]), C(C1_UW0, 256), start=True, stop=False),
                 reads=[dta_, cs], writes=[PM])
            P.pe(lambda e, r=r: e.matmul(PT[:, 256:512], dta_[:, 1, r:r + 1].to_broadcast([128, 128]), C(C1_UW1, 256), start=False, stop=True),
                 reads=[dta_, cs], writes=[PM])
            for st in range(2):
                P.dve(lambda e, r=r, st=st: e.tensor_scalar(out=Dm[:, st, :], in0=PT[:, 256:512], scalar1=negcs[:, st, r:r + 1], scalar2=0.0,
                                                           op0=ALU.add, op1=ALU.min), reads=[PM, negcs], writes=[Dm])
            P.act(lambda e: e.activation(out=dcy[:, :, :], in_=Dm[:, :, :], func=AF.Exp), reads=[Dm], writes=[Dm])
            P.pool(lambda e, sc_=sc_: e.tensor_tensor(out=sc_[:, :, :], in0=Gm[:, :, :], in1=dcy[:, :, :], op=ALU.mult),
                   reads=[Gm, dcy], writes=[sc_])
            if c > 0:
                P.act(lambda e: e.activation(out=E1[:, :], in_=PT[:, 256:512], func=AF.Exp), reads=[PM], writes=[E1])
                P.pool(lambda e, ce_=ce_: e.tensor_tensor(out=ce_[:, :], in0=xcv[:, 5, :], in1=E1[:, :], op=ALU.mult),
                       reads=[xcv, E1], writes=[ce_])
            X = XE if par == 0 else XO
            st_ = stE if par == 0 else stO
            pys = slice((i % 2) * 256, (i % 2 + 1) * 256)
            P.pe(lambda e, X=X, i=i, sc_=sc_, pys=pys, par=par: e.matmul(PY[:, pys], X[:, 0, i * 128:(i + 1) * 128], sc_[:, 0, :],
                                                                      start=(par == 0), stop=False), reads=[X, sc_], writes=[PY])
            P.pe(lambda e, X=X, i=i, sc_=sc_, pys=pys, par=par: e.matmul(PY[:, pys], X[:, 1, i * 128:(i + 1) * 128], sc_[:, 1, :],
                                                                      start=False, stop=(par == 1 and c == 0)), reads=[X, sc_], writes=[PY])
            if c > 0:
                P.pe(lambda e, st_=st_, i=i, ce_=ce_, pys=pys, par=par: e.matmul(PY[:, pys], st_[:, i * 128:(i + 1) * 128], ce_[:, :],
                                                                              start=False, stop=(par == 1)), reads=[st_, ce_], writes=[PY])
            if par == 1:
                P.dve(lambda e, i=i, pys=pys: e.scalar_tensor_tensor(out=yD[:, :], in0=xcv[:, i, :], scalar=pv[:, i:i + 1], in1=PY[:, pys],
                                                                    op0=ALU.mult, op1=ALU.add), reads=[xcv, pv, PY], writes=[yD])
                P.pool(lambda e, i=i: e.tensor_tensor(out=gy[:, i, :], in0=yD[:, :], in1=zs[:, i, :], op=ALU.mult),
                       reads=[yD, zs], writes=[gy])
        P.act(lambda e: e.activation(out=sqg[:, :, :], in_=gy[:, :, :], func=AF.Square), reads=[gy], writes=[sqg])
        for i in range(4):
            P.pe(lambda e, i=i: e.matmul(BJ[:, 0:256], C(C1_ONES, 128), sqg[:, i, :], start=(i == 0), stop=(i == 3)),
                 reads=[cs, sqg], writes=[BJ])
        P.dve(lambda e: e.tensor_scalar(out=rstd[:, :], in0=BJ[:, 0:256], scalar1=1.0 / 512, scalar2=EPS, op0=ALU.mult, op1=ALU.add),
              reads=[BJ], writes=[rstd])
        P.act(lambda e: e.activation(out=rstd[:, :], in_=rstd[:, :], func=AF.Sqrt), reads=[rstd], writes=[rstd])
        P.dve(lambda e: e.reciprocal(out=rstd[:, :], in_=rstd[:, :]), reads=[rstd], writes=[rstd])
        for i in range(4):
            P.dve(lambda e, i=i: e.scalar_tensor_tensor(out=yout[:, i, :], in0=gy[:, i, :], scalar=pv[:, 4 + i:5 + i], in1=rstd[:, :],
                                                       op0=ALU.mult, op1=ALU.mult), reads=[gy, pv, rstd], writes=[yout])
        outs.append(P.dma("sp", YsO, yout[:, :, :], reads=[yout], writes=[Yloc_t]))
        if c < NCH - 1:
            for tt in range(2):
                P.pe(lambda e, tt=tt: e.matmul(PG[:, 0:512], Btok[:, tt, :], xdtw[:, tt, :], start=(tt == 0), stop=(tt == 1)),
                     reads=[Btok, xdtw], writes=[PG])
            if c == 0:
                P.dve(lambda e: e.tensor_copy(state[:, :], PG[:, 0:512]), reads=[PG], writes=[state])
            else:
                P.pool(lambda e: e.tensor_tensor(out=state[:, :].rearrange("p (r q) -> p r q", q=64),
                                                in0=state[:, :].rearrange("p (r q) -> p r q", q=64), in1=bc8(dec[:, 0:8]), op=ALU.mult),
                       reads=[state, dec], writes=[state])
                P.dve(lambda e: e.tensor_tensor(out=state[:, :], in0=state[:, :], in1=PG[:, 0:512], op=ALU.add), reads=[state, PG], writes=[state])
            sv = state[:, :].rearrange("p (i two q) -> p i two q", two=2, q=64)
            P.act(lambda e, sv=sv: e.copy(stE[:, :].rearrange("p (i two q) -> p i two q", two=2, q=64)[:, :, 0, :], sv[:, :, 0, :]),
                  reads=[state], writes=[stE])
            P.act(lambda e, sv=sv: e.copy(stO[:, :].rearrange("p (i two q) -> p i two q", two=2, q=64)[:, :, 1, :], sv[:, :, 1, :]),
                  reads=[state], writes=[stO])
        use_gate = c > 3
        if use_gate:
            for h in range(4):
                i, po = h // 2, (h % 2) * 64
                for tt in range(2):
                    sb_ = selb[tt]
                    P.pe(lambda e, i=i, po=po, tt=tt: e.matmul(PM[:, 0:c], qTf[po:po + 64, i, tt * 128:(tt + 1) * 128], kmean[po:po + 64, i, 0:c],
                                                              start=True, stop=True), reads=[qTf, kmean], writes=[PM])
                    P.dve(lambda e: e.tensor_copy(gate[:, 0:c], PM[:, 0:c]), reads=[PM], writes=[gate])
                    P.dve(lambda e: e.max(out=mx8[:, 0:8], in_=gate[:, 0:32]), reads=[gate], writes=[mx8])
                    P.dve(lambda e: e.tensor_scalar(out=selm[:, 0:c], in0=gate[:, 0:c], scalar1=mx8[:, 2:3], scalar2=None, op0=ALU.is_ge),
                          reads=[gate, mx8], writes=[selm])
                    P.dve(lambda e, sb_=sb_: e.tensor_scalar(out=sb_[:, 0:c], in0=selm[:, 0:c], scalar1=-1.0, scalar2=-NEG, op0=ALU.add, op1=ALU.mult),
                          reads=[selm], writes=[sb_])
                    P.pe(lambda e, sb_=sb_, tt=tt: e.matmul(PM[0:32, 64 + tt * 128:64 + (tt + 1) * 128], sb_[:, 0:32], C(C1_ID, 128), start=True, stop=True),
                         reads=[sb_, cs], writes=[PM])
                P.act(lambda e, h=h: e.copy(selT[:, h, :], PM[0:32, 64:320]), reads=[PM], writes=[selT])
        items = [(h, n) for h in range(4) for n in range(c + 1)]
        PSB = (PS0, PS1, BJ, PG)
        POB = ((PO, PD), (PY, PM))
        LA = 2

        def emit_scores(idx):
            h, n = items[idx]
            i, po = h // 2, (h % 2) * 64
            PSx, pTx = PSB[idx % 4], pT[idx % 4]
            own = (n == c)
            has_bias = own or use_gate
            for kt in range(2):
                ks = slice(n * 256 + kt * 128, n * 256 + (kt + 1) * 128)
                P.pe(lambda e, kt=kt, ks=ks: e.matmul(
                    PSx[:, kt * 256:(kt + 1) * 256], kT_ap[po:po + 64, i, ks], qTb[po:po + 64, i, :], start=True, stop=(not has_bias)),
                    reads=[kTt[n], qTb], writes=[PSx])
                if own:
                    P.pe(lambda e, kt=kt: e.matmul(PSx[:, kt * 256:(kt + 1) * 256], identb[:, :], cbias[:, kt, :], start=False, stop=True),
                         reads=[identb, cbias], writes=[PSx])
                elif use_gate:
                    P.pe(lambda e, kt=kt: e.matmul(PSx[:, kt * 256:(kt + 1) * 256], identb[0:32, n:n + 1].to_broadcast([32, 128]), selT[:, h, :],
                                                   start=False, stop=True), reads=[identb, selT], writes=[PSx])
            P.act(lambda e: e.activation(out=pTx[:, :, :].rearrange("p a t -> p (a t)"), in_=PSx[:, 0:512], func=AF.Exp),
                  reads=[PSx], writes=[pTx])

        def emit_pv(idx):
            h, n = items[idx]
            pTx = pT[idx % 4]
            POx, PDx = POB[h % 2]
            for kt in range(2):
                first = (n == 0 and kt == 0)
                last = (n == c and kt == 1)
                P.pe(lambda e, kt=kt, first=first, last=last: e.matmul(
                    POx[0:64, 0:256], V_ap[:, 2 * n + kt, h * 64:(h + 1) * 64], pTx[:, kt, :], start=first, stop=last),
                    reads=[Vt[n], pTx], writes=[POx])
                P.pe(lambda e, kt=kt, first=first, last=last: e.matmul(
                    PDx[0:64, 0:256], onesb[:, 0:64], pTx[:, kt, :], start=first, stop=last), reads=[onesb, pTx], writes=[PDx])
            if n == c:
                P.dve(lambda e: e.reciprocal(out=rden[:, :], in_=PDx[0:64, 0:256]), reads=[PDx], writes=[rden])
                P.dve(lambda e: e.tensor_tensor(out=yatt[:, h, :], in0=POx[0:64, 0:256], in1=rden[:, :], op=ALU.mult),
                      reads=[POx, rden], writes=[yatt])

        for step in range(len(items) + LA):
            if step < len(items):
                emit_scores(step)
            if step - LA >= 0:
                emit_pv(step - LA)
        outs.append(P.dma("sp", YaO[:, :, col0:col0 + 256], yatt[:, :, :], reads=[yatt], writes=[Yloc_t]))
    for c in range(NCH):
        do_chunk(c)
    return outs


def p1_inputs(inp, l, g):
    w = inp["w_in"][l]
    hq = 5152 + 4 * g * 64
    hk = 5152 + 1024 + 4 * g * 64
    hv = 5152 + 2048 + 4 * g * 64
    swap = np.concatenate([np.arange(h * 64 + 32, h * 64 + 64).tolist() + np.arange(h * 64, h * 64 + 32).tolist() for h in range(4)])
    w1 = np.concatenate([
        w[:, g * 512:(g + 1) * 512],
        w[:, 2048 + g * 512:2048 + (g + 1) * 512],
        w[:, 4096 + g * 128:4096 + (g + 1) * 128],
        w[:, 4608 + g * 128:4608 + (g + 1) * 128],
        w[:, hq:hq + 256], w[:, hk:hk + 256],
        w[:, hq:hq + 256][:, swap], w[:, hk:hk + 256][:, swap],
        w[:, hv:hv + 256],
        w[:, 5120 + g * 8:5120 + (g + 1) * 8],
    ], axis=1)
    ch = np.concatenate([g * 512 + np.arange(512), 2048 + g * 128 + np.arange(128), 2560 + g * 128 + np.arange(128)])
    cwl = inp["conv_w"][l][:, ch]
    convw = np.ascontiguousarray(cwl.reshape(4, 6, 128).transpose(2, 1, 0).reshape(128, 24))
    convb = np.ascontiguousarray(inp["conv_b"][l][ch].reshape(6, 128).T)
    heads = g * 8 + np.arange(8)
    p = np.arange(128)
    dvec = np.stack([inp["d_skip"][l][g * 8 + 2 * i + (p >= 64)] for i in range(4)], axis=1)
    normw = inp["ssd_norm_w"][l][g * 512:(g + 1) * 512].reshape(4, 128).T
    dtb = np.broadcast_to(inp["dt_bias"][l][heads][None, :], (128, 8))
    alog = np.broadcast_to(inp["a_log"][l][heads][None, :], (128, 8))
    pvec = np.ascontiguousarray(np.concatenate([dvec, normw, dtb, alog], axis=1).astype(np.float32))
    return {
        "w_am": inp["w_ada_mix"][l], "b_am": _col(inp["b_ada_mix"][l], 24),
        "w1": np.ascontiguousarray(w1), "convw": convw, "convb": convb, "pvec": pvec,
    }


P1_SHAPES = {"w_am": [D, 3072], "b_am": [128, 24], "w1": [D, 2568], "convw": [128, 24], "convb": [128, 6], "pvec": [128, 24]}
P2_SHAPES = {"w_af": [D, 3072], "b_af": [128, 24], "w_g": [D, 2048], "w_bs": [2048, D], "w_ba": [D, D], "w_o": [D, D],
             "lnp": [128, 32], "w_r": [D, 36], "b_r": [1, 36], "w_eg": [NEXP, 128, 4096], "w_eu": [NEXP, 128, 4096], "w_ed": [NEXP, 128, 4096]}
GROUPS = [[0, 1, 2, 3], [4, 5, 6, 7]]


def _allgather(P, in_ap, out_ap, reads, writes):
    def fn(e):
        return e.collective_compute("AllGather", ALU.bypass, replica_groups=GROUPS, ins=[in_ap.opt()], outs=[out_ap.opt()])
    return P.add("pool", fn, reads=reads, writes=writes, dma=True, inc=1, semgroup="cc")


def build_fused(S, L, nexp=NEXP):
    nc = bass.Bass("TRN2", target_bir_lowering=False)
    NT = S // 4
    H2 = S // 2
    NB = NT // 256
    xT_in = _din(nc, "xT", [S // 256, 128, 2048]); xs_in = _din(nc, "xs", [NB, 128, 2048])
    ccol = _din(nc, "ccol", [128, 8]); pos = _din(nc, "pos", [1, S], I32)
    cst1 = _din(nc, "cst1", [128, C1_N]); cst2 = _din(nc, "cst2", [128, 256])
    lw = []
    for l in range(L):
        dct = {k: _din(nc, "%s_%d" % (k, l), shp) for k, shp in P1_SHAPES.items()}
        dct.update({k: _din(nc, "%s_%d" % (k, l), shp) for k, shp in P2_SHAPES.items()})
        lw.append(dct)
    out = _dout(nc, "xoT", [NB, 128, 2048])
    CB = min(4, NB)
    NCK = NB // CB
    Yls = [nc.dram_tensor("Yls%d" % i, [4, NB, 128, 1024], BF16) for i in range(2)]
    Yla = [nc.dram_tensor("Yla%d" % i, [4, 256, NT], BF16) for i in range(2)]
    Ygs = [nc.dram_tensor("Ygs%d" % i, [4, NCK, 4, CB, 128, 1024], BF16) for i in range(2)]
    Yga = [nc.dram_tensor("Yga%d" % i, [4, 2, 4, 128, NT], BF16) for i in range(2)]
    xo = [nc.dram_tensor("xo%d" % i, [NB, 128, 2048], F32) for i in range(2)]
    xg = [nc.dram_tensor("xg%d" % i, [NB, 4, 128, 2048], F32) for i in range(2)]
    x1scr = nc.dram_tensor("x1scr", [NB, 128, 2048], F32)
    Yms = nc.dram_tensor("Yms", [NCK, 4, CB, 128, 1024], BF16)
    Yma = nc.dram_tensor("Yma", [2, 4, 128, NT], BF16)
    Ym_t = T(None, "Ym")
    Yloc_t = [T(None, "Yloc%d" % i) for i in range(2)]; Yg_t = [T(None, "Yg%d" % i) for i in range(2)]
    xo_t = [T(None, "xo%d" % i) for i in range(2)]; xg_t = [T(None, "xg%d" % i) for i in range(2)]
    x1_t = T(None, "x1scr")

    P = Prog(nc)
    P.use_arena(206 * 1024)
    banks = [P.ps("bank%d" % i, [128, 512]) for i in range(8)]
    outs = []
    for l in range(L):
        par = l % 2
        io = dict(lw[l])
        io.update(ccol=ccol, pos=pos, cst1=cst1, cst2=cst2)
        if l == 0:
            io["x_src"] = lambda c: xT_in[c].rearrange("p (k t) -> p k t", k=8)
            io["x_t"] = None
        else:
            xga = xg[1 - par].ap()
            io["x_src"] = lambda c, xga=xga: xga[c % NB, c // NB].rearrange("p (k t) -> p k t", k=8)
            io["x_t"] = xg_t[1 - par]
        io["Yls"] = Yls[par].ap(); io["Yla"] = Yla[par].ap(); io["Yloc_t"] = Yloc_t[par]
        emit_p1(P, nc, banks, S, io)
        for hh in range(4):
            for ck in range(NCK):
                _allgather(P, Yls[par].ap()[hh, ck * CB:(ck + 1) * CB].rearrange("c p f -> (c p) f"),
                           Ygs[par].ap()[hh, ck].rearrange("g c p f -> (g c p) f"), [Yloc_t[par]], [Yg_t[par]])
            for rt in range(2):
                _allgather(P, Yla[par].ap()[hh, rt * 128:(rt + 1) * 128, :],
                           Yga[par].ap()[hh, rt].rearrange("g p t -> (g p) t"), [Yloc_t[par]], [Yg_t[par]])
        io["Ygs"] = Ygs[par].ap(); io["Yga"] = Yga[par].ap(); io["Yg_t"] = Yg_t[par]; io["CB"] = CB
        io["x1scr"] = x1scr.ap(); io["x1scr_t"] = x1_t
        io["Yms"] = Yms.ap(); io["Yma"] = Yma.ap(); io["Ym_t"] = Ym_t
        if l == 0:
            io["xs"] = xs_in; io["xs_t"] = None
        else:
            io["xs"] = xo[1 - par].ap(); io["xs_t"] = xo_t[1 - par]
        if l == L - 1:
            io["xo"] = out; io["xo_t"] = None
        else:
            io["xo"] = xo[par].ap(); io["xo_t"] = xo_t[par]
        o2 = emit_p2(P, nc, banks, NT, io, nexp=nexp)
        if l == L - 1:
            outs = o2
        else:
            for tb in range(NB):
                _allgather(P, xo[par].ap()[tb], xg[par].ap()[tb].rearrange("g p f -> (g p) f"), [xo_t[par]], [xg_t[par]])
    counts = P.finish(outs)
    return nc, counts


def fused_inputs(inp, r, S):
    b, g = r // 4, r % 4
    NT = S // 4
    L = inp["w_in"].shape[0]
    xblk = _xblocks(np.asarray(inp["x"][b]))
    nb = NT // 256
    m = {"xT": xblk, "xs": np.ascontiguousarray(xblk[g * nb:(g + 1) * nb]), "ccol": _col(inp["c"][b], 8),
         "pos": np.ascontiguousarray(inp["positions"][b][None, :]).astype(np.int32), "cst1": _consts1(), "cst2": _consts()}
    for l in range(L):
        for k, v in p1_inputs(inp, l, g).items():
            m["%s_%d" % (k, l)] = np.ascontiguousarray(v, dtype=np.float32)
        for k, v in p2_inputs(inp, l).items():
            m["%s_%d" % (k, l)] = v
    return m


_NC_CACHE = {}


def kernel(**inputs):
    inp = {k: np.asarray(v) for k, v in inputs.items()}
    B, S, _ = inp["x"].shape
    L = inp["w_in"].shape[0]
    assert B == 2
    key = (S, L)
    if key not in _NC_CACHE:
        _NC_CACHE[key] = build_fused(S, L)[0]
    nc = _NC_CACHE[key]
    import concourse.bass_utils as _bu
    shared = {}
    maps = []
    for r in range(8):
        m = fused_inputs(inp, r, S)
        for k in list(m):
            if k.startswith(("w_a", "b_a", "w_g", "w_b", "w_o", "lnp", "w_r", "b_r", "w_e", "cst")):
                m[k] = shared.setdefault(k, m[k])
        maps.append(m)
    res = _bu.run_bass_kernel_spmd(nc, maps, core_ids=list(range(8))).results
    NT = S // 4
    out = np.zeros((2, S, D), np.float32)
    for r in range(8):
        b, g = r // 4, r % 4
        out[b, g * NT:(g + 1) * NT, :] = _xunblocks(np.asarray(res[r]["xoT"]))
    return out
```

```python
import numpy as np
import concourse.bass as bass
import concourse.mybir as mybir
from concourse.bass_utils import run_bass_kernel_spmd

F32 = mybir.dt.float32
BF16 = mybir.dt.bfloat16
I32 = mybir.dt.int32
AF = mybir.ActivationFunctionType
ALU = mybir.AluOpType
AX = mybir.AxisListType

ENGS = ("pe", "act", "dve", "pool", "sp")
DMA_POOL = 8


class T:
    __slots__ = ("ap", "w", "r", "name")

    def __init__(self, ap, name=""):
        self.ap = ap
        self.w = None
        self.r = []
        self.name = name

    def __getitem__(self, k):
        return self.ap[k]


class Op:
    __slots__ = ("eng", "fn", "deps", "dma", "idx", "needed", "sem", "val", "slot_prev", "inc")

    def __init__(self, eng, fn, deps, dma):
        self.eng = eng
        self.fn = fn
        self.deps = deps
        self.dma = dma
        self.needed = False
        self.sem = None
        self.val = None
        self.slot_prev = None
        self.inc = 16


class Prog:
    def __init__(self, nc):
        self.nc = nc
        self.ops = {e: [] for e in ENGS}
        self.dma_count = {e: 0 for e in ENGS + ("cc",)}
        self.dma_slots = {e: [None] * DMA_POOL for e in ENGS + ("cc",)}
        self._ctx = []
        self.arena = None
        self.off = 0
        self.fence = []
        self._rank = {}

    def rank(self, engine):
        k = id(engine)
        if k not in self._rank:
            self._rank[k] = engine.snap(engine.partition_id() % 4, min_val=0, max_val=3)
        return self._rank[k]

    def use_arena(self, nbytes):
        g = self.nc.sbuf_tensor("arena_all", [128, nbytes // 2], BF16)
        self.arena = g.__enter__()
        self._ctx.append(g)
        self.arena_bytes = nbytes

    def begin_phase(self):
        self.off = 0
        self.fence = _fence(self)

    def sb(self, name, shape, dt=F32):
        if self.arena is not None:
            esz = 2 if dt == BF16 else 4
            free = 1
            for d_ in shape[1:]:
                free *= d_
            nb = (free * esz + 63) // 64 * 64
            assert self.off + nb <= self.arena_bytes, "SBUF arena overflow at %s (%d + %d)" % (name, self.off, nb)
            ap = self.arena[0:shape[0], self.off // 2:self.off // 2 + free * esz // 2]
            self.off += nb
            if dt != BF16:
                ap = ap.bitcast(dt)
            if len(shape) == 3:
                ap = ap.rearrange("p (a b) -> p a b", a=shape[1])
            t = T(ap, name)
            t.r = list(self.fence)
            return t
        g = self.nc.sbuf_tensor(name, list(shape), dt)
        t = g.__enter__()
        self._ctx.append(g)
        return T(t, name)

    def ps(self, name, shape, dt=F32):
        g = self.nc.psum_tensor(name, list(shape), dt)
        t = g.__enter__()
        self._ctx.append(g)
        return T(t, name)

    def alias(self, ap, name=""):
        return T(ap, name)

    def add(self, eng, fn, reads=(), writes=(), dma=False, inc=16, semgroup=None):
        deps = []
        for t in reads:
            if t.w is not None:
                deps.append((t.w, True))
        for t in writes:
            if t.w is not None:
                deps.append((t.w, False))
            for r in t.r:
                deps.append((r, False))
        op = Op(eng, fn, [], dma)
        op.inc = inc
        seen = set()
        for d, raw in deps:
            if d is op or id(d) in seen:
                continue
            if d.eng == eng and not d.dma and not dma:
                if eng == "pe" or not raw:
                    continue
            seen.add(id(d))
            op.deps.append(d)
            d.needed = True
        if dma:
            sg = semgroup or eng
            k = self.dma_count[sg] % DMA_POOL
            self.dma_count[sg] += 1
            prev = self.dma_slots[sg][k]
            op.slot_prev = prev
            if prev is not None:
                prev.needed = True
            self.dma_slots[sg][k] = op
            op.sem = (sg, k)
            op.needed = True
        op.idx = len(self.ops[eng])
        self.ops[eng].append(op)
        for t in reads:
            t.r.append(op)
        for t in writes:
            t.w = op
            t.r = []
        return op

    def pe(self, fn, reads=(), writes=()):
        return self.add("pe", fn, reads, writes)

    def act(self, fn, reads=(), writes=()):
        return self.add("act", fn, reads, writes)

    def dve(self, fn, reads=(), writes=()):
        return self.add("dve", fn, reads, writes)

    def pool(self, fn, reads=(), writes=()):
        return self.add("pool", fn, reads, writes)

    def dma(self, eng, out_ap, in_ap, reads=(), writes=(), **kw):
        return self.add(eng, lambda e: e.dma_start(out=out_ap, in_=in_ap, **kw), reads, writes, dma=True)

    def finish(self, final_waits=()):
        nc = self.nc
        sem_objs = {}
        stack = []

        def getsem(key):
            if key not in sem_objs:
                g = nc.semaphore("s_%s_%s" % key if isinstance(key, tuple) else "s_%s" % key)
                sem_objs[key] = g.__enter__()
                stack.append(g)
            return sem_objs[key]

        for e in ENGS:
            cnt = 0
            dcnt = {}
            for op in self.ops[e]:
                if op.dma:
                    dcnt[op.sem] = dcnt.get(op.sem, 0) + op.inc
                    op.val = dcnt[op.sem]
                elif op.needed:
                    cnt += 1
                    op.sem = e
                    op.val = cnt
        for op in final_waits:
            op.needed = True
        engmap = {"pe": "tensor", "act": "scalar", "dve": "vector", "pool": "gpsimd", "sp": "sync"}
        prog = self

        def emit(e, engine):
            waited = {}
            ops = prog.ops[e]
            for op in ops:
                need = {}
                dl = list(op.deps)
                if op.slot_prev is not None:
                    dl.append(op.slot_prev)
                for d in dl:
                    if waited.get(d.sem, 0) >= d.val:
                        continue
                    if need.get(d.sem, 0) < d.val:
                        need[d.sem] = d.val
                for s, v in need.items():
                    engine.wait_ge(getsem(s), v)
                    waited[s] = v
                ins = op.fn(engine)
                if op.dma:
                    ins.then_inc(getsem(op.sem), op.inc)
                elif op.needed:
                    ins.then_inc(getsem(op.sem), 1)
            if e == "sp":
                for op in final_waits:
                    if waited.get(op.sem, 0) < op.val:
                        engine.wait_ge(getsem(op.sem), op.val)
                        waited[op.sem] = op.val

        for e in ENGS:
            for op in self.ops[e]:
                if op.sem is not None and (op.needed or op.dma):
                    getsem(op.sem)
        with nc.Block() as block:
            for e in ENGS:
                if not self.ops[e] and e != "sp":
                    continue
                getattr(block, engmap[e])(lambda engine, e=e: emit(e, engine))
        for g in reversed(stack):
            g.__exit__(None, None, None)
        for g in reversed(self._ctx):
            g.__exit__(None, None, None)
        self._ctx = []
        n = {e: len(self.ops[e]) for e in ENGS}
        return n


def _fence(P):
    f = []
    for e in ENGS:
        ops = P.ops[e]
        last_c = None
        nd = 0
        for op in reversed(ops):
            if op.dma:
                if nd < DMA_POOL:
                    f.append(op)
                    nd += 1
            elif last_c is None:
                last_c = op
                f.append(op)
            if nd >= DMA_POOL and last_c is not None:
                break
    return f


def _fenced(ap, fence, name=""):
    t = T(ap, name)
    t.r = list(fence)
    return t


D = 1024
KT = 8
ALPHA = float(8 ** 0.25)
EPS = 1e-5
NEG = -1.0e30
NEXP = 32


def _din(nc, name, shape, dt=F32):
    return nc.dram_tensor(name, list(shape), dt, kind="ExternalInput").ap()


def _dout(nc, name, shape, dt=F32):
    return nc.dram_tensor(name, list(shape), dt, kind="ExternalOutput").ap()


def _adaln(P, w_ap, b_sb, sc, wfull, ps, mod):
    for kt in range(KT):
        P.dma("sp", wfull[:, kt, :], w_ap[kt * 128:(kt + 1) * 128, :], writes=[wfull])
    for ft in range(24):
        for kt in range(KT):
            P.pe(lambda e, kt=kt, ft=ft: e.matmul(
                ps[:, ft:ft + 1], wfull[:, kt, ft * 128:(ft + 1) * 128], sc[:, kt:kt + 1],
                start=(kt == 0), stop=(kt == KT - 1)), reads=[wfull, sc], writes=[ps])
    P.dve(lambda e: e.tensor_tensor(out=mod[:, 0:24], in0=ps[:, 0:24], in1=b_sb[:, 0:24], op=ALU.add),
          reads=[ps, b_sb], writes=[mod])


def _layernorm(P, v, sq, ps1, ps2, ones, tmp, gcol, bcol, out, TB):
    P.act(lambda e: e.activation(out=sq[:, :, 0:TB], in_=v[:, :, 0:TB], func=AF.Square), reads=[v], writes=[sq])
    for ft in range(KT):
        P.pe(lambda e, ft=ft: e.matmul(ps1[:, 0:TB], ones[:, 0:128], v[:, ft, 0:TB], start=(ft == 0), stop=(ft == KT - 1)),
             reads=[v, ones], writes=[ps1])
    for ft in range(KT):
        P.pe(lambda e, ft=ft: e.matmul(ps2[:, 0:TB], ones[:, 0:128], sq[:, ft, 0:TB], start=(ft == 0), stop=(ft == KT - 1)),
             reads=[sq, ones], writes=[ps2])
    mean, msq, rstd = tmp
    P.dve(lambda e: e.tensor_scalar(out=mean[:, 0:TB], in0=ps1[:, 0:TB], scalar1=1.0 / D, scalar2=None, op0=ALU.mult),
          reads=[ps1], writes=[mean])
    P.dve(lambda e: e.tensor_tensor(out=msq[:, 0:TB], in0=mean[:, 0:TB], in1=mean[:, 0:TB], op=ALU.mult),
          reads=[mean], writes=[msq])
    P.dve(lambda e: e.scalar_tensor_tensor(out=rstd[:, 0:TB], in0=ps2[:, 0:TB], scalar=1.0 / D, in1=msq[:, 0:TB],
                                           op0=ALU.mult, op1=ALU.subtract), reads=[ps2, msq], writes=[rstd])
    P.dve(lambda e: e.tensor_scalar(out=rstd[:, 0:TB], in0=rstd[:, 0:TB], scalar1=EPS, scalar2=None,
                                    op0=ALU.add), reads=[rstd], writes=[rstd])
    P.act(lambda e: e.activation(out=rstd[:, 0:TB], in_=rstd[:, 0:TB], func=AF.Sqrt), reads=[rstd], writes=[rstd])
    P.dve(lambda e: e.reciprocal(out=rstd[:, 0:TB], in_=rstd[:, 0:TB]), reads=[rstd], writes=[rstd])
    def bc_t(t):
        return t[:, 0:TB].rearrange("p (o t) -> p o t", o=1).to_broadcast([128, KT, TB])

    def bc_f(t):
        return t[:, 0:KT].rearrange("p (k o) -> p k o", o=1).to_broadcast([128, KT, TB])
    P.dve(lambda e: e.tensor_tensor(out=sq[:, :, 0:TB], in0=v[:, :, 0:TB], in1=bc_t(mean), op=ALU.subtract),
          reads=[v, mean], writes=[sq])
    P.pool(lambda e: e.tensor_tensor(out=sq[:, :, 0:TB], in0=sq[:, :, 0:TB], in1=bc_t(rstd), op=ALU.mult),
           reads=[sq, rstd], writes=[sq])
    P.dve(lambda e: e.tensor_tensor(out=sq[:, :, 0:TB], in0=sq[:, :, 0:TB], in1=bc_f(gcol), op=ALU.mult),
          reads=[sq, gcol], writes=[sq])
    P.pool(lambda e: e.tensor_tensor(out=out[:, :, 0:TB], in0=sq[:, :, 0:TB], in1=bc_f(bcol), op=ALU.add),
           reads=[sq, bcol], writes=[out])


def _routing(P, psR, lgs, rt, Wt, ti):
    gmax, ngmax, gsel, gexp, gsum, gval, pen, msk, mx8, dd, ed, w1, w2, wa, wb = rt
    P.dve(lambda e: e.tensor_copy(lgs[:, 0:36], psR[:, 0:36]), reads=[psR], writes=[lgs])
    P.dve(lambda e: e.reduce_max(out=gmax[:, 0:1], in_=lgs[:, 0:4], axis=AX.X), reads=[lgs], writes=[gmax])
    P.dve(lambda e: e.tensor_scalar(out=ngmax[:, 0:1], in0=gmax[:, 0:1], scalar1=-1.0, scalar2=None, op0=ALU.mult),
          reads=[gmax], writes=[ngmax])
    P.dve(lambda e: e.tensor_scalar(out=gsel[:, 0:4], in0=lgs[:, 0:4], scalar1=gmax[:, 0:1], scalar2=None,
                                    op0=ALU.is_equal), reads=[lgs, gmax], writes=[gsel])
    P.act(lambda e: e.activation(out=gexp[:, 0:4], in_=lgs[:, 0:4], func=AF.Exp, bias=ngmax[:, 0:1], scale=1.0),
          reads=[lgs, ngmax], writes=[gexp])
    P.dve(lambda e: e.reduce_sum(out=gsum[:, 0:1], in_=gexp[:, 0:4], axis=AX.X), reads=[gexp], writes=[gsum])
    P.dve(lambda e: e.reciprocal(out=gval[:, 0:1], in_=gsum[:, 0:1]), reads=[gsum], writes=[gval])
    P.dve(lambda e: e.tensor_scalar(out=pen[:, 0:4], in0=gsel[:, 0:4], scalar1=-1.0, scalar2=-NEG,
                                    op0=ALU.add, op1=ALU.mult), reads=[gsel], writes=[pen])
    P.dve(lambda e: e.tensor_tensor(
        out=msk[:, 0:32].rearrange("p (g x) -> p g x", g=4),
        in0=lgs[:, 4:36].rearrange("p (g x) -> p g x", g=4),
        in1=pen[:, 0:4].rearrange("p (g o) -> p g o", o=1).to_broadcast([128, 4, 8]), op=ALU.add),
        reads=[lgs, pen], writes=[msk])
    P.dve(lambda e: e.max(out=mx8[:, 0:8], in_=msk[:, 0:32]), reads=[msk], writes=[mx8])
    P.dve(lambda e: e.tensor_tensor(out=dd[:, 0:1], in0=mx8[:, 1:2], in1=mx8[:, 0:1], op=ALU.subtract),
          reads=[mx8], writes=[dd])
    P.act(lambda e: e.activation(out=ed[:, 0:1], in_=dd[:, 0:1], func=AF.Exp), reads=[dd], writes=[ed])
    P.dve(lambda e: e.tensor_scalar(out=w1[:, 0:1], in0=ed[:, 0:1], scalar1=1.0, scalar2=None, op0=ALU.add),
          reads=[ed], writes=[w1])
    P.dve(lambda e: e.reciprocal(out=w1[:, 0:1], in_=w1[:, 0:1]), reads=[w1], writes=[w1])
    P.dve(lambda e: e.tensor_tensor(out=w1[:, 0:1], in0=w1[:, 0:1], in1=gval[:, 0:1], op=ALU.mult),
          reads=[w1, gval], writes=[w1])
    P.dve(lambda e: e.tensor_tensor(out=w2[:, 0:1], in0=w1[:, 0:1], in1=ed[:, 0:1], op=ALU.mult),
          reads=[w1, ed], writes=[w2])
    P.dve(lambda e: e.tensor_scalar(out=wa[:, 0:32], in0=msk[:, 0:32], scalar1=mx8[:, 0:1], scalar2=w1[:, 0:1],
                                    op0=ALU.is_equal, op1=ALU.mult), reads=[msk, mx8, w1], writes=[wa])
    P.dve(lambda e: e.tensor_scalar(out=wb[:, 0:32], in0=msk[:, 0:32], scalar1=mx8[:, 1:2], scalar2=w2[:, 0:1],
                                    op0=ALU.is_equal, op1=ALU.mult), reads=[msk, mx8, w2], writes=[wb])
    P.dve(lambda e, ti=ti: e.tensor_tensor(out=Wt[:, ti, 0:32], in0=wa[:, 0:32], in1=wb[:, 0:32], op=ALU.add),
          reads=[wa, wb], writes=[Wt])


def emit_p2(P, nc, banks, NT, io, nexp=NEXP):
    TB = 256
    NB = NT // TB
    NTT = NT // 128
    TE = min(512, NT)
    NBE = NT // TE
    xT = io["xs"]; ccol = io["ccol"]
    w_am = io["w_am"]; b_am = io["b_am"]; w_af = io["w_af"]; b_af = io["b_af"]
    w_g = io["w_g"]; w_bs = io["w_bs"]; w_ba = io["w_ba"]; w_o = io["w_o"]
    lnp = io["lnp"]; w_r = io["w_r"]; b_r = io["b_r"]
    w_eg = io["w_eg"]; w_eu = io["w_eu"]; w_ed = io["w_ed"]
    cst = io["cst2"]; xoT = io["xo"]; Yg_t = io["Yg_t"]
    x1scr_ap = io["x1scr"]; x1scr = io["x1scr_t"]
    xs_t = [io["xs_t"]] if io.get("xs_t") is not None else []
    xo_t = [io["xo_t"]] if io.get("xo_t") is not None else []
    P.begin_phase()
    Yms = io["Yms"]; Yma = io["Yma"]; Ygs = io["Ygs"]; Yga = io["Yga"]; Ym_t = io["Ym_t"]
    CB = io["CB"]
    for ch in range((NT // 256) // CB):
        def cps(e, ch=ch):
            return e.dma_start(out=Yms[ch].rearrange("g c p f -> (g c p f)").rearrange("(a b) -> a b", a=128),
                               in_=Ygs[P.rank(e), ch].rearrange("g c p f -> (g c p f)").rearrange("(a b) -> a b", a=128))
        P.add("sp", cps, reads=[Yg_t], writes=[Ym_t], dma=True)

    def cpa(e):
        return e.dma_start(out=Yma.rearrange("i g p t -> (i g p t)").rearrange("(a b) -> a b", a=128),
                           in_=Yga[P.rank(e)].rearrange("i g p t -> (i g p t)").rearrange("(a b) -> a b", a=128))
    P.add("sp", cpa, reads=[Yg_t], writes=[Ym_t], dma=True)
    arena = P.sb("arena", [128, 49152], BF16)
    arena2 = P.sb("arena2", [128, 26624], BF16)
    h2raw = P.sb("h2raw", [128, 8 * NT], BF16)
    ident = P.sb("ident", [128, 128]); ones = P.sb("ones", [128, 128])
    sc = P.sb("sc", [128, 8]); lnv = [P.sb("lnv%d" % i, [128, 8]) for i in range(4)]
    bam = P.sb("bam", [128, 24]); baf = P.sb("baf", [128, 24])
    modm = P.sb("modm", [128, 24]); modf = P.sb("modf", [128, 24])
    sc1m = P.sb("sc1m", [128, 8]); g1pm = P.sb("g1pm", [128, 8]); sc1f = P.sb("sc1f", [128, 8]); g1pf = P.sb("g1pf", [128, 8])
    wr = P.sb("wr", [128, 8, 36]); br = P.sb("br", [1, 36])
    Wt = P.sb("Wt", [128, NTT, 32])
    gs = [P.sb("gs%d" % i, [128, 2, TB]) for i in range(2)]
    m1 = [P.sb("m1%d" % i, [128, TB]) for i in range(2)]
    m2 = [P.sb("m2%d" % i, [128, TB]) for i in range(2)]
    lntmp = [P.sb("lnt%d" % i, [128, TB]) for i in range(3)]
    lgs = P.sb("lgs", [128, 36])
    rt = [P.sb("rt%d" % i, [128, 32]) for i in range(15)]
    A0, A1, B0, B1, G0, G1, L, R = banks

    P.dma("sp", ident[:, :], cst[:, 0:128], writes=[ident])
    P.dma("sp", ones[:, :], cst[:, 128:256], writes=[ones])
    P.dma("sp", sc[:, :], ccol[:, :], writes=[sc])
    for i in range(4):
        P.dma("sp", lnv[i][:, :], lnp[:, i * 8:(i + 1) * 8], writes=[lnv[i]])
    P.dma("sp", bam[:, :], b_am[:, :], writes=[bam])
    P.dma("sp", baf[:, :], b_af[:, :], writes=[baf])
    P.dma("sp", wr[:, :, :], w_r.rearrange("(k p) n -> p k n", p=128), writes=[wr])
    P.dma("sp", br[:, :], b_r[:, :], writes=[br])
    P.act(lambda e: e.activation(out=sc[:, :], in_=sc[:, :], func=AF.Silu), reads=[sc], writes=[sc])
    wfull = T(arena.ap[:, 0:49152].bitcast(F32).rearrange("p (k f) -> p k f", k=8), "wfull")
    P.dma("sp", modm[:, 0:24], io["modscr"], reads=[io["mod_t"]], writes=[modm])
    _adaln(P, w_af, baf, sc, wfull, R, modf)
    for (mod, s1, g1) in ((modm, sc1m, g1pm), (modf, sc1f, g1pf)):
        P.dve(lambda e, mod=mod, s1=s1: e.tensor_scalar(out=s1[:, :], in0=mod[:, 8:16], scalar1=1.0, scalar2=None, op0=ALU.add),
              reads=[mod], writes=[s1])
        P.dve(lambda e, mod=mod, g1=g1: e.tensor_scalar(out=g1[:, :], in0=mod[:, 16:24], scalar1=1.0, scalar2=None, op0=ALU.add),
              reads=[mod], writes=[g1])
    f0 = _fence(P)
    h2b = T(h2raw.ap[:, 0:8 * NT].rearrange("p (k t) -> p k t", k=8), "h2b")

    wg = _fenced(arena.ap[:, 0:16384].rearrange("p (k f) -> p k f", k=8), f0, "wg")
    wbs = _fenced(arena.ap[:, 16384:32768].rearrange("p (k f) -> p k f", k=16), f0, "wbs")
    wba = _fenced(arena.ap[:, 32768:40960].rearrange("p (k f) -> p k f", k=8), f0, "wba")
    wo = _fenced(arena.ap[:, 40960:49152].rearrange("p (k f) -> p k f", k=8), f0, "wo")
    for kt in range(8):
        P.dma("pool", wg[:, kt, :], w_g[kt * 128:(kt + 1) * 128, :], writes=[wg])
    for kt in range(16):
        P.dma("pool", wbs[:, kt, :], w_bs[kt * 128:(kt + 1) * 128, :], writes=[wbs])
    for kt in range(8):
        P.dma("pool", wba[:, kt, :], w_ba[kt * 128:(kt + 1) * 128, :], writes=[wba])
    for kt in range(8):
        P.dma("pool", wo[:, kt, :], w_o[kt * 128:(kt + 1) * 128, :], writes=[wo])

    def a2(off, n, dt, shape_k, name, fence=None):
        ap = arena2.ap[:, off:off + n]
        if dt == F32:
            ap = ap.bitcast(F32)
        ap = ap.rearrange("p (k t) -> p k t", k=shape_k)
        return _fenced(ap, fence, name) if fence is not None else T(ap, name)

    xb = a2(0, 4096, F32, 8, "xb"); v = a2(4096, 4096, F32, 8, "v"); sq = a2(8192, 4096, F32, 8, "sq")
    hT = a2(12288, 2048, BF16, 8, "hT"); ys = a2(14336, 4096, BF16, 16, "ys"); ya = a2(18432, 2048, BF16, 8, "ya")
    mg = a2(20480, 2048, BF16, 8, "mg"); h2f = a2(22528, 4096, F32, 8, "h2f")

    def bc_f(t, lo=0):
        return t[:, lo:lo + 8].rearrange("p (k o) -> p k o", o=1).to_broadcast([128, 8, TB])

    def blk(ap, tb):
        return ap[tb].rearrange("p (k t) -> p k t", k=8)

    for tb in range(NB):
        t0 = tb * TB
        P.dma("sp", xb[:, :, :], blk(xT, tb), reads=xs_t, writes=[xb])
        for gp in range(4):
            P.dma("sp", ys[:, gp * 4:(gp + 1) * 4, :], Yms[tb // CB, gp, tb % CB].rearrange("p (i t) -> p i t", i=4), reads=[Ym_t], writes=[ys])
            P.dma("sp", ya[:, gp * 2:(gp + 1) * 2, :], Yma[0:2, gp, :, t0:t0 + TB].rearrange("i p t -> p i t"), reads=[Ym_t], writes=[ya])
        P.dve(lambda e: e.tensor_tensor(out=v[:, :, :], in0=xb[:, :, :], in1=bc_f(sc1m), op=ALU.mult),
              reads=[xb, sc1m], writes=[v])
        P.dve(lambda e: e.tensor_tensor(out=hT[:, :, :], in0=v[:, :, :], in1=bc_f(modm, 0), op=ALU.add),
              reads=[v, modm], writes=[hT])
        P.pool(lambda e: e.tensor_scalar(out=xb[:, :, :], in0=xb[:, :, :], scalar1=ALPHA, scalar2=None, op0=ALU.mult),
               reads=[xb], writes=[xb])
        for ft in range(8):
            pa, pb, pg = (A0, A1)[ft % 2], (B0, B1)[ft % 2], (G0, G1)[ft % 2]
            gsx, m1x, m2x = gs[ft % 2], m1[ft % 2], m2[ft % 2]
            fs = slice(ft * 128, (ft + 1) * 128)
            fs2 = slice(1024 + ft * 128, 1024 + (ft + 1) * 128)
            for kt in range(16):
                P.pe(lambda e, pa=pa, kt=kt, fs=fs: e.matmul(pa[:, 0:TB], wbs[:, kt, fs], ys[:, kt, :], start=(kt == 0), stop=(kt == 15)),
                     reads=[wbs, ys], writes=[pa])
            for kt in range(8):
                P.pe(lambda e, pb=pb, kt=kt, fs=fs: e.matmul(pb[:, 0:TB], wba[:, kt, fs], ya[:, kt, :], start=(kt == 0), stop=(kt == 7)),
                     reads=[wba, ya], writes=[pb])
            for kt in range(8):
                P.pe(lambda e, pg=pg, kt=kt, fs=fs: e.matmul(pg[:, 0:TB], wg[:, kt, fs], hT[:, kt, :], start=(kt == 0), stop=(kt == 7)),
                     reads=[wg, hT], writes=[pg])
            for kt in range(8):
                P.pe(lambda e, pg=pg, kt=kt, fs2=fs2: e.matmul(pg[:, TB:2 * TB], wg[:, kt, fs2], hT[:, kt, :], start=(kt == 0), stop=(kt == 7)),
                     reads=[wg, hT], writes=[pg])
            P.act(lambda e, pg=pg, gsx=gsx: e.activation(out=gsx[:, :, :].rearrange("p a t -> p (a t)"), in_=pg[:, 0:2 * TB], func=AF.Sigmoid),
                  reads=[pg], writes=[gsx])
            P.dve(lambda e, pa=pa, gsx=gsx, m1x=m1x: e.tensor_tensor(out=m1x[:, :], in0=pa[:, 0:TB], in1=gsx[:, 0, :], op=ALU.mult),
                  reads=[pa, gsx], writes=[m1x])
            P.dve(lambda e, pb=pb, gsx=gsx, m2x=m2x: e.tensor_tensor(out=m2x[:, :], in0=pb[:, 0:TB], in1=gsx[:, 1, :], op=ALU.mult),
                  reads=[pb, gsx], writes=[m2x])
            P.pool(lambda e, ft=ft, m1x=m1x, m2x=m2x: e.tensor_tensor(out=mg[:, ft, :], in0=m1x[:, :], in1=m2x[:, :], op=ALU.add),
                   reads=[m1x, m2x], writes=[mg])
        for ft in range(8):
            pa = (A0, A1)[ft % 2]
            fs = slice(ft * 128, (ft + 1) * 128)
            for kt in range(8):
                P.pe(lambda e, pa=pa, kt=kt, fs=fs: e.matmul(pa[:, 0:TB], wo[:, kt, fs], mg[:, kt, :], start=(kt == 0), stop=(kt == 7)),
                     reads=[wo, mg], writes=[pa])
            P.dve(lambda e, pa=pa, ft=ft: e.scalar_tensor_tensor(out=v[:, ft, :], in0=pa[:, 0:TB], scalar=g1pm[:, ft:ft + 1],
                                                                 in1=xb[:, ft, :], op0=ALU.mult, op1=ALU.add),
                  reads=[pa, g1pm, xb], writes=[v])
        _layernorm(P, v, sq, L, R, ones, lntmp, lnv[0], lnv[1], xb, TB)
        P.dma("sp", blk(x1scr_ap, tb), xb[:, :, :], reads=[xb], writes=[x1scr])
        P.dve(lambda e: e.tensor_tensor(out=v[:, :, :], in0=xb[:, :, :], in1=bc_f(sc1f), op=ALU.mult),
              reads=[xb, sc1f], writes=[v])
        P.dve(lambda e: e.tensor_tensor(out=h2f[:, :, :], in0=v[:, :, :], in1=bc_f(modf, 0), op=ALU.add),
              reads=[v, modf], writes=[h2f])
        P.act(lambda e, t0=t0: e.copy(h2b[:, :, t0:t0 + TB], h2f[:, :, :]), reads=[h2f], writes=[h2b])
        for tt in range(TB // 128):
            ti = tb * (TB // 128) + tt
            for kt in range(8):
                P.pe(lambda e, kt=kt, tt=tt: e.matmul(R[:, 0:36], h2f[:, kt, tt * 128:(tt + 1) * 128], wr[:, kt, :],
                                                     start=(kt == 0), stop=False), reads=[h2f, wr], writes=[R])
            P.pe(lambda e: e.matmul(R[:, 0:36], ones[0:1, 0:128], br[0:1, 0:36], start=False, stop=True),
                 reads=[ones, br], writes=[R])
            _routing(P, R, lgs, rt, Wt, ti)

    fAB = _fence(P)
    acc = _fenced(arena.ap[:, 0:NTT * 2048].bitcast(F32).rearrange("p (t d) -> p t d", t=NTT), fAB, "acc")
    slots = [_fenced(arena.ap[:, 32768:45056], fAB, "slot0"), _fenced(arena2.ap[:, 0:12288], fAB, "slot1")]
    act = _fenced(arena2.ap[:, 12288:12288 + 4 * TE].rearrange("p (k t) -> p k t", k=4), fAB, "act")
    sgs = [_fenced(arena2.ap[:, 14336 + i * 2 * TE:14336 + (i + 1) * 2 * TE].bitcast(F32), fAB, "sg%d" % i) for i in range(2)]
    for ex in range(nexp):
        slot = slots[ex % 2]
        wge = slot.ap[:, 0:4096].rearrange("p (k f) -> p k f", k=8)
        wue = slot.ap[:, 4096:8192].rearrange("p (k f) -> p k f", k=8)
        wde = slot.ap[:, 8192:12288].rearrange("p (k f) -> p k f", k=4)
        P.dma("pool", slot.ap[:, 0:4096], w_eg[ex], writes=[slot])
        P.dma("pool", slot.ap[:, 4096:8192], w_eu[ex], writes=[slot])
        P.dma("pool", slot.ap[:, 8192:12288], w_ed[ex], writes=[slot])
        for tb in range(NBE):
            t0 = tb * TE
            for ff in range(4):
                pg, pu, sg = (A0, A1)[ff % 2], (B0, B1)[ff % 2], sgs[ff % 2]
                fs = slice(ff * 128, (ff + 1) * 128)
                for kt in range(8):
                    P.pe(lambda e, pg=pg, kt=kt, fs=fs, wge=wge, t0=t0: e.matmul(pg[:, 0:TE], wge[:, kt, fs], h2b[:, kt, t0:t0 + TE],
                                                                              start=(kt == 0), stop=(kt == 7)),
                         reads=[slot, h2b], writes=[pg])
                for kt in range(8):
                    P.pe(lambda e, pu=pu, kt=kt, fs=fs, wue=wue, t0=t0: e.matmul(pu[:, 0:TE], wue[:, kt, fs], h2b[:, kt, t0:t0 + TE],
                                                                              start=(kt == 0), stop=(kt == 7)),
                         reads=[slot, h2b], writes=[pu])
                P.act(lambda e, pg=pg, sg=sg: e.activation(out=sg[:, 0:TE], in_=pg[:, 0:TE], func=AF.Silu), reads=[pg], writes=[sg])
                P.dve(lambda e, pu=pu, sg=sg, ff=ff: e.tensor_tensor(out=act[:, ff, :], in0=pu[:, 0:TE], in1=sg[:, 0:TE], op=ALU.mult),
                      reads=[pu, sg], writes=[act])
            for tt in range(TE // 128):
                ti = tb * (TE // 128) + tt
                for dh in range(2):
                    pd = (G0, G1)[dh]
                    for ff in range(4):
                        P.pe(lambda e, pd=pd, ff=ff, tt=tt, dh=dh, wde=wde: e.matmul(
                            pd[:, 0:512], act[:, ff, tt * 128:(tt + 1) * 128], wde[:, ff, dh * 512:(dh + 1) * 512],
                            start=(ff == 0), stop=(ff == 3)), reads=[act, slot], writes=[pd])
                    if ex == 0:
                        P.dve(lambda e, pd=pd, ti=ti, dh=dh, ex=ex: e.tensor_scalar(
                            out=acc[:, ti, dh * 512:(dh + 1) * 512], in0=pd[:, 0:512], scalar1=Wt[:, ti, ex:ex + 1], scalar2=None,
                            op0=ALU.mult), reads=[pd, Wt], writes=[acc])
                    else:
                        P.dve(lambda e, pd=pd, ti=ti, dh=dh, ex=ex: e.scalar_tensor_tensor(
                            out=acc[:, ti, dh * 512:(dh + 1) * 512], in0=pd[:, 0:512], scalar=Wt[:, ti, ex:ex + 1],
                            in1=acc[:, ti, dh * 512:(dh + 1) * 512], op0=ALU.mult, op1=ALU.add), reads=[pd, Wt, acc], writes=[acc])

    fBC = _fence(P)
    xb2 = a2(0, 4096, F32, 8, "xb2", fBC); v2 = a2(4096, 4096, F32, 8, "v2", fBC); sq2 = a2(8192, 4096, F32, 8, "sq2", fBC)
    outs = []
    for tb in range(NB):
        t0 = tb * TB
        P.dma("sp", xb2[:, :, :], blk(x1scr_ap, tb), reads=[x1scr], writes=[xb2])
        P.pool(lambda e: e.tensor_scalar(out=xb2[:, :, :], in0=xb2[:, :, :], scalar1=ALPHA, scalar2=None, op0=ALU.mult),
               reads=[xb2], writes=[xb2])
        for ft in range(8):
            pa = (A0, A1)[ft % 2]
            for tt in range(TB // 128):
                ti = tb * (TB // 128) + tt
                P.pe(lambda e, pa=pa, ti=ti, tt=tt, ft=ft: e.matmul(pa[:, tt * 128:(tt + 1) * 128], acc[:, ti, ft * 128:(ft + 1) * 128],
                                                                   ident[:, 0:128], start=True, stop=True),
                     reads=[acc, ident], writes=[pa])
            P.dve(lambda e, pa=pa, ft=ft: e.scalar_tensor_tensor(out=v2[:, ft, :], in0=pa[:, 0:TB], scalar=g1pf[:, ft:ft + 1],
                                                                 in1=xb2[:, ft, :], op0=ALU.mult, op1=ALU.add),
                  reads=[pa, g1pf, xb2], writes=[v2])
        _layernorm(P, v2, sq2, L, R, ones, lntmp, lnv[2], lnv[3], xb2, TB)
        outs.append(P.dma("sp", blk(xoT, tb), xb2[:, :, :], reads=[xb2], writes=xo_t))
    return outs


def _col(vec, n):
    return np.ascontiguousarray(np.asarray(vec).reshape(n, 128).T)


def _pmajor(w, k):
    E, _, F = w.shape
    return np.ascontiguousarray(w.reshape(E, k, 128, F).transpose(0, 2, 1, 3).reshape(E, 128, k * F))


def _xblocks(xts):
    n = xts.shape[0] // 256
    return np.ascontiguousarray(xts.reshape(n, 256, 8, 128).transpose(0, 3, 2, 1).reshape(n, 128, 2048))


def _xunblocks(xb):
    n = xb.shape[0]
    return np.ascontiguousarray(xb.reshape(n, 128, 8, 256).transpose(0, 3, 2, 1).reshape(n * 256, 1024))


def _consts():
    c = np.zeros((128, 256), np.float32)
    c[:, 0:128] = np.eye(128, dtype=np.float32)
    c[:, 128:256] = 1.0
    return c


def p2_inputs(inp, l):
    return {
        "w_af": inp["w_ada_ffn"][l], "b_af": _col(inp["b_ada_ffn"][l], 24),
        "w_g": np.ascontiguousarray(inp["w_in"][l][:, 8224:10272]),
        "w_bs": inp["w_branch_ssd"][l], "w_ba": inp["w_branch_attn"][l], "w_o": inp["w_out"][l],
        "lnp": np.ascontiguousarray(np.concatenate([_col(inp["ln_mix_g"][l], 8), _col(inp["ln_mix_b"][l], 8),
                                                    _col(inp["ln_ffn_g"][l], 8), _col(inp["ln_ffn_b"][l], 8)], axis=1)),
        "w_r": np.ascontiguousarray(np.concatenate([inp["w_router_group"][l], inp["w_router_expert"][l]], axis=1)),
        "b_r": np.ascontiguousarray(np.concatenate([inp["b_router_group"][l], inp["b_router_expert"][l]])[None, :]),
        "w_eg": _pmajor(inp["w_expert_gate"][l], 8), "w_eu": _pmajor(inp["w_expert_up"][l], 8),
        "w_ed": _pmajor(inp["w_expert_down"][l], 4),
    }


C1_ID, C1_ONES, C1_U, C1_UW0, C1_UW1, C1_CM, C1_CB, C1_MISC, C1_N = 0, 128, 256, 384, 640, 896, 1408, 1920, 1928
W1_NF = 2304
PI = float(np.pi)


def _consts1():
    c = np.zeros((128, C1_N), np.float32)
    c[:, C1_ID:C1_ID + 128] = np.eye(128, dtype=np.float32)
    c[:, C1_ONES:C1_ONES + 128] = 1.0
    U = np.triu(np.ones((128, 128), np.float32))
    c[:, C1_U:C1_U + 128] = U
    c[:, C1_UW0:C1_UW0 + 128] = U
    c[:, C1_UW0 + 128:C1_UW0 + 256] = 1.0
    c[:, C1_UW1 + 128:C1_UW1 + 256] = U
    s = np.arange(128)[:, None]
    l = np.arange(256)[None, :]
    m0 = (l >= s).astype(np.float32)
    m1 = (l >= s + 128).astype(np.float32)
    c[:, C1_CM:C1_CM + 256] = m0
    c[:, C1_CM + 256:C1_CM + 512] = m1
    c[:, C1_CB:C1_CB + 256] = (m0 - 1.0) * 1.0e30
    c[:, C1_CB + 256:C1_CB + 512] = (m1 - 1.0) * 1.0e30
    p = np.arange(128)
    inv_freq = (10000.0 ** (-np.arange(0, 64, 2, dtype=np.float32) / 64)).astype(np.float32)
    c[:, C1_MISC + 0] = inv_freq[p % 32]
    c[:, C1_MISC + 1] = np.where((p % 64) < 32, -1.0, 1.0)
    c[:, C1_MISC + 2] = -PI
    return c


def _esel():
    e = np.zeros((32, 32, 128), np.float32)
    for n in range(32):
        e[n, n, :] = 1.0
    return e.reshape(32, 4096)


def emit_p1(P, nc, banks, S, io):
    NCH = S // 256
    H2 = S // 4
    ccol = io["ccol"]; w_am = io["w_am"]; b_am = io["b_am"]; w1 = io["w1"]
    convw = io["convw"]; convb = io["convb"]; pvec = io["pvec"]; pos = io["pos"]; cst = io["cst1"]
    Yls = io["Yls"]; Yla = io["Yla"]; Yloc_t = io["Yloc_t"]
    NBq = (S // 4) // 256
    x_t = [io["x_t"]] if io.get("x_t") is not None else []
    P.begin_phase()
    big = P.sb("big", [128, max(49152, 20480 + 4 * S)], BF16)
    cs = P.sb("cs", [128, C1_N])
    sc = P.sb("sc", [128, 8]); bam = P.sb("bam", [128, 24]); modm = P.sb("modm", [128, 24]); sc1 = P.sb("sc1", [128, 8])
    cw = P.sb("cw", [128, 24]); cb = P.sb("cb", [128, 6]); pv = P.sb("pv", [128, 24])
    aneg = P.sb("aneg", [128, 8]); wdt = P.sb("wdt", [128, 8, 8])
    BJ, PM, PG, PY, PS0, PS1, PO, PD = banks

    P.dma("sp", cs[:, :], cst[:, :], writes=[cs])
    P.dma("sp", sc[:, :], ccol[:, :], writes=[sc])
    P.dma("sp", bam[:, :], b_am[:, :], writes=[bam])
    P.dma("sp", cw[:, :], convw[:, :], writes=[cw])
    P.dma("sp", cb[:, :], convb[:, :], writes=[cb])
    P.dma("sp", pv[:, :], pvec[:, :], writes=[pv])
    P.dma("sp", wdt[:, :, :], w1.rearrange("(k p) n -> p k n", p=128)[:, :, 2560:2568], writes=[wdt])
    P.act(lambda e: e.activation(out=sc[:, :], in_=sc[:, :], func=AF.Silu), reads=[sc], writes=[sc])
    P.act(lambda e: e.activation(out=aneg[:, :], in_=pv[:, 16:24], func=AF.Exp), reads=[pv], writes=[aneg])
    P.dve(lambda e: e.tensor_scalar(out=aneg[:, :], in0=aneg[:, :], scalar1=-1.0, scalar2=None, op0=ALU.mult),
          reads=[aneg], writes=[aneg])
    wfull = T(big.ap[:, 0:49152].bitcast(F32).rearrange("p (k f) -> p k f", k=8), "wfull")
    _adaln(P, w_am, bam, sc, wfull, PM, modm)
    P.dma("sp", io["modscr"], modm[:, 0:24], reads=[modm], writes=[io["mod_t"]])
    P.dve(lambda e: e.tensor_scalar(out=sc1[:, :], in0=modm[:, 8:16], scalar1=1.0, scalar2=None, op0=ALU.add),
          reads=[modm], writes=[sc1])
    f0 = _fence(P)
    wsb = _fenced(big.ap[:, 0:20480].rearrange("p (k f) -> p k f", k=8), f0, "wsb")
    kT_ap = big.ap[:, 20480:20480 + 2 * S].rearrange("p (i t) -> p i t", i=2)
    V_ap = big.ap[:, 20480 + 2 * S:20480 + 4 * S].rearrange("p (n f) -> p n f", f=256)
    kTt = [_fenced(kT_ap, f0, "kT%d" % c) for c in range(NCH)]
    Vt = [_fenced(V_ap, f0, "V%d" % c) for c in range(NCH)]
    for kt in range(8):
        P.dma("pool", wsb[:, kt, :], w1[kt * 128:(kt + 1) * 128, 0:2560], writes=[wsb])

    xc = P.sb("xc", [128, 8, 256]); hTf = xc; hTb = P.sb("hTb", [128, 8, 256], BF16)
    zs = P.sb("zs", [128, 4, 256]); cin = P.sb("cin", [128, 6, 259]); xcv = P.sb("xcv", [128, 6, 256])
    BTb = P.sb("BTb", [128, 256], BF16); CTb = P.sb("CTb", [128, 256], BF16)
    posi = P.sb("posi", [128, 256], I32); ang = P.sb("ang", [128, 256]); tm = P.sb("tm", [128, 256])
    cosT = P.sb("cosT", [128, 256]); sinT = P.sb("sinT", [128, 256])
    tA = P.sb("tA", [128, 256]); tB = P.sb("tB", [128, 256]); cacc = tA
    qTf = P.sb("qTf", [128, 2, 256]); qTb = P.sb("qTb", [128, 2, 256], BF16); kTf = P.sb("kTf", [128, 2, 256])
    kmean = P.sb("kmean", [128, 2, 32]); ksum = P.sb("ksum", [128, 2])
    dtr = P.sb("dtr", [128, 2, 8]); dta_ = P.sb("dta", [128, 2, 8]); dtt = [P.sb("dtt%d" % i, [128, 2, 8]) for i in range(4)]
    cssb = P.sb("cssb", [128, 24]); negcs = P.sb("negcs", [128, 2, 8]); wend = P.sb("wend", [128, 2, 8])
    dtw = P.sb("dtw", [128, 2, 8]); dec = P.sb("dec", [128, 8])
    xtok = P.sb("xtok", [128, 2, 512]); XE = P.sb("XE", [128, 2, 512], BF16); XO = P.sb("XO", [128, 2, 512], BF16)
    xdtw = P.sb("xdtw", [128, 2, 512], BF16); Btok = P.sb("Btok", [128, 2, 128], BF16)
    Gm = P.sb("Gm", [128, 2, 256]); Dm = P.sb("Dm", [128, 2, 256]); dcy = Dm
    scT = [P.sb("scT%d" % i, [128, 2, 256], BF16) for i in range(2)]
    E1 = P.sb("E1", [128, 256]); CE = [P.sb("CE%d" % i, [128, 256], BF16) for i in range(2)]
    state = P.sb("state", [128, 512]); stE = P.sb("stE", [128, 512], BF16); stO = P.sb("stO", [128, 512], BF16)
    yD = P.sb("yD", [128, 256]); gy = P.sb("gy", [128, 4, 256]); sqg = P.sb("sqg", [128, 4, 256])
    rstd = P.sb("rstd", [128, 256]); yout = P.sb("yout", [128, 4, 256], BF16)
    gate = P.sb("gate", [128, 32]); mx8 = P.sb("mx8", [128, 8]); selm = P.sb("selm", [128, 32])
    selb = [P.sb("selb%d" % i, [128, 32]) for i in range(2)]
    selT = P.sb("selT", [32, 4, 256], BF16)
    pT = [P.sb("pT%d" % i, [128, 2, 256], BF16) for i in range(4)]
    rden = P.sb("rden", [64, 256]); yatt = P.sb("yatt", [64, 4, 256], BF16)
    onesb = P.sb("onesb", [128, 64], BF16); identb = P.sb("identb", [128, 128], BF16)
    cbias = P.sb("cbias", [128, 2, 256], BF16)

    ident = cs; invf = cs
    def C(off, n):
        return cs[:, off:off + n]

    P.dve(lambda e: e.tensor_copy(onesb[:, :], C(C1_ONES, 64)), reads=[cs], writes=[onesb])
    P.dve(lambda e: e.tensor_copy(identb[:, :], C(C1_ID, 128)), reads=[cs], writes=[identb])
    P.dve(lambda e: e.tensor_copy(cbias[:, :, :].rearrange("p a t -> p (a t)"), C(C1_CB, 512)), reads=[cs], writes=[cbias])
    P.dve(lambda e: e.memset(cin[:, :, :], 0.0), writes=[cin])
    P.pool(lambda e: e.memset(XE[:, :, :], 0.0), writes=[XE])
    P.pool(lambda e: e.memset(XO[:, :, :], 0.0), writes=[XO])
    P.pool(lambda e: e.memset(stE[:, :], 0.0), writes=[stE])
    P.pool(lambda e: e.memset(stO[:, :], 0.0), writes=[stO])
    P.dve(lambda e: e.memset(gate[:, :], NEG), writes=[gate])
    for i in range(2):
        P.dve(lambda e, i=i: e.memset(selb[i][:, :], 0.0), writes=[selb[i]])

    outs = []

    def bc8(ap):
        return ap.rearrange("p (r o) -> p r o", o=1).to_broadcast([128, 8, 64])

    def do_chunk(c):
        t0 = c * 256
        hh, col0 = t0 // H2, t0 % H2
        YsO = Yls[c // NBq, c % NBq].rearrange("p (i t) -> p i t", i=4)
        YaO = Yla[hh].rearrange("(h d) t -> d h t", d=64)
        P.dma("sp", xc[:, :, :], io["x_src"](c), reads=x_t, writes=[xc])
        P.dma("sp", posi[:, :], pos[0:1, t0:t0 + 256].to_broadcast([128, 256]), writes=[posi])
        P.dve(lambda e: e.tensor_tensor(out=hTf[:, :, :], in0=xc[:, :, :],
                                        in1=sc1[:, 0:8].rearrange("p (k o) -> p k o", o=1).to_broadcast([128, 8, 256]), op=ALU.mult),
              reads=[xc, sc1], writes=[xc])
        P.dve(lambda e: e.tensor_tensor(out=hTf[:, :, :], in0=hTf[:, :, :],
                                        in1=modm[:, 0:8].rearrange("p (k o) -> p k o", o=1).to_broadcast([128, 8, 256]), op=ALU.add),
              reads=[hTf, modm], writes=[hTf])
        P.act(lambda e: e.copy(hTb[:, :, :], hTf[:, :, :]), reads=[hTf], writes=[hTb])
        P.dve(lambda e: e.tensor_copy(ang[:, :], posi[:, :]), reads=[posi], writes=[ang])
        P.dve(lambda e: e.tensor_scalar(out=ang[:, :], in0=ang[:, :], scalar1=cs[:, C1_MISC:C1_MISC + 1], scalar2=None, op0=ALU.mult),
              reads=[ang, cs], writes=[ang])
        C1_, C2_ = 6.28125, 2 * PI - 6.28125
        P.dve(lambda e: e.tensor_scalar(out=tm[:, :], in0=ang[:, :], scalar1=1.0 / (2 * PI), scalar2=None, op0=ALU.mult), reads=[ang], writes=[tm])
        P.dve(lambda e: e.tensor_copy(posi[:, :], tm[:, :]), reads=[tm], writes=[posi])
        P.dve(lambda e: e.tensor_copy(tm[:, :], posi[:, :]), reads=[posi], writes=[tm])
        P.dve(lambda e: e.scalar_tensor_tensor(out=ang[:, :], in0=tm[:, :], scalar=-C1_, in1=ang[:, :], op0=ALU.mult, op1=ALU.add),
              reads=[tm, ang], writes=[ang])
        P.dve(lambda e: e.scalar_tensor_tensor(out=ang[:, :], in0=tm[:, :], scalar=-C2_, in1=ang[:, :], op0=ALU.mult, op1=ALU.add),
              reads=[tm, ang], writes=[ang])
        for (shift, dstT) in ((0.0, sinT), (0.5 * PI, cosT)):
            if shift != 0.0:
                P.dve(lambda e, shift=shift: e.tensor_scalar(out=ang[:, :], in0=ang[:, :], scalar1=shift, scalar2=None, op0=ALU.add),
                      reads=[ang], writes=[ang])
            P.dve(lambda e: e.tensor_scalar(out=tm[:, :], in0=ang[:, :], scalar1=PI, scalar2=-2 * PI, op0=ALU.is_gt, op1=ALU.mult),
                  reads=[ang], writes=[tm])
            P.dve(lambda e: e.tensor_tensor(out=ang[:, :], in0=ang[:, :], in1=tm[:, :], op=ALU.add), reads=[ang, tm], writes=[ang])
            P.dve(lambda e: e.tensor_scalar(out=tm[:, :], in0=ang[:, :], scalar1=-PI, scalar2=2 * PI, op0=ALU.is_lt, op1=ALU.mult),
                  reads=[ang], writes=[tm])
            P.dve(lambda e: e.tensor_tensor(out=ang[:, :], in0=ang[:, :], in1=tm[:, :], op=ALU.add), reads=[ang, tm], writes=[ang])
            P.act(lambda e, dstT=dstT: e.activation(out=dstT[:, :], in_=ang[:, :], func=AF.Sin), reads=[ang], writes=[dstT])
        P.dve(lambda e: e.tensor_scalar(out=sinT[:, :], in0=sinT[:, :], scalar1=cs[:, C1_MISC + 1:C1_MISC + 2], scalar2=None, op0=ALU.mult),
              reads=[sinT, cs], writes=[sinT])

        def proj(j, half):
            for kt in range(8):
                P.pe(lambda e, kt=kt: e.matmul(BJ[:, half * 256:(half + 1) * 256], wsb[:, kt, j * 128:(j + 1) * 128], hTb[:, kt, :],
                                               start=(kt == 0), stop=(kt == 7)), reads=[wsb, hTb], writes=[BJ])
        for j in range(4):
            proj(j, j % 2)
            P.act(lambda e, j=j: e.activation(out=zs[:, j, :], in_=BJ[:, (j % 2) * 256:(j % 2 + 1) * 256], func=AF.Silu),
                  reads=[BJ], writes=[zs])
        for jj in range(6):
            proj(4 + jj, jj % 2)
            P.act(lambda e, jj=jj: e.copy(cin[:, jj, 3:259], BJ[:, (jj % 2) * 256:(jj % 2 + 1) * 256]), reads=[BJ], writes=[cin])
        for i in range(2):
            for (jq, js, dst) in ((10 + i, 14 + i, "q"), (12 + i, 16 + i, "k")):
                proj(jq, 0)
                proj(js, 1)
                P.dve(lambda e: e.tensor_tensor(out=tA[:, :], in0=BJ[:, 0:256], in1=cosT[:, :], op=ALU.mult),
                      reads=[BJ, cosT], writes=[tA])
                P.dve(lambda e: e.tensor_tensor(out=tB[:, :], in0=BJ[:, 256:512], in1=sinT[:, :], op=ALU.mult),
                      reads=[BJ, sinT], writes=[tB])
                if dst == "q":
                    P.pool(lambda e, i=i: e.tensor_tensor(out=qTf[:, i, :], in0=tA[:, :], in1=tB[:, :], op=ALU.add),
                           reads=[tA, tB], writes=[qTf])
                    P.act(lambda e, i=i: e.mul(qTb[:, i, :], qTf[:, i, :], 0.125), reads=[qTf], writes=[qTb])
                else:
                    P.pool(lambda e, i=i: e.tensor_tensor(out=kTf[:, i, :], in0=tA[:, :], in1=tB[:, :], op=ALU.add),
                           reads=[tA, tB], writes=[kTf])
                    P.act(lambda e, i=i: e.copy(kT_ap[:, i, t0:t0 + 256], kTf[:, i, :]), reads=[kTf], writes=[kTt[c]])
        P.dve(lambda e: e.reduce_sum(out=ksum[:, 0:2], in_=kTf[:, :, :], axis=AX.X), reads=[kTf], writes=[ksum])
        P.dve(lambda e: e.tensor_scalar(out=kmean[:, :, c], in0=ksum[:, 0:2], scalar1=1.0 / 256, scalar2=None, op0=ALU.mult),
              reads=[ksum], writes=[kmean])
        for tt in range(2):
            for kt in range(8):
                P.pe(lambda e, kt=kt, tt=tt: e.matmul(BJ[:, tt * 256:(tt + 1) * 256], hTb[:, kt, tt * 128:(tt + 1) * 128], wsb[:, kt, 2304:2560],
                                                     start=(kt == 0), stop=(kt == 7)), reads=[hTb, wsb], writes=[BJ])
            P.act(lambda e, tt=tt: e.copy(V_ap[:, 2 * c + tt, :], BJ[:, tt * 256:(tt + 1) * 256]), reads=[BJ], writes=[Vt[c]])
        for tt in range(2):
            for kt in range(8):
                P.pe(lambda e, kt=kt, tt=tt: e.matmul(PM[:, tt * 8:(tt + 1) * 8], hTf[:, kt, tt * 128:(tt + 1) * 128], wdt[:, kt, :],
                                                     start=(kt == 0), stop=(kt == 7)), reads=[hTf, wdt], writes=[PM])
        a_, ab_, e_, l_ = dtt
        P.dve(lambda e: e.tensor_tensor(out=a_[:, :, :], in0=PM[:, 0:16].rearrange("p (t r) -> p t r", t=2),
                                        in1=pv[:, 8:16].rearrange("p (o r) -> p o r", o=1).to_broadcast([128, 2, 8]), op=ALU.add),
              reads=[PM, pv], writes=[a_])
        P.dve(lambda e: e.tensor_scalar(out=ab_[:, :, :], in0=a_[:, :, :], scalar1=-1.0, scalar2=None, op0=ALU.mult), reads=[a_], writes=[ab_])
        P.dve(lambda e: e.tensor_tensor(out=ab_[:, :, :], in0=ab_[:, :, :], in1=a_[:, :, :], op=ALU.min), reads=[a_, ab_], writes=[ab_])
        P.act(lambda e: e.activation(out=e_[:, :, :], in_=ab_[:, :, :], func=AF.Exp), reads=[ab_], writes=[e_])
        P.dve(lambda e: e.tensor_scalar(out=e_[:, :, :], in0=e_[:, :, :], scalar1=1.0, scalar2=None, op0=ALU.add), reads=[e_], writes=[e_])
        P.act(lambda e: e.activation(out=l_[:, :, :], in_=e_[:, :, :], func=AF.Ln), reads=[e_], writes=[l_])
        P.dve(lambda e: e.tensor_scalar(out=a_[:, :, :], in0=a_[:, :, :], scalar1=0.0, scalar2=None, op0=ALU.max), reads=[a_], writes=[a_])
        P.dve(lambda e: e.tensor_tensor(out=dtr[:, :, :], in0=a_[:, :, :], in1=l_[:, :, :], op=ALU.add), reads=[a_, l_], writes=[dtr])
        P.dve(lambda e: e.tensor_tensor(out=dta_[:, :, :], in0=dtr[:, :, :],
                                        in1=aneg[:, 0:8].rearrange("p (o r) -> p o r", o=1).to_broadcast([128, 2, 8]), op=ALU.mult),
              reads=[dtr, aneg], writes=[dta_])
        for jj in range(6):
            P.dve(lambda e, jj=jj: e.tensor_scalar(out=cacc[:, :], in0=cin[:, jj, 0:256], scalar1=cw[:, jj * 4:jj * 4 + 1], scalar2=None, op0=ALU.mult),
                  reads=[cin, cw], writes=[cacc])
            for k in range(1, 4):
                P.dve(lambda e, jj=jj, k=k: e.scalar_tensor_tensor(out=cacc[:, :], in0=cin[:, jj, k:k + 256], scalar=cw[:, jj * 4 + k:jj * 4 + k + 1],
                                                                  in1=cacc[:, :], op0=ALU.mult, op1=ALU.add), reads=[cin, cw, cacc], writes=[cacc])
            P.act(lambda e, jj=jj: e.activation(out=xcv[:, jj, :], in_=cacc[:, :], func=AF.Silu, bias=cb[:, jj:jj + 1], scale=1.0),
                  reads=[cacc, cb], writes=[xcv])
        P.pool(lambda e: e.tensor_copy(cin[:, :, 0:3], cin[:, :, 256:259]), reads=[cin], writes=[cin])
        P.act(lambda e: e.copy(BTb[:, :], xcv[:, 4, :]), reads=[xcv], writes=[BTb])
        P.act(lambda e: e.copy(CTb[:, :], xcv[:, 5, :]), reads=[xcv], writes=[CTb])
        Uc = C(C1_U, 128); On = C(C1_ONES, 128)
        P.pe(lambda e: e.matmul(PM[:, 32:40], Uc, dta_[:, 0, :], start=True, stop=True), reads=[cs, dta_], writes=[PM])
        P.pe(lambda e: e.matmul(PM[:, 40:48], On, dta_[:, 0, :], start=True, stop=False), reads=[cs, dta_], writes=[PM])
        P.pe(lambda e: e.matmul(PM[:, 40:48], Uc, dta_[:, 1, :], start=False, stop=True), reads=[cs, dta_], writes=[PM])
        P.pe(lambda e: e.matmul(PM[:, 48:56], On, dta_[:, 0, :], start=True, stop=False), reads=[cs, dta_], writes=[PM])
        P.pe(lambda e: e.matmul(PM[:, 48:56], On, dta_[:, 1, :], start=False, stop=True), reads=[cs, dta_], writes=[PM])
        P.dve(lambda e: e.tensor_copy(cssb[:, 0:24], PM[:, 32:56]), reads=[PM], writes=[cssb])
        csv = cssb[:, 0:16].rearrange("p (t r) -> p t r", t=2)
        clb = cssb[:, 16:24].rearrange("p (o r) -> p o r", o=1).to_broadcast([128, 2, 8])
        P.dve(lambda e: e.tensor_scalar(out=negcs[:, :, :], in0=csv, scalar1=-1.0, scalar2=None, op0=ALU.mult), reads=[cssb], writes=[negcs])
        P.dve(lambda e: e.tensor_tensor(out=wend[:, :, :], in0=clb, in1=csv, op=ALU.subtract), reads=[cssb], writes=[wend])
        P.act(lambda e: e.activation(out=wend[:, :, :], in_=wend[:, :, :], func=AF.Exp), reads=[wend], writes=[wend])
        P.dve(lambda e: e.tensor_tensor(out=dtw[:, :, :], in0=dtr[:, :, :], in1=wend[:, :, :], op=ALU.mult), reads=[dtr, wend], writes=[dtw])
        P.act(lambda e: e.activation(out=dec[:, :], in_=cssb[:, 16:24], func=AF.Exp), reads=[cssb], writes=[dec])
        for tt in range(2):
            for jj in range(4):
                P.pe(lambda e, tt=tt, jj=jj: e.matmul(PG[:, jj * 128:(jj + 1) * 128], xcv[:, jj, tt * 128:(tt + 1) * 128], C(C1_ID, 128),
                                                     start=True, stop=True), reads=[xcv, cs], writes=[PG])
            P.act(lambda e, tt=tt: e.copy(xtok[:, tt, :], PG[:, 0:512]), reads=[PG], writes=[xtok])
        for tt in range(2):
            P.pe(lambda e, tt=tt: e.matmul(PM[:, 64 + tt * 128:64 + (tt + 1) * 128], xcv[:, 4, tt * 128:(tt + 1) * 128], C(C1_ID, 128),
                                           start=True, stop=True), reads=[xcv, cs], writes=[PM])
        P.act(lambda e: e.copy(Btok[:, :, :].rearrange("p t n -> p (t n)"), PM[:, 64:320]), reads=[PM], writes=[Btok])
        for tt in range(2):
            xv = xtok[:, tt, :].rearrange("p (i two q) -> p i two q", two=2, q=64)
            dv = dtr[:, tt, :].rearrange("p (i two) -> p i two", two=2)
            for par, X in ((0, XE), (1, XO)):
                P.dve(lambda e, tt=tt, par=par, X=X, xv=xv, dv=dv: e.tensor_tensor(
                    out=X[:, tt, :].rearrange("p (i two q) -> p i two q", two=2, q=64)[:, :, par, :],
                    in0=xv[:, :, par, :], in1=dv[:, :, par:par + 1].to_broadcast([128, 4, 64]), op=ALU.mult),
                    reads=[xtok, dtr], writes=[X])
            P.pool(lambda e, tt=tt: e.tensor_tensor(out=xdtw[:, tt, :].rearrange("p (r q) -> p r q", q=64),
                                                   in0=xtok[:, tt, :].rearrange("p (r q) -> p r q", q=64),
                                                   in1=bc8(dtw[:, tt, :]), op=ALU.mult), reads=[xtok, dtw], writes=[xdtw])
        for st in range(2):
            P.pe(lambda e, st=st: e.matmul(PG[:, st * 256:(st + 1) * 256], BTb[:, st * 128:(st + 1) * 128], CTb[:, 0:256],
                                           start=True, stop=True), reads=[BTb, CTb], writes=[PG])
        P.dve(lambda e: e.tensor_tensor(out=Gm[:, :, :].rearrange("p a t -> p (a t)"), in0=PG[:, 0:512], in1=C(C1_CM, 512), op=ALU.mult),
              reads=[PG, cs], writes=[Gm])
        for r in range(8):
            i, par = r // 2, r % 2
            sc_, ce_ = scT[r % 2], CE[r % 2]
            PT = PM
            P.pe(lambda e, r=r: e.matmul(PT[:, 256:512], dta_[:, 0, r:r + 1].to_broadcast([128, 128]), C(C1_UW0, 256), start=True, stop=False),
                 reads=[dta_, cs], writes=[PM])
            P.pe(lambda e, r=r: e.matmul(PT[:, 256:512], dta_[:, 1, r:r + 1].to_broadcast([128, 128]), C(C1_UW1, 256), start=False, stop=True),
                 reads=[dta_, cs], writes=[PM])
            for st in range(2):
                P.dve(lambda e, r=r, st=st: e.tensor_scalar(out=Dm[:, st, :], in0=PT[:, 256:512], scalar1=negcs[:, st, r:r + 1], scalar2=0.0,
                                                           op0=ALU.add, op1=ALU.min), reads=[PM, negcs], writes=[Dm])
            P.act(lambda e: e.activation(out=dcy[:, :, :], in_=Dm[:, :, :], func=AF.Exp), reads=[Dm], writes=[Dm])
            P.pool(lambda e, sc_=sc_: e.tensor_tensor(out=sc_[:, :, :], in0=Gm[:, :, :], in1=dcy[:, :, :], op=ALU.mult),
                   reads=[Gm, dcy], writes=[sc_])
            if c > 0:
                P.act(lambda e: e.activation(out=E1[:, :], in_=PT[:, 256:512], func=AF.Exp), reads=[PM], writes=[E1])
                P.pool(lambda e, ce_=ce_: e.tensor_tensor(out=ce_[:, :], in0=xcv[:, 5, :], in1=E1[:, :], op=ALU.mult),
                       reads=[xcv, E1], writes=[ce_])
            X = XE if par == 0 else XO
            st_ = stE if par == 0 else stO
            pys = slice((i % 2) * 256, (i % 2 + 1) * 256)
            P.pe(lambda e, X=X, i=i, sc_=sc_, pys=pys, par=par: e.matmul(PY[:, pys], X[:, 0, i * 128:(i + 1) * 128], sc_[:, 0, :],
                                                                      start=(par == 0), stop=False), reads=[X, sc_], writes=[PY])
            P.pe(lambda e, X=X, i=i, sc_=sc_, pys=pys, par=par: e.matmul(PY[:, pys], X[:, 1, i * 128:(i + 1) * 128], sc_[:, 1, :],
                                                                      start=False, stop=(par == 1 and c == 0)), reads=[X, sc_], writes=[PY])
            if c > 0:
                P.pe(lambda e, st_=st_, i=i, ce_=ce_, pys=pys, par=par: e.matmul(PY[:, pys], st_[:, i * 128:(i + 1) * 128], ce_[:, :],
                                                                              start=False, stop=(par == 1)), reads=[st_, ce_], writes=[PY])
            if par == 1:
                P.dve(lambda e, i=i, pys=pys: e.scalar_tensor_tensor(out=yD[:, :], in0=xcv[:, i, :], scalar=pv[:, i:i + 1], in1=PY[:, pys],
                                                                    op0=ALU.mult, op1=ALU.add), reads=[xcv, pv, PY], writes=[yD])
                P.pool(lambda e, i=i: e.tensor_tensor(out=gy[:, i, :], in0=yD[:, :], in1=zs[:, i, :], op=ALU.mult),
                       reads=[yD, zs], writes=[gy])
        P.act(lambda e: e.activation(out=sqg[:, :, :], in_=gy[:, :, :], func=AF.Square), reads=[gy], writes=[sqg])
        for i in range(4):
            P.pe(lambda e, i=i: e.matmul(BJ[:, 0:256], C(C1_ONES, 128), sqg[:, i, :], start=(i == 0), stop=(i == 3)),
                 reads=[cs, sqg], writes=[BJ])
        P.dve(lambda e: e.tensor_scalar(out=rstd[:, :], in0=BJ[:, 0:256], scalar1=1.0 / 512, scalar2=EPS, op0=ALU.mult, op1=ALU.add),
              reads=[BJ], writes=[rstd])
        P.act(lambda e: e.activation(out=rstd[:, :], in_=rstd[:, :], func=AF.Sqrt), reads=[rstd], writes=[rstd])
        P.dve(lambda e: e.reciprocal(out=rstd[:, :], in_=rstd[:, :]), reads=[rstd], writes=[rstd])
        for i in range(4):
            P.dve(lambda e, i=i: e.scalar_tensor_tensor(out=yout[:, i, :], in0=gy[:, i, :], scalar=pv[:, 4 + i:5 + i], in1=rstd[:, :],
                                                       op0=ALU.mult, op1=ALU.mult), reads=[gy, pv, rstd], writes=[yout])
        outs.append(P.dma("sp", YsO, yout[:, :, :], reads=[yout], writes=[Yloc_t]))
        if c < NCH - 1:
            for tt in range(2):
                P.pe(lambda e, tt=tt: e.matmul(PG[:, 0:512], Btok[:, tt, :], xdtw[:, tt, :], start=(tt == 0), stop=(tt == 1)),
                     reads=[Btok, xdtw], writes=[PG])
            if c == 0:
                P.dve(lambda e: e.tensor_copy(state[:, :], PG[:, 0:512]), reads=[PG], writes=[state])
            else:
                P.pool(lambda e: e.tensor_tensor(out=state[:, :].rearrange("p (r q) -> p r q", q=64),
                                                in0=state[:, :].rearrange("p (r q) -> p r q", q=64), in1=bc8(dec[:, 0:8]), op=ALU.mult),
                       reads=[state, dec], writes=[state])
                P.dve(lambda e: e.tensor_tensor(out=state[:, :], in0=state[:, :], in1=PG[:, 0:512], op=ALU.add), reads=[state, PG], writes=[state])
            sv = state[:, :].rearrange("p (i two q) -> p i two q", two=2, q=64)
            P.act(lambda e, sv=sv: e.copy(stE[:, :].rearrange("p (i two q) -> p i two q", two=2, q=64)[:, :, 0, :], sv[:, :, 0, :]),
                  reads=[state], writes=[stE])
            P.act(lambda e, sv=sv: e.copy(stO[:, :].rearrange("p (i two q) -> p i two q", two=2, q=64)[:, :, 1, :], sv[:, :, 1, :]),
                  reads=[state], writes=[stO])
        use_gate = c > 3
        if use_gate:
            for h in range(4):
                i, po = h // 2, (h % 2) * 64
                for tt in range(2):
                    sb_ = selb[tt]
                    P.pe(lambda e, i=i, po=po, tt=tt: e.matmul(PM[:, 0:c], qTf[po:po + 64, i, tt * 128:(tt + 1) * 128], kmean[po:po + 64, i, 0:c],
                                                              start=True, stop=True), reads=[qTf, kmean], writes=[PM])
                    P.dve(lambda e: e.tensor_copy(gate[:, 0:c], PM[:, 0:c]), reads=[PM], writes=[gate])
                    P.dve(lambda e: e.max(out=mx8[:, 0:8], in_=gate[:, 0:32]), reads=[gate], writes=[mx8])
                    P.dve(lambda e: e.tensor_scalar(out=selm[:, 0:c], in0=gate[:, 0:c], scalar1=mx8[:, 2:3], scalar2=None, op0=ALU.is_ge),
                          reads=[gate, mx8], writes=[selm])
                    P.dve(lambda e, sb_=sb_: e.tensor_scalar(out=sb_[:, 0:c], in0=selm[:, 0:c], scalar1=-1.0, scalar2=-NEG, op0=ALU.add, op1=ALU.mult),
                          reads=[selm], writes=[sb_])
                    P.pe(lambda e, sb_=sb_, tt=tt: e.matmul(PM[0:32, 64 + tt * 128:64 + (tt + 1) * 128], sb_[:, 0:32], C(C1_ID, 128), start=True, stop=True),
                         reads=[sb_, cs], writes=[PM])
                P.act(lambda e, h=h: e.copy(selT[:, h, :], PM[0:32, 64:320]), reads=[PM], writes=[selT])
        items = [(h, n) for h in range(4) for n in range(c + 1)]
        PSB = (PS0, PS1, BJ, PG)
        POB = ((PO, PD), (PY, PM))
        LA = 3

        def emit_scores(idx):
            h, n = items[idx]
            i, po = h // 2, (h % 2) * 64
            PSx, pTx = PSB[idx % 4], pT[idx % 4]
            own = (n == c)
            has_bias = own or use_gate
            for kt in range(2):
                ks = slice(n * 256 + kt * 128, n * 256 + (kt + 1) * 128)
                P.pe(lambda e, kt=kt, ks=ks: e.matmul(
                    PSx[:, kt * 256:(kt + 1) * 256], kT_ap[po:po + 64, i, ks], qTb[po:po + 64, i, :], start=True, stop=(not has_bias)),
                    reads=[kTt[n], qTb], writes=[PSx])
                if own:
                    P.pe(lambda e, kt=kt: e.matmul(PSx[:, kt * 256:(kt + 1) * 256], identb[:, :], cbias[:, kt, :], start=False, stop=True),
                         reads=[identb, cbias], writes=[PSx])
                elif use_gate:
                    P.pe(lambda e, kt=kt: e.matmul(PSx[:, kt * 256:(kt + 1) * 256], identb[0:32, n:n + 1].to_broadcast([32, 128]), selT[:, h, :],
                                                   start=False, stop=True), reads=[identb, selT], writes=[PSx])
            P.act(lambda e: e.activation(out=pTx[:, :, :].rearrange("p a t -> p (a t)"), in_=PSx[:, 0:512], func=AF.Exp),
                  reads=[PSx], writes=[pTx])

        def emit_pv(idx):
            h, n = items[idx]
            pTx = pT[idx % 4]
            POx, PDx = POB[h % 2]
            for kt in range(2):
                first = (n == 0 and kt == 0)
                last = (n == c and kt == 1)
                P.pe(lambda e, kt=kt, first=first, last=last: e.matmul(
                    POx[0:64, 0:256], V_ap[:, 2 * n + kt, h * 64:(h + 1) * 64], pTx[:, kt, :], start=first, stop=last),
                    reads=[Vt[n], pTx], writes=[POx])
                P.pe(lambda e, kt=kt, first=first, last=last: e.matmul(
                    PDx[0:64, 0:256], onesb[:, 0:64], pTx[:, kt, :], start=first, stop=last), reads=[onesb, pTx], writes=[PDx])
            if n == c:
                P.dve(lambda e: e.reciprocal(out=rden[:, :], in_=PDx[0:64, 0:256]), reads=[PDx], writes=[rden])
                P.dve(lambda e: e.tensor_tensor(out=yatt[:, h, :], in0=POx[0:64, 0:256], in1=rden[:, :], op=ALU.mult),
                      reads=[POx, rden], writes=[yatt])

        for step in range(len(items) + LA):
            if step < len(items):
                emit_scores(step)
            if step - LA >= 0:
                emit_pv(step - LA)
        outs.append(P.dma("sp", YaO[:, :, col0:col0 + 256], yatt[:, :, :], reads=[yatt], writes=[Yloc_t]))
    for c in range(NCH):
        do_chunk(c)
    return outs


def p1_inputs(inp, l, g):
    w = inp["w_in"][l]
    hq = 5152 + 4 * g * 64
    hk = 5152 + 1024 + 4 * g * 64
    hv = 5152 + 2048 + 4 * g * 64
    swap = np.concatenate([np.arange(h * 64 + 32, h * 64 + 64).tolist() + np.arange(h * 64, h * 64 + 32).tolist() for h in range(4)])
    w1 = np.concatenate([
        w[:, g * 512:(g + 1) * 512],
        w[:, 2048 + g * 512:2048 + (g + 1) * 512],
        w[:, 4096 + g * 128:4096 + (g + 1) * 128],
        w[:, 4608 + g * 128:4608 + (g + 1) * 128],
        w[:, hq:hq + 256], w[:, hk:hk + 256],
        w[:, hq:hq + 256][:, swap], w[:, hk:hk + 256][:, swap],
        w[:, hv:hv + 256],
        w[:, 5120 + g * 8:5120 + (g + 1) * 8],
    ], axis=1)
    ch = np.concatenate([g * 512 + np.arange(512), 2048 + g * 128 + np.arange(128), 2560 + g * 128 + np.arange(128)])
    cwl = inp["conv_w"][l][:, ch]
    convw = np.ascontiguousarray(cwl.reshape(4, 6, 128).transpose(2, 1, 0).reshape(128, 24))
    convb = np.ascontiguousarray(inp["conv_b"][l][ch].reshape(6, 128).T)
    heads = g * 8 + np.arange(8)
    p = np.arange(128)
    dvec = np.stack([inp["d_skip"][l][g * 8 + 2 * i + (p >= 64)] for i in range(4)], axis=1)
    normw = inp["ssd_norm_w"][l][g * 512:(g + 1) * 512].reshape(4, 128).T
    dtb = np.broadcast_to(inp["dt_bias"][l][heads][None, :], (128, 8))
    alog = np.broadcast_to(inp["a_log"][l][heads][None, :], (128, 8))
    pvec = np.ascontiguousarray(np.concatenate([dvec, normw, dtb, alog], axis=1).astype(np.float32))
    return {
        "w_am": inp["w_ada_mix"][l], "b_am": _col(inp["b_ada_mix"][l], 24),
        "w1": np.ascontiguousarray(w1), "convw": convw, "convb": convb, "pvec": pvec,
    }


P1_SHAPES = {"w_am": [D, 3072], "b_am": [128, 24], "w1": [D, 2568], "convw": [128, 24], "convb": [128, 6], "pvec": [128, 24]}
P2_SHAPES = {"w_af": [D, 3072], "b_af": [128, 24], "w_g": [D, 2048], "w_bs": [2048, D], "w_ba": [D, D], "w_o": [D, D],
             "lnp": [128, 32], "w_r": [D, 36], "b_r": [1, 36], "w_eg": [NEXP, 128, 4096], "w_eu": [NEXP, 128, 4096], "w_ed": [NEXP, 128, 4096]}
GROUPS = [[0, 1, 2, 3], [4, 5, 6, 7]]


def _allgather(P, in_ap, out_ap, reads, writes):
    def fn(e):
        return e.collective_compute("AllGather", ALU.bypass, replica_groups=GROUPS, ins=[in_ap.opt()], outs=[out_ap.opt()])
    return P.add("pool", fn, reads=reads, writes=writes, dma=True, inc=1, semgroup="cc")


def build_fused(S, L, nexp=NEXP):
    nc = bass.Bass("TRN2", target_bir_lowering=False)
    NT = S // 4
    H2 = S // 2
    NB = NT // 256
    xT_in = _din(nc, "xT", [S // 256, 128, 2048]); xs_in = _din(nc, "xs", [NB, 128, 2048])
    ccol = _din(nc, "ccol", [128, 8]); pos = _din(nc, "pos", [1, S], I32)
    cst1 = _din(nc, "cst1", [128, C1_N]); cst2 = _din(nc, "cst2", [128, 256])
    lw = []
    for l in range(L):
        dct = {k: _din(nc, "%s_%d" % (k, l), shp) for k, shp in P1_SHAPES.items()}
        dct.update({k: _din(nc, "%s_%d" % (k, l), shp) for k, shp in P2_SHAPES.items()})
        lw.append(dct)
    out = _dout(nc, "xoT", [NB, 128, 2048])
    CB = min(4, NB)
    NCK = NB // CB
    Yls = [nc.dram_tensor("Yls%d" % i, [4, NB, 128, 1024], BF16) for i in range(2)]
    Yla = [nc.dram_tensor("Yla%d" % i, [4, 256, NT], BF16) for i in range(2)]
    Ygs = [nc.dram_tensor("Ygs%d" % i, [4, NCK, 4, CB, 128, 1024], BF16) for i in range(2)]
    Yga = [nc.dram_tensor("Yga%d" % i, [4, 2, 4, 128, NT], BF16) for i in range(2)]
    xo = [nc.dram_tensor("xo%d" % i, [NB, 128, 2048], F32) for i in range(2)]
    xg = [nc.dram_tensor("xg%d" % i, [NB, 4, 128, 2048], F32) for i in range(2)]
    x1scr = nc.dram_tensor("x1scr", [NB, 128, 2048], F32)
    Yms = nc.dram_tensor("Yms", [NCK, 4, CB, 128, 1024], BF16)
    Yma = nc.dram_tensor("Yma", [2, 4, 128, NT], BF16)
    Ym_t = T(None, "Ym")
    modscr = nc.dram_tensor("modscr", [128, 24], F32)
    mod_t = T(None, "modscr")
    Yloc_t = [T(None, "Yloc%d" % i) for i in range(2)]; Yg_t = [T(None, "Yg%d" % i) for i in range(2)]
    xo_t = [T(None, "xo%d" % i) for i in range(2)]; xg_t = [T(None, "xg%d" % i) for i in range(2)]
    x1_t = T(None, "x1scr")

    P = Prog(nc)
    P.use_arena(206 * 1024)
    banks = [P.ps("bank%d" % i, [128, 512]) for i in range(8)]
    outs = []
    for l in range(L):
        par = l % 2
        io = dict(lw[l])
        io.update(ccol=ccol, pos=pos, cst1=cst1, cst2=cst2, modscr=modscr.ap(), mod_t=mod_t)
        if l == 0:
            io["x_src"] = lambda c: xT_in[c].rearrange("p (k t) -> p k t", k=8)
            io["x_t"] = None
        else:
            xga = xg[1 - par].ap()
            io["x_src"] = lambda c, xga=xga: xga[c % NB, c // NB].rearrange("p (k t) -> p k t", k=8)
            io["x_t"] = xg_t[1 - par]
        io["Yls"] = Yls[par].ap(); io["Yla"] = Yla[par].ap(); io["Yloc_t"] = Yloc_t[par]
        emit_p1(P, nc, banks, S, io)
        for hh in range(4):
            for ck in range(NCK):
                _allgather(P, Yls[par].ap()[hh, ck * CB:(ck + 1) * CB].rearrange("c p f -> (c p) f"),
                           Ygs[par].ap()[hh, ck].rearrange("g c p f -> (g c p) f"), [Yloc_t[par]], [Yg_t[par]])
            for rt in range(2):
                _allgather(P, Yla[par].ap()[hh, rt * 128:(rt + 1) * 128, :],
                           Yga[par].ap()[hh, rt].rearrange("g p t -> (g p) t"), [Yloc_t[par]], [Yg_t[par]])
        io["Ygs"] = Ygs[par].ap(); io["Yga"] = Yga[par].ap(); io["Yg_t"] = Yg_t[par]; io["CB"] = CB
        io["x1scr"] = x1scr.ap(); io["x1scr_t"] = x1_t
        io["Yms"] = Yms.ap(); io["Yma"] = Yma.ap(); io["Ym_t"] = Ym_t
        if l == 0:
            io["xs"] = xs_in; io["xs_t"] = None
        else:
            io["xs"] = xo[1 - par].ap(); io["xs_t"] = xo_t[1 - par]
        if l == L - 1:
            io["xo"] = out; io["xo_t"] = None
        else:
            io["xo"] = xo[par].ap(); io["xo_t"] = xo_t[par]
        o2 = emit_p2(P, nc, banks, NT, io, nexp=nexp)
        if l == L - 1:
            outs = o2
        else:
            for tb in range(NB):
                _allgather(P, xo[par].ap()[tb], xg[par].ap()[tb].rearrange("g p f -> (g p) f"), [xo_t[par]], [xg_t[par]])
    counts = P.finish(outs)
    return nc, counts


def fused_inputs(inp, r, S):
    b, g = r // 4, r % 4
    NT = S // 4
    L = inp["w_in"].shape[0]
    xblk = _xblocks(np.asarray(inp["x"][b]))
    nb = NT // 256
    m = {"xT": xblk, "xs": np.ascontiguousarray(xblk[g * nb:(g + 1) * nb]), "ccol": _col(inp["c"][b], 8),
         "pos": np.ascontiguousarray(inp["positions"][b][None, :]).astype(np.int32), "cst1": _consts1(), "cst2": _consts()}
    for l in range(L):
        for k, v in p1_inputs(inp, l, g).items():
            m["%s_%d" % (k, l)] = np.ascontiguousarray(v, dtype=np.float32)
        for k, v in p2_inputs(inp, l).items():
            m["%s_%d" % (k, l)] = v
    return m


_NC_CACHE = {}


def kernel(**inputs):
    inp = {k: np.asarray(v) for k, v in inputs.items()}
    B, S, _ = inp["x"].shape
    L = inp["w_in"].shape[0]
    assert B == 2
    key = (S, L)
    if key not in _NC_CACHE:
        _NC_CACHE[key] = build_fused(S, L)[0]
    nc = _NC_CACHE[key]
    import concourse.bass_utils as _bu
    shared = {}
    maps = []
    for r in range(8):
        m = fused_inputs(inp, r, S)
        for k in list(m):
            if k.startswith(("w_a", "b_a", "w_g", "w_b", "w_o", "lnp", "w_r", "b_r", "w_e", "cst")):
                m[k] = shared.setdefault(k, m[k])
        maps.append(m)
    res = _bu.run_bass_kernel_spmd(nc, maps, core_ids=list(range(8))).results
    NT = S // 4
    out = np.zeros((2, S, D), np.float32)
    for r in range(8):
        b, g = r // 4, r % 4
        out[b, g * NT:(g + 1) * NT, :] = _xunblocks(np.asarray(res[r]["xoT"]))
    return out
```
